# Optimizing a Trainium2 kernel written in Bass

```python
import math
import jax, jax.numpy as jnp
from jax import lax
import numpy as np

D_MODEL = 1024
BATCH = 2
SEQ = 16384
DEPTH = 2

N_A_LAYERS = DEPTH // 2
N_B_LAYERS = DEPTH - N_A_LAYERS
RMS_EPS = 1e-6

RET_HEADS = 4
RET_QK_DIM = D_MODEL // RET_HEADS
RET_V_DIM = 2 * D_MODEL // RET_HEADS
RET_CHUNK = 128
RET_IN = 2 * RET_HEADS * RET_QK_DIM + 2 * RET_HEADS * RET_V_DIM

FFN_HIDDEN = ((8 * D_MODEL + 3 * 256 - 1) // (3 * 256)) * 256

NSA_HEADS = 16
NSA_GROUPS = 4
NSA_REP = NSA_HEADS // NSA_GROUPS
NSA_HEAD_DIM = 64
CMP_LEN = 32
CMP_STRIDE = 16
CMP_HIDDEN = 256
SEL_LEN = 64
SEL_TOPK = 16
WINDOW = 512
Q_BLOCK = 128
NSA_IN = NSA_HEADS * NSA_HEAD_DIM + 3 * NSA_HEADS
KV_SHARED_OUT = 6 * NSA_GROUPS * NSA_HEAD_DIM

REL_BUCKETS = 32
REL_MAX_DIST = 128

kernel_name = "yoco_retnet_nsa_hybrid"


def rms_norm(x, g):
    xf = x.astype(jnp.float32)
    y = xf * lax.rsqrt(jnp.mean(xf * xf, axis=-1, keepdims=True) + RMS_EPS)
    return (y * g.astype(jnp.float32)).astype(x.dtype)


def swiglu_ffn(x, w_in, w_out):
    gate, up = jnp.split(x @ w_in, 2, axis=-1)
    return (jax.nn.silu(gate) * up) @ w_out


def rotate_pairs(x, cos, sin):
    x1 = x[..., 0::2]
    x2 = x[..., 1::2]
    return jnp.stack([x1 * cos - x2 * sin, x1 * sin + x2 * cos], axis=-1).reshape(x.shape)


def t5_bucket(dist):
    n = jnp.maximum(dist, 0)
    max_exact = REL_BUCKETS // 2
    nf = jnp.maximum(n, 1).astype(jnp.float32)
    large = max_exact + (jnp.log(nf / max_exact) / math.log(REL_MAX_DIST / max_exact)
                         * (REL_BUCKETS - max_exact)).astype(jnp.int32)
    large = jnp.minimum(large, REL_BUCKETS - 1)
    return jnp.where(n < max_exact, n, large)


def masked_softmax(logits, mask):
    logits = jnp.where(mask, logits, -jnp.inf)
    m = jnp.max(logits, axis=-1, keepdims=True)
    m = jnp.where(jnp.isfinite(m), m, 0.0)
    p = jnp.exp(logits - m)
    return p / jnp.maximum(jnp.sum(p, axis=-1, keepdims=True), jnp.finfo(jnp.float32).tiny)


def retention_mixer(x, w_in, w_out):
    B, S, _ = x.shape
    h, dk, dv, C = RET_HEADS, RET_QK_DIM, RET_V_DIM, RET_CHUNK
    proj = x @ w_in
    q, k, v, g = jnp.split(proj, [h * dk, 2 * h * dk, 2 * h * dk + h * dv], axis=-1)
    q = q.reshape(B, S, h, dk).astype(jnp.float32)
    k = k.reshape(B, S, h, dk).astype(jnp.float32)
    v = v.reshape(B, S, h, dv).astype(jnp.float32)
    pos = jnp.arange(S, dtype=jnp.float32)
    theta = 1.0 / (10000.0 ** jnp.linspace(0.0, 1.0, dk // 2, dtype=jnp.float32))
    ang = pos[:, None] * theta[None, :]
    cos = jnp.cos(ang)[:, None, :]
    sin = jnp.sin(ang)[:, None, :]
    q = rotate_pairs(q, cos, sin)
    k = rotate_pairs(k, cos, sin) * (dk ** -0.5)
    log_gamma = jnp.log(1.0 - 2.0 ** (-5.0 - jnp.arange(h, dtype=jnp.float32)))
    idx = jnp.arange(C, dtype=jnp.float32)
    rel = idx[:, None] - idx[None, :]
    intra_decay = jnp.where(rel >= 0, jnp.exp(jnp.maximum(rel, 0.0) * log_gamma[:, None, None]), 0.0)
    q_decay = jnp.exp((idx + 1.0)[None, :] * log_gamma[:, None])
    k_decay = jnp.exp((C - 1.0 - idx)[None, :] * log_gamma[:, None])
    chunk_decay = jnp.exp(C * log_gamma)
    nC = S // C

    def to_chunks(t):
        return t.reshape(B, nC, C, h, t.shape[-1]).transpose(1, 0, 3, 2, 4)

    def step(state, inp):
        qc, kc, vc = inp
        scores = jnp.einsum('bhid,bhjd->bhij', qc, kc) * intra_decay
        o = (jnp.einsum('bhij,bhje->bhie', scores, vc)
             + jnp.einsum('bhid,bhde->bhie', qc * q_decay[..., None], state))
        state = (state * chunk_decay[:, None, None]
                 + jnp.einsum('bhjd,bhje->bhde', kc * k_decay[..., None], vc))
        return state, o

    state0 = jnp.zeros((B, h, dk, dv), jnp.float32)
    _, o = lax.scan(step, state0, (to_chunks(q), to_chunks(k), to_chunks(v)))
    o = o.transpose(1, 0, 3, 2, 4).reshape(B, S, h, dv)
    o = o * lax.rsqrt(jnp.mean(o * o, axis=-1, keepdims=True) + RMS_EPS)
    o = (o.reshape(B, S, h * dv) * jax.nn.silu(g.astype(jnp.float32))).astype(x.dtype)
    return o @ w_out


def nsa_shared_kv(h, kv_norm, kv_w, cmp_pe_k, cmp_w1_k, cmp_w2_k, cmp_pe_v, cmp_w1_v, cmp_w2_v):
    B, S, _ = h.shape
    G, d = NSA_GROUPS, NSA_HEAD_DIM
    kv = rms_norm(h, kv_norm) @ kv_w
    k_c, v_c, k_s, v_s, k_w, v_w = [t.reshape(B, S, G, d) for t in jnp.split(kv, 6, axis=-1)]
    n_cmp = (S - CMP_LEN) // CMP_STRIDE + 1
    blk_idx = np.arange(n_cmp)[:, None] * CMP_STRIDE + np.arange(CMP_LEN)[None, :]

    def compress(t, pe, w1, w2):
        blocks = t[:, blk_idx] + pe[:, None, :]
        blocks = blocks.transpose(0, 1, 3, 2, 4).reshape(B, n_cmp, G, CMP_LEN * d)
        return jax.nn.silu(blocks @ w1) @ w2

    kc = compress(k_c, cmp_pe_k, cmp_w1_k, cmp_w2_k)
    vc = compress(v_c, cmp_pe_v, cmp_w1_v, cmp_w2_v)
    n_sel = S // SEL_LEN
    ks = k_s.reshape(B, n_sel, SEL_LEN, G, d).transpose(0, 3, 1, 2, 4)
    vs = v_s.reshape(B, n_sel, SEL_LEN, G, d).transpose(0, 3, 1, 2, 4)
    pad = ((0, 0), (WINDOW, 0), (0, 0), (0, 0))
    kw = jnp.pad(k_w, pad)
    vw = jnp.pad(v_w, pad)
    return (kc, vc, ks, vs, kw, vw)


def nsa_mixer(x, w_in, w_out, rel_bias, shared):
    B, S, _ = x.shape
    H, G, R, d = NSA_HEADS, NSA_GROUPS, NSA_REP, NSA_HEAD_DIM
    Q = Q_BLOCK
    kc, vc, ks, vs, kw, vw = shared
    proj = x @ w_in
    q = proj[..., :H * d].reshape(B, S, G, R, d)
    gate = jax.nn.sigmoid(proj[..., H * d:].astype(jnp.float32)).reshape(B, S, G, R, 3)
    n_cmp = kc.shape[1]
    n_sel = ks.shape[2]
    n_top = min(SEL_TOPK, n_sel)
    cmp_end = jnp.arange(n_cmp, dtype=jnp.int32) * CMP_STRIDE + CMP_LEN - 1
    ratio = SEL_LEN // CMP_STRIDE
    lead = CMP_LEN // CMP_STRIDE - 1
    span = ratio + lead
    sel_map = ratio * np.arange(n_sel)[:, None] - lead + np.arange(span)[None, :]
    sel_map = np.where((sel_map >= 0) & (sel_map < n_cmp), sel_map, n_cmp)
    bias_tab = rel_bias.astype(jnp.float32)
    bias_gr = bias_tab.reshape(REL_BUCKETS, G, R).transpose(1, 0, 2)
    scale = d ** -0.5
    gather_blocks = jax.vmap(jax.vmap(lambda kb, ib: kb[ib]))
    kc32 = kc.astype(jnp.float32)
    vc32 = vc.astype(jnp.float32)
    sel_ids = jnp.arange(n_sel, dtype=jnp.int32)
    blk_start = sel_ids * SEL_LEN
    win_off = jnp.arange(WINDOW + Q, dtype=jnp.int32)
    g_ids = jnp.arange(G)[None, :, None, None, None]

    def block(qb):
        start = qb * Q
        t = start + jnp.arange(Q, dtype=jnp.int32)
        qblk = lax.dynamic_slice_in_dim(q, start, Q, 1).astype(jnp.float32) * scale
        gg = lax.dynamic_slice_in_dim(gate, start, Q, 1)
        dist_c = t[:, None] - cmp_end[None, :]
        s = jnp.einsum('bqgrd,bngd->bgrqn', qblk, kc32)
        s = s + bias_tab[t5_bucket(dist_c)].reshape(Q, n_cmp, G, R).transpose(2, 3, 0, 1)
        p_c = masked_softmax(s, dist_c >= 0)
        o_c = jnp.einsum('bgrqn,bngd->bqgrd', p_c, vc32)
        imp_c = jnp.sum(p_c, axis=2)
        imp_c = jnp.concatenate([imp_c, jnp.zeros(imp_c.shape[:-1] + (1,), imp_c.dtype)], axis=-1)
        imp = jnp.sum(imp_c[..., sel_map], axis=-1)
        cur = t // SEL_LEN
        valid = blk_start[None, :] <= t[:, None]
        forced = ((sel_ids[None, :] == 0) | (sel_ids[None, :] == cur[:, None])
                  | (sel_ids[None, :] == cur[:, None] - 1))
        imp = jnp.where(forced, jnp.inf, jnp.where(valid, imp, -jnp.inf))
        _, sel_idx = lax.top_k(imp, n_top)
        k_sel = gather_blocks(ks, sel_idx).astype(jnp.float32)
        v_sel = gather_blocks(vs, sel_idx).astype(jnp.float32)
        pos = sel_idx[..., None] * SEL_LEN + jnp.arange(SEL_LEN, dtype=jnp.int32)
        dist_s = t[None, None, :, None, None] - pos
        s = jnp.einsum('bqgrd,bgqnld->bgrqnl', qblk, k_sel)
        s = s + jnp.moveaxis(bias_gr[g_ids, t5_bucket(dist_s)], -1, 2)
        s = s.reshape(B, G, R, Q, n_top * SEL_LEN)
        mask_s = (dist_s >= 0).reshape(B, G, 1, Q, n_top * SEL_LEN)
        p_s = masked_softmax(s, mask_s).reshape(B, G, R, Q, n_top, SEL_LEN)
        o_s = jnp.einsum('bgrqnl,bgqnld->bqgrd', p_s, v_sel)
        kwin = lax.dynamic_slice_in_dim(kw, start, WINDOW + Q, 1).astype(jnp.float32)
        vwin = lax.dynamic_slice_in_dim(vw, start, WINDOW + Q, 1).astype(jnp.float32)
        kpos = start - WINDOW + win_off
        dist_w = t[:, None] - kpos[None, :]
        s = jnp.einsum('bqgrd,bkgd->bgrqk', qblk, kwin)
        s = s + bias_tab[t5_bucket(dist_w)].reshape(Q, WINDOW + Q, G, R).transpose(2, 3, 0, 1)
        mask_w = (dist_w >= 0) & (dist_w < WINDOW) & (kpos[None, :] >= 0)
        p_w = masked_softmax(s, mask_w)
        o_w = jnp.einsum('bgrqk,bkgd->bqgrd', p_w, vwin)
        o = gg[..., 0:1] * o_c + gg[..., 1:2] * o_s + gg[..., 2:3] * o_w
        return o.reshape(B, Q, H * d)

    o = lax.map(block, jnp.arange(S // Q, dtype=jnp.int32))
    o = o.transpose(1, 0, 2, 3).reshape(B, S, H * d).astype(x.dtype)
    return o @ w_out


def setup_inputs(seed: int = 0) -> dict:
    key = jax.random.key(seed)
    ks = jax.random.split(key, 24)
    f32 = jnp.float32

    def dense(k, shape, fan_in):
        return jax.random.normal(k, shape, f32) * (fan_in ** -0.5)

    def gain(k, shape):
        return 1.0 + 0.1 * jax.random.normal(k, shape, f32)

    d = NSA_HEAD_DIM
    return {
        "x": jax.random.normal(ks[0], (BATCH, SEQ, D_MODEL), f32),
        "mix_norm_pre": gain(ks[1], (DEPTH, D_MODEL)),
        "mix_norm_post": gain(ks[2], (DEPTH, D_MODEL)),
        "ffn_norm_pre": gain(ks[3], (DEPTH, D_MODEL)),
        "ffn_norm_post": gain(ks[4], (DEPTH, D_MODEL)),
        "ffn_w_in": dense(ks[5], (DEPTH, D_MODEL, 2 * FFN_HIDDEN), D_MODEL),
        "ffn_w_out": dense(ks[6], (DEPTH, FFN_HIDDEN, D_MODEL), FFN_HIDDEN),
        "ret_w_in": dense(ks[7], (N_A_LAYERS, D_MODEL, RET_IN), D_MODEL),
        "ret_w_out": dense(ks[8], (N_A_LAYERS, RET_HEADS * RET_V_DIM, D_MODEL), RET_HEADS * RET_V_DIM),
        "kv_norm": gain(ks[9], (D_MODEL,)),
        "kv_w": dense(ks[10], (D_MODEL, KV_SHARED_OUT), D_MODEL),
        "cmp_pe_k": 0.5 * jax.random.normal(ks[11], (CMP_LEN, d), f32),
        "cmp_w1_k": dense(ks[12], (CMP_LEN * d, CMP_HIDDEN), CMP_LEN * d),
        "cmp_w2_k": dense(ks[13], (CMP_HIDDEN, d), CMP_HIDDEN),
        "cmp_pe_v": 0.5 * jax.random.normal(ks[14], (CMP_LEN, d), f32),
        "cmp_w1_v": dense(ks[15], (CMP_LEN * d, CMP_HIDDEN), CMP_LEN * d),
        "cmp_w2_v": dense(ks[16], (CMP_HIDDEN, d), CMP_HIDDEN),
        "nsa_w_in": dense(ks[17], (N_B_LAYERS, D_MODEL, NSA_IN), D_MODEL),
        "nsa_w_out": dense(ks[18], (N_B_LAYERS, NSA_HEADS * d, D_MODEL), NSA_HEADS * d),
        "rel_bias": 0.5 * jax.random.normal(ks[19], (REL_BUCKETS, NSA_HEADS), f32),
    }


def reference(x, mix_norm_pre, mix_norm_post, ffn_norm_pre, ffn_norm_post, ffn_w_in, ffn_w_out,
              ret_w_in, ret_w_out, kv_norm, kv_w, cmp_pe_k, cmp_w1_k, cmp_w2_k,
              cmp_pe_v, cmp_w1_v, cmp_w2_v, nsa_w_in, nsa_w_out, rel_bias):
    h = x
    shared = None
    for layer in range(DEPTH):
        if layer == N_A_LAYERS:
            shared = nsa_shared_kv(h, kv_norm, kv_w, cmp_pe_k, cmp_w1_k, cmp_w2_k,
                                   cmp_pe_v, cmp_w1_v, cmp_w2_v)
        normed = rms_norm(h, mix_norm_pre[layer])
        if layer < N_A_LAYERS:
            mixed = retention_mixer(normed, ret_w_in[layer], ret_w_out[layer])
        else:
            j = layer - N_A_LAYERS
            mixed = nsa_mixer(normed, nsa_w_in[j], nsa_w_out[j], rel_bias, shared)
        h = h + rms_norm(mixed, mix_norm_post[layer])
        normed = rms_norm(h, ffn_norm_pre[layer])
        h = h + rms_norm(swiglu_ffn(normed, ffn_w_in[layer], ffn_w_out[layer]), ffn_norm_post[layer])
    return h
```

```python
import math
import numpy as np
import ml_dtypes
from contextlib import ExitStack
import concourse.bass as bass
import concourse.mybir as mybir
from concourse.bass_utils import run_bass_kernel_spmd

F32 = mybir.dt.float32
BF16 = mybir.dt.bfloat16
ACT = mybir.ActivationFunctionType
ALU = mybir.AluOpType
AX = mybir.AxisListType

COMPUTE = ("pe", "dve", "act", "pool")


class Buf:
    def __init__(self, kb, t, name, dram=False):
        self.kb = kb
        self.t = t
        self.name = name
        self.dram = dram
        self.last_w = None
        self.reads = []
        self.sem = None
        self.semcnt = 0
        self.psum = False

    def __getitem__(self, idx):
        return self.t[idx]

    def dsem(self, q="sp"):
        if self.sem is None:
            self.sw = (q == "pool")
            if self.kb.sem_pool and not self.sw:
                self.sem, self.semcnt = self.kb.sem_pool.pop()
            else:
                self.sem = self.kb.new_sem("d")
            self.kb.phase_bufs.append(self)
        return self.sem


class KB:
    def __init__(self, nc, same_eng_sync=True):
        self.nc = nc
        self.es = ExitStack()
        self.engs = {"pe": nc.tensor, "dve": nc.vector, "act": nc.scalar,
                     "pool": nc.gpsimd, "sp": nc.sync}
        self.esem = {}
        self.ecnt = {}
        for e in COMPUTE:
            self.esem[e] = self.es.enter_context(nc.semaphore("e_" + e))
            self.ecnt[e] = 0
        self.waited = {e: {} for e in self.engs}
        self.same = same_eng_sync
        self.nsem = 4
        self.nbuf = 0
        self.ninstr = 0
        self.final_tokens = []
        self.cur = self.es
        self.sem_pool = []
        self.phase_bufs = []

    def new_sem(self, name):
        self.nsem += 1
        return self.es.enter_context(self.nc.semaphore(name + "_%d" % self.nsem))

    def sb(self, shape, dtype, name=None):
        self.nbuf += 1
        name = (name or "b") + "_%d" % self.nbuf
        t = self.cur.enter_context(self.nc.sbuf_tensor(name, list(shape), dtype))
        return Buf(self, t, name)

    def ps(self, shape, dtype, name=None):
        self.nbuf += 1
        name = (name or "p") + "_%d" % self.nbuf
        t = self.cur.enter_context(self.nc.psum_tensor(name, list(shape), dtype))
        b = Buf(self, t, name)
        b.psum = True
        return b

    def dram(self, ap, name):
        return Buf(self, ap, name, dram=True)

    def _wait(self, eng, tok):
        if tok is None:
            return
        sem, val, peng = tok
        if peng == eng and (eng == "pe" or not self.same):
            return
        key = id(sem)
        if self.waited[eng].get(key, 0) >= val:
            return
        self.engs[eng].wait_ge(sem, val)
        self.waited[eng][key] = val

    def _deps(self, eng, reads, writes):
        for b in reads:
            self._wait(eng, b.last_w)
            if b.psum:
                for r in self._compact(b.reads):
                    if r[2] != eng:
                        self._wait(eng, r)
        for b in writes:
            self._wait(eng, b.last_w)
            for r in self._compact(b.reads):
                self._wait(eng, r)

    def op(self, eng, fn, reads=(), writes=()):
        self._deps(eng, reads, writes)
        ins = fn()
        self.ecnt[eng] += 1
        ins.then_inc(self.esem[eng], 1)
        tok = (self.esem[eng], self.ecnt[eng], eng)
        for b in reads:
            b.reads.append(tok)
            if len(b.reads) > 24:
                b.reads = self._compact(b.reads)
        for b in writes:
            b.last_w = tok
            b.reads = []
        self.ninstr += 1
        return tok

    @staticmethod
    def _compact(reads):
        best = {}
        for (s, v, e) in reads:
            k = id(s)
            if k not in best or best[k][1] < v:
                best[k] = (s, v, e)
        return list(best.values())

    def dma(self, q, out_ap, in_ap, reads=(), writes=(), **kw):
        self._deps(q, reads, writes)
        owner = writes[0] if writes else reads[0]
        sem = owner.dsem(q)
        owner.semcnt += 16
        self.engs[q].dma_start(out=out_ap, in_=in_ap, **kw).then_inc(sem, 16)
        tok = (sem, owner.semcnt, "dma")
        for b in reads:
            b.reads.append(tok)
        for b in writes:
            b.last_w = tok
            b.reads = []
        self.ninstr += 1
        return tok

    def finish(self, toks, eng="sp"):
        for t in self._compact(toks):
            sem, val, _ = t
            key = id(sem)
            if self.waited[eng].get(key, 0) >= val:
                continue
            self.engs[eng].wait_ge(sem, val)
            self.waited[eng][key] = val

    def barrier(self):
        toks = [(self.esem[e], self.ecnt[e], "x") for e in COMPUTE if self.ecnt[e] > 0]
        toks += [(b.sem, b.semcnt, "dma") for b in self.phase_bufs if b.semcnt > 0]
        for e in self.engs:
            self.finish(toks, eng=e)

    def begin_phase(self):
        self.cur = ExitStack()
        self.phase_bufs = [b for b in self.phase_bufs if b.dram]

    def end_phase(self):
        self.barrier()
        for b in self.phase_bufs:
            if not b.dram and b.sem is not None:
                if not getattr(b, "sw", False):
                    self.sem_pool.append((b.sem, b.semcnt))
                b.sem = None
        self.phase_bufs = [b for b in self.phase_bufs if b.dram]
        self.cur.close()
        self.cur = self.es

    def close(self):
        self.es.close()


D = 1024; H = 2816; HC = 22; KC = 8

def load_ident(kb, ident_ap):
    idb = kb.sb([128, 128], BF16, "ident")
    kb.dma("pool", idb[:], ident_ap, writes=[idb])
    return idb

def rstd_from_ms(kb, rs, ms, eps=1e-6):
    nc = kb.nc
    kb.op("act", lambda: nc.scalar.activation(out=rs[:], in_=ms[:], func=ACT.Sqrt, bias=eps, scale=1.0),
          reads=[ms], writes=[rs])
    kb.op("dve", lambda: nc.vector.reciprocal(out=rs[:], in_=rs[:]), reads=[rs], writes=[rs])

def build_ffn(kb, NT, hin, w_in, w_out, g_pre, g_post, hout, ident, TT=256):
    nc = kb.nc
    NS = TT // 128
    Win = kb.sb([128, KC, 2 * H], BF16, "Win")
    Wout = kb.sb([128, HC, D], BF16, "Wout")
    Gpre = kb.sb([128, D], F32, "Gpre")
    Gpost = kb.sb([128, D], F32, "Gpost")
    for kc in range(KC):
        kb.dma("pool", Win[:, kc, :], w_in[kc * 128:(kc + 1) * 128, :], writes=[Win])
    for hc in range(HC):
        kb.dma("pool", Wout[:, hc, :], w_out[hc * 128:(hc + 1) * 128, :], writes=[Wout])
    kb.dma("sp", Gpre[:], g_pre, writes=[Gpre])
    kb.dma("sp", Gpost[:], g_post, writes=[Gpost])
    XA = [kb.sb([128, D], F32, "xa") for _ in range(2)]
    XB = [kb.sb([128, D], F32, "xb") for _ in range(2)]
    XN = [kb.sb([128, D], BF16, "xn") for _ in range(2)]
    OT = [kb.sb([128, D], F32, "ot") for _ in range(2)]
    junk = kb.sb([128, D], BF16, "junk")
    SS = [kb.sb([128, 1], F32, "ss") for _ in range(2)]
    RS = [kb.sb([128, 1], F32, "rs") for _ in range(2)]
    SS2 = [kb.sb([128, 1], F32, "ss2") for _ in range(2)]
    RS2 = [kb.sb([128, 1], F32, "rs2") for _ in range(2)]
    xnT = kb.sb([128, KC, TT], BF16, "xnT")
    hT = kb.sb([128, HC, TT], BF16, "hT")
    SG = [kb.sb([128, TT], F32, "sg") for _ in range(2)]
    tp = kb.ps([128, KC, 128], BF16, "tp")
    PG = [kb.ps([128, 2, TT], F32, "pg") for _ in range(3)]
    PY = [kb.ps([128, D], F32, "py") for _ in range(NS)]
    outs = []
    n = 0
    for ti in range(NT // TT):
        for s in range(NS):
            r0 = ti * TT + s * 128
            xa = XA[n % 2]; xn = XN[n % 2]; ss = SS[n % 2]; rs = RS[n % 2]
            n += 1
            kb.dma("sp", xa[:], hin[r0:r0 + 128, :], writes=[xa])
            kb.op("dve", lambda: nc.vector.memset(ss[:], 0.0), writes=[ss])
            kb.op("act", lambda: nc.scalar.activation(out=junk[:], in_=xa[:], func=ACT.Square, scale=float(D) ** -0.5,
                                                      accum_out=ss[:, 0:1]), reads=[xa, ss], writes=[junk, ss])
            rstd_from_ms(kb, rs, ss)
            kb.op("dve", lambda: nc.vector.scalar_tensor_tensor(out=xn[:], in0=xa[:], scalar=rs[:, 0:1], in1=Gpre[:],
                                                                op0=ALU.mult, op1=ALU.mult),
                  reads=[xa, rs, Gpre], writes=[xn])
            for kc in range(KC):
                kb.op("pe", lambda: nc.tensor.transpose(out=tp[:, kc, :], in_=xn[:, kc * 128:(kc + 1) * 128],
                                                        identity=ident[:]), reads=[xn, ident], writes=[tp])
            kb.op("act", lambda: nc.scalar.copy(out=xnT[:, :, s * 128:(s + 1) * 128], in_=tp[:]),
                  reads=[tp], writes=[xnT])
        for hc in range(HC):
            pg = PG[hc % 3]; sg = SG[hc % 2]
            for half in range(2):
                c0 = half * H + hc * 128
                for kc in range(KC):
                    kb.op("pe", lambda: nc.tensor.matmul(out=pg[:, half, :], lhsT=Win[:, kc, c0:c0 + 128],
                                                         rhs=xnT[:, kc, :], start=(kc == 0), stop=(kc == KC - 1)),
                          reads=[Win, xnT], writes=[pg])
            kb.op("act", lambda: nc.scalar.activation(out=sg[:], in_=pg[:, 0, :], func=ACT.Silu),
                  reads=[pg], writes=[sg])
            kb.op("dve", lambda: nc.vector.tensor_tensor(out=hT[:, hc, :], in0=sg[:], in1=pg[:, 1, :], op=ALU.mult),
                  reads=[sg, pg], writes=[hT])
        for s in range(NS):
            py = PY[s]
            for cb in range(2):
                for hc in range(HC):
                    kb.op("pe", lambda: nc.tensor.matmul(out=py[:, cb * 512:(cb + 1) * 512],
                                                         lhsT=hT[:, hc, s * 128:(s + 1) * 128],
                                                         rhs=Wout[:, hc, cb * 512:(cb + 1) * 512],
                                                         start=(hc == 0), stop=(hc == HC - 1)),
                          reads=[hT, Wout], writes=[py])
        for s in range(NS):
            r0 = ti * TT + s * 128
            py = PY[s]; xb = XB[s % 2]; ot = OT[s % 2]; ss2 = SS2[s % 2]; rs2 = RS2[s % 2]
            kb.dma("sp", xb[:], hin[r0:r0 + 128, :], writes=[xb])
            kb.op("dve", lambda: nc.vector.memset(ss2[:], 0.0), writes=[ss2])
            kb.op("act", lambda: nc.scalar.activation(out=junk[:], in_=py[:], func=ACT.Square, scale=float(D) ** -0.5,
                                                      accum_out=ss2[:, 0:1]), reads=[py, ss2], writes=[junk, ss2])
            rstd_from_ms(kb, rs2, ss2)
            kb.op("dve", lambda: nc.vector.scalar_tensor_tensor(out=ot[:], in0=py[:], scalar=rs2[:, 0:1], in1=Gpost[:],
                                                                op0=ALU.mult, op1=ALU.mult),
                  reads=[py, rs2, Gpost], writes=[ot])
            kb.op("pool", lambda: nc.gpsimd.tensor_tensor(out=ot[:], in0=ot[:], in1=xb[:], op=ALU.add),
                  reads=[ot, xb], writes=[ot])
            outs.append(kb.dma("sp", hout[r0:r0 + 128, :], ot[:], reads=[ot]))
    return outs


D = 1024
RH = 4; DK = 256; DV = 512
GAM = [1.0 - 2.0 ** (-5.0 - h) for h in range(RH)]


def build_ret(kb, NT, x_ap, w_in, g_pre, cosT, sinT, cosk, sink, gi, g2i, maskT_ap, s_slots, coef, og_out, s_out,
              ident, state_only=False, stage=9):
    nc = kb.nc
    NCH = NT // 128
    KC = 8
    if state_only:
        c_lo, c_hi = 1024, 4096
    else:
        c_lo, c_hi = 0, 6144
    WC = c_hi - c_lo
    Win = kb.sb([128, KC, WC], BF16, "Win")
    for kc in range(KC):
        for cc in range(0, WC, 1024):
            kb.dma("pool", Win[:, kc, cc:cc + 1024], w_in[kc * 128:(kc + 1) * 128, c_lo + cc:c_lo + cc + 1024],
                   writes=[Win])
    KCOL = 1024 - c_lo; VCOL = 2048 - c_lo; GCOL = 4096 - c_lo
    Gpre = kb.sb([128, D], F32, "Gpre")
    kb.dma("sp", Gpre[:], g_pre, writes=[Gpre])
    S = kb.sb([128, 2, RH, DV], F32, "S")
    if state_only:
        kb.op("dve", lambda: nc.vector.memset(S[:], 0.0), writes=[S])
    else:
        osb = kb.sb([128, RH, DV], F32, "osb")
        coef_sb = kb.sb([128, 3 * RH], F32, "coef")
        kb.dma("sp", coef_sb[:], coef, writes=[coef_sb])
        for dc in range(2):
            for slot in range(3):
                kb.dma("sp", osb[:], s_slots[slot, dc], writes=[osb])
                for h in range(RH):
                    cs = coef_sb[:, slot * RH + h:slot * RH + h + 1]
                    if slot == 0:
                        kb.op("dve", lambda: nc.vector.tensor_scalar(out=S[:, dc, h, :], in0=osb[:, h, :], scalar1=cs, scalar2=None,
                                                                     op0=ALU.mult), reads=[osb, coef_sb], writes=[S])
                    else:
                        kb.op("dve", lambda: nc.vector.scalar_tensor_tensor(out=S[:, dc, h, :], in0=osb[:, h, :], scalar=cs,
                                                                            in1=S[:, dc, h, :], op0=ALU.mult, op1=ALU.add),
                              reads=[osb, coef_sb, S], writes=[S])
    XA = [kb.sb([128, D], F32, "xa") for _ in range(2)]
    xn = kb.sb([128, D], BF16, "xn")
    xnT = kb.sb([128, KC, 128], BF16, "xnT")
    junk = kb.sb([128, D], BF16, "junk")
    ss = kb.sb([128, 1], F32, "ss"); rs = kb.sb([128, 1], F32, "rs")
    COSK = [kb.sb([128, RH, 128], F32, "cosk") for _ in range(2)]
    SINK = [kb.sb([128, RH, 128], F32, "sink") for _ in range(2)]
    kraw = kb.sb([128, 2, 2, 128], F32, "kraw")
    kt = kb.sb([128, RH, 2, 128], BF16, "kt")
    v_sb = kb.sb([128, RH, DV], BF16, "v")
    T = [kb.sb([128, 2, 128], F32, "t%d" % i) for i in range(4)]
    P = [kb.ps([128, 512], F32, "bank%d" % i) for i in range(8)]
    p0b = P[0][:].bitcast(BF16).rearrange("p (a b) -> p a b", b=128)
    if not state_only:
        Sb = kb.sb([128, 2, RH, DV], BF16, "Sb")
        for dc in range(2):
            for h in range(RH):
                kb.op("act", lambda: nc.scalar.activation(out=Sb[:, dc, h, :], in_=S[:, dc, h, :], func=ACT.Copy,
                                                          scale=GAM[h]), reads=[S], writes=[Sb])
        COST = [kb.sb([128, 128], F32, "cosT") for _ in range(2)]
        SINT = [kb.sb([128, 128], F32, "sinT") for _ in range(2)]
        GI = kb.sb([128, RH], F32, "gi"); G2I = kb.sb([128, RH], F32, "g2i")
        kb.dma("sp", GI[:], gi, writes=[GI]); kb.dma("sp", G2I[:], g2i, writes=[G2I])
        maskT = kb.sb([128, 128], F32, "maskT")
        kb.dma("sp", maskT[:], maskT_ap, writes=[maskT])
        qraw = kb.sb([128, 2, 2, 128], F32, "qraw")
        qT = kb.sb([128, RH, 2, 128], BF16, "qT")
        kT = kb.sb([128, RH, 2, 128], BF16, "kT")
        gs = kb.sb([128, RH * DV], BF16, "gs")
        PT = kb.sb([128, RH, 128], BF16, "PT")
        OG = [kb.sb([128, RH * DV], BF16, "og") for _ in range(2)]
        ms4 = kb.sb([128, RH], F32, "ms4"); f4 = kb.sb([128, RH], F32, "f4")
    outs = []
    bk = 0
    for c in range(NCH):
        r0 = c * 128
        xa = XA[c % 2]; cosk_t = COSK[c % 2]; sink_t = SINK[c % 2]
        kb.dma("sp", xa[:], x_ap[r0:r0 + 128, :], writes=[xa])
        kb.dma("sp", cosk_t[:], cosk[r0:r0 + 128], writes=[cosk_t])
        kb.dma("sp", sink_t[:], sink[r0:r0 + 128], writes=[sink_t])
        if not state_only:
            cosT_t = COST[c % 2]; sinT_t = SINT[c % 2]
            kb.dma("sp", cosT_t[:], cosT[:, r0:r0 + 128], writes=[cosT_t])
            kb.dma("sp", sinT_t[:], sinT[:, r0:r0 + 128], writes=[sinT_t])
        kb.op("dve", lambda: nc.vector.memset(ss[:], 0.0), writes=[ss])
        kb.op("act", lambda: nc.scalar.activation(out=junk[:], in_=xa[:], func=ACT.Square, scale=float(D) ** -0.5,
                                                  accum_out=ss[:, 0:1]), reads=[xa, ss], writes=[junk, ss])
        rstd_from_ms(kb, rs, ss)
        kb.op("dve", lambda: nc.vector.scalar_tensor_tensor(out=xn[:], in0=xa[:], scalar=rs[:, 0:1], in1=Gpre[:],
                                                            op0=ALU.mult, op1=ALU.mult),
              reads=[xa, rs, Gpre], writes=[xn])
        for kc in range(KC):
            kb.op("pe", lambda: nc.tensor.transpose(out=p0b[:, kc, :], in_=xn[:, kc * 128:(kc + 1) * 128],
                                                    identity=ident[:]), reads=[xn, ident], writes=[P[0]])
        kb.op("act", lambda: nc.scalar.copy(out=xnT[:], in_=p0b), reads=[P[0]], writes=[xnT])
        if not state_only:
            for hb in range(2):
                pq = P[1 + hb]
                pqv = pq[:].rearrange("p (a b t) -> p a b t", a=2, b=2)
                for hh in range(2):
                    h = 2 * hb + hh
                    for blk in range(2):
                        col0 = h * DK + blk * 128
                        for kc in range(KC):
                            kb.op("pe", lambda: nc.tensor.matmul(out=pqv[:, hh, blk, :], lhsT=Win[:, kc, col0:col0 + 128],
                                                                 rhs=xnT[:, kc, :], start=(kc == 0), stop=(kc == KC - 1)),
                                  reads=[Win, xnT], writes=[pq])
                kb.op("act", lambda: nc.scalar.copy(out=qraw[:].rearrange("p a b t -> p (a b t)"), in_=pq[:]),
                      reads=[pq], writes=[qraw])
                A = qraw[:, :, 0, :]; B = qraw[:, :, 1, :]
                cb = cosT_t[:].unsqueeze(1).broadcast_to([128, 2, 128])
                sb_ = sinT_t[:].unsqueeze(1).broadcast_to([128, 2, 128])
                kb.op("dve", lambda: nc.vector.tensor_tensor(out=T[0][:], in0=A, in1=cb, op=ALU.mult),
                      reads=[qraw, cosT_t], writes=[T[0]])
                kb.op("dve", lambda: nc.vector.tensor_tensor(out=T[1][:], in0=B, in1=sb_, op=ALU.mult),
                      reads=[qraw, sinT_t], writes=[T[1]])
                kb.op("dve", lambda: nc.vector.tensor_tensor(out=T[2][:], in0=A, in1=sb_, op=ALU.mult),
                      reads=[qraw, sinT_t], writes=[T[2]])
                kb.op("dve", lambda: nc.vector.tensor_tensor(out=T[3][:], in0=B, in1=cb, op=ALU.mult),
                      reads=[qraw, cosT_t], writes=[T[3]])
                kb.op("dve", lambda: nc.vector.tensor_tensor(out=qT[:, 2 * hb:2 * hb + 2, 0, :], in0=T[0][:], in1=T[1][:],
                                                             op=ALU.subtract), reads=[T[0], T[1]], writes=[qT])
                kb.op("dve", lambda: nc.vector.tensor_tensor(out=qT[:, 2 * hb:2 * hb + 2, 1, :], in0=T[2][:], in1=T[3][:],
                                                              op=ALU.add), reads=[T[2], T[3]], writes=[qT])
        for kb_ in range(2):
            pk = P[3 + bk % 2]; bk += 1
            for kc in range(KC):
                kb.op("pe", lambda: nc.tensor.matmul(out=pk[:], lhsT=xnT[:, kc, :],
                                                     rhs=Win[:, kc, KCOL + kb_ * 512:KCOL + (kb_ + 1) * 512],
                                                     start=(kc == 0), stop=(kc == KC - 1)),
                      reads=[Win, xnT], writes=[pk])
            kb.op("act", lambda: nc.scalar.copy(out=kraw[:].rearrange("p a b t -> p (a b t)"), in_=pk[:]),
                  reads=[pk], writes=[kraw])
            A = kraw[:, :, 0, :]; B = kraw[:, :, 1, :]
            ck = cosk_t[:, 2 * kb_:2 * kb_ + 2, :]; sk = sink_t[:, 2 * kb_:2 * kb_ + 2, :]
            kb.op("dve", lambda: nc.vector.tensor_tensor(out=T[0][:], in0=A, in1=ck, op=ALU.mult),
                  reads=[kraw, cosk_t], writes=[T[0]])
            kb.op("dve", lambda: nc.vector.tensor_tensor(out=T[1][:], in0=B, in1=sk, op=ALU.mult),
                  reads=[kraw, sink_t], writes=[T[1]])
            kb.op("dve", lambda: nc.vector.tensor_tensor(out=T[2][:], in0=A, in1=sk, op=ALU.mult),
                  reads=[kraw, sink_t], writes=[T[2]])
            kb.op("dve", lambda: nc.vector.tensor_tensor(out=T[3][:], in0=B, in1=ck, op=ALU.mult),
                  reads=[kraw, cosk_t], writes=[T[3]])
            kb.op("dve", lambda: nc.vector.tensor_tensor(out=kt[:, 2 * kb_:2 * kb_ + 2, 0, :], in0=T[0][:], in1=T[1][:],
                                                         op=ALU.subtract), reads=[T[0], T[1]], writes=[kt])
            kb.op("dve", lambda: nc.vector.tensor_tensor(out=kt[:, 2 * kb_:2 * kb_ + 2, 1, :], in0=T[2][:], in1=T[3][:],
                                                          op=ALU.add), reads=[T[2], T[3]], writes=[kt])
        for h in range(RH):
            pv = P[3 + bk % 2]; bk += 1
            for kc in range(KC):
                kb.op("pe", lambda: nc.tensor.matmul(out=pv[:], lhsT=xnT[:, kc, :],
                                                     rhs=Win[:, kc, VCOL + h * 512:VCOL + (h + 1) * 512],
                                                     start=(kc == 0), stop=(kc == KC - 1)),
                      reads=[Win, xnT], writes=[pv])
            kb.op("act", lambda: nc.scalar.copy(out=v_sb[:, h, :], in_=pv[:]), reads=[pv], writes=[v_sb])
        if not state_only:
            for h in range(RH if stage >= 2 else 0):
                pg = P[3 + bk % 2]; bk += 1
                for kc in range(KC):
                    kb.op("pe", lambda: nc.tensor.matmul(out=pg[:], lhsT=xnT[:, kc, :],
                                                         rhs=Win[:, kc, GCOL + h * 512:GCOL + (h + 1) * 512],
                                                         start=(kc == 0), stop=(kc == KC - 1)),
                          reads=[Win, xnT], writes=[pg])
                kb.op("act", lambda: nc.scalar.activation(out=gs[:, h * 512:(h + 1) * 512], in_=pg[:], func=ACT.Silu),
                      reads=[pg], writes=[gs])
            for h in range(RH if stage >= 2 else 0):
                for blk in range(2):
                    kb.op("pe", lambda: nc.tensor.transpose(out=p0b[:, h * 2 + blk, :], in_=kt[:, h, blk, :],
                                                            identity=ident[:]), reads=[kt, ident], writes=[P[0]])
            if stage >= 2:
                kb.op("dve", lambda: nc.vector.tensor_copy(out=kT[:].rearrange("p h b t -> p (h b) t"), in_=p0b),
                      reads=[P[0]], writes=[kT])
            p5 = P[5]; p5v = p5[:].rearrange("p (h t) -> p h t", h=RH)
            for h in range(RH if stage >= 3 else 0):
                for blk in range(2):
                    kb.op("pe", lambda: nc.tensor.matmul(out=p5v[:, h, :], lhsT=kT[:, h, blk, :], rhs=qT[:, h, blk, :],
                                                         start=(blk == 0), stop=(blk == 1)),
                          reads=[kT, qT], writes=[p5])
            if stage >= 3:
              kb.op("dve", lambda: nc.vector.tensor_tensor(out=PT[:], in0=p5v,
                                                         in1=maskT[:].unsqueeze(1).broadcast_to([128, RH, 128]),
                                                         op=ALU.mult), reads=[p5, maskT], writes=[PT])
            kb.op("dve", lambda: nc.vector.memset(ms4[:], 0.0), writes=[ms4])
            for h in range(RH if stage >= 4 else 0):
                po = P[6 + h % 2]
                kb.op("pe", lambda: nc.tensor.matmul(out=po[:], lhsT=PT[:, h, :], rhs=v_sb[:, h, :], start=True, stop=False),
                      reads=[PT, v_sb], writes=[po])
                kb.op("pe", lambda: nc.tensor.matmul(out=po[:], lhsT=qT[:, h, 0, :], rhs=Sb[:, 0, h, :], start=False, stop=False),
                      reads=[qT, Sb], writes=[po])
                kb.op("pe", lambda: nc.tensor.matmul(out=po[:], lhsT=qT[:, h, 1, :], rhs=Sb[:, 1, h, :], start=False, stop=True),
                      reads=[qT, Sb], writes=[po])
                kb.op("act", lambda: nc.scalar.activation(out=junk[:, 0:DV], in_=po[:], func=ACT.Square, scale=float(DV) ** -0.5,
                                                          accum_out=ms4[:, h:h + 1]), reads=[po, ms4], writes=[junk, ms4])
                kb.op("dve", lambda: nc.vector.tensor_copy(out=osb[:, h, :], in_=po[:]), reads=[po], writes=[osb])
        for h in range(RH):
            for dc in range(2):
                pkv = P[3 + bk % 2]; bk += 1
                kb.op("pe", lambda: nc.tensor.matmul(out=pkv[:], lhsT=kt[:, h, dc, :], rhs=v_sb[:, h, :], start=True, stop=True),
                      reads=[kt, v_sb], writes=[pkv])
                kb.op("act", lambda: nc.scalar.activation(out=S[:, dc, h, :], in_=S[:, dc, h, :], func=ACT.Copy,
                                                          scale=GAM[h] ** 128), reads=[S], writes=[S])
                kb.op("dve", lambda: nc.vector.scalar_tensor_tensor(out=S[:, dc, h, :], in0=pkv[:], scalar=GAM[h] ** 127,
                                                                    in1=S[:, dc, h, :], op0=ALU.mult, op1=ALU.add),
                      reads=[pkv, S], writes=[S])
                if not state_only:
                    kb.op("act", lambda: nc.scalar.activation(out=Sb[:, dc, h, :], in_=S[:, dc, h, :], func=ACT.Copy,
                                                              scale=GAM[h]), reads=[S], writes=[Sb])
        if not state_only and stage >= 5:
            og = OG[c % 2]
            kb.op("dve", lambda: nc.vector.tensor_tensor(out=f4[:], in0=ms4[:], in1=G2I[:], op=ALU.mult),
                  reads=[ms4, G2I], writes=[f4])
            rstd_from_ms(kb, f4, f4)
            kb.op("dve", lambda: nc.vector.tensor_tensor(out=f4[:], in0=f4[:], in1=GI[:], op=ALU.mult),
                  reads=[f4, GI], writes=[f4])
            for h in range(RH):
                kb.op("dve", lambda: nc.vector.scalar_tensor_tensor(out=og[:, h * DV:(h + 1) * DV], in0=osb[:, h, :], scalar=f4[:, h:h + 1],
                                                          in1=gs[:, h * DV:(h + 1) * DV], op0=ALU.mult, op1=ALU.mult),
                      reads=[osb, f4, gs], writes=[og])
            outs.append(kb.dma("sp", og_out[r0:r0 + 128, :], og[:], reads=[og]))
    for dc in range(2):
        outs.append(kb.dma("sp", s_out[dc], S[:, dc, :, :], reads=[S]))
    return outs

D = 1024

def post_norm_residual(kb, py, x_rows_ap, Gpost, out_rows_ap, xb, ot, ss2, rs2, junk):
    nc = kb.nc
    kb.dma("sp", xb[:], x_rows_ap, writes=[xb])
    kb.op("dve", lambda: nc.vector.memset(ss2[:], 0.0), writes=[ss2])
    kb.op("act", lambda: nc.scalar.activation(out=junk[:], in_=py[:], func=ACT.Square, scale=float(D) ** -0.5,
                                              accum_out=ss2[:, 0:1]), reads=[py, ss2], writes=[junk, ss2])
    rstd_from_ms(kb, rs2, ss2)
    kb.op("dve", lambda: nc.vector.scalar_tensor_tensor(out=ot[:], in0=py[:], scalar=rs2[:, 0:1], in1=Gpost[:],
                                                        op0=ALU.mult, op1=ALU.mult), reads=[py, rs2, Gpost], writes=[ot])
    kb.op("pool", lambda: nc.gpsimd.tensor_tensor(out=ot[:], in0=ot[:], in1=xb[:], op=ALU.add), reads=[ot, xb], writes=[ot])
    return kb.dma("sp", out_rows_ap, ot[:], reads=[ot])


def build_outproj(kb, NT, KD, og_in, w_out, g_post, x_in, h_out, ident, og_buf=None, x_buf=None, h_buf=None):
    nc = kb.nc
    KC = KD // 128
    Wout = kb.sb([128, KC, D], BF16, "Wo")
    for kc in range(KC):
        kb.dma("pool", Wout[:, kc, :], w_out[kc * 128:(kc + 1) * 128, :], writes=[Wout])
    Gpost = kb.sb([128, D], F32, "Gpost")
    kb.dma("sp", Gpost[:], g_post, writes=[Gpost])
    OGT = [kb.sb([128, KD], BF16, "ogt") for _ in range(2)]
    ogT = [kb.sb([128, KC, 128], BF16, "ogT") for _ in range(2)]
    XB = [kb.sb([128, D], F32, "xb") for _ in range(2)]
    OT = [kb.sb([128, D], F32, "ot") for _ in range(2)]
    SS = [kb.sb([128, 1], F32, "ss") for _ in range(2)]
    RS = [kb.sb([128, 1], F32, "rs") for _ in range(2)]
    junk = kb.sb([128, D], BF16, "junk")
    TP = [kb.ps([128, 512], F32, "tp") for _ in range(2)]
    PY = [kb.ps([128, D], F32, "py") for _ in range(2)]
    outs = []
    rd = [og_buf] if og_buf is not None else []
    rdx = [x_buf] if x_buf is not None else []
    for t in range(NT // 128):
        r0 = t * 128
        og = OGT[t % 2]; oT = ogT[t % 2]; py = PY[t % 2]
        kb.dma("sp", og[:], og_in[r0:r0 + 128, :], reads=rd, writes=[og])
        for kc in range(KC):
            tp = TP[(kc // 8) % 2]
            tpb = tp[:].bitcast(BF16).rearrange("p (a b) -> p a b", b=128)
            kb.op("pe", lambda: nc.tensor.transpose(out=tpb[:, kc % 8, :], in_=og[:, kc * 128:(kc + 1) * 128],
                                                    identity=ident[:]), reads=[og, ident], writes=[tp])
            if kc % 8 == 7:
                kb.op("act", lambda: nc.scalar.copy(out=oT[:, kc - 7:kc + 1, :], in_=tpb), reads=[tp], writes=[oT])
        for cb in range(2):
            for kc in range(KC):
                kb.op("pe", lambda: nc.tensor.matmul(out=py[:, cb * 512:(cb + 1) * 512], lhsT=oT[:, kc, :],
                                                     rhs=Wout[:, kc, cb * 512:(cb + 1) * 512],
                                                     start=(kc == 0), stop=(kc == KC - 1)), reads=[oT, Wout], writes=[py])
        if x_buf is not None:
            pass
        tok = post_norm_residual(kb, py, x_in[r0:r0 + 128, :], Gpost, h_out[r0:r0 + 128, :], XB[t % 2], OT[t % 2],
                                 SS[t % 2], RS[t % 2], junk)
        if h_buf is not None:
            h_buf.last_w = tok
        outs.append(tok)
    return outs


def build_normproj(kb, NT, x_in, g_pre, w, C, out_ap, ident, sig_from=None, sig_out=None, x_buf=None):
    nc = kb.nc
    KC = 8
    W = kb.sb([128, KC, C], BF16, "Wp")
    for kc in range(KC):
        kb.dma("pool", W[:, kc, :], w[kc * 128:(kc + 1) * 128, :], writes=[W])
    G = kb.sb([128, D], F32, "G")
    kb.dma("sp", G[:], g_pre, writes=[G])
    XA = [kb.sb([128, D], F32, "xa") for _ in range(2)]
    xn = kb.sb([128, D], BF16, "xn")
    xnT = kb.sb([128, KC, 128], BF16, "xnT")
    junk = kb.sb([128, D], BF16, "junk")
    ss = kb.sb([128, 1], F32, "ss"); rs = kb.sb([128, 1], F32, "rs")
    CM = C if sig_from is None else sig_from
    OB = [kb.sb([128, CM], BF16, "ob") for _ in range(2)]
    if sig_from is not None:
        SG = [kb.sb([128, C - sig_from], F32, "sgo") for _ in range(2)]
    tp = kb.ps([128, 512], F32, "tp")
    tpb = tp[:].bitcast(BF16).rearrange("p (a b) -> p a b", b=128)
    PB = [kb.ps([128, 512], F32, "pb") for _ in range(3)]
    outs = []
    nb = 0
    rdx = [x_buf] if x_buf is not None else []
    for t in range(NT // 128):
        r0 = t * 128
        xa = XA[t % 2]; ob = OB[t % 2]
        kb.dma("sp", xa[:], x_in[r0:r0 + 128, :], reads=rdx, writes=[xa])
        kb.op("dve", lambda: nc.vector.memset(ss[:], 0.0), writes=[ss])
        kb.op("act", lambda: nc.scalar.activation(out=junk[:], in_=xa[:], func=ACT.Square, scale=float(D) ** -0.5,
                                                  accum_out=ss[:, 0:1]), reads=[xa, ss], writes=[junk, ss])
        rstd_from_ms(kb, rs, ss)
        kb.op("dve", lambda: nc.vector.scalar_tensor_tensor(out=xn[:], in0=xa[:], scalar=rs[:, 0:1], in1=G[:],
                                                            op0=ALU.mult, op1=ALU.mult), reads=[xa, rs, G], writes=[xn])
        for kc in range(KC):
            kb.op("pe", lambda: nc.tensor.transpose(out=tpb[:, kc, :], in_=xn[:, kc * 128:(kc + 1) * 128],
                                                    identity=ident[:]), reads=[xn, ident], writes=[tp])
        kb.op("act", lambda: nc.scalar.copy(out=xnT[:], in_=tpb), reads=[tp], writes=[xnT])
        c0 = 0
        while c0 < C:
            cw = min(512, C - c0)
            if sig_from is not None and c0 < sig_from:
                cw = min(cw, sig_from - c0)
            pb = PB[nb % 3]; nb += 1
            for kc in range(KC):
                kb.op("pe", lambda: nc.tensor.matmul(out=pb[:, 0:cw], lhsT=xnT[:, kc, :], rhs=W[:, kc, c0:c0 + cw],
                                                     start=(kc == 0), stop=(kc == KC - 1)), reads=[xnT, W], writes=[pb])
            if sig_from is not None and c0 >= sig_from:
                sg = SG[t % 2]
                kb.op("act", lambda: nc.scalar.activation(out=sg[:, c0 - sig_from:c0 - sig_from + cw], in_=pb[:, 0:cw],
                                                          func=ACT.Sigmoid), reads=[pb], writes=[sg])
            else:
                kb.op("act", lambda: nc.scalar.copy(out=ob[:, c0:c0 + cw], in_=pb[:, 0:cw]), reads=[pb], writes=[ob])
            c0 += cw
        outs.append(kb.dma("sp", out_ap[r0:r0 + 128, :], ob[:], reads=[ob]))
        if sig_from is not None:
            outs.append(kb.dma("sp", sig_out[r0:r0 + 128, :], SG[t % 2][:], reads=[SG[t % 2]]))
    return outs


NEG = -30000.0
HD = 64; GR = 4; NG = 4


def build_compress(kb, NB, XT, W1, W2, PEcol, kcv_out):
    nc = kb.nc
    W1s = kb.sb([128, 2, 16, 256], BF16, "W1s")
    W2s = kb.sb([128, 2, 2, 64], BF16, "W2s")
    PEc = kb.sb([128, 2, 16], BF16, "PEc")
    for kv in range(2):
        kb.dma("pool", W1s[:, kv, :, :], W1[kv].rearrange("(c p) n -> p c n", p=128), writes=[W1s])
        kb.dma("pool", W2s[:, kv, :, :], W2[kv].rearrange("(c p) n -> p c n", p=128), writes=[W2s])
        kb.dma("pool", PEc[:, kv, :], PEcol[kv], writes=[PEc])
    pebs = kb.sb([128, 2, 2], F32, "pebs")
    pp = kb.ps([128, 512], F32, "pp")
    for kv in range(2):
        for hc in range(2):
            for cc in range(16):
                kb.op("pe", lambda: nc.tensor.matmul(out=pp[:, 0:1], lhsT=W1s[:, kv, cc, hc * 128:(hc + 1) * 128],
                                                     rhs=PEc[:, kv, cc:cc + 1], start=(cc == 0), stop=(cc == 15)),
                      reads=[W1s, PEc], writes=[pp])
            kb.op("dve", lambda: nc.vector.tensor_copy(out=pebs[:, kv, hc:hc + 1], in_=pp[:, 0:1]), reads=[pp], writes=[pebs])
    NW = min(128, NB)
    XTt = [kb.sb([128, 16, NW], BF16, "XTt") for _ in range(2)]
    hT = [kb.sb([128, 2, NW], BF16, "hT") for _ in range(2)]
    ob = [kb.sb([128, 64], BF16, "ob") for _ in range(2)]
    PH = [kb.ps([128, 512], F32, "ph") for _ in range(2)]
    PO = [kb.ps([128, 512], F32, "po") for _ in range(2)]
    outs = []
    it = 0
    for kv in range(2):
        for g in range(NG):
            for nt in range((NB + 127) // 128):
                xt = XTt[it % 2]; ht = hT[it % 2]; o = ob[it % 2]; ph = PH[it % 2]; po = PO[it % 2]; it += 1
                kb.dma("sp", xt[:], XT[kv, g, :, :, nt * 128:nt * 128 + NW].rearrange("c p n -> p c n"), writes=[xt])
                for hc in range(2):
                    for cc in range(16):
                        kb.op("pe", lambda: nc.tensor.matmul(out=ph[:, hc * 128:hc * 128 + NW],
                                                             lhsT=W1s[:, kv, cc, hc * 128:(hc + 1) * 128], rhs=xt[:, cc, :],
                                                             start=(cc == 0), stop=(cc == 15)), reads=[W1s, xt], writes=[ph])
                    kb.op("act", lambda: nc.scalar.activation(out=ht[:, hc, :], in_=ph[:, hc * 128:hc * 128 + NW],
                                                              func=ACT.Silu, bias=pebs[:, kv, hc:hc + 1]),
                          reads=[ph, pebs], writes=[ht])
                for hc in range(2):
                    kb.op("pe", lambda: nc.tensor.matmul(out=po[0:NW, 0:64], lhsT=ht[:, hc, :], rhs=W2s[:, kv, hc, :],
                                                         start=(hc == 0), stop=(hc == 1)), reads=[ht, W2s], writes=[po])
                kb.op("dve", lambda: nc.vector.tensor_copy(out=o[0:NW, :], in_=po[0:NW, 0:64]), reads=[po], writes=[o])
                outs.append(kb.dma("sp", kcv_out[kv, nt * 128:nt * 128 + NW, g, :], o[0:NW, :], reads=[o]))
    return outs


def build_nsa_attn(kb, NQT, CPB, qin, gates, A, o_out, identb, q_buf=None):
    nc = kb.nc
    NVT = NQT * CPB + CPB - 1
    NSB = (NVT + 15) // 16
    NBLK = NSB * 32
    NCT = (8 * NVT + 16 + 127) // 128
    NKV = NVT * 128
    rdq = [q_buf] if q_buf is not None else []
    identf = kb.sb([128, 128], F32, "identf")
    kb.dma("sp", identf[:], A["identf"], writes=[identf])
    Mmap = kb.sb([128, NCT, NBLK], BF16, "Mmap")
    kb.dma("pool", Mmap[:], A["Mmap"], writes=[Mmap])
    MNear = kb.sb([16, NQT, NBLK], BF16, "MNear")
    kb.dma("pool", MNear[:], A["MNear"], writes=[MNear])
    bmax = kb.sb([128, 1], F32, "bmax")
    rbf = kb.sb([128, 512], F32, "rbf")
    kb.dma("sp", rbf[:], A["relb_rep"], writes=[rbf])
    kb.op("dve", lambda: nc.vector.tensor_reduce(out=bmax[:], in_=rbf[:], axis=AX.X, op=ALU.max, apply_absolute_value=True),
          reads=[rbf], writes=[bmax])
    ones64 = kb.sb([64, 128], F32, "ones64")
    kb.op("dve", lambda: nc.vector.memset(ones64[:], 1.0), writes=[ones64])
    KsT = kb.sb([128, NKV], BF16, "KsT")
    Vs = kb.sb([128, NVT, 65], BF16, "Vs")
    KcT = kb.sb([128, NCT * 128], BF16, "KcT")
    Vc = kb.sb([128, NCT, 65], BF16, "Vc")
    VcN = kb.sb([16, NQT, 65], BF16, "VcN")
    TS = kb.sb([128, 2, 2, 512], BF16, "TS")
    TW = kb.sb([128, 5, 2, 512], BF16, "TW")
    TC = kb.sb([16, 2, 512], BF16, "TC")
    tf = kb.sb([128, 512], F32, "tf"); tr = kb.sb([128, 512], F32, "tr")
    kd = kb.sb([64, 3], F32, "kd"); kd1 = kb.sb([64, 1], F32, "kd1"); dg = kb.sb([64, 64], F32, "dg")
    KDrow = kb.sb([128, 64], F32, "KDrow")
    kwscr = kb.sb([64, 2048], BF16, "kwscr"); kdw = kb.sb([64, 16], F32, "kdw")
    b31 = kb.sb([128, 4], F32, "b31"); b31h = kb.sb([128, 4], BF16, "b31h"); b31r = kb.sb([128, 4], F32, "b31r")
    AUG1 = kb.sb([128, GR, 128], BF16, "AUG1")
    AUG2 = kb.sb([128, NSB, 128], BF16, "AUG2")
    QA = [kb.sb([128, 512], BF16, "QA%d" % i) for i in range(NSB)]
    QA0 = kb.sb([128, 512], BF16, "QA0")
    QT = [kb.sb([128, GR * HD], BF16, "qt") for _ in range(2)]
    GT = [kb.sb([128, 48], F32, "gt") for _ in range(2)]
    MS = [kb.sb([128, 3, NBLK], F32, "ms") for _ in range(2)]
    KwT = [kb.sb([128, 5 * 128], BF16, "KwT") for _ in range(2)]
    Vw = [kb.sb([128, 5, 65], BF16, "Vw") for _ in range(2)]
    absq = kb.sb([128, GR, HD], F32, "absq")
    U4 = kb.sb([128, GR], F32, "U4")
    PTc = kb.sb([128, NCT + 1, 512], BF16, "PTc")
    PTn = kb.sb([16, 512], BF16, "PTn")
    PTr = [kb.sb([128, 512], BF16, "PTr") for _ in range(3)]
    oT = kb.sb([65, 512], F32, "oT")
    otok = kb.sb([128, 3, GR, 65], F32, "otok")
    rinv = kb.sb([128, 3, GR], F32, "rinv")
    fb = kb.sb([128, 3, GR], F32, "fb")
    imp = kb.sb([128, NBLK], F32, "imp")
    imw = kb.sb([128, NBLK], F32, "imw")
    m8 = kb.sb([128, 8], F32, "m8"); thr = kb.sb([128, 1], F32, "thr")
    oacc = kb.sb([128, GR, HD], F32, "oacc"); otmp = kb.sb([128, GR, HD], F32, "otmp")
    OB = [kb.sb([128, GR * HD], BF16, "obo") for _ in range(2)]
    PSB = [kb.ps([128, 512], F32, "psS%d" % i) for i in range(3)]
    PO_ = [kb.ps([128, 512], F32, "psO%d" % i) for i in range(2)]
    PY = kb.ps([128, 512], F32, "psY")
    PQ = kb.ps([128, 512], F32, "psQ")
    PX = kb.ps([128, 512], F32, "psX")
    outs = []
    cnt = {"s": 0, "p": 0, "it": 0}

    def split_hilo(dst_hi, dst_lo, src_f32, rows):
        kb.op("dve", lambda: nc.vector.tensor_copy(out=dst_hi, in_=src_f32[0:rows, :]), reads=[tf], writes=[TS, TW, TC])
        kb.op("dve", lambda: nc.vector.tensor_copy(out=tr[0:rows, :], in_=dst_hi), reads=[TS, TW, TC], writes=[tr])
        kb.op("dve", lambda: nc.vector.tensor_tensor(out=dst_lo, in0=src_f32[0:rows, :], in1=tr[0:rows, :], op=ALU.subtract),
              reads=[tf, tr], writes=[TS, TW, TC])

    def softmax_tile(lhsT_ap, rhs_ap, KR, rows, toep=None, identrows=None):
        ps = PSB[cnt["s"] % 3]; cnt["s"] += 1
        kb.op("pe", lambda: nc.tensor.matmul(out=ps[0:rows, :], lhsT=lhsT_ap, rhs=rhs_ap, start=True, stop=(toep is None)),
              reads=[KsT, KcT, QA0] + QA + KwT, writes=[ps])
        if toep is not None:
            hi, lo = toep
            kb.op("pe", lambda: nc.tensor.matmul(out=ps[0:rows, :], lhsT=identb[0:rows, 0:rows], rhs=hi, start=False, stop=False),
                  reads=[identb, TS, TW, TC], writes=[ps])
            kb.op("pe", lambda: nc.tensor.matmul(out=ps[0:rows, :], lhsT=identb[0:rows, 0:rows], rhs=lo, start=False, stop=True),
                  reads=[identb, TS, TW, TC], writes=[ps])
        return ps

    for g in range(NG):
        kb.dma("sp", KsT[:], A["KsT"][g], writes=[KsT])
        kb.dma("sp", Vs[:], A["Vs"][g].rearrange("(t p) e -> p t e", p=128), writes=[Vs])
        kb.dma("sp", KcT[:], A["KcT"][g], writes=[KcT])
        kb.dma("sp", Vc[:], A["Vc"][g].rearrange("(t p) e -> p t e", p=128), writes=[Vc])
        kb.dma("sp", VcN[:], A["VcN"][g].rearrange("l u e -> u l e"), writes=[VcN])
        for m in range(2):
            kb.dma("sp", tf[:], A["TOEPS"][g, m], writes=[tf])
            split_hilo(TS[:, m, 0, :], TS[:, m, 1, :], tf, 128)
        for m in range(5):
            kb.dma("sp", tf[:], A["TOEPW"][g, m], writes=[tf])
            split_hilo(TW[:, m, 0, :], TW[:, m, 1, :], tf, 128)
        kb.dma("sp", tf[0:16, :], A["TOEPC"][g], writes=[tf])
        split_hilo(TC[:, 0, :], TC[:, 1, :], tf, 16)
        kb.dma("sp", b31[:], A["B31"][g], writes=[b31])
        kb.op("dve", lambda: nc.vector.tensor_copy(out=b31h[:], in_=b31[:]), reads=[b31], writes=[b31h])
        kb.op("dve", lambda: nc.vector.tensor_copy(out=b31r[:], in_=b31h[:]), reads=[b31h], writes=[b31r])
        kb.op("dve", lambda: nc.vector.tensor_tensor(out=b31r[:], in0=b31[:], in1=b31r[:], op=ALU.subtract),
              reads=[b31, b31r], writes=[b31r])
        kb.op("dve", lambda: nc.vector.memset(AUG1[:], 0.0), writes=[AUG1])
        kb.op("dve", lambda: nc.vector.memset(AUG2[:], 0.0), writes=[AUG2])
        kb.op("dve", lambda: nc.vector.memset(AUG1[:, :, 97:98], 1.0), writes=[AUG1])
        kb.op("dve", lambda: nc.vector.tensor_copy(out=AUG1[:, :, 98:99], in_=b31h[:].unsqueeze(2)), reads=[b31h], writes=[AUG1])
        kb.op("dve", lambda: nc.vector.tensor_copy(out=AUG1[:, :, 99:100], in_=b31r[:].unsqueeze(2)), reads=[b31r], writes=[AUG1])
        kb.op("dve", lambda: nc.vector.tensor_reduce(out=kd[:, 0:1], in_=KsT[0:64, :], axis=AX.X, op=ALU.max, apply_absolute_value=True),
              reads=[KsT], writes=[kd])
        kb.op("dve", lambda: nc.vector.tensor_reduce(out=kd[:, 1:2], in_=KcT[0:64, :], axis=AX.X, op=ALU.max, apply_absolute_value=True),
              reads=[KcT], writes=[kd])
        nch = (NKV + 2047) // 2048
        for c in range(nch):
            w = min(2048, NKV - c * 2048)
            kb.dma("sp", kwscr[:, 0:w], A["KwT"][g][0:64, c * 2048:c * 2048 + w], writes=[kwscr])
            kb.op("dve", lambda: nc.vector.tensor_reduce(out=kdw[:, c:c + 1], in_=kwscr[:, 0:w], axis=AX.X, op=ALU.max,
                                                         apply_absolute_value=True), reads=[kwscr], writes=[kdw])
        kb.op("dve", lambda: nc.vector.tensor_reduce(out=kd[:, 2:3], in_=kdw[:, 0:nch], axis=AX.X, op=ALU.max), reads=[kdw], writes=[kd])
        kb.op("dve", lambda: nc.vector.tensor_reduce(out=kd1[:], in_=kd[:], axis=AX.X, op=ALU.max), reads=[kd], writes=[kd1])
        kb.op("dve", lambda: nc.vector.tensor_scalar(out=dg[:], in0=identf[0:64, 0:64], scalar1=kd1[:, 0:1], scalar2=None, op0=ALU.mult),
              reads=[identf, kd1], writes=[dg])
        kb.op("pe", lambda: nc.tensor.matmul(out=PX[:, 0:64], lhsT=ones64[:], rhs=dg[:], start=True, stop=True),
              reads=[ones64, dg], writes=[PX])
        kb.op("dve", lambda: nc.vector.tensor_copy(out=KDrow[:], in_=PX[:, 0:64]), reads=[PX], writes=[KDrow])
        for l in range(NQT):
            V = CPB * l + CPB - 1
            it = cnt["it"]; cnt["it"] += 1
            qt = QT[it % 2]; gt = GT[it % 2]; ms = MS[it % 2]; kw = KwT[it % 2]; vw = Vw[it % 2]; ob = OB[it % 2]
            kb.dma("sp", qt[:], qin[l * 128:(l + 1) * 128, g * 256:(g + 1) * 256], reads=rdq, writes=[qt])
            kb.dma("sp", gt[:], gates[l * 128:(l + 1) * 128, :], reads=rdq, writes=[gt])
            kb.dma("sp", ms[:], A["MSEL"][l].rearrange("c q b -> q c b"), writes=[ms])
            wt0 = max(0, V - 4); nwt = V - wt0 + 1
            kb.dma("sp", kw[:, 0:nwt * 128], A["KwT"][g][:, wt0 * 128:(V + 1) * 128], writes=[kw])
            kb.dma("sp", vw[:, 0:nwt, :], A["Vw"][g][wt0 * 128:(V + 1) * 128, :].rearrange("(t p) e -> p t e", p=128), writes=[vw])
            qv = qt[:].rearrange("p (h d) -> p h d", h=GR)
            kb.op("act", lambda: nc.scalar.activation(out=AUG1[:, :, 0:HD], in_=qv, func=ACT.Copy, scale=HD ** -0.5),
                  reads=[qt], writes=[AUG1])
            kb.op("act", lambda: nc.scalar.activation(out=absq[:], in_=qv, func=ACT.Abs), reads=[qt], writes=[absq])
            kb.op("dve", lambda: nc.vector.tensor_tensor(out=absq[:], in0=absq[:], in1=KDrow[:].unsqueeze(1).broadcast_to([128, GR, HD]),
                                                         op=ALU.mult), reads=[absq, KDrow], writes=[absq])
            kb.op("dve", lambda: nc.vector.tensor_reduce(out=U4[:], in_=absq[:], axis=AX.X, op=ALU.add), reads=[absq], writes=[U4])
            kb.op("dve", lambda: nc.vector.tensor_scalar(out=U4[:], in0=U4[:], scalar1=-(HD ** -0.5), scalar2=bmax[:, 0:1],
                                                         op0=ALU.mult, op1=ALU.subtract), reads=[U4, bmax], writes=[U4])
            kb.op("dve", lambda: nc.vector.tensor_copy(out=AUG1[:, :, 96:97], in_=U4[:].unsqueeze(2)), reads=[U4], writes=[AUG1])
            for h in range(GR):
                kb.op("pe", lambda: nc.tensor.matmul(out=PQ[:, h * 128:(h + 1) * 128], lhsT=AUG1[:, h, :], rhs=identb[:],
                                                     start=True, stop=True), reads=[AUG1, identb], writes=[PQ])
            kb.op("act", lambda: nc.scalar.copy(out=QA0[:], in_=PQ[:]), reads=[PQ], writes=[QA0])
            nfar = 8 * V - 9
            oc = PO_[cnt["p"] % 2]; cnt["p"] += 1
            tiles = []
            n0 = 0
            while n0 < nfar:
                rows = min(128, nfar - n0)
                tiles.append((n0 // 128, rows))
                n0 += 128
            first = True
            for (tix, rows) in tiles:
                ps = softmax_tile(KcT[0:100, tix * 128:tix * 128 + rows], QA0[0:100, :], 100, rows)
                kb.op("act", lambda: nc.scalar.activation(out=PTc[0:rows, tix, :], in_=ps[0:rows, :], func=ACT.Exp),
                      reads=[ps], writes=[PTc])
                kb.op("pe", lambda: nc.tensor.matmul(out=oc[0:65, :], lhsT=Vc[0:rows, tix, :], rhs=PTc[0:rows, tix, :],
                                                     start=first, stop=False), reads=[Vc, PTc], writes=[oc])
                first = False
            ps = softmax_tile(KcT[0:98, nfar:nfar + 16], QA0[0:98, :], 98, 16, toep=(TC[:, 0, :], TC[:, 1, :]))
            kb.op("act", lambda: nc.scalar.activation(out=PTn[:], in_=ps[0:16, :], func=ACT.Exp), reads=[ps], writes=[PTn])
            kb.op("pe", lambda: nc.tensor.matmul(out=oc[0:65, :], lhsT=VcN[:, l, :], rhs=PTn[:], start=first, stop=True),
                  reads=[VcN, PTn], writes=[oc])

            def finish_branch(b, acc):
                kb.op("act", lambda: nc.scalar.copy(out=oT[:], in_=acc[0:65, :]), reads=[acc], writes=[oT])
                pxv = PX[:, 0:GR * 65].rearrange("p (h e) -> p h e", h=GR)
                for h in range(GR):
                    kb.op("pe", lambda: nc.tensor.matmul(out=pxv[:, h, :], lhsT=oT[:, h * 128:(h + 1) * 128], rhs=identf[0:65, 0:65],
                                                         start=True, stop=True), reads=[oT, identf], writes=[PX])
                kb.op("dve", lambda: nc.vector.tensor_copy(out=otok[:, b, :, :], in_=pxv), reads=[PX], writes=[otok])
                kb.op("dve", lambda: nc.vector.tensor_scalar(out=rinv[:, b, :], in0=otok[:, b, :, 64], scalar1=1e-30, scalar2=None,
                                                             op0=ALU.max), reads=[otok], writes=[rinv])
                kb.op("dve", lambda: nc.vector.reciprocal(out=rinv[:, b, :], in_=rinv[:, b, :]), reads=[rinv], writes=[rinv])

            finish_branch(0, oc)
            for h in range(GR):
                fst = True
                for (tix, rows) in tiles:
                    kb.op("pe", lambda: nc.tensor.matmul(out=PY[:, 0:NBLK], lhsT=PTc[0:rows, tix, h * 128:(h + 1) * 128],
                                                         rhs=Mmap[0:rows, tix, :], start=fst, stop=False),
                          reads=[PTc, Mmap], writes=[PY])
                    fst = False
                assert not fst
                kb.op("pe", lambda: nc.tensor.matmul(out=PY[:, 0:NBLK], lhsT=PTn[:, h * 128:(h + 1) * 128], rhs=MNear[:, l, :],
                                                     start=False, stop=True), reads=[PTn, MNear], writes=[PY])
                if h == 0:
                    kb.op("dve", lambda: nc.vector.tensor_scalar(out=imp[:], in0=PY[:, 0:NBLK], scalar1=rinv[:, 0, 0:1], scalar2=None,
                                                                 op0=ALU.mult), reads=[PY, rinv], writes=[imp])
                else:
                    kb.op("dve", lambda: nc.vector.scalar_tensor_tensor(out=imp[:], in0=PY[:, 0:NBLK], scalar=rinv[:, 0, h:h + 1],
                                                                        in1=imp[:], op0=ALU.mult, op1=ALU.add),
                          reads=[PY, rinv, imp], writes=[imp])
            kb.op("dve", lambda: nc.vector.tensor_tensor(out=imp[:], in0=imp[:], in1=ms[:, 0, :], op=ALU.mult), reads=[imp, ms], writes=[imp])
            kb.op("dve", lambda: nc.vector.tensor_tensor(out=imp[:], in0=imp[:], in1=ms[:, 1, :], op=ALU.add), reads=[imp, ms], writes=[imp])
            kb.op("dve", lambda: nc.vector.max(out=m8[:], in_=imp[:]), reads=[imp], writes=[m8])
            kb.op("dve", lambda: nc.vector.match_replace(out=imw[:], in_to_replace=m8[:], in_values=imp[:], imm_value=-3.0e38),
                  reads=[m8, imp], writes=[imw])
            kb.op("dve", lambda: nc.vector.max(out=m8[:], in_=imw[:]), reads=[imw], writes=[m8])
            kb.op("dve", lambda: nc.vector.tensor_reduce(out=thr[:], in_=m8[:], axis=AX.X, op=ALU.min), reads=[m8], writes=[thr])
            kb.op("dve", lambda: nc.vector.tensor_scalar(out=imw[:], in0=imp[:], scalar1=thr[:, 0:1], scalar2=None, op0=ALU.is_ge),
                  reads=[imp, thr], writes=[imw])
            kb.op("dve", lambda: nc.vector.tensor_tensor(out=imw[:], in0=imw[:], in1=ms[:, 2, :], op=ALU.mult), reads=[imw, ms], writes=[imw])
            kb.op("dve", lambda: nc.vector.tensor_scalar(out=AUG2[:, :, 64:96], in0=imw[:].rearrange("p (s j) -> p s j", j=32),
                                                         scalar1=-NEG, scalar2=NEG, op0=ALU.mult, op1=ALU.add),
                  reads=[imw], writes=[AUG2])
            nsb = (V + 1 + 15) // 16
            for sb in range(nsb):
                for h in range(GR):
                    kb.op("pe", lambda: nc.tensor.matmul(out=PQ[:, h * 128:(h + 1) * 128], lhsT=AUG1[:, h, :], rhs=identb[:],
                                                         start=True, stop=False), reads=[AUG1, identb], writes=[PQ])
                    kb.op("pe", lambda: nc.tensor.matmul(out=PQ[:, h * 128:(h + 1) * 128], lhsT=AUG2[:, sb, :], rhs=identb[:],
                                                         start=False, stop=True), reads=[AUG2, identb], writes=[PQ])
                kb.op("dve", lambda: nc.vector.tensor_copy(out=QA[sb][:], in_=PQ[:]), reads=[PQ], writes=[QA[sb]])
            osel = PO_[cnt["p"] % 2]; cnt["p"] += 1
            for v in range(V + 1):
                sb = v // 16
                near = v >= V - 1
                if near:
                    m = v - (V - 1)
                    ps = softmax_tile(KsT[0:98, v * 128:(v + 1) * 128], QA[sb][0:98, :], 98, 128, toep=(TS[:, m, 0, :], TS[:, m, 1, :]))
                else:
                    ps = softmax_tile(KsT[0:100, v * 128:(v + 1) * 128], QA[sb][0:100, :], 100, 128)
                pt = PTr[v % 3]
                kb.op("act", lambda: nc.scalar.activation(out=pt[:], in_=ps[:], func=ACT.Exp), reads=[ps], writes=[pt])
                kb.op("pe", lambda: nc.tensor.matmul(out=osel[0:65, :], lhsT=Vs[:, v, :], rhs=pt[:], start=(v == 0), stop=(v == V)),
                      reads=[Vs, pt], writes=[osel])
            finish_branch(1, osel)
            ow = PO_[cnt["p"] % 2]; cnt["p"] += 1
            for i in range(nwt):
                v = wt0 + i
                m = v - (V - 4)
                ps = softmax_tile(kw[0:98, i * 128:(i + 1) * 128], QA0[0:98, :], 98, 128, toep=(TW[:, m, 0, :], TW[:, m, 1, :]))
                pt = PTr[i % 3]
                kb.op("act", lambda: nc.scalar.activation(out=pt[:], in_=ps[:], func=ACT.Exp), reads=[ps], writes=[pt])
                kb.op("pe", lambda: nc.tensor.matmul(out=ow[0:65, :], lhsT=vw[:, i, :], rhs=pt[:], start=(i == 0), stop=(i == nwt - 1)),
                      reads=[vw, pt], writes=[ow])
            finish_branch(2, ow)
            gv = gt[:, g * 12:(g + 1) * 12].rearrange("p (r b) -> p b r", b=3)
            kb.op("dve", lambda: nc.vector.tensor_tensor(out=fb[:], in0=rinv[:], in1=gv, op=ALU.mult), reads=[rinv, gt], writes=[fb])
            for b in range(3):
                fbb = fb[:, b, :].unsqueeze(2).broadcast_to([128, GR, HD])
                dst = oacc if b == 0 else otmp
                kb.op("dve", lambda: nc.vector.tensor_tensor(out=dst[:], in0=otok[:, b, :, 0:HD], in1=fbb, op=ALU.mult),
                      reads=[otok, fb], writes=[dst])
                if b > 0:
                    kb.op("dve", lambda: nc.vector.tensor_tensor(out=oacc[:], in0=oacc[:], in1=otmp[:], op=ALU.add),
                          reads=[oacc, otmp], writes=[oacc])
            kb.op("dve", lambda: nc.vector.tensor_copy(out=ob[:].rearrange("p (h d) -> p h d", h=GR), in_=oacc[:]),
                  reads=[oacc], writes=[ob])
            outs.append(kb.dma("sp", o_out[l * 128:(l + 1) * 128, g * 256:(g + 1) * 256], ob[:], reads=[ob]))
    return outs

RH = 4; DK = 256; DV = 512
LOGG = np.log(1.0 - 2.0 ** (-5.0 - np.arange(RH, dtype=np.float64)))

def perm_ret_w_in(w):
    idx = []
    for blk in range(2):
        for h in range(RH):
            base = blk * 1024 + h * DK
            idx += [base + 2 * m for m in range(128)] + [base + 2 * m + 1 for m in range(128)]
    idx += list(range(2048, 6144))
    return np.ascontiguousarray(w[:, idx])

def ret_tables(pos):
    theta = (1.0 / (10000.0 ** np.linspace(0.0, 1.0, DK // 2, dtype=np.float32))).astype(np.float32)
    ang = pos.astype(np.float32)[:, None] * theta[None, :]
    cos = np.cos(ang).astype(np.float32); sin = np.sin(ang).astype(np.float32)
    jloc = (pos % 128).astype(np.float64)
    sc = (DK ** -0.5) * np.exp(-jloc[:, None] * LOGG[None, :])
    cosk = (cos[:, None, :].astype(np.float64) * sc[:, :, None]).astype(np.float32)
    sink = (sin[:, None, :].astype(np.float64) * sc[:, :, None]).astype(np.float32)
    return (np.ascontiguousarray(cos.T), np.ascontiguousarray(sin.T), np.ascontiguousarray(cosk),
            np.ascontiguousarray(sink))

def ret_consts():
    i = np.arange(128, dtype=np.float64)
    gi = np.exp(i[:, None] * LOGG[None, :]).astype(np.float32)
    g2i = np.exp(2 * i[:, None] * LOGG[None, :]).astype(np.float32)
    maskT = (np.arange(128)[None, :] >= np.arange(128)[:, None]).astype(np.float32)
    return gi, g2i, maskT

def state_to_dev(S):
    return np.ascontiguousarray(S.reshape(RH, 128, 2, DV).transpose(2, 1, 0, 3))

def state_from_dev(Sd):
    return np.ascontiguousarray(Sd.transpose(2, 1, 0, 3).reshape(RH, DK, DV))

def bcast128(v):
    return np.ascontiguousarray(np.broadcast_to(v.reshape(1, -1), (128, v.size))).astype(np.float32)

BF = ml_dtypes.bfloat16
NEG = -30000.0

def t5_bucket_np(dist):
    n = np.maximum(dist, 0).astype(np.int64)
    nf = np.maximum(n, 1).astype(np.float32)
    val = (np.log(nf / np.float32(16)) / np.float32(math.log(128 / 16))).astype(np.float32) * np.float32(16)
    large = 16 + val.astype(np.int32)
    large = np.minimum(large, 31)
    return np.where(n < 16, n, large).astype(np.int64)

def nsa_consts(NQT, CPB, rel_bias):
    NVT = NQT * CPB + CPB - 1
    NSB = (NVT + 15) // 16; NBLK = NSB * 32
    NCT = (8 * NVT + 16 + 127) // 128
    rb = np.asarray(rel_bias, np.float32)
    k = np.arange(128)[:, None]; q = np.arange(128)[None, :]
    TOEPS = np.zeros((4, 2, 128, 4, 128), np.float32)
    TOEPW = np.zeros((4, 5, 128, 4, 128), np.float32)
    TOEPC = np.zeros((4, 16, 4, 128), np.float32)
    for g in range(4):
        for r in range(4):
            h = g * 4 + r
            for m in range(2):
                d = 128 * (1 - m) + q - k
                TOEPS[g, m, :, r, :] = np.where(d >= 0, rb[t5_bucket_np(d), h], NEG)
            for m in range(5):
                d = 128 * (4 - m) + q - k
                TOEPW[g, m, :, r, :] = np.where((d >= 0) & (d < 512), rb[t5_bucket_np(d), h], NEG)
            u = np.arange(16)[:, None]; qq = np.arange(128)[None, :]
            d = qq + 113 - 16 * u
            TOEPC[g, :, r, :] = np.where(d >= 0, rb[t5_bucket_np(d), h], NEG)
    B31 = np.zeros((4, 128, 4), np.float32)
    for g in range(4):
        B31[g] = rb[31, g * 4:(g + 1) * 4][None, :]
    relb_rep = np.ascontiguousarray(np.broadcast_to(rb.reshape(1, 512), (128, 512)))
    npr = (np.arange(NCT)[None, :] * 128 + np.arange(128)[:, None])
    b = np.arange(NBLK)[None, None, :]
    Mmap = ((npr[:, :, None] >= 4 * b - 1) & (npr[:, :, None] <= 4 * b + 3)).astype(np.float32)
    MNear = np.zeros((16, NQT, NBLK), np.float32)
    for l in range(NQT):
        V = CPB * l + CPB - 1
        npn = 8 * V - 9 + np.arange(16)[:, None]
        bb = np.arange(NBLK)[None, :]
        MNear[:, l, :] = ((npn >= 4 * bb - 1) & (npn <= 4 * bb + 3))
    return dict(TOEPS=TOEPS.reshape(4, 2, 128, 512), TOEPW=TOEPW.reshape(4, 5, 128, 512), TOEPC=TOEPC.reshape(4, 16, 512),
                B31=B31, relb_rep=relb_rep, Mmap=Mmap, MNear=MNear, identf=np.eye(128, dtype=np.float32))

def nsa_core_inputs(NQT, CPB, j, S, kv_tok, kc, vc):
    shift = CPB - 1 - j
    NVT = NQT * CPB + CPB - 1
    NSB = (NVT + 15) // 16; NBLK = NSB * 32
    NCT = (8 * NVT + 16 + 127) // 128
    NKV = NVT * 128
    n_cmp = kc.shape[0]
    kvt = kv_tok.reshape(S, 6, 4, 64)
    KsT = np.zeros((4, 128, NKV), BF); KwT = np.zeros((4, 128, NKV), BF)
    Vs = np.zeros((4, NKV, 65), BF); Vw = np.zeros((4, NKV, 65), BF)
    KcT = np.zeros((4, 128, NCT * 128), BF); Vc = np.zeros((4, NCT * 128, 65), BF)
    t0 = 128 * shift
    tp = np.arange(NKV)
    ind = ((tp // 64) % 32)
    for g in range(4):
        KsT[g, 0:64, t0:t0 + S] = kvt[:, 2, g, :].T
        KwT[g, 0:64, t0:t0 + S] = kvt[:, 4, g, :].T
        Vs[g, t0:t0 + S, 0:64] = kvt[:, 3, g, :]
        Vw[g, t0:t0 + S, 0:64] = kvt[:, 5, g, :]
        Vs[g, :, 64] = 1.0; Vw[g, :, 64] = 1.0
        for jj in range(32):
            KsT[g, 64 + jj, :] = (ind == jj).astype(np.float32)
        KsT[g, 96] = 1.0; KsT[g, 98] = 1.0; KsT[g, 99] = 1.0
        KwT[g, 96] = 1.0
        KwT[g, 97, :t0] = NEG
        c0 = 8 * shift
        KcT[g, 0:64, c0:c0 + n_cmp] = kc[:, g, :].T
        Vc[g, c0:c0 + n_cmp, 0:64] = vc[:, g, :]
        Vc[g, :, 64] = 1.0
        KcT[g, 96] = 1.0; KcT[g, 98] = 1.0; KcT[g, 99] = 1.0
        KcT[g, 97, :] = NEG
        KcT[g, 97, c0:c0 + n_cmp] = 0.0
    VcN = np.zeros((4, NQT, 16, 65), BF)
    for l in range(NQT):
        V = CPB * l + CPB - 1
        VcN[:, l] = Vc[:, 8 * V - 9:8 * V + 7, :]
    MSEL = np.zeros((NQT, 3, 128, NBLK), np.float32)
    n_sel = S // 64
    for l in range(NQT):
        T = CPB * l + j
        t = 128 * T + np.arange(128)[:, None]
        b = np.arange(NBLK)[None, :] - 2 * shift
        valid = (b >= 0) & (b < n_sel) & (64 * b <= t)
        cur = t // 64
        f0 = (b == 0); f1 = (b == cur); f2 = (b == cur - 1)
        forced = (f0 | f1 | f2) & valid
        MSEL[l, 0] = (valid & ~forced)
        add = np.where(valid, 0.0, -1e30)
        add = np.where(f2 & valid, 1e30, add); add = np.where(f1 & valid, 2e30, add); add = np.where(f0 & valid, 3e30, add)
        MSEL[l, 1] = add
        MSEL[l, 2] = valid
    return dict(KsT=KsT, KwT=KwT, Vs=Vs, Vw=Vw, KcT=KcT, Vc=Vc, VcN=VcN, MSEL=MSEL)

def im2col(kvtok, n0, NB):
    S = kvtok.shape[0]
    idx = 16 * (n0 + np.arange(NB))[None, None, :] + 2 * np.arange(16)[:, None, None] + np.arange(2)[None, :, None]
    ok = idx < S
    g = kvtok[np.minimum(idx, S - 1)]
    g = np.where(ok[..., None, None, None], g, np.zeros((), kvtok.dtype))
    return np.ascontiguousarray(g.transpose(3, 4, 0, 1, 5, 2).reshape(2, 4, 16, 128, NB))

def pecol(pe):
    return np.ascontiguousarray(pe.reshape(2, 16, 2, 64).transpose(0, 2, 3, 1).reshape(2, 128, 16))


def pecol(pe):
    return np.ascontiguousarray(pe.reshape(2, 16, 2, 64).transpose(0, 2, 3, 1).reshape(2, 128, 16))

CPB = 4; FH = 2816


def _di(nc, n, s, dt=F32):
    return nc.dram_tensor(n, list(s), dt, kind="ExternalInput").ap()


def _do(nc, n, s, dt=F32):
    return nc.dram_tensor(n, list(s), dt, kind="ExternalOutput").ap()


def _dint(nc, n, s, dt=F32):
    return nc.dram_tensor(n, list(s), dt, kind="Internal").ap()


def _ret_inputs(nc, NT):
    return dict(x=_di(nc, "x", [NT, D]), w_in=_di(nc, "ret_w_in", [D, 6144]), g_pre=_di(nc, "g_mix_pre", [128, D]),
                cosT=_di(nc, "cosT", [128, NT]), sinT=_di(nc, "sinT", [128, NT]), cosk=_di(nc, "cosk", [NT, 4, 128]),
                sink=_di(nc, "sink", [NT, 4, 128]), gi=_di(nc, "gi", [128, 4]), g2i=_di(nc, "g2i", [128, 4]),
                maskT=_di(nc, "maskT", [128, 128]), ident=_di(nc, "ident", [128, 128]))


def prog_state(NT):
    nc = bass.Bass("TRN2", target_bir_lowering=False)
    a = _ret_inputs(nc, NT)
    s_out = _do(nc, "s_out", [2, 128, 4, 512])
    kb = KB(nc)
    idb = load_ident(kb, a["ident"])
    kb.begin_phase()
    outs = build_ret(kb, NT, a["x"], a["w_in"], a["g_pre"], a["cosT"], a["sinT"], a["cosk"], a["sink"], a["gi"], a["g2i"],
                     a["maskT"], None, None, None, s_out, idb, state_only=True)
    kb.end_phase()
    kb.finish(outs); kb.close()
    return nc


def prog_layer0(NT):
    nc = bass.Bass("TRN2", target_bir_lowering=False)
    a = _ret_inputs(nc, NT)
    s_slots = _di(nc, "s_slots", [3, 2, 128, 4, 512]); coef = _di(nc, "coef", [128, 12])
    ret_w_out = _di(nc, "ret_w_out", [2048, D]); g_mix_post = _di(nc, "g_mix_post", [128, D])
    fw_in = _di(nc, "ffn_w_in", [D, 2 * FH]); fw_out = _di(nc, "ffn_w_out", [FH, D])
    fg_pre = _di(nc, "g_ffn_pre", [128, D]); fg_post = _di(nc, "g_ffn_post", [128, D])
    kv_g = _di(nc, "g_kv", [128, D]); kv_w = _di(nc, "kv_w", [D, 1536])
    og = _dint(nc, "og_scr", [NT, 2048], BF16); hmid = _dint(nc, "hmid_scr", [NT, D])
    s_dummy = _dint(nc, "s_scr", [2, 128, 4, 512])
    h1 = _do(nc, "h1", [NT, D]); kv = _do(nc, "kv", [NT, 1536], BF16)
    kb = KB(nc)
    idb = load_ident(kb, a["ident"])
    kb.begin_phase()
    build_ret(kb, NT, a["x"], a["w_in"], a["g_pre"], a["cosT"], a["sinT"], a["cosk"], a["sink"], a["gi"], a["g2i"],
              a["maskT"], s_slots, coef, og, s_dummy, idb)
    kb.end_phase()
    kb.begin_phase()
    build_outproj(kb, NT, 2048, og, ret_w_out, g_mix_post, a["x"], hmid, idb)
    kb.end_phase()
    kb.begin_phase()
    o1 = build_ffn(kb, NT, hmid, fw_in, fw_out, fg_pre, fg_post, h1, idb)
    kb.end_phase()
    kb.begin_phase()
    o2 = build_normproj(kb, NT, h1, kv_g, kv_w, 1536, kv, idb)
    kb.end_phase()
    kb.finish(o1 + o2); kb.close()
    return nc


def prog_compress(NB=256):
    nc = bass.Bass("TRN2", target_bir_lowering=False)
    XT = _di(nc, "XT", (2, 4, 16, 128, NB), BF16); W1 = _di(nc, "W1", (2, 2048, 256)); W2 = _di(nc, "W2", (2, 256, 64))
    PEc = _di(nc, "PEc", (2, 128, 16))
    out = _do(nc, "kcv", [2, NB, 4, 64], BF16)
    kb = KB(nc)
    kb.begin_phase()
    outs = build_compress(kb, NB, XT, W1, W2, PEc, out)
    kb.end_phase()
    kb.finish(outs); kb.close()
    return nc


def prog_layer1(nqt, cpb=CPB):
    nc = bass.Bass("TRN2", target_bir_lowering=False)
    NT = nqt * 128
    NVT = nqt * cpb + cpb - 1; NSB = (NVT + 15) // 16; NBLK = NSB * 32; NCT = (8 * NVT + 16 + 127) // 128; NKV = NVT * 128
    h1 = _di(nc, "h1rr", [NT, D]); g_pre = _di(nc, "g_mix_pre", [128, D]); w_in = _di(nc, "nsa_w_in", [D, 1072])
    ident = _di(nc, "ident", [128, 128])
    A = {}
    A["KsT"] = _di(nc, "KsT", (4, 128, NKV), BF16); A["KwT"] = _di(nc, "KwT", (4, 128, NKV), BF16)
    A["Vs"] = _di(nc, "Vs", (4, NKV, 65), BF16); A["Vw"] = _di(nc, "Vw", (4, NKV, 65), BF16)
    A["KcT"] = _di(nc, "KcT", (4, 128, NCT * 128), BF16); A["Vc"] = _di(nc, "Vc", (4, NCT * 128, 65), BF16)
    A["VcN"] = _di(nc, "VcN", (4, nqt, 16, 65), BF16); A["MSEL"] = _di(nc, "MSEL", (nqt, 3, 128, NBLK))
    A["TOEPS"] = _di(nc, "TOEPS", (4, 2, 128, 512)); A["TOEPW"] = _di(nc, "TOEPW", (4, 5, 128, 512)); A["TOEPC"] = _di(nc, "TOEPC", (4, 16, 512))
    A["B31"] = _di(nc, "B31", (4, 128, 4)); A["relb_rep"] = _di(nc, "relb_rep", (128, 512)); A["Mmap"] = _di(nc, "Mmap", (128, NCT, NBLK))
    A["MNear"] = _di(nc, "MNear", (16, nqt, NBLK)); A["identf"] = _di(nc, "identf", (128, 128))
    w_out = _di(nc, "nsa_w_out", [1024, D]); g_post = _di(nc, "g_mix_post", [128, D])
    fw_in = _di(nc, "ffn_w_in", [D, 2 * FH]); fw_out = _di(nc, "ffn_w_out", [FH, D])
    fg_pre = _di(nc, "g_ffn_pre", [128, D]); fg_post = _di(nc, "g_ffn_post", [128, D])
    qin = _dint(nc, "q_scr", [NT, 1024], BF16); gates = _dint(nc, "gate_scr", [NT, 48])
    o = _dint(nc, "o_scr", [NT, 1024], BF16); hmid = _dint(nc, "hmid_scr", [NT, D])
    out = _do(nc, "out", [NT, D])
    kb = KB(nc)
    idb = load_ident(kb, ident)
    kb.begin_phase()
    build_normproj(kb, NT, h1, g_pre, w_in, 1072, qin, idb, sig_from=1024, sig_out=gates)
    kb.end_phase()
    kb.begin_phase()
    build_nsa_attn(kb, nqt, cpb, qin, gates, A, o, idb)
    kb.end_phase()
    kb.begin_phase()
    build_outproj(kb, NT, 1024, o, w_out, g_post, h1, hmid, idb)
    kb.end_phase()
    kb.begin_phase()
    outs = build_ffn(kb, NT, hmid, fw_in, fw_out, fg_pre, fg_post, out, idb)
    kb.end_phase()
    kb.finish(outs); kb.close()
    return nc


def _run(nc, in_maps):
    res = run_bass_kernel_spmd(nc, in_maps, core_ids=list(range(len(in_maps))))
    return res.results


def kernel(x, mix_norm_pre, mix_norm_post, ffn_norm_pre, ffn_norm_post, ffn_w_in, ffn_w_out, ret_w_in, ret_w_out,
           kv_norm, kv_w, cmp_pe_k, cmp_w1_k, cmp_w2_k, cmp_pe_v, cmp_w1_v, cmp_w2_v, nsa_w_in, nsa_w_out, rel_bias):
    f = lambda a: np.ascontiguousarray(np.asarray(a, dtype=np.float32))
    x = f(x)
    B, SEQ = x.shape[0], x.shape[1]
    NCORE = B * CPB; NTC = SEQ // CPB; NQT = NTC // 128
    assert NCORE <= 8 and NTC % 256 == 0
    eye = np.eye(128, dtype=np.float32)
    gi, g2i, maskT = ret_consts()
    w_in_p = perm_ret_w_in(f(ret_w_in)[0])
    base = []
    for c in range(NCORE):
        b, j = divmod(c, CPB)
        pos = np.arange(j * NTC, (j + 1) * NTC)
        cT, sT, ck, sk = ret_tables(pos)
        base.append({"x": np.ascontiguousarray(x[b, j * NTC:(j + 1) * NTC]), "ret_w_in": w_in_p,
                     "g_mix_pre": bcast128(f(mix_norm_pre)[0]), "cosT": cT, "sinT": sT, "cosk": ck, "sink": sk,
                     "gi": gi, "g2i": g2i, "maskT": maskT, "ident": eye})
    r1 = _run(prog_state(NTC), base)
    L = [np.asarray(r["s_out"], np.float32) for r in r1]
    gam128 = np.exp(128.0 * LOGG)
    ims = []
    for c in range(NCORE):
        b, j = divmod(c, CPB)
        slots = np.zeros((3, 2, 128, 4, 512), np.float32); coef = np.zeros((3, 4), np.float64)
        for i in range(j):
            slots[i] = L[b * CPB + i]
            coef[i] = gam128 ** (NQT * (j - 1 - i))
        im = dict(base[c])
        im.update({"s_slots": slots, "coef": bcast128(coef.reshape(-1).astype(np.float32)), "ret_w_out": f(ret_w_out)[0],
                   "g_mix_post": bcast128(f(mix_norm_post)[0]), "ffn_w_in": f(ffn_w_in)[0], "ffn_w_out": f(ffn_w_out)[0],
                   "g_ffn_pre": bcast128(f(ffn_norm_pre)[0]), "g_ffn_post": bcast128(f(ffn_norm_post)[0]),
                   "g_kv": bcast128(f(kv_norm)), "kv_w": f(kv_w)})
        ims.append(im)
    r2 = _run(prog_layer0(NTC), ims)
    h1 = np.stack([np.concatenate([np.asarray(r2[b * CPB + j]["h1"]) for j in range(CPB)]) for b in range(B)])
    kv = np.stack([np.concatenate([np.asarray(r2[b * CPB + j]["kv"]) for j in range(CPB)]) for b in range(B)])
    W1 = np.stack([f(cmp_w1_k), f(cmp_w1_v)]); W2 = np.stack([f(cmp_w2_k), f(cmp_w2_v)])
    PEc = pecol(np.stack([f(cmp_pe_k), f(cmp_pe_v)]))
    ims = []
    for c in range(NCORE):
        b, j = divmod(c, CPB)
        kcv_tok = kv[b].reshape(SEQ, 6, 4, 64)[:, 0:2]
        ims.append({"XT": im2col(kcv_tok, (NTC // 16) * j, NTC // 16), "W1": W1, "W2": W2, "PEc": PEc})
    r3 = _run(prog_compress(NTC // 16), ims)
    n_cmp = (SEQ - 32) // 16 + 1
    kcs = [np.concatenate([np.asarray(r3[b * CPB + j]["kcv"]) for j in range(CPB)], axis=1)[:, :n_cmp] for b in range(B)]
    consts = nsa_consts(NQT, CPB, f(rel_bias))
    ims = []; rows_all = []
    for c in range(NCORE):
        b, j = divmod(c, CPB)
        rows = np.concatenate([np.arange(128) + 128 * (CPB * l + j) for l in range(NQT)])
        rows_all.append(rows)
        im = nsa_core_inputs(NQT, CPB, j, SEQ, kv[b], kcs[b][0], kcs[b][1])
        im.update(consts)
        im.update({"h1rr": np.ascontiguousarray(h1[b][rows]), "g_mix_pre": bcast128(f(mix_norm_pre)[1]), "nsa_w_in": f(nsa_w_in)[0],
                   "ident": eye, "nsa_w_out": f(nsa_w_out)[0], "g_mix_post": bcast128(f(mix_norm_post)[1]),
                   "ffn_w_in": f(ffn_w_in)[1], "ffn_w_out": f(ffn_w_out)[1], "g_ffn_pre": bcast128(f(ffn_norm_pre)[1]),
                   "g_ffn_post": bcast128(f(ffn_norm_post)[1])})
        ims.append(im)
    r4 = _run(prog_layer1(NQT), ims)
    out = np.zeros((B, SEQ, D), np.float32)
    for c in range(NCORE):
        b, j = divmod(c, CPB)
        out[b, rows_all[c]] = np.asarray(r4[c]["out"])
    return out
```

```python
import math
import numpy as np
import ml_dtypes
from contextlib import ExitStack
import concourse.bass as bass
import concourse.mybir as mybir
from concourse.bass_utils import run_bass_kernel_spmd

F32 = mybir.dt.float32
BF16 = mybir.dt.bfloat16
ACT = mybir.ActivationFunctionType
ALU = mybir.AluOpType
AX = mybir.AxisListType

COMPUTE = ("pe", "dve", "act", "pool")


class Buf:
    def __init__(self, kb, t, name, dram=False):
        self.kb = kb
        self.t = t
        self.name = name
        self.dram = dram
        self.last_w = None
        self.reads = []
        self.sem = None
        self.semcnt = 0
        self.psum = False

    def __getitem__(self, idx):
        return self.t[idx]

    def dsem(self, q="sp"):
        if self.sem is None:
            self.sw = (q == "pool")
            if self.kb.sem_pool and not self.sw:
                self.sem, self.semcnt = self.kb.sem_pool.pop()
            else:
                self.sem = self.kb.new_sem("d")
            self.kb.phase_bufs.append(self)
        return self.sem


class KB:
    def __init__(self, nc, same_eng_sync=True):
        self.nc = nc
        self.es = ExitStack()
        self.engs = {"pe": nc.tensor, "dve": nc.vector, "act": nc.scalar,
                     "pool": nc.gpsimd, "sp": nc.sync}
        self.esem = {}
        self.ecnt = {}
        for e in COMPUTE:
            self.esem[e] = self.es.enter_context(nc.semaphore("e_" + e))
            self.ecnt[e] = 0
        self.waited = {e: {} for e in self.engs}
        self.same = same_eng_sync
        self.nsem = 4
        self.nbuf = 0
        self.ninstr = 0
        self.final_tokens = []
        self.cur = self.es
        self.sem_pool = []
        self.phase_bufs = []

    def new_sem(self, name):
        self.nsem += 1
        return self.es.enter_context(self.nc.semaphore(name + "_%d" % self.nsem))

    def sb(self, shape, dtype, name=None):
        self.nbuf += 1
        name = (name or "b") + "_%d" % self.nbuf
        t = self.cur.enter_context(self.nc.sbuf_tensor(name, list(shape), dtype))
        return Buf(self, t, name)

    def ps(self, shape, dtype, name=None):
        self.nbuf += 1
        name = (name or "p") + "_%d" % self.nbuf
        t = self.cur.enter_context(self.nc.psum_tensor(name, list(shape), dtype))
        b = Buf(self, t, name)
        b.psum = True
        return b

    def dram(self, ap, name):
        return Buf(self, ap, name, dram=True)

    def _wait(self, eng, tok):
        if tok is None:
            return
        sem, val, peng = tok
        if peng == eng and (eng == "pe" or not self.same):
            return
        key = id(sem)
        if self.waited[eng].get(key, 0) >= val:
            return
        self.engs[eng].wait_ge(sem, val)
        self.waited[eng][key] = val

    def _deps(self, eng, reads, writes):
        for b in reads:
            self._wait(eng, b.last_w)
            if b.psum:
                for r in self._compact(b.reads):
                    if r[2] != eng:
                        self._wait(eng, r)
        for b in writes:
            self._wait(eng, b.last_w)
            for r in self._compact(b.reads):
                self._wait(eng, r)

    def op(self, eng, fn, reads=(), writes=()):
        self._deps(eng, reads, writes)
        ins = fn()
        self.ecnt[eng] += 1
        ins.then_inc(self.esem[eng], 1)
        tok = (self.esem[eng], self.ecnt[eng], eng)
        for b in reads:
            b.reads.append(tok)
            if len(b.reads) > 24:
                b.reads = self._compact(b.reads)
        for b in writes:
            b.last_w = tok
            b.reads = []
        self.ninstr += 1
        return tok

    @staticmethod
    def _compact(reads):
        best = {}
        for (s, v, e) in reads:
            k = id(s)
            if k not in best or best[k][1] < v:
                best[k] = (s, v, e)
        return list(best.values())

    def dma(self, q, out_ap, in_ap, reads=(), writes=(), **kw):
        self._deps(q, reads, writes)
        owner = writes[0] if writes else reads[0]
        sem = owner.dsem(q)
        owner.semcnt += 16
        self.engs[q].dma_start(out=out_ap, in_=in_ap, **kw).then_inc(sem, 16)
        tok = (sem, owner.semcnt, "dma")
        for b in reads:
            b.reads.append(tok)
        for b in writes:
            b.last_w = tok
            b.reads = []
        self.ninstr += 1
        return tok

    def finish(self, toks, eng="sp"):
        for t in self._compact(toks):
            sem, val, _ = t
            key = id(sem)
            if self.waited[eng].get(key, 0) >= val:
                continue
            self.engs[eng].wait_ge(sem, val)
            self.waited[eng][key] = val

    def barrier(self):
        toks = [(self.esem[e], self.ecnt[e], "x") for e in COMPUTE if self.ecnt[e] > 0]
        toks += [(b.sem, b.semcnt, "dma") for b in self.phase_bufs if b.semcnt > 0]
        for e in self.engs:
            self.finish(toks, eng=e)

    def begin_phase(self):
        self.cur = ExitStack()
        self.phase_bufs = [b for b in self.phase_bufs if b.dram]

    def end_phase(self):
        self.barrier()
        for b in self.phase_bufs:
            if not b.dram and b.sem is not None:
                if not getattr(b, "sw", False):
                    self.sem_pool.append((b.sem, b.semcnt))
                b.sem = None
        self.phase_bufs = [b for b in self.phase_bufs if b.dram]
        self.cur.close()
        self.cur = self.es

    def close(self):
        self.es.close()


D = 1024; H = 2816; HC = 22; KC = 8

def load_ident(kb, ident_ap):
    idb = kb.sb([128, 128], BF16, "ident")
    kb.dma("pool", idb[:], ident_ap, writes=[idb])
    return idb

def rstd_from_ms(kb, rs, ms, eps=1e-6):
    nc = kb.nc
    kb.op("act", lambda: nc.scalar.activation(out=rs[:], in_=ms[:], func=ACT.Sqrt, bias=eps, scale=1.0),
          reads=[ms], writes=[rs])
    kb.op("dve", lambda: nc.vector.reciprocal(out=rs[:], in_=rs[:]), reads=[rs], writes=[rs])

def build_ffn(kb, NT, hin, w_in, w_out, g_pre, g_post, hout, ident, TT=256):
    nc = kb.nc
    NS = TT // 128
    Win = kb.sb([128, KC, 2 * H], BF16, "Win")
    Wout = kb.sb([128, HC, D], BF16, "Wout")
    Gpre = kb.sb([128, D], F32, "Gpre")
    Gpost = kb.sb([128, D], F32, "Gpost")
    for kc in range(KC):
        kb.dma("pool", Win[:, kc, :], w_in[kc * 128:(kc + 1) * 128, :], writes=[Win])
    for hc in range(HC):
        kb.dma("pool", Wout[:, hc, :], w_out[hc * 128:(hc + 1) * 128, :], writes=[Wout])
    kb.dma("sp", Gpre[:], g_pre, writes=[Gpre])
    kb.dma("sp", Gpost[:], g_post, writes=[Gpost])
    XA = [kb.sb([128, D], F32, "xa") for _ in range(2)]
    XB = [kb.sb([128, D], F32, "xb") for _ in range(2)]
    XN = [kb.sb([128, D], BF16, "xn") for _ in range(2)]
    OT = [kb.sb([128, D], F32, "ot") for _ in range(2)]
    junk = kb.sb([128, D], BF16, "junk")
    SS = [kb.sb([128, 1], F32, "ss") for _ in range(2)]
    RS = [kb.sb([128, 1], F32, "rs") for _ in range(2)]
    SS2 = [kb.sb([128, 1], F32, "ss2") for _ in range(2)]
    RS2 = [kb.sb([128, 1], F32, "rs2") for _ in range(2)]
    xnT = kb.sb([128, KC, TT], BF16, "xnT")
    hT = kb.sb([128, HC, TT], BF16, "hT")
    SG = [kb.sb([128, TT], F32, "sg") for _ in range(2)]
    tp = kb.ps([128, KC, 128], BF16, "tp")
    PG = [kb.ps([128, 2, TT], F32, "pg") for _ in range(3)]
    PY = [kb.ps([128, D], F32, "py") for _ in range(NS)]
    outs = []
    n = 0
    for ti in range(NT // TT):
        for s in range(NS):
            r0 = ti * TT + s * 128
            xa = XA[n % 2]; xn = XN[n % 2]; ss = SS[n % 2]; rs = RS[n % 2]
            n += 1
            kb.dma("sp", xa[:], hin[r0:r0 + 128, :], writes=[xa])
            kb.op("dve", lambda: nc.vector.memset(ss[:], 0.0), writes=[ss])
            kb.op("act", lambda: nc.scalar.activation(out=junk[:], in_=xa[:], func=ACT.Square, scale=float(D) ** -0.5,
                                                      accum_out=ss[:, 0:1]), reads=[xa, ss], writes=[junk, ss])
            rstd_from_ms(kb, rs, ss)
            kb.op("dve", lambda: nc.vector.scalar_tensor_tensor(out=xn[:], in0=xa[:], scalar=rs[:, 0:1], in1=Gpre[:],
                                                                op0=ALU.mult, op1=ALU.mult),
                  reads=[xa, rs, Gpre], writes=[xn])
            for kc in range(KC):
                kb.op("pe", lambda: nc.tensor.transpose(out=tp[:, kc, :], in_=xn[:, kc * 128:(kc + 1) * 128],
                                                        identity=ident[:]), reads=[xn, ident], writes=[tp])
            kb.op("act", lambda: nc.scalar.copy(out=xnT[:, :, s * 128:(s + 1) * 128], in_=tp[:]),
                  reads=[tp], writes=[xnT])
        for hc in range(HC):
            pg = PG[hc % 3]; sg = SG[hc % 2]
            for half in range(2):
                c0 = half * H + hc * 128
                for kc in range(KC):
                    kb.op("pe", lambda: nc.tensor.matmul(out=pg[:, half, :], lhsT=Win[:, kc, c0:c0 + 128],
                                                         rhs=xnT[:, kc, :], start=(kc == 0), stop=(kc == KC - 1)),
                          reads=[Win, xnT], writes=[pg])
            kb.op("act", lambda: nc.scalar.activation(out=sg[:], in_=pg[:, 0, :], func=ACT.Silu),
                  reads=[pg], writes=[sg])
            kb.op("dve", lambda: nc.vector.tensor_tensor(out=hT[:, hc, :], in0=sg[:], in1=pg[:, 1, :], op=ALU.mult),
                  reads=[sg, pg], writes=[hT])
        for s in range(NS):
            py = PY[s]
            for cb in range(2):
                for hc in range(HC):
                    kb.op("pe", lambda: nc.tensor.matmul(out=py[:, cb * 512:(cb + 1) * 512],
                                                         lhsT=hT[:, hc, s * 128:(s + 1) * 128],
                                                         rhs=Wout[:, hc, cb * 512:(cb + 1) * 512],
                                                         start=(hc == 0), stop=(hc == HC - 1)),
                          reads=[hT, Wout], writes=[py])
        for s in range(NS):
            r0 = ti * TT + s * 128
            py = PY[s]; xb = XB[s % 2]; ot = OT[s % 2]; ss2 = SS2[s % 2]; rs2 = RS2[s % 2]
            kb.dma("sp", xb[:], hin[r0:r0 + 128, :], writes=[xb])
            kb.op("dve", lambda: nc.vector.memset(ss2[:], 0.0), writes=[ss2])
            kb.op("act", lambda: nc.scalar.activation(out=junk[:], in_=py[:], func=ACT.Square, scale=float(D) ** -0.5,
                                                      accum_out=ss2[:, 0:1]), reads=[py, ss2], writes=[junk, ss2])
            rstd_from_ms(kb, rs2, ss2)
            kb.op("dve", lambda: nc.vector.scalar_tensor_tensor(out=ot[:], in0=py[:], scalar=rs2[:, 0:1], in1=Gpost[:],
                                                                op0=ALU.mult, op1=ALU.mult),
                  reads=[py, rs2, Gpost], writes=[ot])
            kb.op("pool", lambda: nc.gpsimd.tensor_tensor(out=ot[:], in0=ot[:], in1=xb[:], op=ALU.add),
                  reads=[ot, xb], writes=[ot])
            outs.append(kb.dma("sp", hout[r0:r0 + 128, :], ot[:], reads=[ot]))
    return outs


D = 1024
RH = 4; DK = 256; DV = 512
GAM = [1.0 - 2.0 ** (-5.0 - h) for h in range(RH)]


def build_ret(kb, NT, x_ap, w_in, g_pre, cosT, sinT, cosk, sink, gi, g2i, maskT_ap, s_slots, coef, og_out, s_out,
              ident, state_only=False, stage=9):
    nc = kb.nc
    NCH = NT // 128
    KC = 8
    if state_only:
        c_lo, c_hi = 1024, 4096
    else:
        c_lo, c_hi = 0, 6144
    WC = c_hi - c_lo
    Win = kb.sb([128, KC, WC], BF16, "Win")
    for kc in range(KC):
        for cc in range(0, WC, 1024):
            kb.dma("pool", Win[:, kc, cc:cc + 1024], w_in[kc * 128:(kc + 1) * 128, c_lo + cc:c_lo + cc + 1024],
                   writes=[Win])
    KCOL = 1024 - c_lo; VCOL = 2048 - c_lo; GCOL = 4096 - c_lo
    Gpre = kb.sb([128, D], F32, "Gpre")
    kb.dma("sp", Gpre[:], g_pre, writes=[Gpre])
    S = kb.sb([128, 2, RH, DV], F32, "S")
    if state_only:
        kb.op("dve", lambda: nc.vector.memset(S[:], 0.0), writes=[S])
    else:
        osb = kb.sb([128, RH, DV], F32, "osb")
        coef_sb = kb.sb([128, 3 * RH], F32, "coef")
        kb.dma("sp", coef_sb[:], coef, writes=[coef_sb])
        for dc in range(2):
            for slot in range(3):
                kb.dma("sp", osb[:], s_slots[slot, dc], writes=[osb])
                for h in range(RH):
                    cs = coef_sb[:, slot * RH + h:slot * RH + h + 1]
                    if slot == 0:
                        kb.op("dve", lambda: nc.vector.tensor_scalar(out=S[:, dc, h, :], in0=osb[:, h, :], scalar1=cs, scalar2=None,
                                                                     op0=ALU.mult), reads=[osb, coef_sb], writes=[S])
                    else:
                        kb.op("dve", lambda: nc.vector.scalar_tensor_tensor(out=S[:, dc, h, :], in0=osb[:, h, :], scalar=cs,
                                                                            in1=S[:, dc, h, :], op0=ALU.mult, op1=ALU.add),
                              reads=[osb, coef_sb, S], writes=[S])
    XA = [kb.sb([128, D], F32, "xa") for _ in range(2)]
    xn = kb.sb([128, D], BF16, "xn")
    xnT = kb.sb([128, KC, 128], BF16, "xnT")
    junk = kb.sb([128, D], BF16, "junk")
    ss = kb.sb([128, 1], F32, "ss"); rs = kb.sb([128, 1], F32, "rs")
    COSK = [kb.sb([128, RH, 128], F32, "cosk") for _ in range(2)]
    SINK = [kb.sb([128, RH, 128], F32, "sink") for _ in range(2)]
    kraw = kb.sb([128, 2, 2, 128], F32, "kraw")
    kt = kb.sb([128, RH, 2, 128], BF16, "kt")
    v_sb = kb.sb([128, RH, DV], BF16, "v")
    T = [kb.sb([128, 2, 128], F32, "t%d" % i) for i in range(4)]
    P = [kb.ps([128, 512], F32, "bank%d" % i) for i in range(8)]
    p0b = P[0][:].bitcast(BF16).rearrange("p (a b) -> p a b", b=128)
    if not state_only:
        Sb = kb.sb([128, 2, RH, DV], BF16, "Sb")
        for dc in range(2):
            for h in range(RH):
                kb.op("act", lambda: nc.scalar.activation(out=Sb[:, dc, h, :], in_=S[:, dc, h, :], func=ACT.Copy,
                                                          scale=GAM[h]), reads=[S], writes=[Sb])
        COST = [kb.sb([128, 128], F32, "cosT") for _ in range(2)]
        SINT = [kb.sb([128, 128], F32, "sinT") for _ in range(2)]
        GI = kb.sb([128, RH], F32, "gi"); G2I = kb.sb([128, RH], F32, "g2i")
        kb.dma("sp", GI[:], gi, writes=[GI]); kb.dma("sp", G2I[:], g2i, writes=[G2I])
        maskT = kb.sb([128, 128], F32, "maskT")
        kb.dma("sp", maskT[:], maskT_ap, writes=[maskT])
        qraw = kb.sb([128, 2, 2, 128], F32, "qraw")
        qT = kb.sb([128, RH, 2, 128], BF16, "qT")
        kT = kb.sb([128, RH, 2, 128], BF16, "kT")
        gs = kb.sb([128, RH * DV], BF16, "gs")
        PT = kb.sb([128, RH, 128], BF16, "PT")
        OG = [kb.sb([128, RH * DV], BF16, "og") for _ in range(2)]
        ms4 = kb.sb([128, RH], F32, "ms4"); f4 = kb.sb([128, RH], F32, "f4")
    outs = []
    bk = 0
    for c in range(NCH):
        r0 = c * 128
        xa = XA[c % 2]; cosk_t = COSK[c % 2]; sink_t = SINK[c % 2]
        kb.dma("sp", xa[:], x_ap[r0:r0 + 128, :], writes=[xa])
        kb.dma("sp", cosk_t[:], cosk[r0:r0 + 128], writes=[cosk_t])
        kb.dma("sp", sink_t[:], sink[r0:r0 + 128], writes=[sink_t])
        if not state_only:
            cosT_t = COST[c % 2]; sinT_t = SINT[c % 2]
            kb.dma("sp", cosT_t[:], cosT[:, r0:r0 + 128], writes=[cosT_t])
            kb.dma("sp", sinT_t[:], sinT[:, r0:r0 + 128], writes=[sinT_t])
        kb.op("dve", lambda: nc.vector.memset(ss[:], 0.0), writes=[ss])
        kb.op("act", lambda: nc.scalar.activation(out=junk[:], in_=xa[:], func=ACT.Square, scale=float(D) ** -0.5,
                                                  accum_out=ss[:, 0:1]), reads=[xa, ss], writes=[junk, ss])
        rstd_from_ms(kb, rs, ss)
        kb.op("dve", lambda: nc.vector.scalar_tensor_tensor(out=xn[:], in0=xa[:], scalar=rs[:, 0:1], in1=Gpre[:],
                                                            op0=ALU.mult, op1=ALU.mult),
              reads=[xa, rs, Gpre], writes=[xn])
        for kc in range(KC):
            kb.op("pe", lambda: nc.tensor.transpose(out=p0b[:, kc, :], in_=xn[:, kc * 128:(kc + 1) * 128],
                                                    identity=ident[:]), reads=[xn, ident], writes=[P[0]])
        kb.op("act", lambda: nc.scalar.copy(out=xnT[:], in_=p0b), reads=[P[0]], writes=[xnT])
        if not state_only:
            for hb in range(2):
                pq = P[1 + hb]
                pqv = pq[:].rearrange("p (a b t) -> p a b t", a=2, b=2)
                for hh in range(2):
                    h = 2 * hb + hh
                    for blk in range(2):
                        col0 = h * DK + blk * 128
                        for kc in range(KC):
                            kb.op("pe", lambda: nc.tensor.matmul(out=pqv[:, hh, blk, :], lhsT=Win[:, kc, col0:col0 + 128],
                                                                 rhs=xnT[:, kc, :], start=(kc == 0), stop=(kc == KC - 1)),
                                  reads=[Win, xnT], writes=[pq])
                kb.op("act", lambda: nc.scalar.copy(out=qraw[:].rearrange("p a b t -> p (a b t)"), in_=pq[:]),
                      reads=[pq], writes=[qraw])
                A = qraw[:, :, 0, :]; B = qraw[:, :, 1, :]
                cb = cosT_t[:].unsqueeze(1).broadcast_to([128, 2, 128])
                sb_ = sinT_t[:].unsqueeze(1).broadcast_to([128, 2, 128])
                kb.op("dve", lambda: nc.vector.tensor_tensor(out=T[0][:], in0=A, in1=cb, op=ALU.mult),
                      reads=[qraw, cosT_t], writes=[T[0]])
                kb.op("dve", lambda: nc.vector.tensor_tensor(out=T[1][:], in0=B, in1=sb_, op=ALU.mult),
                      reads=[qraw, sinT_t], writes=[T[1]])
                kb.op("dve", lambda: nc.vector.tensor_tensor(out=T[2][:], in0=A, in1=sb_, op=ALU.mult),
                      reads=[qraw, sinT_t], writes=[T[2]])
                kb.op("dve", lambda: nc.vector.tensor_tensor(out=T[3][:], in0=B, in1=cb, op=ALU.mult),
                      reads=[qraw, cosT_t], writes=[T[3]])
                kb.op("dve", lambda: nc.vector.tensor_tensor(out=qT[:, 2 * hb:2 * hb + 2, 0, :], in0=T[0][:], in1=T[1][:],
                                                             op=ALU.subtract), reads=[T[0], T[1]], writes=[qT])
                kb.op("dve", lambda: nc.vector.tensor_tensor(out=qT[:, 2 * hb:2 * hb + 2, 1, :], in0=T[2][:], in1=T[3][:],
                                                              op=ALU.add), reads=[T[2], T[3]], writes=[qT])
        for kb_ in range(2):
            pk = P[3 + bk % 2]; bk += 1
            for kc in range(KC):
                kb.op("pe", lambda: nc.tensor.matmul(out=pk[:], lhsT=xnT[:, kc, :],
                                                     rhs=Win[:, kc, KCOL + kb_ * 512:KCOL + (kb_ + 1) * 512],
                                                     start=(kc == 0), stop=(kc == KC - 1)),
                      reads=[Win, xnT], writes=[pk])
            kb.op("act", lambda: nc.scalar.copy(out=kraw[:].rearrange("p a b t -> p (a b t)"), in_=pk[:]),
                  reads=[pk], writes=[kraw])
            A = kraw[:, :, 0, :]; B = kraw[:, :, 1, :]
            ck = cosk_t[:, 2 * kb_:2 * kb_ + 2, :]; sk = sink_t[:, 2 * kb_:2 * kb_ + 2, :]
            kb.op("dve", lambda: nc.vector.tensor_tensor(out=T[0][:], in0=A, in1=ck, op=ALU.mult),
                  reads=[kraw, cosk_t], writes=[T[0]])
            kb.op("dve", lambda: nc.vector.tensor_tensor(out=T[1][:], in0=B, in1=sk, op=ALU.mult),
                  reads=[kraw, sink_t], writes=[T[1]])
            kb.op("dve", lambda: nc.vector.tensor_tensor(out=T[2][:], in0=A, in1=sk, op=ALU.mult),
                  reads=[kraw, sink_t], writes=[T[2]])
            kb.op("dve", lambda: nc.vector.tensor_tensor(out=T[3][:], in0=B, in1=ck, op=ALU.mult),
                  reads=[kraw, cosk_t], writes=[T[3]])
            kb.op("dve", lambda: nc.vector.tensor_tensor(out=kt[:, 2 * kb_:2 * kb_ + 2, 0, :], in0=T[0][:], in1=T[1][:],
                                                         op=ALU.subtract), reads=[T[0], T[1]], writes=[kt])
            kb.op("dve", lambda: nc.vector.tensor_tensor(out=kt[:, 2 * kb_:2 * kb_ + 2, 1, :], in0=T[2][:], in1=T[3][:],
                                                          op=ALU.add), reads=[T[2], T[3]], writes=[kt])
        for h in range(RH):
            pv = P[3 + bk % 2]; bk += 1
            for kc in range(KC):
                kb.op("pe", lambda: nc.tensor.matmul(out=pv[:], lhsT=xnT[:, kc, :],
                                                     rhs=Win[:, kc, VCOL + h * 512:VCOL + (h + 1) * 512],
                                                     start=(kc == 0), stop=(kc == KC - 1)),
                      reads=[Win, xnT], writes=[pv])
            kb.op("act", lambda: nc.scalar.copy(out=v_sb[:, h, :], in_=pv[:]), reads=[pv], writes=[v_sb])
        if not state_only:
            for h in range(RH if stage >= 2 else 0):
                pg = P[3 + bk % 2]; bk += 1
                for kc in range(KC):
                    kb.op("pe", lambda: nc.tensor.matmul(out=pg[:], lhsT=xnT[:, kc, :],
                                                         rhs=Win[:, kc, GCOL + h * 512:GCOL + (h + 1) * 512],
                                                         start=(kc == 0), stop=(kc == KC - 1)),
                          reads=[Win, xnT], writes=[pg])
                kb.op("act", lambda: nc.scalar.activation(out=gs[:, h * 512:(h + 1) * 512], in_=pg[:], func=ACT.Silu),
                      reads=[pg], writes=[gs])
            for h in range(RH if stage >= 2 else 0):
                for blk in range(2):
                    kb.op("pe", lambda: nc.tensor.transpose(out=p0b[:, h * 2 + blk, :], in_=kt[:, h, blk, :],
                                                            identity=ident[:]), reads=[kt, ident], writes=[P[0]])
            if stage >= 2:
                kb.op("dve", lambda: nc.vector.tensor_copy(out=kT[:].rearrange("p h b t -> p (h b) t"), in_=p0b),
                      reads=[P[0]], writes=[kT])
            p5 = P[5]; p5v = p5[:].rearrange("p (h t) -> p h t", h=RH)
            for h in range(RH if stage >= 3 else 0):
                for blk in range(2):
                    kb.op("pe", lambda: nc.tensor.matmul(out=p5v[:, h, :], lhsT=kT[:, h, blk, :], rhs=qT[:, h, blk, :],
                                                         start=(blk == 0), stop=(blk == 1)),
                          reads=[kT, qT], writes=[p5])
            if stage >= 3:
              kb.op("dve", lambda: nc.vector.tensor_tensor(out=PT[:], in0=p5v,
                                                         in1=maskT[:].unsqueeze(1).broadcast_to([128, RH, 128]),
                                                         op=ALU.mult), reads=[p5, maskT], writes=[PT])
            kb.op("dve", lambda: nc.vector.memset(ms4[:], 0.0), writes=[ms4])
            for h in range(RH if stage >= 4 else 0):
                po = P[6 + h % 2]
                kb.op("pe", lambda: nc.tensor.matmul(out=po[:], lhsT=PT[:, h, :], rhs=v_sb[:, h, :], start=True, stop=False),
                      reads=[PT, v_sb], writes=[po])
                kb.op("pe", lambda: nc.tensor.matmul(out=po[:], lhsT=qT[:, h, 0, :], rhs=Sb[:, 0, h, :], start=False, stop=False),
                      reads=[qT, Sb], writes=[po])
                kb.op("pe", lambda: nc.tensor.matmul(out=po[:], lhsT=qT[:, h, 1, :], rhs=Sb[:, 1, h, :], start=False, stop=True),
                      reads=[qT, Sb], writes=[po])
                kb.op("act", lambda: nc.scalar.activation(out=junk[:, 0:DV], in_=po[:], func=ACT.Square, scale=float(DV) ** -0.5,
                                                          accum_out=ms4[:, h:h + 1]), reads=[po, ms4], writes=[junk, ms4])
                kb.op("dve", lambda: nc.vector.tensor_copy(out=osb[:, h, :], in_=po[:]), reads=[po], writes=[osb])
        for h in range(RH):
            for dc in range(2):
                pkv = P[3 + bk % 2]; bk += 1
                kb.op("pe", lambda: nc.tensor.matmul(out=pkv[:], lhsT=kt[:, h, dc, :], rhs=v_sb[:, h, :], start=True, stop=True),
                      reads=[kt, v_sb], writes=[pkv])
                kb.op("act", lambda: nc.scalar.activation(out=S[:, dc, h, :], in_=S[:, dc, h, :], func=ACT.Copy,
                                                          scale=GAM[h] ** 128), reads=[S], writes=[S])
                kb.op("dve", lambda: nc.vector.scalar_tensor_tensor(out=S[:, dc, h, :], in0=pkv[:], scalar=GAM[h] ** 127,
                                                                    in1=S[:, dc, h, :], op0=ALU.mult, op1=ALU.add),
                      reads=[pkv, S], writes=[S])
                if not state_only:
                    kb.op("act", lambda: nc.scalar.activation(out=Sb[:, dc, h, :], in_=S[:, dc, h, :], func=ACT.Copy,
                                                              scale=GAM[h]), reads=[S], writes=[Sb])
        if not state_only and stage >= 5:
            og = OG[c % 2]
            kb.op("dve", lambda: nc.vector.tensor_tensor(out=f4[:], in0=ms4[:], in1=G2I[:], op=ALU.mult),
                  reads=[ms4, G2I], writes=[f4])
            rstd_from_ms(kb, f4, f4)
            kb.op("dve", lambda: nc.vector.tensor_tensor(out=f4[:], in0=f4[:], in1=GI[:], op=ALU.mult),
                  reads=[f4, GI], writes=[f4])
            for h in range(RH):
                kb.op("dve", lambda: nc.vector.scalar_tensor_tensor(out=og[:, h * DV:(h + 1) * DV], in0=osb[:, h, :], scalar=f4[:, h:h + 1],
                                                          in1=gs[:, h * DV:(h + 1) * DV], op0=ALU.mult, op1=ALU.mult),
                      reads=[osb, f4, gs], writes=[og])
            outs.append(kb.dma("sp", og_out[r0:r0 + 128, :], og[:], reads=[og]))
    for dc in range(2):
        outs.append(kb.dma("sp", s_out[dc], S[:, dc, :, :], reads=[S]))
    return outs

D = 1024

def post_norm_residual(kb, py, x_rows_ap, Gpost, out_rows_ap, xb, ot, ss2, rs2, junk):
    nc = kb.nc
    kb.dma("sp", xb[:], x_rows_ap, writes=[xb])
    kb.op("dve", lambda: nc.vector.memset(ss2[:], 0.0), writes=[ss2])
    kb.op("act", lambda: nc.scalar.activation(out=junk[:], in_=py[:], func=ACT.Square, scale=float(D) ** -0.5,
                                              accum_out=ss2[:, 0:1]), reads=[py, ss2], writes=[junk, ss2])
    rstd_from_ms(kb, rs2, ss2)
    kb.op("dve", lambda: nc.vector.scalar_tensor_tensor(out=ot[:], in0=py[:], scalar=rs2[:, 0:1], in1=Gpost[:],
                                                        op0=ALU.mult, op1=ALU.mult), reads=[py, rs2, Gpost], writes=[ot])
    kb.op("pool", lambda: nc.gpsimd.tensor_tensor(out=ot[:], in0=ot[:], in1=xb[:], op=ALU.add), reads=[ot, xb], writes=[ot])
    return kb.dma("sp", out_rows_ap, ot[:], reads=[ot])


def build_outproj(kb, NT, KD, og_in, w_out, g_post, x_in, h_out, ident, og_buf=None, x_buf=None, h_buf=None):
    nc = kb.nc
    KC = KD // 128
    Wout = kb.sb([128, KC, D], BF16, "Wo")
    for kc in range(KC):
        kb.dma("pool", Wout[:, kc, :], w_out[kc * 128:(kc + 1) * 128, :], writes=[Wout])
    Gpost = kb.sb([128, D], F32, "Gpost")
    kb.dma("sp", Gpost[:], g_post, writes=[Gpost])
    OGT = [kb.sb([128, KD], BF16, "ogt") for _ in range(2)]
    ogT = [kb.sb([128, KC, 128], BF16, "ogT") for _ in range(2)]
    XB = [kb.sb([128, D], F32, "xb") for _ in range(2)]
    OT = [kb.sb([128, D], F32, "ot") for _ in range(2)]
    SS = [kb.sb([128, 1], F32, "ss") for _ in range(2)]
    RS = [kb.sb([128, 1], F32, "rs") for _ in range(2)]
    junk = kb.sb([128, D], BF16, "junk")
    TP = [kb.ps([128, 512], F32, "tp") for _ in range(2)]
    PY = [kb.ps([128, D], F32, "py") for _ in range(2)]
    outs = []
    rd = [og_buf] if og_buf is not None else []
    rdx = [x_buf] if x_buf is not None else []
    for t in range(NT // 128):
        r0 = t * 128
        og = OGT[t % 2]; oT = ogT[t % 2]; py = PY[t % 2]
        kb.dma("sp", og[:], og_in[r0:r0 + 128, :], reads=rd, writes=[og])
        for kc in range(KC):
            tp = TP[(kc // 8) % 2]
            tpb = tp[:].bitcast(BF16).rearrange("p (a b) -> p a b", b=128)
            kb.op("pe", lambda: nc.tensor.transpose(out=tpb[:, kc % 8, :], in_=og[:, kc * 128:(kc + 1) * 128],
                                                    identity=ident[:]), reads=[og, ident], writes=[tp])
            if kc % 8 == 7:
                kb.op("act", lambda: nc.scalar.copy(out=oT[:, kc - 7:kc + 1, :], in_=tpb), reads=[tp], writes=[oT])
        for cb in range(2):
            for kc in range(KC):
                kb.op("pe", lambda: nc.tensor.matmul(out=py[:, cb * 512:(cb + 1) * 512], lhsT=oT[:, kc, :],
                                                     rhs=Wout[:, kc, cb * 512:(cb + 1) * 512],
                                                     start=(kc == 0), stop=(kc == KC - 1)), reads=[oT, Wout], writes=[py])
        if x_buf is not None:
            pass
        tok = post_norm_residual(kb, py, x_in[r0:r0 + 128, :], Gpost, h_out[r0:r0 + 128, :], XB[t % 2], OT[t % 2],
                                 SS[t % 2], RS[t % 2], junk)
        if h_buf is not None:
            h_buf.last_w = tok
        outs.append(tok)
    return outs


def build_normproj(kb, NT, x_in, g_pre, w, C, out_ap, ident, sig_from=None, sig_out=None, x_buf=None):
    nc = kb.nc
    KC = 8
    W = kb.sb([128, KC, C], BF16, "Wp")
    for kc in range(KC):
        kb.dma("pool", W[:, kc, :], w[kc * 128:(kc + 1) * 128, :], writes=[W])
    G = kb.sb([128, D], F32, "G")
    kb.dma("sp", G[:], g_pre, writes=[G])
    XA = [kb.sb([128, D], F32, "xa") for _ in range(2)]
    xn = kb.sb([128, D], BF16, "xn")
    xnT = kb.sb([128, KC, 128], BF16, "xnT")
    junk = kb.sb([128, D], BF16, "junk")
    ss = kb.sb([128, 1], F32, "ss"); rs = kb.sb([128, 1], F32, "rs")
    CM = C if sig_from is None else sig_from
    OB = [kb.sb([128, CM], BF16, "ob") for _ in range(2)]
    if sig_from is not None:
        SG = [kb.sb([128, C - sig_from], F32, "sgo") for _ in range(2)]
    tp = kb.ps([128, 512], F32, "tp")
    tpb = tp[:].bitcast(BF16).rearrange("p (a b) -> p a b", b=128)
    PB = [kb.ps([128, 512], F32, "pb") for _ in range(3)]
    outs = []
    nb = 0
    rdx = [x_buf] if x_buf is not None else []
    for t in range(NT // 128):
        r0 = t * 128
        xa = XA[t % 2]; ob = OB[t % 2]
        kb.dma("sp", xa[:], x_in[r0:r0 + 128, :], reads=rdx, writes=[xa])
        kb.op("dve", lambda: nc.vector.memset(ss[:], 0.0), writes=[ss])
        kb.op("act", lambda: nc.scalar.activation(out=junk[:], in_=xa[:], func=ACT.Square, scale=float(D) ** -0.5,
                                                  accum_out=ss[:, 0:1]), reads=[xa, ss], writes=[junk, ss])
        rstd_from_ms(kb, rs, ss)
        kb.op("dve", lambda: nc.vector.scalar_tensor_tensor(out=xn[:], in0=xa[:], scalar=rs[:, 0:1], in1=G[:],
                                                            op0=ALU.mult, op1=ALU.mult), reads=[xa, rs, G], writes=[xn])
        for kc in range(KC):
            kb.op("pe", lambda: nc.tensor.transpose(out=tpb[:, kc, :], in_=xn[:, kc * 128:(kc + 1) * 128],
                                                    identity=ident[:]), reads=[xn, ident], writes=[tp])
        kb.op("act", lambda: nc.scalar.copy(out=xnT[:], in_=tpb), reads=[tp], writes=[xnT])
        c0 = 0
        while c0 < C:
            cw = min(512, C - c0)
            if sig_from is not None and c0 < sig_from:
                cw = min(cw, sig_from - c0)
            pb = PB[nb % 3]; nb += 1
            for kc in range(KC):
                kb.op("pe", lambda: nc.tensor.matmul(out=pb[:, 0:cw], lhsT=xnT[:, kc, :], rhs=W[:, kc, c0:c0 + cw],
                                                     start=(kc == 0), stop=(kc == KC - 1)), reads=[xnT, W], writes=[pb])
            if sig_from is not None and c0 >= sig_from:
                sg = SG[t % 2]
                kb.op("act", lambda: nc.scalar.activation(out=sg[:, c0 - sig_from:c0 - sig_from + cw], in_=pb[:, 0:cw],
                                                          func=ACT.Sigmoid), reads=[pb], writes=[sg])
            else:
                kb.op("act", lambda: nc.scalar.copy(out=ob[:, c0:c0 + cw], in_=pb[:, 0:cw]), reads=[pb], writes=[ob])
            c0 += cw
        outs.append(kb.dma("sp", out_ap[r0:r0 + 128, :], ob[:], reads=[ob]))
        if sig_from is not None:
            outs.append(kb.dma("sp", sig_out[r0:r0 + 128, :], SG[t % 2][:], reads=[SG[t % 2]]))
    return outs


NEG = -30000.0
HD = 64; GR = 4; NG = 4


def build_compress(kb, NB, XT, W1, W2, PEcol, kcv_out):
    nc = kb.nc
    W1s = kb.sb([128, 2, 16, 256], BF16, "W1s")
    W2s = kb.sb([128, 2, 2, 64], BF16, "W2s")
    PEc = kb.sb([128, 2, 16], BF16, "PEc")
    for kv in range(2):
        kb.dma("pool", W1s[:, kv, :, :], W1[kv].rearrange("(c p) n -> p c n", p=128), writes=[W1s])
        kb.dma("pool", W2s[:, kv, :, :], W2[kv].rearrange("(c p) n -> p c n", p=128), writes=[W2s])
        kb.dma("pool", PEc[:, kv, :], PEcol[kv], writes=[PEc])
    pebs = kb.sb([128, 2, 2], F32, "pebs")
    pp = kb.ps([128, 512], F32, "pp")
    for kv in range(2):
        for hc in range(2):
            for cc in range(16):
                kb.op("pe", lambda: nc.tensor.matmul(out=pp[:, 0:1], lhsT=W1s[:, kv, cc, hc * 128:(hc + 1) * 128],
                                                     rhs=PEc[:, kv, cc:cc + 1], start=(cc == 0), stop=(cc == 15)),
                      reads=[W1s, PEc], writes=[pp])
            kb.op("dve", lambda: nc.vector.tensor_copy(out=pebs[:, kv, hc:hc + 1], in_=pp[:, 0:1]), reads=[pp], writes=[pebs])
    NW = min(128, NB)
    XTt = [kb.sb([128, 16, NW], BF16, "XTt") for _ in range(2)]
    hT = [kb.sb([128, 2, NW], BF16, "hT") for _ in range(2)]
    ob = [kb.sb([128, 64], BF16, "ob") for _ in range(2)]
    PH = [kb.ps([128, 512], F32, "ph") for _ in range(2)]
    PO = [kb.ps([128, 512], F32, "po") for _ in range(2)]
    outs = []
    it = 0
    for kv in range(2):
        for g in range(NG):
            for nt in range((NB + 127) // 128):
                xt = XTt[it % 2]; ht = hT[it % 2]; o = ob[it % 2]; ph = PH[it % 2]; po = PO[it % 2]; it += 1
                kb.dma("sp", xt[:], XT[kv, g, :, :, nt * 128:nt * 128 + NW].rearrange("c p n -> p c n"), writes=[xt])
                for hc in range(2):
                    for cc in range(16):
                        kb.op("pe", lambda: nc.tensor.matmul(out=ph[:, hc * 128:hc * 128 + NW],
                                                             lhsT=W1s[:, kv, cc, hc * 128:(hc + 1) * 128], rhs=xt[:, cc, :],
                                                             start=(cc == 0), stop=(cc == 15)), reads=[W1s, xt], writes=[ph])
                    kb.op("act", lambda: nc.scalar.activation(out=ht[:, hc, :], in_=ph[:, hc * 128:hc * 128 + NW],
                                                              func=ACT.Silu, bias=pebs[:, kv, hc:hc + 1]),
                          reads=[ph, pebs], writes=[ht])
                for hc in range(2):
                    kb.op("pe", lambda: nc.tensor.matmul(out=po[0:NW, 0:64], lhsT=ht[:, hc, :], rhs=W2s[:, kv, hc, :],
                                                         start=(hc == 0), stop=(hc == 1)), reads=[ht, W2s], writes=[po])
                kb.op("dve", lambda: nc.vector.tensor_copy(out=o[0:NW, :], in_=po[0:NW, 0:64]), reads=[po], writes=[o])
                outs.append(kb.dma("sp", kcv_out[kv, nt * 128:nt * 128 + NW, g, :], o[0:NW, :], reads=[o]))
    return outs


def build_nsa_attn(kb, NQT, CPB, qin, gates, A, o_out, identb, q_buf=None):
    nc = kb.nc
    NVT = NQT * CPB + CPB - 1
    NSB = (NVT + 15) // 16
    NBLK = NSB * 32
    NCT = (8 * NVT + 16 + 127) // 128
    NKV = NVT * 128
    rdq = [q_buf] if q_buf is not None else []
    identf = kb.sb([128, 128], F32, "identf")
    kb.dma("sp", identf[:], A["identf"], writes=[identf])
    Mmap = kb.sb([128, NCT, NBLK], BF16, "Mmap")
    kb.dma("pool", Mmap[:], A["Mmap"], writes=[Mmap])
    MNear = kb.sb([16, NQT, NBLK], BF16, "MNear")
    kb.dma("pool", MNear[:], A["MNear"], writes=[MNear])
    bmax = kb.sb([128, 1], F32, "bmax")
    rbf = kb.sb([128, 512], F32, "rbf")
    kb.dma("sp", rbf[:], A["relb_rep"], writes=[rbf])
    kb.op("dve", lambda: nc.vector.tensor_reduce(out=bmax[:], in_=rbf[:], axis=AX.X, op=ALU.max, apply_absolute_value=True),
          reads=[rbf], writes=[bmax])
    ones64 = kb.sb([64, 128], F32, "ones64")
    kb.op("dve", lambda: nc.vector.memset(ones64[:], 1.0), writes=[ones64])
    KsT = kb.sb([128, NKV], BF16, "KsT")
    Vs = kb.sb([128, NVT, 65], BF16, "Vs")
    KcT = kb.sb([128, NCT * 128], BF16, "KcT")
    Vc = kb.sb([128, NCT, 65], BF16, "Vc")
    VcN = kb.sb([16, NQT, 65], BF16, "VcN")
    TS = kb.sb([128, 2, 2, 512], BF16, "TS")
    TW = kb.sb([128, 5, 2, 512], BF16, "TW")
    TC = kb.sb([16, 2, 512], BF16, "TC")
    tf = kb.sb([128, 512], F32, "tf"); tr = kb.sb([128, 512], F32, "tr")
    kd = kb.sb([64, 3], F32, "kd"); kd1 = kb.sb([64, 1], F32, "kd1"); dg = kb.sb([64, 64], F32, "dg")
    KDrow = kb.sb([128, 64], F32, "KDrow")
    kwscr = kb.sb([64, 2048], BF16, "kwscr"); kdw = kb.sb([64, 16], F32, "kdw")
    b31 = kb.sb([128, 4], F32, "b31"); b31h = kb.sb([128, 4], BF16, "b31h"); b31r = kb.sb([128, 4], F32, "b31r")
    AUG1 = kb.sb([128, GR, 128], BF16, "AUG1")
    AUG2 = kb.sb([128, NSB, 128], BF16, "AUG2")
    QA = [kb.sb([128, 512], BF16, "QA%d" % i) for i in range(NSB)]
    QA0 = kb.sb([128, 512], BF16, "QA0")
    QT = [kb.sb([128, GR * HD], BF16, "qt") for _ in range(2)]
    GT = [kb.sb([128, 48], F32, "gt") for _ in range(2)]
    MS = [kb.sb([128, 3, NBLK], F32, "ms") for _ in range(2)]
    KwT = [kb.sb([128, 5 * 128], BF16, "KwT") for _ in range(2)]
    Vw = [kb.sb([128, 5, 65], BF16, "Vw") for _ in range(2)]
    absq = kb.sb([128, GR, HD], F32, "absq")
    U4 = kb.sb([128, GR], F32, "U4")
    PTc = kb.sb([128, NCT + 1, 512], BF16, "PTc")
    PTn = kb.sb([16, 512], BF16, "PTn")
    PTr = [kb.sb([128, 512], BF16, "PTr") for _ in range(3)]
    oT = kb.sb([65, 512], F32, "oT")
    otok = kb.sb([128, 3, GR, 65], F32, "otok")
    rinv = kb.sb([128, 3, GR], F32, "rinv")
    fb = kb.sb([128, 3, GR], F32, "fb")
    imp = kb.sb([128, NBLK], F32, "imp")
    imw = kb.sb([128, NBLK], F32, "imw")
    m8 = kb.sb([128, 8], F32, "m8"); thr = kb.sb([128, 1], F32, "thr")
    oacc = kb.sb([128, GR, HD], F32, "oacc"); otmp = kb.sb([128, GR, HD], F32, "otmp")
    OB = [kb.sb([128, GR * HD], BF16, "obo") for _ in range(2)]
    PSB = [kb.ps([128, 512], F32, "psS%d" % i) for i in range(3)]
    PO_ = [kb.ps([128, 512], F32, "psO%d" % i) for i in range(2)]
    PY = kb.ps([128, 512], F32, "psY")
    PQ = kb.ps([128, 512], F32, "psQ")
    PX = kb.ps([128, 512], F32, "psX")
    outs = []
    cnt = {"s": 0, "p": 0, "it": 0}

    def split_hilo(dst_hi, dst_lo, src_f32, rows):
        kb.op("dve", lambda: nc.vector.tensor_copy(out=dst_hi, in_=src_f32[0:rows, :]), reads=[tf], writes=[TS, TW, TC])
        kb.op("dve", lambda: nc.vector.tensor_copy(out=tr[0:rows, :], in_=dst_hi), reads=[TS, TW, TC], writes=[tr])
        kb.op("dve", lambda: nc.vector.tensor_tensor(out=dst_lo, in0=src_f32[0:rows, :], in1=tr[0:rows, :], op=ALU.subtract),
              reads=[tf, tr], writes=[TS, TW, TC])

    def softmax_tile(lhsT_ap, rhs_ap, KR, rows, toep=None, identrows=None, rd=None):
        ps = PSB[cnt["s"] % 3]; cnt["s"] += 1
        kb.op("pe", lambda: nc.tensor.matmul(out=ps[0:rows, :], lhsT=lhsT_ap, rhs=rhs_ap, start=True, stop=(toep is None)),
              reads=(rd if rd is not None else [KsT, KcT, QA0] + QA + KwT), writes=[ps])
        if toep is not None:
            hi, lo = toep
            kb.op("pe", lambda: nc.tensor.matmul(out=ps[0:rows, :], lhsT=identb[0:rows, 0:rows], rhs=hi, start=False, stop=False),
                  reads=[identb, TS, TW, TC], writes=[ps])
            kb.op("pe", lambda: nc.tensor.matmul(out=ps[0:rows, :], lhsT=identb[0:rows, 0:rows], rhs=lo, start=False, stop=True),
                  reads=[identb, TS, TW, TC], writes=[ps])
        return ps

    for g in range(NG):
        kb.dma("sp", KsT[:], A["KsT"][g], writes=[KsT])
        kb.dma("sp", Vs[:], A["Vs"][g].rearrange("(t p) e -> p t e", p=128), writes=[Vs])
        kb.dma("sp", KcT[:], A["KcT"][g], writes=[KcT])
        kb.dma("sp", Vc[:], A["Vc"][g].rearrange("(t p) e -> p t e", p=128), writes=[Vc])
        kb.dma("sp", VcN[:], A["VcN"][g].rearrange("l u e -> u l e"), writes=[VcN])
        for m in range(2):
            kb.dma("sp", tf[:], A["TOEPS"][g, m], writes=[tf])
            split_hilo(TS[:, m, 0, :], TS[:, m, 1, :], tf, 128)
        for m in range(5):
            kb.dma("sp", tf[:], A["TOEPW"][g, m], writes=[tf])
            split_hilo(TW[:, m, 0, :], TW[:, m, 1, :], tf, 128)
        kb.dma("sp", tf[0:16, :], A["TOEPC"][g], writes=[tf])
        split_hilo(TC[:, 0, :], TC[:, 1, :], tf, 16)
        kb.dma("sp", b31[:], A["B31"][g], writes=[b31])
        kb.op("dve", lambda: nc.vector.tensor_copy(out=b31h[:], in_=b31[:]), reads=[b31], writes=[b31h])
        kb.op("dve", lambda: nc.vector.tensor_copy(out=b31r[:], in_=b31h[:]), reads=[b31h], writes=[b31r])
        kb.op("dve", lambda: nc.vector.tensor_tensor(out=b31r[:], in0=b31[:], in1=b31r[:], op=ALU.subtract),
              reads=[b31, b31r], writes=[b31r])
        kb.op("dve", lambda: nc.vector.memset(AUG1[:], 0.0), writes=[AUG1])
        kb.op("dve", lambda: nc.vector.memset(AUG2[:], 0.0), writes=[AUG2])
        kb.op("dve", lambda: nc.vector.memset(AUG1[:, :, 97:98], 1.0), writes=[AUG1])
        kb.op("dve", lambda: nc.vector.tensor_copy(out=AUG1[:, :, 98:99], in_=b31h[:].unsqueeze(2)), reads=[b31h], writes=[AUG1])
        kb.op("dve", lambda: nc.vector.tensor_copy(out=AUG1[:, :, 99:100], in_=b31r[:].unsqueeze(2)), reads=[b31r], writes=[AUG1])
        kb.op("dve", lambda: nc.vector.tensor_reduce(out=kd[:, 0:1], in_=KsT[0:64, :], axis=AX.X, op=ALU.max, apply_absolute_value=True),
              reads=[KsT], writes=[kd])
        kb.op("dve", lambda: nc.vector.tensor_reduce(out=kd[:, 1:2], in_=KcT[0:64, :], axis=AX.X, op=ALU.max, apply_absolute_value=True),
              reads=[KcT], writes=[kd])
        nch = (NKV + 2047) // 2048
        for c in range(nch):
            w = min(2048, NKV - c * 2048)
            kb.dma("sp", kwscr[:, 0:w], A["KwT"][g][0:64, c * 2048:c * 2048 + w], writes=[kwscr])
            kb.op("dve", lambda: nc.vector.tensor_reduce(out=kdw[:, c:c + 1], in_=kwscr[:, 0:w], axis=AX.X, op=ALU.max,
                                                         apply_absolute_value=True), reads=[kwscr], writes=[kdw])
        kb.op("dve", lambda: nc.vector.tensor_reduce(out=kd[:, 2:3], in_=kdw[:, 0:nch], axis=AX.X, op=ALU.max), reads=[kdw], writes=[kd])
        kb.op("dve", lambda: nc.vector.tensor_reduce(out=kd1[:], in_=kd[:], axis=AX.X, op=ALU.max), reads=[kd], writes=[kd1])
        kb.op("dve", lambda: nc.vector.tensor_scalar(out=dg[:], in0=identf[0:64, 0:64], scalar1=kd1[:, 0:1], scalar2=None, op0=ALU.mult),
              reads=[identf, kd1], writes=[dg])
        kb.op("pe", lambda: nc.tensor.matmul(out=PX[:, 0:64], lhsT=ones64[:], rhs=dg[:], start=True, stop=True),
              reads=[ones64, dg], writes=[PX])
        kb.op("dve", lambda: nc.vector.tensor_copy(out=KDrow[:], in_=PX[:, 0:64]), reads=[PX], writes=[KDrow])
        for l in range(NQT):
            V = CPB * l + CPB - 1
            it = cnt["it"]; cnt["it"] += 1
            qt = QT[it % 2]; gt = GT[it % 2]; ms = MS[it % 2]; kw = KwT[it % 2]; vw = Vw[it % 2]; ob = OB[it % 2]
            kb.dma("sp", qt[:], qin[l * 128:(l + 1) * 128, g * 256:(g + 1) * 256], reads=rdq, writes=[qt])
            kb.dma("sp", gt[:], gates[l * 128:(l + 1) * 128, :], reads=rdq, writes=[gt])
            kb.dma("sp", ms[:], A["MSEL"][l].rearrange("c q b -> q c b"), writes=[ms])
            wt0 = max(0, V - 4); nwt = V - wt0 + 1
            kb.dma("sp", kw[:, 0:nwt * 128], A["KwT"][g][:, wt0 * 128:(V + 1) * 128], writes=[kw])
            kb.dma("sp", vw[:, 0:nwt, :], A["Vw"][g][wt0 * 128:(V + 1) * 128, :].rearrange("(t p) e -> p t e", p=128), writes=[vw])
            qv = qt[:].rearrange("p (h d) -> p h d", h=GR)
            kb.op("act", lambda: nc.scalar.activation(out=AUG1[:, :, 0:HD], in_=qv, func=ACT.Copy, scale=HD ** -0.5),
                  reads=[qt], writes=[AUG1])
            kb.op("act", lambda: nc.scalar.activation(out=absq[:], in_=qv, func=ACT.Abs), reads=[qt], writes=[absq])
            kb.op("dve", lambda: nc.vector.tensor_tensor(out=absq[:], in0=absq[:], in1=KDrow[:].unsqueeze(1).broadcast_to([128, GR, HD]),
                                                         op=ALU.mult), reads=[absq, KDrow], writes=[absq])
            kb.op("dve", lambda: nc.vector.tensor_reduce(out=U4[:], in_=absq[:], axis=AX.X, op=ALU.add), reads=[absq], writes=[U4])
            kb.op("dve", lambda: nc.vector.tensor_scalar(out=U4[:], in0=U4[:], scalar1=-(HD ** -0.5), scalar2=bmax[:, 0:1],
                                                         op0=ALU.mult, op1=ALU.subtract), reads=[U4, bmax], writes=[U4])
            kb.op("dve", lambda: nc.vector.tensor_copy(out=AUG1[:, :, 96:97], in_=U4[:].unsqueeze(2)), reads=[U4], writes=[AUG1])
            for h in range(GR):
                kb.op("pe", lambda: nc.tensor.matmul(out=PQ[:, h * 128:(h + 1) * 128], lhsT=AUG1[:, h, :], rhs=identb[:],
                                                     start=True, stop=True), reads=[AUG1, identb], writes=[PQ])
            kb.op("act", lambda: nc.scalar.copy(out=QA0[:], in_=PQ[:]), reads=[PQ], writes=[QA0])
            nfar = 8 * V - 9
            oc = PO_[cnt["p"] % 2]; cnt["p"] += 1
            tiles = []
            n0 = 0
            while n0 < nfar:
                rows = min(128, nfar - n0)
                tiles.append((n0 // 128, rows))
                n0 += 128
            first = True
            for (tix, rows) in tiles:
                ps = softmax_tile(KcT[0:100, tix * 128:tix * 128 + rows], QA0[0:100, :], 100, rows)
                kb.op("act", lambda: nc.scalar.activation(out=PTc[0:rows, tix, :], in_=ps[0:rows, :], func=ACT.Exp),
                      reads=[ps], writes=[PTc])
                kb.op("pe", lambda: nc.tensor.matmul(out=oc[0:65, :], lhsT=Vc[0:rows, tix, :], rhs=PTc[0:rows, tix, :],
                                                     start=first, stop=False), reads=[Vc, PTc], writes=[oc])
                first = False
            ps = softmax_tile(KcT[0:98, nfar:nfar + 16], QA0[0:98, :], 98, 16, toep=(TC[:, 0, :], TC[:, 1, :]))
            kb.op("act", lambda: nc.scalar.activation(out=PTn[:], in_=ps[0:16, :], func=ACT.Exp), reads=[ps], writes=[PTn])
            kb.op("pe", lambda: nc.tensor.matmul(out=oc[0:65, :], lhsT=VcN[:, l, :], rhs=PTn[:], start=first, stop=True),
                  reads=[VcN, PTn], writes=[oc])

            def finish_branch(b, acc):
                kb.op("act", lambda: nc.scalar.copy(out=oT[:], in_=acc[0:65, :]), reads=[acc], writes=[oT])
                pxv = PX[:, 0:GR * 65].rearrange("p (h e) -> p h e", h=GR)
                for h in range(GR):
                    kb.op("pe", lambda: nc.tensor.matmul(out=pxv[:, h, :], lhsT=oT[:, h * 128:(h + 1) * 128], rhs=identf[0:65, 0:65],
                                                         start=True, stop=True), reads=[oT, identf], writes=[PX])
                kb.op("dve", lambda: nc.vector.tensor_copy(out=otok[:, b, :, :], in_=pxv), reads=[PX], writes=[otok])
                kb.op("dve", lambda: nc.vector.tensor_scalar(out=rinv[:, b, :], in0=otok[:, b, :, 64], scalar1=1e-30, scalar2=None,
                                                             op0=ALU.max), reads=[otok], writes=[rinv])
                kb.op("dve", lambda: nc.vector.reciprocal(out=rinv[:, b, :], in_=rinv[:, b, :]), reads=[rinv], writes=[rinv])

            finish_branch(0, oc)
            for h in range(GR):
                fst = True
                for (tix, rows) in tiles:
                    kb.op("pe", lambda: nc.tensor.matmul(out=PY[:, 0:NBLK], lhsT=PTc[0:rows, tix, h * 128:(h + 1) * 128],
                                                         rhs=Mmap[0:rows, tix, :], start=fst, stop=False),
                          reads=[PTc, Mmap], writes=[PY])
                    fst = False
                assert not fst
                kb.op("pe", lambda: nc.tensor.matmul(out=PY[:, 0:NBLK], lhsT=PTn[:, h * 128:(h + 1) * 128], rhs=MNear[:, l, :],
                                                     start=False, stop=True), reads=[PTn, MNear], writes=[PY])
                if h == 0:
                    kb.op("dve", lambda: nc.vector.tensor_scalar(out=imp[:], in0=PY[:, 0:NBLK], scalar1=rinv[:, 0, 0:1], scalar2=None,
                                                                 op0=ALU.mult), reads=[PY, rinv], writes=[imp])
                else:
                    kb.op("dve", lambda: nc.vector.scalar_tensor_tensor(out=imp[:], in0=PY[:, 0:NBLK], scalar=rinv[:, 0, h:h + 1],
                                                                        in1=imp[:], op0=ALU.mult, op1=ALU.add),
                          reads=[PY, rinv, imp], writes=[imp])
            kb.op("dve", lambda: nc.vector.tensor_tensor(out=imp[:], in0=imp[:], in1=ms[:, 0, :], op=ALU.mult), reads=[imp, ms], writes=[imp])
            kb.op("dve", lambda: nc.vector.tensor_tensor(out=imp[:], in0=imp[:], in1=ms[:, 1, :], op=ALU.add), reads=[imp, ms], writes=[imp])
            kb.op("dve", lambda: nc.vector.max(out=m8[:], in_=imp[:]), reads=[imp], writes=[m8])
            kb.op("dve", lambda: nc.vector.match_replace(out=imw[:], in_to_replace=m8[:], in_values=imp[:], imm_value=-3.0e38),
                  reads=[m8, imp], writes=[imw])
            kb.op("dve", lambda: nc.vector.max(out=m8[:], in_=imw[:]), reads=[imw], writes=[m8])
            kb.op("dve", lambda: nc.vector.tensor_reduce(out=thr[:], in_=m8[:], axis=AX.X, op=ALU.min), reads=[m8], writes=[thr])
            kb.op("dve", lambda: nc.vector.tensor_scalar(out=imw[:], in0=imp[:], scalar1=thr[:, 0:1], scalar2=None, op0=ALU.is_ge),
                  reads=[imp, thr], writes=[imw])
            kb.op("dve", lambda: nc.vector.tensor_tensor(out=imw[:], in0=imw[:], in1=ms[:, 2, :], op=ALU.mult), reads=[imw, ms], writes=[imw])
            kb.op("dve", lambda: nc.vector.tensor_scalar(out=AUG2[:, :, 64:96], in0=imw[:].rearrange("p (s j) -> p s j", j=32),
                                                         scalar1=-NEG, scalar2=NEG, op0=ALU.mult, op1=ALU.add),
                  reads=[imw], writes=[AUG2])
            nsb = (V + 1 + 15) // 16
            for sb in range(nsb):
                for h in range(GR):
                    kb.op("pe", lambda: nc.tensor.matmul(out=PQ[:, h * 128:(h + 1) * 128], lhsT=AUG1[:, h, :], rhs=identb[:],
                                                         start=True, stop=False), reads=[AUG1, identb], writes=[PQ])
                    kb.op("pe", lambda: nc.tensor.matmul(out=PQ[:, h * 128:(h + 1) * 128], lhsT=AUG2[:, sb, :], rhs=identb[:],
                                                         start=False, stop=True), reads=[AUG2, identb], writes=[PQ])
                kb.op("dve", lambda: nc.vector.tensor_copy(out=QA[sb][:], in_=PQ[:]), reads=[PQ], writes=[QA[sb]])
            osel = PO_[cnt["p"] % 2]; cnt["p"] += 1

            def sel_tail(v, ps):
                pt = PTr[v % 3]
                kb.op("act", lambda: nc.scalar.activation(out=pt[:], in_=ps[:], func=ACT.Exp), reads=[ps], writes=[pt])
                kb.op("pe", lambda: nc.tensor.matmul(out=osel[0:65, :], lhsT=Vs[:, v, :], rhs=pt[:], start=(v == 0), stop=(v == V)),
                      reads=[Vs, pt], writes=[osel])

            pend = None
            for v in range(V + 1):
                sb = v // 16
                near = v >= V - 1
                if near:
                    m = v - (V - 1)
                    ps = softmax_tile(KsT[0:98, v * 128:(v + 1) * 128], QA[sb][0:98, :], 98, 128, toep=(TS[:, m, 0, :], TS[:, m, 1, :]))
                else:
                    ps = softmax_tile(KsT[0:100, v * 128:(v + 1) * 128], QA[sb][0:100, :], 100, 128)
                if pend is not None:
                    sel_tail(*pend)
                pend = (v, ps)
            sel_tail(*pend)
            finish_branch(1, osel)
            ow = PO_[cnt["p"] % 2]; cnt["p"] += 1

            def win_tail(i, ps):
                pt = PTr[i % 3]
                kb.op("act", lambda: nc.scalar.activation(out=pt[:], in_=ps[:], func=ACT.Exp), reads=[ps], writes=[pt])
                kb.op("pe", lambda: nc.tensor.matmul(out=ow[0:65, :], lhsT=vw[:, i, :], rhs=pt[:], start=(i == 0), stop=(i == nwt - 1)),
                      reads=[vw, pt], writes=[ow])

            pend = None
            for i in range(nwt):
                v = wt0 + i
                m = v - (V - 4)
                ps = softmax_tile(kw[0:98, i * 128:(i + 1) * 128], QA0[0:98, :], 98, 128, toep=(TW[:, m, 0, :], TW[:, m, 1, :]))
                if pend is not None:
                    win_tail(*pend)
                pend = (i, ps)
            win_tail(*pend)
            finish_branch(2, ow)
            gv = gt[:, g * 12:(g + 1) * 12].rearrange("p (r b) -> p b r", b=3)
            kb.op("dve", lambda: nc.vector.tensor_tensor(out=fb[:], in0=rinv[:], in1=gv, op=ALU.mult), reads=[rinv, gt], writes=[fb])
            for b in range(3):
                fbb = fb[:, b, :].unsqueeze(2).broadcast_to([128, GR, HD])
                dst = oacc if b == 0 else otmp
                kb.op("dve", lambda: nc.vector.tensor_tensor(out=dst[:], in0=otok[:, b, :, 0:HD], in1=fbb, op=ALU.mult),
                      reads=[otok, fb], writes=[dst])
                if b > 0:
                    kb.op("dve", lambda: nc.vector.tensor_tensor(out=oacc[:], in0=oacc[:], in1=otmp[:], op=ALU.add),
                          reads=[oacc, otmp], writes=[oacc])
            kb.op("dve", lambda: nc.vector.tensor_copy(out=ob[:].rearrange("p (h d) -> p h d", h=GR), in_=oacc[:]),
                  reads=[oacc], writes=[ob])
            outs.append(kb.dma("sp", o_out[l * 128:(l + 1) * 128, g * 256:(g + 1) * 256], ob[:], reads=[ob]))
    return outs

RH = 4; DK = 256; DV = 512
LOGG = np.log(1.0 - 2.0 ** (-5.0 - np.arange(RH, dtype=np.float64)))

def perm_ret_w_in(w):
    idx = []
    for blk in range(2):
        for h in range(RH):
            base = blk * 1024 + h * DK
            idx += [base + 2 * m for m in range(128)] + [base + 2 * m + 1 for m in range(128)]
    idx += list(range(2048, 6144))
    return np.ascontiguousarray(w[:, idx])

def ret_tables(pos):
    theta = (1.0 / (10000.0 ** np.linspace(0.0, 1.0, DK // 2, dtype=np.float32))).astype(np.float32)
    ang = pos.astype(np.float32)[:, None] * theta[None, :]
    cos = np.cos(ang).astype(np.float32); sin = np.sin(ang).astype(np.float32)
    jloc = (pos % 128).astype(np.float64)
    sc = (DK ** -0.5) * np.exp(-jloc[:, None] * LOGG[None, :])
    cosk = (cos[:, None, :].astype(np.float64) * sc[:, :, None]).astype(np.float32)
    sink = (sin[:, None, :].astype(np.float64) * sc[:, :, None]).astype(np.float32)
    return (np.ascontiguousarray(cos.T), np.ascontiguousarray(sin.T), np.ascontiguousarray(cosk),
            np.ascontiguousarray(sink))

def ret_consts():
    i = np.arange(128, dtype=np.float64)
    gi = np.exp(i[:, None] * LOGG[None, :]).astype(np.float32)
    g2i = np.exp(2 * i[:, None] * LOGG[None, :]).astype(np.float32)
    maskT = (np.arange(128)[None, :] >= np.arange(128)[:, None]).astype(np.float32)
    return gi, g2i, maskT

def state_to_dev(S):
    return np.ascontiguousarray(S.reshape(RH, 128, 2, DV).transpose(2, 1, 0, 3))

def state_from_dev(Sd):
    return np.ascontiguousarray(Sd.transpose(2, 1, 0, 3).reshape(RH, DK, DV))

def bcast128(v):
    return np.ascontiguousarray(np.broadcast_to(v.reshape(1, -1), (128, v.size))).astype(np.float32)

BF = ml_dtypes.bfloat16
NEG = -30000.0

def t5_bucket_np(dist):
    n = np.maximum(dist, 0).astype(np.int64)
    nf = np.maximum(n, 1).astype(np.float32)
    val = (np.log(nf / np.float32(16)) / np.float32(math.log(128 / 16))).astype(np.float32) * np.float32(16)
    large = 16 + val.astype(np.int32)
    large = np.minimum(large, 31)
    return np.where(n < 16, n, large).astype(np.int64)

def nsa_consts(NQT, CPB, rel_bias):
    NVT = NQT * CPB + CPB - 1
    NSB = (NVT + 15) // 16; NBLK = NSB * 32
    NCT = (8 * NVT + 16 + 127) // 128
    rb = np.asarray(rel_bias, np.float32)
    k = np.arange(128)[:, None]; q = np.arange(128)[None, :]
    TOEPS = np.zeros((4, 2, 128, 4, 128), np.float32)
    TOEPW = np.zeros((4, 5, 128, 4, 128), np.float32)
    TOEPC = np.zeros((4, 16, 4, 128), np.float32)
    for g in range(4):
        for r in range(4):
            h = g * 4 + r
            for m in range(2):
                d = 128 * (1 - m) + q - k
                TOEPS[g, m, :, r, :] = np.where(d >= 0, rb[t5_bucket_np(d), h], NEG)
            for m in range(5):
                d = 128 * (4 - m) + q - k
                TOEPW[g, m, :, r, :] = np.where((d >= 0) & (d < 512), rb[t5_bucket_np(d), h], NEG)
            u = np.arange(16)[:, None]; qq = np.arange(128)[None, :]
            d = qq + 113 - 16 * u
            TOEPC[g, :, r, :] = np.where(d >= 0, rb[t5_bucket_np(d), h], NEG)
    B31 = np.zeros((4, 128, 4), np.float32)
    for g in range(4):
        B31[g] = rb[31, g * 4:(g + 1) * 4][None, :]
    relb_rep = np.ascontiguousarray(np.broadcast_to(rb.reshape(1, 512), (128, 512)))
    npr = (np.arange(NCT)[None, :] * 128 + np.arange(128)[:, None])
    b = np.arange(NBLK)[None, None, :]
    Mmap = ((npr[:, :, None] >= 4 * b - 1) & (npr[:, :, None] <= 4 * b + 3)).astype(np.float32)
    MNear = np.zeros((16, NQT, NBLK), np.float32)
    for l in range(NQT):
        V = CPB * l + CPB - 1
        npn = 8 * V - 9 + np.arange(16)[:, None]
        bb = np.arange(NBLK)[None, :]
        MNear[:, l, :] = ((npn >= 4 * bb - 1) & (npn <= 4 * bb + 3))
    return dict(TOEPS=TOEPS.reshape(4, 2, 128, 512), TOEPW=TOEPW.reshape(4, 5, 128, 512), TOEPC=TOEPC.reshape(4, 16, 512),
                B31=B31, relb_rep=relb_rep, Mmap=Mmap, MNear=MNear, identf=np.eye(128, dtype=np.float32))

def nsa_core_inputs(NQT, CPB, j, S, kv_tok, kc, vc):
    shift = CPB - 1 - j
    NVT = NQT * CPB + CPB - 1
    NSB = (NVT + 15) // 16; NBLK = NSB * 32
    NCT = (8 * NVT + 16 + 127) // 128
    NKV = NVT * 128
    n_cmp = kc.shape[0]
    kvt = kv_tok.reshape(S, 6, 4, 64)
    KsT = np.zeros((4, 128, NKV), BF); KwT = np.zeros((4, 128, NKV), BF)
    Vs = np.zeros((4, NKV, 65), BF); Vw = np.zeros((4, NKV, 65), BF)
    KcT = np.zeros((4, 128, NCT * 128), BF); Vc = np.zeros((4, NCT * 128, 65), BF)
    t0 = 128 * shift
    tp = np.arange(NKV)
    ind = ((tp // 64) % 32)
    for g in range(4):
        KsT[g, 0:64, t0:t0 + S] = kvt[:, 2, g, :].T
        KwT[g, 0:64, t0:t0 + S] = kvt[:, 4, g, :].T
        Vs[g, t0:t0 + S, 0:64] = kvt[:, 3, g, :]
        Vw[g, t0:t0 + S, 0:64] = kvt[:, 5, g, :]
        Vs[g, :, 64] = 1.0; Vw[g, :, 64] = 1.0
        for jj in range(32):
            KsT[g, 64 + jj, :] = (ind == jj).astype(np.float32)
        KsT[g, 96] = 1.0; KsT[g, 98] = 1.0; KsT[g, 99] = 1.0
        KwT[g, 96] = 1.0
        KwT[g, 97, :t0] = NEG
        c0 = 8 * shift
        KcT[g, 0:64, c0:c0 + n_cmp] = kc[:, g, :].T
        Vc[g, c0:c0 + n_cmp, 0:64] = vc[:, g, :]
        Vc[g, :, 64] = 1.0
        KcT[g, 96] = 1.0; KcT[g, 98] = 1.0; KcT[g, 99] = 1.0
        KcT[g, 97, :] = NEG
        KcT[g, 97, c0:c0 + n_cmp] = 0.0
    VcN = np.zeros((4, NQT, 16, 65), BF)
    for l in range(NQT):
        V = CPB * l + CPB - 1
        VcN[:, l] = Vc[:, 8 * V - 9:8 * V + 7, :]
    MSEL = np.zeros((NQT, 3, 128, NBLK), np.float32)
    n_sel = S // 64
    for l in range(NQT):
        T = CPB * l + j
        t = 128 * T + np.arange(128)[:, None]
        b = np.arange(NBLK)[None, :] - 2 * shift
        valid = (b >= 0) & (b < n_sel) & (64 * b <= t)
        cur = t // 64
        f0 = (b == 0); f1 = (b == cur); f2 = (b == cur - 1)
        forced = (f0 | f1 | f2) & valid
        MSEL[l, 0] = (valid & ~forced)
        add = np.where(valid, 0.0, -1e30)
        add = np.where(f2 & valid, 1e30, add); add = np.where(f1 & valid, 2e30, add); add = np.where(f0 & valid, 3e30, add)
        MSEL[l, 1] = add
        MSEL[l, 2] = valid
    return dict(KsT=KsT, KwT=KwT, Vs=Vs, Vw=Vw, KcT=KcT, Vc=Vc, VcN=VcN, MSEL=MSEL)

def im2col(kvtok, n0, NB):
    S = kvtok.shape[0]
    idx = 16 * (n0 + np.arange(NB))[None, None, :] + 2 * np.arange(16)[:, None, None] + np.arange(2)[None, :, None]
    ok = idx < S
    g = kvtok[np.minimum(idx, S - 1)]
    g = np.where(ok[..., None, None, None], g, np.zeros((), kvtok.dtype))
    return np.ascontiguousarray(g.transpose(3, 4, 0, 1, 5, 2).reshape(2, 4, 16, 128, NB))

def pecol(pe):
    return np.ascontiguousarray(pe.reshape(2, 16, 2, 64).transpose(0, 2, 3, 1).reshape(2, 128, 16))


def pecol(pe):
    return np.ascontiguousarray(pe.reshape(2, 16, 2, 64).transpose(0, 2, 3, 1).reshape(2, 128, 16))

CPB = 4; FH = 2816


def _di(nc, n, s, dt=F32):
    return nc.dram_tensor(n, list(s), dt, kind="ExternalInput").ap()


def _do(nc, n, s, dt=F32):
    return nc.dram_tensor(n, list(s), dt, kind="ExternalOutput").ap()


def _dint(nc, n, s, dt=F32):
    return nc.dram_tensor(n, list(s), dt, kind="Internal").ap()


def _ret_inputs(nc, NT):
    return dict(x=_di(nc, "x", [NT, D]), w_in=_di(nc, "ret_w_in", [D, 6144]), g_pre=_di(nc, "g_mix_pre", [128, D]),
                cosT=_di(nc, "cosT", [128, NT]), sinT=_di(nc, "sinT", [128, NT]), cosk=_di(nc, "cosk", [NT, 4, 128]),
                sink=_di(nc, "sink", [NT, 4, 128]), gi=_di(nc, "gi", [128, 4]), g2i=_di(nc, "g2i", [128, 4]),
                maskT=_di(nc, "maskT", [128, 128]), ident=_di(nc, "ident", [128, 128]))


def prog_state(NT):
    nc = bass.Bass("TRN2", target_bir_lowering=False)
    a = _ret_inputs(nc, NT)
    s_out = _do(nc, "s_out", [2, 128, 4, 512])
    kb = KB(nc)
    idb = load_ident(kb, a["ident"])
    kb.begin_phase()
    outs = build_ret(kb, NT, a["x"], a["w_in"], a["g_pre"], a["cosT"], a["sinT"], a["cosk"], a["sink"], a["gi"], a["g2i"],
                     a["maskT"], None, None, None, s_out, idb, state_only=True)
    kb.end_phase()
    kb.finish(outs); kb.close()
    return nc


def prog_layer0(NT):
    nc = bass.Bass("TRN2", target_bir_lowering=False)
    a = _ret_inputs(nc, NT)
    s_slots = _di(nc, "s_slots", [3, 2, 128, 4, 512]); coef = _di(nc, "coef", [128, 12])
    ret_w_out = _di(nc, "ret_w_out", [2048, D]); g_mix_post = _di(nc, "g_mix_post", [128, D])
    fw_in = _di(nc, "ffn_w_in", [D, 2 * FH]); fw_out = _di(nc, "ffn_w_out", [FH, D])
    fg_pre = _di(nc, "g_ffn_pre", [128, D]); fg_post = _di(nc, "g_ffn_post", [128, D])
    kv_g = _di(nc, "g_kv", [128, D]); kv_w = _di(nc, "kv_w", [D, 1536])
    og = _dint(nc, "og_scr", [NT, 2048], BF16); hmid = _dint(nc, "hmid_scr", [NT, D])
    s_dummy = _dint(nc, "s_scr", [2, 128, 4, 512])
    h1 = _do(nc, "h1", [NT, D]); kv = _do(nc, "kv", [NT, 1536], BF16)
    kb = KB(nc)
    idb = load_ident(kb, a["ident"])
    kb.begin_phase()
    build_ret(kb, NT, a["x"], a["w_in"], a["g_pre"], a["cosT"], a["sinT"], a["cosk"], a["sink"], a["gi"], a["g2i"],
              a["maskT"], s_slots, coef, og, s_dummy, idb)
    kb.end_phase()
    kb.begin_phase()
    build_outproj(kb, NT, 2048, og, ret_w_out, g_mix_post, a["x"], hmid, idb)
    kb.end_phase()
    kb.begin_phase()
    o1 = build_ffn(kb, NT, hmid, fw_in, fw_out, fg_pre, fg_post, h1, idb)
    kb.end_phase()
    kb.begin_phase()
    o2 = build_normproj(kb, NT, h1, kv_g, kv_w, 1536, kv, idb)
    kb.end_phase()
    kb.finish(o1 + o2); kb.close()
    return nc


def prog_compress(NB=256):
    nc = bass.Bass("TRN2", target_bir_lowering=False)
    XT = _di(nc, "XT", (2, 4, 16, 128, NB), BF16); W1 = _di(nc, "W1", (2, 2048, 256)); W2 = _di(nc, "W2", (2, 256, 64))
    PEc = _di(nc, "PEc", (2, 128, 16))
    out = _do(nc, "kcv", [2, NB, 4, 64], BF16)
    kb = KB(nc)
    kb.begin_phase()
    outs = build_compress(kb, NB, XT, W1, W2, PEc, out)
    kb.end_phase()
    kb.finish(outs); kb.close()
    return nc


def prog_layer1(nqt, cpb=CPB):
    nc = bass.Bass("TRN2", target_bir_lowering=False)
    NT = nqt * 128
    NVT = nqt * cpb + cpb - 1; NSB = (NVT + 15) // 16; NBLK = NSB * 32; NCT = (8 * NVT + 16 + 127) // 128; NKV = NVT * 128
    h1 = _di(nc, "h1rr", [NT, D]); g_pre = _di(nc, "g_mix_pre", [128, D]); w_in = _di(nc, "nsa_w_in", [D, 1072])
    ident = _di(nc, "ident", [128, 128])
    A = {}
    A["KsT"] = _di(nc, "KsT", (4, 128, NKV), BF16); A["KwT"] = _di(nc, "KwT", (4, 128, NKV), BF16)
    A["Vs"] = _di(nc, "Vs", (4, NKV, 65), BF16); A["Vw"] = _di(nc, "Vw", (4, NKV, 65), BF16)
    A["KcT"] = _di(nc, "KcT", (4, 128, NCT * 128), BF16); A["Vc"] = _di(nc, "Vc", (4, NCT * 128, 65), BF16)
    A["VcN"] = _di(nc, "VcN", (4, nqt, 16, 65), BF16); A["MSEL"] = _di(nc, "MSEL", (nqt, 3, 128, NBLK))
    A["TOEPS"] = _di(nc, "TOEPS", (4, 2, 128, 512)); A["TOEPW"] = _di(nc, "TOEPW", (4, 5, 128, 512)); A["TOEPC"] = _di(nc, "TOEPC", (4, 16, 512))
    A["B31"] = _di(nc, "B31", (4, 128, 4)); A["relb_rep"] = _di(nc, "relb_rep", (128, 512)); A["Mmap"] = _di(nc, "Mmap", (128, NCT, NBLK))
    A["MNear"] = _di(nc, "MNear", (16, nqt, NBLK)); A["identf"] = _di(nc, "identf", (128, 128))
    w_out = _di(nc, "nsa_w_out", [1024, D]); g_post = _di(nc, "g_mix_post", [128, D])
    fw_in = _di(nc, "ffn_w_in", [D, 2 * FH]); fw_out = _di(nc, "ffn_w_out", [FH, D])
    fg_pre = _di(nc, "g_ffn_pre", [128, D]); fg_post = _di(nc, "g_ffn_post", [128, D])
    qin = _dint(nc, "q_scr", [NT, 1024], BF16); gates = _dint(nc, "gate_scr", [NT, 48])
    o = _dint(nc, "o_scr", [NT, 1024], BF16); hmid = _dint(nc, "hmid_scr", [NT, D])
    out = _do(nc, "out", [NT, D])
    kb = KB(nc)
    idb = load_ident(kb, ident)
    kb.begin_phase()
    build_normproj(kb, NT, h1, g_pre, w_in, 1072, qin, idb, sig_from=1024, sig_out=gates)
    kb.end_phase()
    kb.begin_phase()
    build_nsa_attn(kb, nqt, cpb, qin, gates, A, o, idb)
    kb.end_phase()
    kb.begin_phase()
    build_outproj(kb, NT, 1024, o, w_out, g_post, h1, hmid, idb)
    kb.end_phase()
    kb.begin_phase()
    outs = build_ffn(kb, NT, hmid, fw_in, fw_out, fg_pre, fg_post, out, idb)
    kb.end_phase()
    kb.finish(outs); kb.close()
    return nc


def _run(nc, in_maps):
    res = run_bass_kernel_spmd(nc, in_maps, core_ids=list(range(len(in_maps))))
    return res.results


def kernel(x, mix_norm_pre, mix_norm_post, ffn_norm_pre, ffn_norm_post, ffn_w_in, ffn_w_out, ret_w_in, ret_w_out,
           kv_norm, kv_w, cmp_pe_k, cmp_w1_k, cmp_w2_k, cmp_pe_v, cmp_w1_v, cmp_w2_v, nsa_w_in, nsa_w_out, rel_bias):
    f = lambda a: np.ascontiguousarray(np.asarray(a, dtype=np.float32))
    x = f(x)
    B, SEQ = x.shape[0], x.shape[1]
    NCORE = B * CPB; NTC = SEQ // CPB; NQT = NTC // 128
    assert NCORE <= 8 and NTC % 256 == 0
    eye = np.eye(128, dtype=np.float32)
    gi, g2i, maskT = ret_consts()
    w_in_p = perm_ret_w_in(f(ret_w_in)[0])
    base = []
    for c in range(NCORE):
        b, j = divmod(c, CPB)
        pos = np.arange(j * NTC, (j + 1) * NTC)
        cT, sT, ck, sk = ret_tables(pos)
        base.append({"x": np.ascontiguousarray(x[b, j * NTC:(j + 1) * NTC]), "ret_w_in": w_in_p,
                     "g_mix_pre": bcast128(f(mix_norm_pre)[0]), "cosT": cT, "sinT": sT, "cosk": ck, "sink": sk,
                     "gi": gi, "g2i": g2i, "maskT": maskT, "ident": eye})
    r1 = _run(prog_state(NTC), base)
    L = [np.asarray(r["s_out"], np.float32) for r in r1]
    gam128 = np.exp(128.0 * LOGG)
    ims = []
    for c in range(NCORE):
        b, j = divmod(c, CPB)
        slots = np.zeros((3, 2, 128, 4, 512), np.float32); coef = np.zeros((3, 4), np.float64)
        for i in range(j):
            slots[i] = L[b * CPB + i]
            coef[i] = gam128 ** (NQT * (j - 1 - i))
        im = dict(base[c])
        im.update({"s_slots": slots, "coef": bcast128(coef.reshape(-1).astype(np.float32)), "ret_w_out": f(ret_w_out)[0],
                   "g_mix_post": bcast128(f(mix_norm_post)[0]), "ffn_w_in": f(ffn_w_in)[0], "ffn_w_out": f(ffn_w_out)[0],
                   "g_ffn_pre": bcast128(f(ffn_norm_pre)[0]), "g_ffn_post": bcast128(f(ffn_norm_post)[0]),
                   "g_kv": bcast128(f(kv_norm)), "kv_w": f(kv_w)})
        ims.append(im)
    r2 = _run(prog_layer0(NTC), ims)
    h1 = np.stack([np.concatenate([np.asarray(r2[b * CPB + j]["h1"]) for j in range(CPB)]) for b in range(B)])
    kv = np.stack([np.concatenate([np.asarray(r2[b * CPB + j]["kv"]) for j in range(CPB)]) for b in range(B)])
    W1 = np.stack([f(cmp_w1_k), f(cmp_w1_v)]); W2 = np.stack([f(cmp_w2_k), f(cmp_w2_v)])
    PEc = pecol(np.stack([f(cmp_pe_k), f(cmp_pe_v)]))
    ims = []
    for c in range(NCORE):
        b, j = divmod(c, CPB)
        kcv_tok = kv[b].reshape(SEQ, 6, 4, 64)[:, 0:2]
        ims.append({"XT": im2col(kcv_tok, (NTC // 16) * j, NTC // 16), "W1": W1, "W2": W2, "PEc": PEc})
    r3 = _run(prog_compress(NTC // 16), ims)
    n_cmp = (SEQ - 32) // 16 + 1
    kcs = [np.concatenate([np.asarray(r3[b * CPB + j]["kcv"]) for j in range(CPB)], axis=1)[:, :n_cmp] for b in range(B)]
    consts = nsa_consts(NQT, CPB, f(rel_bias))
    ims = []; rows_all = []
    for c in range(NCORE):
        b, j = divmod(c, CPB)
        rows = np.concatenate([np.arange(128) + 128 * (CPB * l + j) for l in range(NQT)])
        rows_all.append(rows)
        im = nsa_core_inputs(NQT, CPB, j, SEQ, kv[b], kcs[b][0], kcs[b][1])
        im.update(consts)
        im.update({"h1rr": np.ascontiguousarray(h1[b][rows]), "g_mix_pre": bcast128(f(mix_norm_pre)[1]), "nsa_w_in": f(nsa_w_in)[0],
                   "ident": eye, "nsa_w_out": f(nsa_w_out)[0], "g_mix_post": bcast128(f(mix_norm_post)[1]),
                   "ffn_w_in": f(ffn_w_in)[1], "ffn_w_out": f(ffn_w_out)[1], "g_ffn_pre": bcast128(f(ffn_norm_pre)[1]),
                   "g_ffn_post": bcast128(f(ffn_norm_post)[1])})
        ims.append(im)
    r4 = _run(prog_layer1(NQT), ims)
    out = np.zeros((B, SEQ, D), np.float32)
    for c in range(NCORE):
        b, j = divmod(c, CPB)
        out[b, rows_all[c]] = np.asarray(r4[c]["out"])
    return out
```

```python
import math
import numpy as np
import ml_dtypes
from contextlib import ExitStack
import concourse.bass as bass
import concourse.mybir as mybir
from concourse.bass_utils import run_bass_kernel_spmd

F32 = mybir.dt.float32
BF16 = mybir.dt.bfloat16
ACT = mybir.ActivationFunctionType
ALU = mybir.AluOpType
AX = mybir.AxisListType

COMPUTE = ("pe", "dve", "act", "pool")


class Buf:
    def __init__(self, kb, t, name, dram=False):
        self.kb = kb
        self.t = t
        self.name = name
        self.dram = dram
        self.last_w = None
        self.reads = []
        self.sem = None
        self.semcnt = 0
        self.psum = False

    def __getitem__(self, idx):
        return self.t[idx]

    def dsem(self, q="sp"):
        if self.sem is None:
            self.sw = (q == "pool")
            if self.kb.sem_pool and not self.sw:
                self.sem, self.semcnt = self.kb.sem_pool.pop()
            else:
                self.sem = self.kb.new_sem("d")
            self.kb.phase_bufs.append(self)
        return self.sem


class KB:
    def __init__(self, nc, same_eng_sync=True):
        self.nc = nc
        self.es = ExitStack()
        self.engs = {"pe": nc.tensor, "dve": nc.vector, "act": nc.scalar,
                     "pool": nc.gpsimd, "sp": nc.sync}
        self.esem = {}
        self.ecnt = {}
        for e in COMPUTE:
            self.esem[e] = self.es.enter_context(nc.semaphore("e_" + e))
            self.ecnt[e] = 0
        self.waited = {e: {} for e in self.engs}
        self.same = same_eng_sync
        self.nsem = 4
        self.nbuf = 0
        self.ninstr = 0
        self.final_tokens = []
        self.cur = self.es
        self.sem_pool = []
        self.phase_bufs = []

    def new_sem(self, name):
        self.nsem += 1
        return self.es.enter_context(self.nc.semaphore(name + "_%d" % self.nsem))

    def sb(self, shape, dtype, name=None):
        self.nbuf += 1
        name = (name or "b") + "_%d" % self.nbuf
        t = self.cur.enter_context(self.nc.sbuf_tensor(name, list(shape), dtype))
        return Buf(self, t, name)

    def ps(self, shape, dtype, name=None):
        self.nbuf += 1
        name = (name or "p") + "_%d" % self.nbuf
        t = self.cur.enter_context(self.nc.psum_tensor(name, list(shape), dtype))
        b = Buf(self, t, name)
        b.psum = True
        return b

    def dram(self, ap, name):
        return Buf(self, ap, name, dram=True)

    def _wait(self, eng, tok):
        if tok is None:
            return
        sem, val, peng = tok
        if peng == eng and (eng == "pe" or not self.same):
            return
        key = id(sem)
        if self.waited[eng].get(key, 0) >= val:
            return
        self.engs[eng].wait_ge(sem, val)
        self.waited[eng][key] = val

    def _deps(self, eng, reads, writes):
        for b in reads:
            self._wait(eng, b.last_w)
            if b.psum:
                for r in self._compact(b.reads):
                    if r[2] != eng:
                        self._wait(eng, r)
        for b in writes:
            self._wait(eng, b.last_w)
            for r in self._compact(b.reads):
                self._wait(eng, r)

    def op(self, eng, fn, reads=(), writes=()):
        self._deps(eng, reads, writes)
        ins = fn()
        self.ecnt[eng] += 1
        ins.then_inc(self.esem[eng], 1)
        tok = (self.esem[eng], self.ecnt[eng], eng)
        for b in reads:
            b.reads.append(tok)
            if len(b.reads) > 24:
                b.reads = self._compact(b.reads)
        for b in writes:
            b.last_w = tok
            b.reads = []
        self.ninstr += 1
        return tok

    @staticmethod
    def _compact(reads):
        best = {}
        for (s, v, e) in reads:
            k = id(s)
            if k not in best or best[k][1] < v:
                best[k] = (s, v, e)
        return list(best.values())

    def dma(self, q, out_ap, in_ap, reads=(), writes=(), **kw):
        self._deps(q, reads, writes)
        owner = writes[0] if writes else reads[0]
        sem = owner.dsem(q)
        owner.semcnt += 16
        self.engs[q].dma_start(out=out_ap, in_=in_ap, **kw).then_inc(sem, 16)
        tok = (sem, owner.semcnt, "dma")
        for b in reads:
            b.reads.append(tok)
        for b in writes:
            b.last_w = tok
            b.reads = []
        self.ninstr += 1
        return tok

    def finish(self, toks, eng="sp"):
        for t in self._compact(toks):
            sem, val, _ = t
            key = id(sem)
            if self.waited[eng].get(key, 0) >= val:
                continue
            self.engs[eng].wait_ge(sem, val)
            self.waited[eng][key] = val

    def barrier(self):
        toks = [(self.esem[e], self.ecnt[e], "x") for e in COMPUTE if self.ecnt[e] > 0]
        toks += [(b.sem, b.semcnt, "dma") for b in self.phase_bufs if b.semcnt > 0]
        for e in self.engs:
            self.finish(toks, eng=e)

    def begin_phase(self):
        self.cur = ExitStack()
        self.phase_bufs = [b for b in self.phase_bufs if b.dram]

    def end_phase(self):
        self.barrier()
        for b in self.phase_bufs:
            if not b.dram and b.sem is not None:
                if not getattr(b, "sw", False):
                    self.sem_pool.append((b.sem, b.semcnt))
                b.sem = None
        self.phase_bufs = [b for b in self.phase_bufs if b.dram]
        self.cur.close()
        self.cur = self.es

    def close(self):
        self.es.close()


D = 1024; H = 2816; HC = 22; KC = 8

def load_ident(kb, ident_ap):
    idb = kb.sb([128, 128], BF16, "ident")
    kb.dma("pool", idb[:], ident_ap, writes=[idb])
    return idb

def rstd_from_ms(kb, rs, ms, eps=1e-6):
    nc = kb.nc
    kb.op("act", lambda: nc.scalar.activation(out=rs[:], in_=ms[:], func=ACT.Sqrt, bias=eps, scale=1.0),
          reads=[ms], writes=[rs])
    kb.op("dve", lambda: nc.vector.reciprocal(out=rs[:], in_=rs[:]), reads=[rs], writes=[rs])

def build_ffn(kb, NT, hin, w_in, w_out, g_pre, g_post, hout, ident, TT=256):
    nc = kb.nc
    NS = TT // 128
    Win = kb.sb([128, KC, 2 * H], BF16, "Win")
    Wout = kb.sb([128, HC, D], BF16, "Wout")
    Gpre = kb.sb([128, D], F32, "Gpre")
    Gpost = kb.sb([128, D], F32, "Gpost")
    for kc in range(KC):
        kb.dma("pool", Win[:, kc, :], w_in[kc * 128:(kc + 1) * 128, :], writes=[Win])
    for hc in range(HC):
        kb.dma("pool", Wout[:, hc, :], w_out[hc * 128:(hc + 1) * 128, :], writes=[Wout])
    kb.dma("sp", Gpre[:], g_pre, writes=[Gpre])
    kb.dma("sp", Gpost[:], g_post, writes=[Gpost])
    XA = [kb.sb([128, D], F32, "xa") for _ in range(2)]
    XB = [kb.sb([128, D], F32, "xb") for _ in range(2)]
    XN = [kb.sb([128, D], BF16, "xn") for _ in range(2)]
    OT = [kb.sb([128, D], F32, "ot") for _ in range(2)]
    junk = kb.sb([128, D], BF16, "junk")
    SS = [kb.sb([128, 1], F32, "ss") for _ in range(2)]
    RS = [kb.sb([128, 1], F32, "rs") for _ in range(2)]
    SS2 = [kb.sb([128, 1], F32, "ss2") for _ in range(2)]
    RS2 = [kb.sb([128, 1], F32, "rs2") for _ in range(2)]
    xnT = kb.sb([128, KC, TT], BF16, "xnT")
    hT = kb.sb([128, HC, TT], BF16, "hT")
    SG = [kb.sb([128, TT], F32, "sg") for _ in range(2)]
    tp = kb.ps([128, KC, 128], BF16, "tp")
    PG = [kb.ps([128, 2, TT], F32, "pg") for _ in range(3)]
    PY = [kb.ps([128, D], F32, "py") for _ in range(NS)]
    outs = []
    n = 0
    for ti in range(NT // TT):
        for s in range(NS):
            r0 = ti * TT + s * 128
            xa = XA[n % 2]; xn = XN[n % 2]; ss = SS[n % 2]; rs = RS[n % 2]
            n += 1
            kb.dma("sp", xa[:], hin[r0:r0 + 128, :], writes=[xa])
            kb.op("dve", lambda: nc.vector.memset(ss[:], 0.0), writes=[ss])
            kb.op("act", lambda: nc.scalar.activation(out=junk[:], in_=xa[:], func=ACT.Square, scale=float(D) ** -0.5,
                                                      accum_out=ss[:, 0:1]), reads=[xa, ss], writes=[junk, ss])
            rstd_from_ms(kb, rs, ss)
            kb.op("dve", lambda: nc.vector.scalar_tensor_tensor(out=xn[:], in0=xa[:], scalar=rs[:, 0:1], in1=Gpre[:],
                                                                op0=ALU.mult, op1=ALU.mult),
                  reads=[xa, rs, Gpre], writes=[xn])
            for kc in range(KC):
                kb.op("pe", lambda: nc.tensor.transpose(out=tp[:, kc, :], in_=xn[:, kc * 128:(kc + 1) * 128],
                                                        identity=ident[:]), reads=[xn, ident], writes=[tp])
            kb.op("act", lambda: nc.scalar.copy(out=xnT[:, :, s * 128:(s + 1) * 128], in_=tp[:]),
                  reads=[tp], writes=[xnT])
        for hc in range(HC):
            pg = PG[hc % 3]; sg = SG[hc % 2]
            for half in range(2):
                c0 = half * H + hc * 128
                for kc in range(KC):
                    kb.op("pe", lambda: nc.tensor.matmul(out=pg[:, half, :], lhsT=Win[:, kc, c0:c0 + 128],
                                                         rhs=xnT[:, kc, :], start=(kc == 0), stop=(kc == KC - 1)),
                          reads=[Win, xnT], writes=[pg])
            kb.op("act", lambda: nc.scalar.activation(out=sg[:], in_=pg[:, 0, :], func=ACT.Silu),
                  reads=[pg], writes=[sg])
            kb.op("dve", lambda: nc.vector.tensor_tensor(out=hT[:, hc, :], in0=sg[:], in1=pg[:, 1, :], op=ALU.mult),
                  reads=[sg, pg], writes=[hT])
        for s in range(NS):
            py = PY[s]
            for cb in range(2):
                for hc in range(HC):
                    kb.op("pe", lambda: nc.tensor.matmul(out=py[:, cb * 512:(cb + 1) * 512],
                                                         lhsT=hT[:, hc, s * 128:(s + 1) * 128],
                                                         rhs=Wout[:, hc, cb * 512:(cb + 1) * 512],
                                                         start=(hc == 0), stop=(hc == HC - 1)),
                          reads=[hT, Wout], writes=[py])
        for s in range(NS):
            r0 = ti * TT + s * 128
            py = PY[s]; xb = XB[s % 2]; ot = OT[s % 2]; ss2 = SS2[s % 2]; rs2 = RS2[s % 2]
            kb.dma("sp", xb[:], hin[r0:r0 + 128, :], writes=[xb])
            kb.op("dve", lambda: nc.vector.memset(ss2[:], 0.0), writes=[ss2])
            kb.op("act", lambda: nc.scalar.activation(out=junk[:], in_=py[:], func=ACT.Square, scale=float(D) ** -0.5,
                                                      accum_out=ss2[:, 0:1]), reads=[py, ss2], writes=[junk, ss2])
            rstd_from_ms(kb, rs2, ss2)
            kb.op("dve", lambda: nc.vector.scalar_tensor_tensor(out=ot[:], in0=py[:], scalar=rs2[:, 0:1], in1=Gpost[:],
                                                                op0=ALU.mult, op1=ALU.mult),
                  reads=[py, rs2, Gpost], writes=[ot])
            kb.op("pool", lambda: nc.gpsimd.tensor_tensor(out=ot[:], in0=ot[:], in1=xb[:], op=ALU.add),
                  reads=[ot, xb], writes=[ot])
            outs.append(kb.dma("sp", hout[r0:r0 + 128, :], ot[:], reads=[ot]))
    return outs


D = 1024
RH = 4; DK = 256; DV = 512
GAM = [1.0 - 2.0 ** (-5.0 - h) for h in range(RH)]


def build_ret(kb, NT, x_ap, w_in, g_pre, cosT, sinT, cosk, sink, gi, g2i, maskT_ap, s_slots, coef, og_out, s_out,
              ident, state_only=False, stage=9):
    nc = kb.nc
    NCH = NT // 128
    KC = 8
    if state_only:
        c_lo, c_hi = 1024, 4096
    else:
        c_lo, c_hi = 0, 6144
    WC = c_hi - c_lo
    Win = kb.sb([128, KC, WC], BF16, "Win")
    for kc in range(KC):
        for cc in range(0, WC, 1024):
            kb.dma("pool", Win[:, kc, cc:cc + 1024], w_in[kc * 128:(kc + 1) * 128, c_lo + cc:c_lo + cc + 1024],
                   writes=[Win])
    KCOL = 1024 - c_lo; VCOL = 2048 - c_lo; GCOL = 4096 - c_lo
    Gpre = kb.sb([128, D], F32, "Gpre")
    kb.dma("sp", Gpre[:], g_pre, writes=[Gpre])
    S = kb.sb([128, 2, RH, DV], F32, "S")
    if state_only:
        kb.op("dve", lambda: nc.vector.memset(S[:], 0.0), writes=[S])
    else:
        osb = kb.sb([128, RH, DV], F32, "osb")
        coef_sb = kb.sb([128, 3 * RH], F32, "coef")
        kb.dma("sp", coef_sb[:], coef, writes=[coef_sb])
        for dc in range(2):
            for slot in range(3):
                kb.dma("sp", osb[:], s_slots[slot, dc], writes=[osb])
                for h in range(RH):
                    cs = coef_sb[:, slot * RH + h:slot * RH + h + 1]
                    if slot == 0:
                        kb.op("dve", lambda: nc.vector.tensor_scalar(out=S[:, dc, h, :], in0=osb[:, h, :], scalar1=cs, scalar2=None,
                                                                     op0=ALU.mult), reads=[osb, coef_sb], writes=[S])
                    else:
                        kb.op("dve", lambda: nc.vector.scalar_tensor_tensor(out=S[:, dc, h, :], in0=osb[:, h, :], scalar=cs,
                                                                            in1=S[:, dc, h, :], op0=ALU.mult, op1=ALU.add),
                              reads=[osb, coef_sb, S], writes=[S])
    XA = [kb.sb([128, D], F32, "xa") for _ in range(2)]
    xn = kb.sb([128, D], BF16, "xn")
    xnT = kb.sb([128, KC, 128], BF16, "xnT")
    junk = kb.sb([128, D], BF16, "junk")
    ss = kb.sb([128, 1], F32, "ss"); rs = kb.sb([128, 1], F32, "rs")
    COSK = [kb.sb([128, RH, 128], F32, "cosk") for _ in range(2)]
    SINK = [kb.sb([128, RH, 128], F32, "sink") for _ in range(2)]
    kraw = kb.sb([128, 2, 2, 128], F32, "kraw")
    kt = kb.sb([128, RH, 2, 128], BF16, "kt")
    v_sb = kb.sb([128, RH, DV], BF16, "v")
    T = [kb.sb([128, 2, 128], F32, "t%d" % i) for i in range(4)]
    P = [kb.ps([128, 512], F32, "bank%d" % i) for i in range(8)]
    p0b = P[0][:].bitcast(BF16).rearrange("p (a b) -> p a b", b=128)
    if not state_only:
        Sb = kb.sb([128, 2, RH, DV], BF16, "Sb")
        for dc in range(2):
            for h in range(RH):
                kb.op("act", lambda: nc.scalar.activation(out=Sb[:, dc, h, :], in_=S[:, dc, h, :], func=ACT.Copy,
                                                          scale=GAM[h]), reads=[S], writes=[Sb])
        COST = [kb.sb([128, 128], F32, "cosT") for _ in range(2)]
        SINT = [kb.sb([128, 128], F32, "sinT") for _ in range(2)]
        GI = kb.sb([128, RH], F32, "gi"); G2I = kb.sb([128, RH], F32, "g2i")
        kb.dma("sp", GI[:], gi, writes=[GI]); kb.dma("sp", G2I[:], g2i, writes=[G2I])
        maskT = kb.sb([128, 128], F32, "maskT")
        kb.dma("sp", maskT[:], maskT_ap, writes=[maskT])
        qraw = kb.sb([128, 2, 2, 128], F32, "qraw")
        qT = kb.sb([128, RH, 2, 128], BF16, "qT")
        kT = kb.sb([128, RH, 2, 128], BF16, "kT")
        gs = kb.sb([128, RH * DV], BF16, "gs")
        PT = kb.sb([128, RH, 128], BF16, "PT")
        OG = [kb.sb([128, RH * DV], BF16, "og") for _ in range(2)]
        ms4 = kb.sb([128, RH], F32, "ms4"); f4 = kb.sb([128, RH], F32, "f4")
    outs = []
    bk = 0
    for c in range(NCH):
        r0 = c * 128
        xa = XA[c % 2]; cosk_t = COSK[c % 2]; sink_t = SINK[c % 2]
        kb.dma("sp", xa[:], x_ap[r0:r0 + 128, :], writes=[xa])
        kb.dma("sp", cosk_t[:], cosk[r0:r0 + 128], writes=[cosk_t])
        kb.dma("sp", sink_t[:], sink[r0:r0 + 128], writes=[sink_t])
        if not state_only:
            cosT_t = COST[c % 2]; sinT_t = SINT[c % 2]
            kb.dma("sp", cosT_t[:], cosT[:, r0:r0 + 128], writes=[cosT_t])
            kb.dma("sp", sinT_t[:], sinT[:, r0:r0 + 128], writes=[sinT_t])
        kb.op("dve", lambda: nc.vector.memset(ss[:], 0.0), writes=[ss])
        kb.op("act", lambda: nc.scalar.activation(out=junk[:], in_=xa[:], func=ACT.Square, scale=float(D) ** -0.5,
                                                  accum_out=ss[:, 0:1]), reads=[xa, ss], writes=[junk, ss])
        rstd_from_ms(kb, rs, ss)
        kb.op("dve", lambda: nc.vector.scalar_tensor_tensor(out=xn[:], in0=xa[:], scalar=rs[:, 0:1], in1=Gpre[:],
                                                            op0=ALU.mult, op1=ALU.mult),
              reads=[xa, rs, Gpre], writes=[xn])
        for kc in range(KC):
            kb.op("pe", lambda: nc.tensor.transpose(out=p0b[:, kc, :], in_=xn[:, kc * 128:(kc + 1) * 128],
                                                    identity=ident[:]), reads=[xn, ident], writes=[P[0]])
        kb.op("act", lambda: nc.scalar.copy(out=xnT[:], in_=p0b), reads=[P[0]], writes=[xnT])
        if not state_only:
            for hb in range(2):
                pq = P[1 + hb]
                pqv = pq[:].rearrange("p (a b t) -> p a b t", a=2, b=2)
                for hh in range(2):
                    h = 2 * hb + hh
                    for blk in range(2):
                        col0 = h * DK + blk * 128
                        for kc in range(KC):
                            kb.op("pe", lambda: nc.tensor.matmul(out=pqv[:, hh, blk, :], lhsT=Win[:, kc, col0:col0 + 128],
                                                                 rhs=xnT[:, kc, :], start=(kc == 0), stop=(kc == KC - 1)),
                                  reads=[Win, xnT], writes=[pq])
                kb.op("act", lambda: nc.scalar.copy(out=qraw[:].rearrange("p a b t -> p (a b t)"), in_=pq[:]),
                      reads=[pq], writes=[qraw])
                A = qraw[:, :, 0, :]; B = qraw[:, :, 1, :]
                cb = cosT_t[:].unsqueeze(1).broadcast_to([128, 2, 128])
                sb_ = sinT_t[:].unsqueeze(1).broadcast_to([128, 2, 128])
                kb.op("dve", lambda: nc.vector.tensor_tensor(out=T[0][:], in0=A, in1=cb, op=ALU.mult),
                      reads=[qraw, cosT_t], writes=[T[0]])
                kb.op("dve", lambda: nc.vector.tensor_tensor(out=T[1][:], in0=B, in1=sb_, op=ALU.mult),
                      reads=[qraw, sinT_t], writes=[T[1]])
                kb.op("dve", lambda: nc.vector.tensor_tensor(out=T[2][:], in0=A, in1=sb_, op=ALU.mult),
                      reads=[qraw, sinT_t], writes=[T[2]])
                kb.op("dve", lambda: nc.vector.tensor_tensor(out=T[3][:], in0=B, in1=cb, op=ALU.mult),
                      reads=[qraw, cosT_t], writes=[T[3]])
                kb.op("dve", lambda: nc.vector.tensor_tensor(out=qT[:, 2 * hb:2 * hb + 2, 0, :], in0=T[0][:], in1=T[1][:],
                                                             op=ALU.subtract), reads=[T[0], T[1]], writes=[qT])
                kb.op("dve", lambda: nc.vector.tensor_tensor(out=qT[:, 2 * hb:2 * hb + 2, 1, :], in0=T[2][:], in1=T[3][:],
                                                              op=ALU.add), reads=[T[2], T[3]], writes=[qT])
        for kb_ in range(2):
            pk = P[3 + bk % 2]; bk += 1
            for kc in range(KC):
                kb.op("pe", lambda: nc.tensor.matmul(out=pk[:], lhsT=xnT[:, kc, :],
                                                     rhs=Win[:, kc, KCOL + kb_ * 512:KCOL + (kb_ + 1) * 512],
                                                     start=(kc == 0), stop=(kc == KC - 1)),
                      reads=[Win, xnT], writes=[pk])
            kb.op("act", lambda: nc.scalar.copy(out=kraw[:].rearrange("p a b t -> p (a b t)"), in_=pk[:]),
                  reads=[pk], writes=[kraw])
            A = kraw[:, :, 0, :]; B = kraw[:, :, 1, :]
            ck = cosk_t[:, 2 * kb_:2 * kb_ + 2, :]; sk = sink_t[:, 2 * kb_:2 * kb_ + 2, :]
            kb.op("dve", lambda: nc.vector.tensor_tensor(out=T[0][:], in0=A, in1=ck, op=ALU.mult),
                  reads=[kraw, cosk_t], writes=[T[0]])
            kb.op("dve", lambda: nc.vector.tensor_tensor(out=T[1][:], in0=B, in1=sk, op=ALU.mult),
                  reads=[kraw, sink_t], writes=[T[1]])
            kb.op("dve", lambda: nc.vector.tensor_tensor(out=T[2][:], in0=A, in1=sk, op=ALU.mult),
                  reads=[kraw, sink_t], writes=[T[2]])
            kb.op("dve", lambda: nc.vector.tensor_tensor(out=T[3][:], in0=B, in1=ck, op=ALU.mult),
                  reads=[kraw, cosk_t], writes=[T[3]])
            kb.op("dve", lambda: nc.vector.tensor_tensor(out=kt[:, 2 * kb_:2 * kb_ + 2, 0, :], in0=T[0][:], in1=T[1][:],
                                                         op=ALU.subtract), reads=[T[0], T[1]], writes=[kt])
            kb.op("dve", lambda: nc.vector.tensor_tensor(out=kt[:, 2 * kb_:2 * kb_ + 2, 1, :], in0=T[2][:], in1=T[3][:],
                                                          op=ALU.add), reads=[T[2], T[3]], writes=[kt])
        for h in range(RH):
            pv = P[3 + bk % 2]; bk += 1
            for kc in range(KC):
                kb.op("pe", lambda: nc.tensor.matmul(out=pv[:], lhsT=xnT[:, kc, :],
                                                     rhs=Win[:, kc, VCOL + h * 512:VCOL + (h + 1) * 512],
                                                     start=(kc == 0), stop=(kc == KC - 1)),
                      reads=[Win, xnT], writes=[pv])
            kb.op("act", lambda: nc.scalar.copy(out=v_sb[:, h, :], in_=pv[:]), reads=[pv], writes=[v_sb])
        if not state_only:
            for h in range(RH if stage >= 2 else 0):
                pg = P[3 + bk % 2]; bk += 1
                for kc in range(KC):
                    kb.op("pe", lambda: nc.tensor.matmul(out=pg[:], lhsT=xnT[:, kc, :],
                                                         rhs=Win[:, kc, GCOL + h * 512:GCOL + (h + 1) * 512],
                                                         start=(kc == 0), stop=(kc == KC - 1)),
                          reads=[Win, xnT], writes=[pg])
                kb.op("act", lambda: nc.scalar.activation(out=gs[:, h * 512:(h + 1) * 512], in_=pg[:], func=ACT.Silu),
                      reads=[pg], writes=[gs])
            for h in range(RH if stage >= 2 else 0):
                for blk in range(2):
                    kb.op("pe", lambda: nc.tensor.transpose(out=p0b[:, h * 2 + blk, :], in_=kt[:, h, blk, :],
                                                            identity=ident[:]), reads=[kt, ident], writes=[P[0]])
            if stage >= 2:
                kb.op("dve", lambda: nc.vector.tensor_copy(out=kT[:].rearrange("p h b t -> p (h b) t"), in_=p0b),
                      reads=[P[0]], writes=[kT])
            p5 = P[5]; p5v = p5[:].rearrange("p (h t) -> p h t", h=RH)
            for h in range(RH if stage >= 3 else 0):
                for blk in range(2):
                    kb.op("pe", lambda: nc.tensor.matmul(out=p5v[:, h, :], lhsT=kT[:, h, blk, :], rhs=qT[:, h, blk, :],
                                                         start=(blk == 0), stop=(blk == 1)),
                          reads=[kT, qT], writes=[p5])
            if stage >= 3:
              kb.op("dve", lambda: nc.vector.tensor_tensor(out=PT[:], in0=p5v,
                                                         in1=maskT[:].unsqueeze(1).broadcast_to([128, RH, 128]),
                                                         op=ALU.mult), reads=[p5, maskT], writes=[PT])
            kb.op("dve", lambda: nc.vector.memset(ms4[:], 0.0), writes=[ms4])
            for h in range(RH if stage >= 4 else 0):
                po = P[6 + h % 2]
                kb.op("pe", lambda: nc.tensor.matmul(out=po[:], lhsT=PT[:, h, :], rhs=v_sb[:, h, :], start=True, stop=False),
                      reads=[PT, v_sb], writes=[po])
                kb.op("pe", lambda: nc.tensor.matmul(out=po[:], lhsT=qT[:, h, 0, :], rhs=Sb[:, 0, h, :], start=False, stop=False),
                      reads=[qT, Sb], writes=[po])
                kb.op("pe", lambda: nc.tensor.matmul(out=po[:], lhsT=qT[:, h, 1, :], rhs=Sb[:, 1, h, :], start=False, stop=True),
                      reads=[qT, Sb], writes=[po])
                kb.op("act", lambda: nc.scalar.activation(out=junk[:, 0:DV], in_=po[:], func=ACT.Square, scale=float(DV) ** -0.5,
                                                          accum_out=ms4[:, h:h + 1]), reads=[po, ms4], writes=[junk, ms4])
                kb.op("dve", lambda: nc.vector.tensor_copy(out=osb[:, h, :], in_=po[:]), reads=[po], writes=[osb])
        for h in range(RH):
            for dc in range(2):
                pkv = P[3 + bk % 2]; bk += 1
                kb.op("pe", lambda: nc.tensor.matmul(out=pkv[:], lhsT=kt[:, h, dc, :], rhs=v_sb[:, h, :], start=True, stop=True),
                      reads=[kt, v_sb], writes=[pkv])
                kb.op("act", lambda: nc.scalar.activation(out=S[:, dc, h, :], in_=S[:, dc, h, :], func=ACT.Copy,
                                                          scale=GAM[h] ** 128), reads=[S], writes=[S])
                kb.op("dve", lambda: nc.vector.scalar_tensor_tensor(out=S[:, dc, h, :], in0=pkv[:], scalar=GAM[h] ** 127,
                                                                    in1=S[:, dc, h, :], op0=ALU.mult, op1=ALU.add),
                      reads=[pkv, S], writes=[S])
                if not state_only:
                    kb.op("act", lambda: nc.scalar.activation(out=Sb[:, dc, h, :], in_=S[:, dc, h, :], func=ACT.Copy,
                                                              scale=GAM[h]), reads=[S], writes=[Sb])
        if not state_only and stage >= 5:
            og = OG[c % 2]
            kb.op("dve", lambda: nc.vector.tensor_tensor(out=f4[:], in0=ms4[:], in1=G2I[:], op=ALU.mult),
                  reads=[ms4, G2I], writes=[f4])
            rstd_from_ms(kb, f4, f4)
            kb.op("dve", lambda: nc.vector.tensor_tensor(out=f4[:], in0=f4[:], in1=GI[:], op=ALU.mult),
                  reads=[f4, GI], writes=[f4])
            for h in range(RH):
                kb.op("dve", lambda: nc.vector.scalar_tensor_tensor(out=og[:, h * DV:(h + 1) * DV], in0=osb[:, h, :], scalar=f4[:, h:h + 1],
                                                          in1=gs[:, h * DV:(h + 1) * DV], op0=ALU.mult, op1=ALU.mult),
                      reads=[osb, f4, gs], writes=[og])
            outs.append(kb.dma("sp", og_out[r0:r0 + 128, :], og[:], reads=[og]))
    for dc in range(2):
        outs.append(kb.dma("sp", s_out[dc], S[:, dc, :, :], reads=[S]))
    return outs

D = 1024

def post_norm_residual(kb, py, x_rows_ap, Gpost, out_rows_ap, xb, ot, ss2, rs2, junk):
    nc = kb.nc
    kb.dma("sp", xb[:], x_rows_ap, writes=[xb])
    kb.op("dve", lambda: nc.vector.memset(ss2[:], 0.0), writes=[ss2])
    kb.op("act", lambda: nc.scalar.activation(out=junk[:], in_=py[:], func=ACT.Square, scale=float(D) ** -0.5,
                                              accum_out=ss2[:, 0:1]), reads=[py, ss2], writes=[junk, ss2])
    rstd_from_ms(kb, rs2, ss2)
    kb.op("dve", lambda: nc.vector.scalar_tensor_tensor(out=ot[:], in0=py[:], scalar=rs2[:, 0:1], in1=Gpost[:],
                                                        op0=ALU.mult, op1=ALU.mult), reads=[py, rs2, Gpost], writes=[ot])
    kb.op("pool", lambda: nc.gpsimd.tensor_tensor(out=ot[:], in0=ot[:], in1=xb[:], op=ALU.add), reads=[ot, xb], writes=[ot])
    return kb.dma("sp", out_rows_ap, ot[:], reads=[ot])


def build_outproj(kb, NT, KD, og_in, w_out, g_post, x_in, h_out, ident, og_buf=None, x_buf=None, h_buf=None):
    nc = kb.nc
    KC = KD // 128
    Wout = kb.sb([128, KC, D], BF16, "Wo")
    for kc in range(KC):
        kb.dma("pool", Wout[:, kc, :], w_out[kc * 128:(kc + 1) * 128, :], writes=[Wout])
    Gpost = kb.sb([128, D], F32, "Gpost")
    kb.dma("sp", Gpost[:], g_post, writes=[Gpost])
    OGT = [kb.sb([128, KD], BF16, "ogt") for _ in range(2)]
    ogT = [kb.sb([128, KC, 128], BF16, "ogT") for _ in range(2)]
    XB = [kb.sb([128, D], F32, "xb") for _ in range(2)]
    OT = [kb.sb([128, D], F32, "ot") for _ in range(2)]
    SS = [kb.sb([128, 1], F32, "ss") for _ in range(2)]
    RS = [kb.sb([128, 1], F32, "rs") for _ in range(2)]
    junk = kb.sb([128, D], BF16, "junk")
    TP = [kb.ps([128, 512], F32, "tp") for _ in range(2)]
    PY = [kb.ps([128, D], F32, "py") for _ in range(2)]
    outs = []
    rd = [og_buf] if og_buf is not None else []
    rdx = [x_buf] if x_buf is not None else []
    for t in range(NT // 128):
        r0 = t * 128
        og = OGT[t % 2]; oT = ogT[t % 2]; py = PY[t % 2]
        kb.dma("sp", og[:], og_in[r0:r0 + 128, :], reads=rd, writes=[og])
        for kc in range(KC):
            tp = TP[(kc // 8) % 2]
            tpb = tp[:].bitcast(BF16).rearrange("p (a b) -> p a b", b=128)
            kb.op("pe", lambda: nc.tensor.transpose(out=tpb[:, kc % 8, :], in_=og[:, kc * 128:(kc + 1) * 128],
                                                    identity=ident[:]), reads=[og, ident], writes=[tp])
            if kc % 8 == 7:
                kb.op("act", lambda: nc.scalar.copy(out=oT[:, kc - 7:kc + 1, :], in_=tpb), reads=[tp], writes=[oT])
        for cb in range(2):
            for kc in range(KC):
                kb.op("pe", lambda: nc.tensor.matmul(out=py[:, cb * 512:(cb + 1) * 512], lhsT=oT[:, kc, :],
                                                     rhs=Wout[:, kc, cb * 512:(cb + 1) * 512],
                                                     start=(kc == 0), stop=(kc == KC - 1)), reads=[oT, Wout], writes=[py])
        if x_buf is not None:
            pass
        tok = post_norm_residual(kb, py, x_in[r0:r0 + 128, :], Gpost, h_out[r0:r0 + 128, :], XB[t % 2], OT[t % 2],
                                 SS[t % 2], RS[t % 2], junk)
        if h_buf is not None:
            h_buf.last_w = tok
        outs.append(tok)
    return outs


def build_normproj(kb, NT, x_in, g_pre, w, C, out_ap, ident, sig_from=None, sig_out=None, x_buf=None):
    nc = kb.nc
    KC = 8
    W = kb.sb([128, KC, C], BF16, "Wp")
    for kc in range(KC):
        kb.dma("pool", W[:, kc, :], w[kc * 128:(kc + 1) * 128, :], writes=[W])
    G = kb.sb([128, D], F32, "G")
    kb.dma("sp", G[:], g_pre, writes=[G])
    XA = [kb.sb([128, D], F32, "xa") for _ in range(2)]
    xn = kb.sb([128, D], BF16, "xn")
    xnT = kb.sb([128, KC, 128], BF16, "xnT")
    junk = kb.sb([128, D], BF16, "junk")
    ss = kb.sb([128, 1], F32, "ss"); rs = kb.sb([128, 1], F32, "rs")
    CM = C if sig_from is None else sig_from
    OB = [kb.sb([128, CM], BF16, "ob") for _ in range(2)]
    if sig_from is not None:
        SG = [kb.sb([128, C - sig_from], F32, "sgo") for _ in range(2)]
    tp = kb.ps([128, 512], F32, "tp")
    tpb = tp[:].bitcast(BF16).rearrange("p (a b) -> p a b", b=128)
    PB = [kb.ps([128, 512], F32, "pb") for _ in range(3)]
    outs = []
    nb = 0
    rdx = [x_buf] if x_buf is not None else []
    for t in range(NT // 128):
        r0 = t * 128
        xa = XA[t % 2]; ob = OB[t % 2]
        kb.dma("sp", xa[:], x_in[r0:r0 + 128, :], reads=rdx, writes=[xa])
        kb.op("dve", lambda: nc.vector.memset(ss[:], 0.0), writes=[ss])
        kb.op("act", lambda: nc.scalar.activation(out=junk[:], in_=xa[:], func=ACT.Square, scale=float(D) ** -0.5,
                                                  accum_out=ss[:, 0:1]), reads=[xa, ss], writes=[junk, ss])
        rstd_from_ms(kb, rs, ss)
        kb.op("dve", lambda: nc.vector.scalar_tensor_tensor(out=xn[:], in0=xa[:], scalar=rs[:, 0:1], in1=G[:],
                                                            op0=ALU.mult, op1=ALU.mult), reads=[xa, rs, G], writes=[xn])
        for kc in range(KC):
            kb.op("pe", lambda: nc.tensor.transpose(out=tpb[:, kc, :], in_=xn[:, kc * 128:(kc + 1) * 128],
                                                    identity=ident[:]), reads=[xn, ident], writes=[tp])
        kb.op("act", lambda: nc.scalar.copy(out=xnT[:], in_=tpb), reads=[tp], writes=[xnT])
        c0 = 0
        while c0 < C:
            cw = min(512, C - c0)
            if sig_from is not None and c0 < sig_from:
                cw = min(cw, sig_from - c0)
            pb = PB[nb % 3]; nb += 1
            for kc in range(KC):
                kb.op("pe", lambda: nc.tensor.matmul(out=pb[:, 0:cw], lhsT=xnT[:, kc, :], rhs=W[:, kc, c0:c0 + cw],
                                                     start=(kc == 0), stop=(kc == KC - 1)), reads=[xnT, W], writes=[pb])
            if sig_from is not None and c0 >= sig_from:
                sg = SG[t % 2]
                kb.op("act", lambda: nc.scalar.activation(out=sg[:, c0 - sig_from:c0 - sig_from + cw], in_=pb[:, 0:cw],
                                                          func=ACT.Sigmoid), reads=[pb], writes=[sg])
            else:
                kb.op("act", lambda: nc.scalar.copy(out=ob[:, c0:c0 + cw], in_=pb[:, 0:cw]), reads=[pb], writes=[ob])
            c0 += cw
        outs.append(kb.dma("sp", out_ap[r0:r0 + 128, :], ob[:], reads=[ob]))
        if sig_from is not None:
            outs.append(kb.dma("sp", sig_out[r0:r0 + 128, :], SG[t % 2][:], reads=[SG[t % 2]]))
    return outs


NEG = -30000.0
HD = 64; GR = 4; NG = 4


def build_compress(kb, NB, XT, W1, W2, PEcol, kcv_out):
    nc = kb.nc
    W1s = kb.sb([128, 2, 16, 256], BF16, "W1s")
    W2s = kb.sb([128, 2, 2, 64], BF16, "W2s")
    PEc = kb.sb([128, 2, 16], BF16, "PEc")
    for kv in range(2):
        kb.dma("pool", W1s[:, kv, :, :], W1[kv].rearrange("(c p) n -> p c n", p=128), writes=[W1s])
        kb.dma("pool", W2s[:, kv, :, :], W2[kv].rearrange("(c p) n -> p c n", p=128), writes=[W2s])
        kb.dma("pool", PEc[:, kv, :], PEcol[kv], writes=[PEc])
    pebs = kb.sb([128, 2, 2], F32, "pebs")
    pp = kb.ps([128, 512], F32, "pp")
    for kv in range(2):
        for hc in range(2):
            for cc in range(16):
                kb.op("pe", lambda: nc.tensor.matmul(out=pp[:, 0:1], lhsT=W1s[:, kv, cc, hc * 128:(hc + 1) * 128],
                                                     rhs=PEc[:, kv, cc:cc + 1], start=(cc == 0), stop=(cc == 15)),
                      reads=[W1s, PEc], writes=[pp])
            kb.op("dve", lambda: nc.vector.tensor_copy(out=pebs[:, kv, hc:hc + 1], in_=pp[:, 0:1]), reads=[pp], writes=[pebs])
    NW = min(128, NB)
    XTt = [kb.sb([128, 16, NW], BF16, "XTt") for _ in range(2)]
    hT = [kb.sb([128, 2, NW], BF16, "hT") for _ in range(2)]
    ob = [kb.sb([128, 64], BF16, "ob") for _ in range(2)]
    PH = [kb.ps([128, 512], F32, "ph") for _ in range(2)]
    PO = [kb.ps([128, 512], F32, "po") for _ in range(2)]
    outs = []
    it = 0
    for kv in range(2):
        for g in range(NG):
            for nt in range((NB + 127) // 128):
                xt = XTt[it % 2]; ht = hT[it % 2]; o = ob[it % 2]; ph = PH[it % 2]; po = PO[it % 2]; it += 1
                kb.dma("sp", xt[:], XT[kv, g, :, :, nt * 128:nt * 128 + NW].rearrange("c p n -> p c n"), writes=[xt])
                for hc in range(2):
                    for cc in range(16):
                        kb.op("pe", lambda: nc.tensor.matmul(out=ph[:, hc * 128:hc * 128 + NW],
                                                             lhsT=W1s[:, kv, cc, hc * 128:(hc + 1) * 128], rhs=xt[:, cc, :],
                                                             start=(cc == 0), stop=(cc == 15)), reads=[W1s, xt], writes=[ph])
                    kb.op("act", lambda: nc.scalar.activation(out=ht[:, hc, :], in_=ph[:, hc * 128:hc * 128 + NW],
                                                              func=ACT.Silu, bias=pebs[:, kv, hc:hc + 1]),
                          reads=[ph, pebs], writes=[ht])
                for hc in range(2):
                    kb.op("pe", lambda: nc.tensor.matmul(out=po[0:NW, 0:64], lhsT=ht[:, hc, :], rhs=W2s[:, kv, hc, :],
                                                         start=(hc == 0), stop=(hc == 1)), reads=[ht, W2s], writes=[po])
                kb.op("dve", lambda: nc.vector.tensor_copy(out=o[0:NW, :], in_=po[0:NW, 0:64]), reads=[po], writes=[o])
                outs.append(kb.dma("sp", kcv_out[kv, nt * 128:nt * 128 + NW, g, :], o[0:NW, :], reads=[o]))
    return outs


def build_nsa_attn(kb, NQT, CPB, qin, gates, A, o_out, identb, q_buf=None):
    nc = kb.nc
    NVT = NQT * CPB + CPB - 1
    NSB = (NVT + 15) // 16
    NBLK = NSB * 32
    NCT = (8 * NVT + 16 + 127) // 128
    NKV = NVT * 128
    rdq = [q_buf] if q_buf is not None else []
    identf = kb.sb([128, 128], F32, "identf")
    kb.dma("sp", identf[:], A["identf"], writes=[identf])
    Mmap = kb.sb([128, NCT, NBLK], BF16, "Mmap")
    kb.dma("pool", Mmap[:], A["Mmap"], writes=[Mmap])
    MNear = kb.sb([16, NQT, NBLK], BF16, "MNear")
    kb.dma("pool", MNear[:], A["MNear"], writes=[MNear])
    bmax = kb.sb([128, 1], F32, "bmax")
    rbf = kb.sb([128, 512], F32, "rbf")
    kb.dma("sp", rbf[:], A["relb_rep"], writes=[rbf])
    kb.op("dve", lambda: nc.vector.tensor_reduce(out=bmax[:], in_=rbf[:], axis=AX.X, op=ALU.max, apply_absolute_value=True),
          reads=[rbf], writes=[bmax])
    ones64 = kb.sb([64, 128], F32, "ones64")
    kb.op("dve", lambda: nc.vector.memset(ones64[:], 1.0), writes=[ones64])
    KsT = kb.sb([128, NKV], BF16, "KsT")
    Vs = kb.sb([128, NVT, 65], BF16, "Vs")
    KcT = kb.sb([128, NCT * 128], BF16, "KcT")
    Vc = kb.sb([128, NCT, 65], BF16, "Vc")
    VcN = kb.sb([16, NQT, 65], BF16, "VcN")
    TS = kb.sb([128, 2, 2, 512], BF16, "TS")
    TW = kb.sb([128, 5, 2, 512], BF16, "TW")
    TC = kb.sb([16, 2, 512], BF16, "TC")
    tf = kb.sb([128, 512], F32, "tf"); tr = kb.sb([128, 512], F32, "tr")
    kd = kb.sb([64, 3], F32, "kd"); kd1 = kb.sb([64, 1], F32, "kd1"); dg = kb.sb([64, 64], F32, "dg")
    KDrow = kb.sb([128, 64], F32, "KDrow")
    kwscr = kb.sb([64, 2048], BF16, "kwscr"); kdw = kb.sb([64, 16], F32, "kdw")
    b31 = kb.sb([128, 4], F32, "b31"); b31h = kb.sb([128, 4], BF16, "b31h"); b31r = kb.sb([128, 4], F32, "b31r")
    AUG1 = [kb.sb([128, GR, 128], BF16, "AUG1") for _ in range(2)]
    AUG2 = [kb.sb([128, NSB, 128], BF16, "AUG2") for _ in range(2)]
    QA = [[kb.sb([128, 512], BF16, "QA%d" % i) for i in range(NSB)] for _ in range(2)]
    QA0 = [kb.sb([128, 512], BF16, "QA0") for _ in range(2)]
    QT = [kb.sb([128, GR * HD], BF16, "qt") for _ in range(2)]
    GT = [kb.sb([128, 48], F32, "gt") for _ in range(2)]
    MS = [kb.sb([128, 3, NBLK], F32, "ms") for _ in range(2)]
    KwT = [kb.sb([128, 5 * 128], BF16, "KwT") for _ in range(2)]
    Vw = [kb.sb([128, 5, 65], BF16, "Vw") for _ in range(2)]
    absq = kb.sb([128, GR, HD], F32, "absq")
    U4 = kb.sb([128, GR], F32, "U4")
    PTc = kb.sb([128, NCT + 1, 512], BF16, "PTc")
    PTn = kb.sb([16, 512], BF16, "PTn")
    PTr = [kb.sb([128, 512], BF16, "PTr") for _ in range(3)]
    oTs = [kb.sb([65, 512], F32, "oT") for _ in range(2)]
    otok = [kb.sb([128, 3, GR, 65], F32, "otok") for _ in range(2)]
    rinv = [kb.sb([128, 3, GR], F32, "rinv") for _ in range(2)]
    fb = kb.sb([128, 3, GR], F32, "fb")
    imp = kb.sb([128, NBLK], F32, "imp")
    imw = kb.sb([128, NBLK], F32, "imw")
    m8 = kb.sb([128, 8], F32, "m8"); thr = kb.sb([128, 1], F32, "thr")
    oacc = kb.sb([128, GR, HD], F32, "oacc"); otmp = kb.sb([128, GR, HD], F32, "otmp")
    OB = [kb.sb([128, GR * HD], BF16, "obo") for _ in range(2)]
    PSB = [kb.ps([128, 512], F32, "psS%d" % i) for i in range(3)]
    PO_ = [kb.ps([128, 512], F32, "psO%d" % i) for i in range(2)]
    PY = kb.ps([128, 512], F32, "psY")
    PQ = kb.ps([128, 512], F32, "psQ")
    PX = kb.ps([128, 512], F32, "psX")
    outs = []
    cnt = {"s": 0, "p": 0, "it": 0}

    def split_hilo(dst_hi, dst_lo, src_f32, rows):
        kb.op("dve", lambda: nc.vector.tensor_copy(out=dst_hi, in_=src_f32[0:rows, :]), reads=[tf], writes=[TS, TW, TC])
        kb.op("dve", lambda: nc.vector.tensor_copy(out=tr[0:rows, :], in_=dst_hi), reads=[TS, TW, TC], writes=[tr])
        kb.op("dve", lambda: nc.vector.tensor_tensor(out=dst_lo, in0=src_f32[0:rows, :], in1=tr[0:rows, :], op=ALU.subtract),
              reads=[tf, tr], writes=[TS, TW, TC])

    def softmax_tile(lhsT_ap, rhs_ap, KR, rows, toep=None, identrows=None, rd=None):
        ps = PSB[cnt["s"] % 3]; cnt["s"] += 1
        kb.op("pe", lambda: nc.tensor.matmul(out=ps[0:rows, :], lhsT=lhsT_ap, rhs=rhs_ap, start=True, stop=(toep is None)),
              reads=rd, writes=[ps])
        if toep is not None:
            hi, lo = toep
            kb.op("pe", lambda: nc.tensor.matmul(out=ps[0:rows, :], lhsT=identb[0:rows, 0:rows], rhs=hi, start=False, stop=False),
                  reads=[identb, TS, TW, TC], writes=[ps])
            kb.op("pe", lambda: nc.tensor.matmul(out=ps[0:rows, :], lhsT=identb[0:rows, 0:rows], rhs=lo, start=False, stop=True),
                  reads=[identb, TS, TW, TC], writes=[ps])
        return ps

    for g in range(NG):
        kb.dma("sp", KsT[:], A["KsT"][g], writes=[KsT])
        kb.dma("sp", Vs[:], A["Vs"][g].rearrange("(t p) e -> p t e", p=128), writes=[Vs])
        kb.dma("sp", KcT[:], A["KcT"][g], writes=[KcT])
        kb.dma("sp", Vc[:], A["Vc"][g].rearrange("(t p) e -> p t e", p=128), writes=[Vc])
        kb.dma("sp", VcN[:], A["VcN"][g].rearrange("l u e -> u l e"), writes=[VcN])
        for m in range(2):
            kb.dma("sp", tf[:], A["TOEPS"][g, m], writes=[tf])
            split_hilo(TS[:, m, 0, :], TS[:, m, 1, :], tf, 128)
        for m in range(5):
            kb.dma("sp", tf[:], A["TOEPW"][g, m], writes=[tf])
            split_hilo(TW[:, m, 0, :], TW[:, m, 1, :], tf, 128)
        kb.dma("sp", tf[0:16, :], A["TOEPC"][g], writes=[tf])
        split_hilo(TC[:, 0, :], TC[:, 1, :], tf, 16)
        kb.dma("sp", b31[:], A["B31"][g], writes=[b31])
        kb.op("dve", lambda: nc.vector.tensor_copy(out=b31h[:], in_=b31[:]), reads=[b31], writes=[b31h])
        kb.op("dve", lambda: nc.vector.tensor_copy(out=b31r[:], in_=b31h[:]), reads=[b31h], writes=[b31r])
        kb.op("dve", lambda: nc.vector.tensor_tensor(out=b31r[:], in0=b31[:], in1=b31r[:], op=ALU.subtract),
              reads=[b31, b31r], writes=[b31r])
        for pp_ in range(2):
            a1 = AUG1[pp_]; a2 = AUG2[pp_]
            kb.op("dve", lambda: nc.vector.memset(a1[:], 0.0), writes=[a1])
            kb.op("dve", lambda: nc.vector.memset(a2[:], 0.0), writes=[a2])
            kb.op("dve", lambda: nc.vector.memset(a1[:, :, 97:98], 1.0), writes=[a1])
            kb.op("dve", lambda: nc.vector.tensor_copy(out=a1[:, :, 98:99], in_=b31h[:].unsqueeze(2)), reads=[b31h], writes=[a1])
            kb.op("dve", lambda: nc.vector.tensor_copy(out=a1[:, :, 99:100], in_=b31r[:].unsqueeze(2)), reads=[b31r], writes=[a1])
        kb.op("dve", lambda: nc.vector.tensor_reduce(out=kd[:, 0:1], in_=KsT[0:64, :], axis=AX.X, op=ALU.max, apply_absolute_value=True),
              reads=[KsT], writes=[kd])
        kb.op("dve", lambda: nc.vector.tensor_reduce(out=kd[:, 1:2], in_=KcT[0:64, :], axis=AX.X, op=ALU.max, apply_absolute_value=True),
              reads=[KcT], writes=[kd])
        nch = (NKV + 2047) // 2048
        for c in range(nch):
            w = min(2048, NKV - c * 2048)
            kb.dma("sp", kwscr[:, 0:w], A["KwT"][g][0:64, c * 2048:c * 2048 + w], writes=[kwscr])
            kb.op("dve", lambda: nc.vector.tensor_reduce(out=kdw[:, c:c + 1], in_=kwscr[:, 0:w], axis=AX.X, op=ALU.max,
                                                         apply_absolute_value=True), reads=[kwscr], writes=[kdw])
        kb.op("dve", lambda: nc.vector.tensor_reduce(out=kd[:, 2:3], in_=kdw[:, 0:nch], axis=AX.X, op=ALU.max), reads=[kdw], writes=[kd])
        kb.op("dve", lambda: nc.vector.tensor_reduce(out=kd1[:], in_=kd[:], axis=AX.X, op=ALU.max), reads=[kd], writes=[kd1])
        kb.op("dve", lambda: nc.vector.tensor_scalar(out=dg[:], in0=identf[0:64, 0:64], scalar1=kd1[:, 0:1], scalar2=None, op0=ALU.mult),
              reads=[identf, kd1], writes=[dg])
        kb.op("pe", lambda: nc.tensor.matmul(out=PX[:, 0:64], lhsT=ones64[:], rhs=dg[:], start=True, stop=True),
              reads=[ones64, dg], writes=[PX])
        kb.op("dve", lambda: nc.vector.tensor_copy(out=KDrow[:], in_=PX[:, 0:64]), reads=[PX], writes=[KDrow])
        def pre(l, par):
            V = CPB * l + CPB - 1
            qt = QT[par]; gt = GT[par]; ms = MS[par]; kw = KwT[par]; vw = Vw[par]
            aug1 = AUG1[par]; aug2 = AUG2[par]; qa0 = QA0[par]; qa = QA[par]; otk = otok[par]; rnv = rinv[par]
            kb.dma("sp", qt[:], qin[l * 128:(l + 1) * 128, g * 256:(g + 1) * 256], reads=rdq, writes=[qt])
            kb.dma("sp", gt[:], gates[l * 128:(l + 1) * 128, :], reads=rdq, writes=[gt])
            kb.dma("sp", ms[:], A["MSEL"][l].rearrange("c q b -> q c b"), writes=[ms])
            wt0 = max(0, V - 4); nwt = V - wt0 + 1
            kb.dma("sp", kw[:, 0:nwt * 128], A["KwT"][g][:, wt0 * 128:(V + 1) * 128], writes=[kw])
            kb.dma("sp", vw[:, 0:nwt, :], A["Vw"][g][wt0 * 128:(V + 1) * 128, :].rearrange("(t p) e -> p t e", p=128), writes=[vw])
            qv = qt[:].rearrange("p (h d) -> p h d", h=GR)
            kb.op("act", lambda: nc.scalar.activation(out=aug1[:, :, 0:HD], in_=qv, func=ACT.Copy, scale=HD ** -0.5),
                  reads=[qt], writes=[aug1])
            kb.op("act", lambda: nc.scalar.activation(out=absq[:], in_=qv, func=ACT.Abs), reads=[qt], writes=[absq])
            kb.op("dve", lambda: nc.vector.tensor_tensor(out=absq[:], in0=absq[:], in1=KDrow[:].unsqueeze(1).broadcast_to([128, GR, HD]),
                                                         op=ALU.mult), reads=[absq, KDrow], writes=[absq])
            kb.op("dve", lambda: nc.vector.tensor_reduce(out=U4[:], in_=absq[:], axis=AX.X, op=ALU.add), reads=[absq], writes=[U4])
            kb.op("dve", lambda: nc.vector.tensor_scalar(out=U4[:], in0=U4[:], scalar1=-(HD ** -0.5), scalar2=bmax[:, 0:1],
                                                         op0=ALU.mult, op1=ALU.subtract), reads=[U4, bmax], writes=[U4])
            kb.op("dve", lambda: nc.vector.tensor_copy(out=aug1[:, :, 96:97], in_=U4[:].unsqueeze(2)), reads=[U4], writes=[aug1])
            yield
            for h in range(GR):
                kb.op("pe", lambda: nc.tensor.matmul(out=PQ[:, h * 128:(h + 1) * 128], lhsT=aug1[:, h, :], rhs=identb[:],
                                                     start=True, stop=True), reads=[aug1, identb], writes=[PQ])
            kb.op("act", lambda: nc.scalar.copy(out=qa0[:], in_=PQ[:]), reads=[PQ], writes=[qa0])
            yield
            nfar = 8 * V - 9
            oc = PO_[cnt["p"] % 2]; cnt["p"] += 1
            tiles = []
            n0 = 0
            while n0 < nfar:
                rows = min(128, nfar - n0)
                tiles.append((n0 // 128, rows))
                n0 += 128
            first = True
            for (tix, rows) in tiles:
                ps = softmax_tile(KcT[0:100, tix * 128:tix * 128 + rows], qa0[0:100, :], 100, rows, rd=[KcT, qa0])
                kb.op("act", lambda: nc.scalar.activation(out=PTc[0:rows, tix, :], in_=ps[0:rows, :], func=ACT.Exp),
                      reads=[ps], writes=[PTc])
                kb.op("pe", lambda: nc.tensor.matmul(out=oc[0:65, :], lhsT=Vc[0:rows, tix, :], rhs=PTc[0:rows, tix, :],
                                                     start=first, stop=False), reads=[Vc, PTc], writes=[oc])
                first = False
                yield
            ps = softmax_tile(KcT[0:98, nfar:nfar + 16], qa0[0:98, :], 98, 16, toep=(TC[:, 0, :], TC[:, 1, :]), rd=[KcT, qa0])
            kb.op("act", lambda: nc.scalar.activation(out=PTn[:], in_=ps[0:16, :], func=ACT.Exp), reads=[ps], writes=[PTn])
            kb.op("pe", lambda: nc.tensor.matmul(out=oc[0:65, :], lhsT=VcN[:, l, :], rhs=PTn[:], start=first, stop=True),
                  reads=[VcN, PTn], writes=[oc])
            yield
            finish_branch(0, oc, otk, rnv, oTs[0])
            yield
            for h in range(GR):
                fst = True
                for (tix, rows) in tiles:
                    kb.op("pe", lambda: nc.tensor.matmul(out=PY[:, 0:NBLK], lhsT=PTc[0:rows, tix, h * 128:(h + 1) * 128],
                                                         rhs=Mmap[0:rows, tix, :], start=fst, stop=False),
                          reads=[PTc, Mmap], writes=[PY])
                    fst = False
                assert not fst
                kb.op("pe", lambda: nc.tensor.matmul(out=PY[:, 0:NBLK], lhsT=PTn[:, h * 128:(h + 1) * 128], rhs=MNear[:, l, :],
                                                     start=False, stop=True), reads=[PTn, MNear], writes=[PY])
                if h == 0:
                    kb.op("dve", lambda: nc.vector.tensor_scalar(out=imp[:], in0=PY[:, 0:NBLK], scalar1=rnv[:, 0, 0:1], scalar2=None,
                                                                 op0=ALU.mult), reads=[PY, rnv], writes=[imp])
                else:
                    kb.op("dve", lambda: nc.vector.scalar_tensor_tensor(out=imp[:], in0=PY[:, 0:NBLK], scalar=rnv[:, 0, h:h + 1],
                                                                        in1=imp[:], op0=ALU.mult, op1=ALU.add),
                          reads=[PY, rnv, imp], writes=[imp])
                yield
            kb.op("dve", lambda: nc.vector.tensor_tensor(out=imp[:], in0=imp[:], in1=ms[:, 0, :], op=ALU.mult), reads=[imp, ms], writes=[imp])
            kb.op("dve", lambda: nc.vector.tensor_tensor(out=imp[:], in0=imp[:], in1=ms[:, 1, :], op=ALU.add), reads=[imp, ms], writes=[imp])
            kb.op("dve", lambda: nc.vector.max(out=m8[:], in_=imp[:]), reads=[imp], writes=[m8])
            kb.op("dve", lambda: nc.vector.match_replace(out=imw[:], in_to_replace=m8[:], in_values=imp[:], imm_value=-3.0e38),
                  reads=[m8, imp], writes=[imw])
            yield
            kb.op("dve", lambda: nc.vector.max(out=m8[:], in_=imw[:]), reads=[imw], writes=[m8])
            kb.op("dve", lambda: nc.vector.tensor_reduce(out=thr[:], in_=m8[:], axis=AX.X, op=ALU.min), reads=[m8], writes=[thr])
            kb.op("dve", lambda: nc.vector.tensor_scalar(out=imw[:], in0=imp[:], scalar1=thr[:, 0:1], scalar2=None, op0=ALU.is_ge),
                  reads=[imp, thr], writes=[imw])
            kb.op("dve", lambda: nc.vector.tensor_tensor(out=imw[:], in0=imw[:], in1=ms[:, 2, :], op=ALU.mult), reads=[imw, ms], writes=[imw])
            kb.op("dve", lambda: nc.vector.tensor_scalar(out=aug2[:, :, 64:96], in0=imw[:].rearrange("p (s j) -> p s j", j=32),
                                                         scalar1=-NEG, scalar2=NEG, op0=ALU.mult, op1=ALU.add),
                  reads=[imw], writes=[aug2])
            yield
            nsb = (V + 1 + 15) // 16
            for sb in range(nsb):
                for h in range(GR):
                    kb.op("pe", lambda: nc.tensor.matmul(out=PQ[:, h * 128:(h + 1) * 128], lhsT=aug1[:, h, :], rhs=identb[:],
                                                         start=True, stop=False), reads=[aug1, identb], writes=[PQ])
                    kb.op("pe", lambda: nc.tensor.matmul(out=PQ[:, h * 128:(h + 1) * 128], lhsT=aug2[:, sb, :], rhs=identb[:],
                                                         start=False, stop=True), reads=[aug2, identb], writes=[PQ])
                kb.op("dve", lambda: nc.vector.tensor_copy(out=qa[sb][:], in_=PQ[:]), reads=[PQ], writes=[qa[sb]])
                yield

        def finish_branch(b, acc, otk, rnv, oT):
            kb.op("act", lambda: nc.scalar.copy(out=oT[:], in_=acc[0:65, :]), reads=[acc], writes=[oT])
            pxv = PX[:, 0:GR * 65].rearrange("p (h e) -> p h e", h=GR)
            for h in range(GR):
                kb.op("pe", lambda: nc.tensor.matmul(out=pxv[:, h, :], lhsT=oT[:, h * 128:(h + 1) * 128], rhs=identf[0:65, 0:65],
                                                     start=True, stop=True), reads=[oT, identf], writes=[PX])
            kb.op("dve", lambda: nc.vector.tensor_copy(out=otk[:, b, :, :], in_=pxv), reads=[PX], writes=[otk])
            kb.op("dve", lambda: nc.vector.tensor_scalar(out=rnv[:, b, :], in0=otk[:, b, :, 64], scalar1=1e-30, scalar2=None,
                                                         op0=ALU.max), reads=[otk], writes=[rnv])
            kb.op("dve", lambda: nc.vector.reciprocal(out=rnv[:, b, :], in_=rnv[:, b, :]), reads=[rnv], writes=[rnv])

        def step(gen):
            if gen is not None:
                next(gen, None)

        def post(l, par, nxt):
            V = CPB * l + CPB - 1
            gt = GT[par]; kw = KwT[par]; vw = Vw[par]; ob = OB[par]
            qa0 = QA0[par]; qa = QA[par]; otk = otok[par]; rnv = rinv[par]
            wt0 = max(0, V - 4); nwt = V - wt0 + 1
            osel = PO_[cnt["p"] % 2]; cnt["p"] += 1

            def sel_tail(v, ps):
                pt = PTr[v % 3]
                kb.op("act", lambda: nc.scalar.activation(out=pt[:], in_=ps[:], func=ACT.Exp), reads=[ps], writes=[pt])
                kb.op("pe", lambda: nc.tensor.matmul(out=osel[0:65, :], lhsT=Vs[:, v, :], rhs=pt[:], start=(v == 0), stop=(v == V)),
                      reads=[Vs, pt], writes=[osel])

            pend = None
            for v in range(V + 1):
                sb = v // 16
                if v >= V - 1:
                    m = v - (V - 1)
                    ps = softmax_tile(KsT[0:98, v * 128:(v + 1) * 128], qa[sb][0:98, :], 98, 128, toep=(TS[:, m, 0, :], TS[:, m, 1, :]),
                                      rd=[KsT, qa[sb]])
                else:
                    ps = softmax_tile(KsT[0:100, v * 128:(v + 1) * 128], qa[sb][0:100, :], 100, 128, rd=[KsT, qa[sb]])
                if pend is not None:
                    sel_tail(*pend)
                pend = (v, ps)
                step(nxt)
            sel_tail(*pend)
            finish_branch(1, osel, otk, rnv, oTs[1])
            ow = PO_[cnt["p"] % 2]; cnt["p"] += 1

            def win_tail(i, ps):
                pt = PTr[i % 3]
                kb.op("act", lambda: nc.scalar.activation(out=pt[:], in_=ps[:], func=ACT.Exp), reads=[ps], writes=[pt])
                kb.op("pe", lambda: nc.tensor.matmul(out=ow[0:65, :], lhsT=vw[:, i, :], rhs=pt[:], start=(i == 0), stop=(i == nwt - 1)),
                      reads=[vw, pt], writes=[ow])

            pend = None
            for i in range(nwt):
                v = wt0 + i
                m = v - (V - 4)
                ps = softmax_tile(kw[0:98, i * 128:(i + 1) * 128], qa0[0:98, :], 98, 128, toep=(TW[:, m, 0, :], TW[:, m, 1, :]),
                                  rd=[kw, qa0])
                if pend is not None:
                    win_tail(*pend)
                pend = (i, ps)
                step(nxt)
            win_tail(*pend)
            finish_branch(2, ow, otk, rnv, oTs[1])
            gv = gt[:, g * 12:(g + 1) * 12].rearrange("p (r b) -> p b r", b=3)
            kb.op("dve", lambda: nc.vector.tensor_tensor(out=fb[:], in0=rnv[:], in1=gv, op=ALU.mult), reads=[rnv, gt], writes=[fb])
            for b in range(3):
                fbb = fb[:, b, :].unsqueeze(2).broadcast_to([128, GR, HD])
                dst = oacc if b == 0 else otmp
                kb.op("dve", lambda: nc.vector.tensor_tensor(out=dst[:], in0=otk[:, b, :, 0:HD], in1=fbb, op=ALU.mult),
                      reads=[otk, fb], writes=[dst])
                if b > 0:
                    kb.op("dve", lambda: nc.vector.tensor_tensor(out=oacc[:], in0=oacc[:], in1=otmp[:], op=ALU.add),
                          reads=[oacc, otmp], writes=[oacc])
            kb.op("dve", lambda: nc.vector.tensor_copy(out=ob[:].rearrange("p (h d) -> p h d", h=GR), in_=oacc[:]),
                  reads=[oacc], writes=[ob])
            outs.append(kb.dma("sp", o_out[l * 128:(l + 1) * 128, g * 256:(g + 1) * 256], ob[:], reads=[ob]))
            if nxt is not None:
                for _ in nxt:
                    pass

        par = 0
        for _ in pre(0, par):
            pass
        for l in range(NQT):
            nxt = pre(l + 1, 1 - par) if l + 1 < NQT else None
            post(l, par, nxt)
            par = 1 - par
    return outs

RH = 4; DK = 256; DV = 512
LOGG = np.log(1.0 - 2.0 ** (-5.0 - np.arange(RH, dtype=np.float64)))

def perm_ret_w_in(w):
    idx = []
    for blk in range(2):
        for h in range(RH):
            base = blk * 1024 + h * DK
            idx += [base + 2 * m for m in range(128)] + [base + 2 * m + 1 for m in range(128)]
    idx += list(range(2048, 6144))
    return np.ascontiguousarray(w[:, idx])

def ret_tables(pos):
    theta = (1.0 / (10000.0 ** np.linspace(0.0, 1.0, DK // 2, dtype=np.float32))).astype(np.float32)
    ang = pos.astype(np.float32)[:, None] * theta[None, :]
    cos = np.cos(ang).astype(np.float32); sin = np.sin(ang).astype(np.float32)
    jloc = (pos % 128).astype(np.float64)
    sc = (DK ** -0.5) * np.exp(-jloc[:, None] * LOGG[None, :])
    cosk = (cos[:, None, :].astype(np.float64) * sc[:, :, None]).astype(np.float32)
    sink = (sin[:, None, :].astype(np.float64) * sc[:, :, None]).astype(np.float32)
    return (np.ascontiguousarray(cos.T), np.ascontiguousarray(sin.T), np.ascontiguousarray(cosk),
            np.ascontiguousarray(sink))

def ret_consts():
    i = np.arange(128, dtype=np.float64)
    gi = np.exp(i[:, None] * LOGG[None, :]).astype(np.float32)
    g2i = np.exp(2 * i[:, None] * LOGG[None, :]).astype(np.float32)
    maskT = (np.arange(128)[None, :] >= np.arange(128)[:, None]).astype(np.float32)
    return gi, g2i, maskT

def state_to_dev(S):
    return np.ascontiguousarray(S.reshape(RH, 128, 2, DV).transpose(2, 1, 0, 3))

def state_from_dev(Sd):
    return np.ascontiguousarray(Sd.transpose(2, 1, 0, 3).reshape(RH, DK, DV))

def bcast128(v):
    return np.ascontiguousarray(np.broadcast_to(v.reshape(1, -1), (128, v.size))).astype(np.float32)

BF = ml_dtypes.bfloat16
NEG = -30000.0

def t5_bucket_np(dist):
    n = np.maximum(dist, 0).astype(np.int64)
    nf = np.maximum(n, 1).astype(np.float32)
    val = (np.log(nf / np.float32(16)) / np.float32(math.log(128 / 16))).astype(np.float32) * np.float32(16)
    large = 16 + val.astype(np.int32)
    large = np.minimum(large, 31)
    return np.where(n < 16, n, large).astype(np.int64)

def nsa_consts(NQT, CPB, rel_bias):
    NVT = NQT * CPB + CPB - 1
    NSB = (NVT + 15) // 16; NBLK = NSB * 32
    NCT = (8 * NVT + 16 + 127) // 128
    rb = np.asarray(rel_bias, np.float32)
    k = np.arange(128)[:, None]; q = np.arange(128)[None, :]
    TOEPS = np.zeros((4, 2, 128, 4, 128), np.float32)
    TOEPW = np.zeros((4, 5, 128, 4, 128), np.float32)
    TOEPC = np.zeros((4, 16, 4, 128), np.float32)
    for g in range(4):
        for r in range(4):
            h = g * 4 + r
            for m in range(2):
                d = 128 * (1 - m) + q - k
                TOEPS[g, m, :, r, :] = np.where(d >= 0, rb[t5_bucket_np(d), h], NEG)
            for m in range(5):
                d = 128 * (4 - m) + q - k
                TOEPW[g, m, :, r, :] = np.where((d >= 0) & (d < 512), rb[t5_bucket_np(d), h], NEG)
            u = np.arange(16)[:, None]; qq = np.arange(128)[None, :]
            d = qq + 113 - 16 * u
            TOEPC[g, :, r, :] = np.where(d >= 0, rb[t5_bucket_np(d), h], NEG)
    B31 = np.zeros((4, 128, 4), np.float32)
    for g in range(4):
        B31[g] = rb[31, g * 4:(g + 1) * 4][None, :]
    relb_rep = np.ascontiguousarray(np.broadcast_to(rb.reshape(1, 512), (128, 512)))
    npr = (np.arange(NCT)[None, :] * 128 + np.arange(128)[:, None])
    b = np.arange(NBLK)[None, None, :]
    Mmap = ((npr[:, :, None] >= 4 * b - 1) & (npr[:, :, None] <= 4 * b + 3)).astype(np.float32)
    MNear = np.zeros((16, NQT, NBLK), np.float32)
    for l in range(NQT):
        V = CPB * l + CPB - 1
        npn = 8 * V - 9 + np.arange(16)[:, None]
        bb = np.arange(NBLK)[None, :]
        MNear[:, l, :] = ((npn >= 4 * bb - 1) & (npn <= 4 * bb + 3))
    return dict(TOEPS=TOEPS.reshape(4, 2, 128, 512), TOEPW=TOEPW.reshape(4, 5, 128, 512), TOEPC=TOEPC.reshape(4, 16, 512),
                B31=B31, relb_rep=relb_rep, Mmap=Mmap, MNear=MNear, identf=np.eye(128, dtype=np.float32))

def nsa_core_inputs(NQT, CPB, j, S, kv_tok, kc, vc):
    shift = CPB - 1 - j
    NVT = NQT * CPB + CPB - 1
    NSB = (NVT + 15) // 16; NBLK = NSB * 32
    NCT = (8 * NVT + 16 + 127) // 128
    NKV = NVT * 128
    n_cmp = kc.shape[0]
    kvt = kv_tok.reshape(S, 6, 4, 64)
    KsT = np.zeros((4, 128, NKV), BF); KwT = np.zeros((4, 128, NKV), BF)
    Vs = np.zeros((4, NKV, 65), BF); Vw = np.zeros((4, NKV, 65), BF)
    KcT = np.zeros((4, 128, NCT * 128), BF); Vc = np.zeros((4, NCT * 128, 65), BF)
    t0 = 128 * shift
    tp = np.arange(NKV)
    ind = ((tp // 64) % 32)
    for g in range(4):
        KsT[g, 0:64, t0:t0 + S] = kvt[:, 2, g, :].T
        KwT[g, 0:64, t0:t0 + S] = kvt[:, 4, g, :].T
        Vs[g, t0:t0 + S, 0:64] = kvt[:, 3, g, :]
        Vw[g, t0:t0 + S, 0:64] = kvt[:, 5, g, :]
        Vs[g, :, 64] = 1.0; Vw[g, :, 64] = 1.0
        for jj in range(32):
            KsT[g, 64 + jj, :] = (ind == jj).astype(np.float32)
        KsT[g, 96] = 1.0; KsT[g, 98] = 1.0; KsT[g, 99] = 1.0
        KwT[g, 96] = 1.0
        KwT[g, 97, :t0] = NEG
        c0 = 8 * shift
        KcT[g, 0:64, c0:c0 + n_cmp] = kc[:, g, :].T
        Vc[g, c0:c0 + n_cmp, 0:64] = vc[:, g, :]
        Vc[g, :, 64] = 1.0
        KcT[g, 96] = 1.0; KcT[g, 98] = 1.0; KcT[g, 99] = 1.0
        KcT[g, 97, :] = NEG
        KcT[g, 97, c0:c0 + n_cmp] = 0.0
    VcN = np.zeros((4, NQT, 16, 65), BF)
    for l in range(NQT):
        V = CPB * l + CPB - 1
        VcN[:, l] = Vc[:, 8 * V - 9:8 * V + 7, :]
    MSEL = np.zeros((NQT, 3, 128, NBLK), np.float32)
    n_sel = S // 64
    for l in range(NQT):
        T = CPB * l + j
        t = 128 * T + np.arange(128)[:, None]
        b = np.arange(NBLK)[None, :] - 2 * shift
        valid = (b >= 0) & (b < n_sel) & (64 * b <= t)
        cur = t // 64
        f0 = (b == 0); f1 = (b == cur); f2 = (b == cur - 1)
        forced = (f0 | f1 | f2) & valid
        MSEL[l, 0] = (valid & ~forced)
        add = np.where(valid, 0.0, -1e30)
        add = np.where(f2 & valid, 1e30, add); add = np.where(f1 & valid, 2e30, add); add = np.where(f0 & valid, 3e30, add)
        MSEL[l, 1] = add
        MSEL[l, 2] = valid
    return dict(KsT=KsT, KwT=KwT, Vs=Vs, Vw=Vw, KcT=KcT, Vc=Vc, VcN=VcN, MSEL=MSEL)

def im2col(kvtok, n0, NB):
    S = kvtok.shape[0]
    idx = 16 * (n0 + np.arange(NB))[None, None, :] + 2 * np.arange(16)[:, None, None] + np.arange(2)[None, :, None]
    ok = idx < S
    g = kvtok[np.minimum(idx, S - 1)]
    g = np.where(ok[..., None, None, None], g, np.zeros((), kvtok.dtype))
    return np.ascontiguousarray(g.transpose(3, 4, 0, 1, 5, 2).reshape(2, 4, 16, 128, NB))

def pecol(pe):
    return np.ascontiguousarray(pe.reshape(2, 16, 2, 64).transpose(0, 2, 3, 1).reshape(2, 128, 16))


def pecol(pe):
    return np.ascontiguousarray(pe.reshape(2, 16, 2, 64).transpose(0, 2, 3, 1).reshape(2, 128, 16))

CPB = 4; FH = 2816


def _di(nc, n, s, dt=F32):
    return nc.dram_tensor(n, list(s), dt, kind="ExternalInput").ap()


def _do(nc, n, s, dt=F32):
    return nc.dram_tensor(n, list(s), dt, kind="ExternalOutput").ap()


def _dint(nc, n, s, dt=F32):
    return nc.dram_tensor(n, list(s), dt, kind="Internal").ap()


def _ret_inputs(nc, NT):
    return dict(x=_di(nc, "x", [NT, D]), w_in=_di(nc, "ret_w_in", [D, 6144]), g_pre=_di(nc, "g_mix_pre", [128, D]),
                cosT=_di(nc, "cosT", [128, NT]), sinT=_di(nc, "sinT", [128, NT]), cosk=_di(nc, "cosk", [NT, 4, 128]),
                sink=_di(nc, "sink", [NT, 4, 128]), gi=_di(nc, "gi", [128, 4]), g2i=_di(nc, "g2i", [128, 4]),
                maskT=_di(nc, "maskT", [128, 128]), ident=_di(nc, "ident", [128, 128]))


def prog_state(NT):
    nc = bass.Bass("TRN2", target_bir_lowering=False)
    a = _ret_inputs(nc, NT)
    s_out = _do(nc, "s_out", [2, 128, 4, 512])
    kb = KB(nc)
    idb = load_ident(kb, a["ident"])
    kb.begin_phase()
    outs = build_ret(kb, NT, a["x"], a["w_in"], a["g_pre"], a["cosT"], a["sinT"], a["cosk"], a["sink"], a["gi"], a["g2i"],
                     a["maskT"], None, None, None, s_out, idb, state_only=True)
    kb.end_phase()
    kb.finish(outs); kb.close()
    return nc


def prog_layer0(NT):
    nc = bass.Bass("TRN2", target_bir_lowering=False)
    a = _ret_inputs(nc, NT)
    s_slots = _di(nc, "s_slots", [3, 2, 128, 4, 512]); coef = _di(nc, "coef", [128, 12])
    ret_w_out = _di(nc, "ret_w_out", [2048, D]); g_mix_post = _di(nc, "g_mix_post", [128, D])
    fw_in = _di(nc, "ffn_w_in", [D, 2 * FH]); fw_out = _di(nc, "ffn_w_out", [FH, D])
    fg_pre = _di(nc, "g_ffn_pre", [128, D]); fg_post = _di(nc, "g_ffn_post", [128, D])
    kv_g = _di(nc, "g_kv", [128, D]); kv_w = _di(nc, "kv_w", [D, 1536])
    og = _dint(nc, "og_scr", [NT, 2048], BF16); hmid = _dint(nc, "hmid_scr", [NT, D])
    s_dummy = _dint(nc, "s_scr", [2, 128, 4, 512])
    h1 = _do(nc, "h1", [NT, D]); kv = _do(nc, "kv", [NT, 1536], BF16)
    kb = KB(nc)
    idb = load_ident(kb, a["ident"])
    kb.begin_phase()
    build_ret(kb, NT, a["x"], a["w_in"], a["g_pre"], a["cosT"], a["sinT"], a["cosk"], a["sink"], a["gi"], a["g2i"],
              a["maskT"], s_slots, coef, og, s_dummy, idb)
    kb.end_phase()
    kb.begin_phase()
    build_outproj(kb, NT, 2048, og, ret_w_out, g_mix_post, a["x"], hmid, idb)
    kb.end_phase()
    kb.begin_phase()
    o1 = build_ffn(kb, NT, hmid, fw_in, fw_out, fg_pre, fg_post, h1, idb)
    kb.end_phase()
    kb.begin_phase()
    o2 = build_normproj(kb, NT, h1, kv_g, kv_w, 1536, kv, idb)
    kb.end_phase()
    kb.finish(o1 + o2); kb.close()
    return nc


def prog_compress(NB=256):
    nc = bass.Bass("TRN2", target_bir_lowering=False)
    XT = _di(nc, "XT", (2, 4, 16, 128, NB), BF16); W1 = _di(nc, "W1", (2, 2048, 256)); W2 = _di(nc, "W2", (2, 256, 64))
    PEc = _di(nc, "PEc", (2, 128, 16))
    out = _do(nc, "kcv", [2, NB, 4, 64], BF16)
    kb = KB(nc)
    kb.begin_phase()
    outs = build_compress(kb, NB, XT, W1, W2, PEc, out)
    kb.end_phase()
    kb.finish(outs); kb.close()
    return nc


def prog_layer1(nqt, cpb=CPB):
    nc = bass.Bass("TRN2", target_bir_lowering=False)
    NT = nqt * 128
    NVT = nqt * cpb + cpb - 1; NSB = (NVT + 15) // 16; NBLK = NSB * 32; NCT = (8 * NVT + 16 + 127) // 128; NKV = NVT * 128
    h1 = _di(nc, "h1rr", [NT, D]); g_pre = _di(nc, "g_mix_pre", [128, D]); w_in = _di(nc, "nsa_w_in", [D, 1072])
    ident = _di(nc, "ident", [128, 128])
    A = {}
    A["KsT"] = _di(nc, "KsT", (4, 128, NKV), BF16); A["KwT"] = _di(nc, "KwT", (4, 128, NKV), BF16)
    A["Vs"] = _di(nc, "Vs", (4, NKV, 65), BF16); A["Vw"] = _di(nc, "Vw", (4, NKV, 65), BF16)
    A["KcT"] = _di(nc, "KcT", (4, 128, NCT * 128), BF16); A["Vc"] = _di(nc, "Vc", (4, NCT * 128, 65), BF16)
    A["VcN"] = _di(nc, "VcN", (4, nqt, 16, 65), BF16); A["MSEL"] = _di(nc, "MSEL", (nqt, 3, 128, NBLK))
    A["TOEPS"] = _di(nc, "TOEPS", (4, 2, 128, 512)); A["TOEPW"] = _di(nc, "TOEPW", (4, 5, 128, 512)); A["TOEPC"] = _di(nc, "TOEPC", (4, 16, 512))
    A["B31"] = _di(nc, "B31", (4, 128, 4)); A["relb_rep"] = _di(nc, "relb_rep", (128, 512)); A["Mmap"] = _di(nc, "Mmap", (128, NCT, NBLK))
    A["MNear"] = _di(nc, "MNear", (16, nqt, NBLK)); A["identf"] = _di(nc, "identf", (128, 128))
    w_out = _di(nc, "nsa_w_out", [1024, D]); g_post = _di(nc, "g_mix_post", [128, D])
    fw_in = _di(nc, "ffn_w_in", [D, 2 * FH]); fw_out = _di(nc, "ffn_w_out", [FH, D])
    fg_pre = _di(nc, "g_ffn_pre", [128, D]); fg_post = _di(nc, "g_ffn_post", [128, D])
    qin = _dint(nc, "q_scr", [NT, 1024], BF16); gates = _dint(nc, "gate_scr", [NT, 48])
    o = _dint(nc, "o_scr", [NT, 1024], BF16); hmid = _dint(nc, "hmid_scr", [NT, D])
    out = _do(nc, "out", [NT, D])
    kb = KB(nc)
    idb = load_ident(kb, ident)
    kb.begin_phase()
    build_normproj(kb, NT, h1, g_pre, w_in, 1072, qin, idb, sig_from=1024, sig_out=gates)
    kb.end_phase()
    kb.begin_phase()
    build_nsa_attn(kb, nqt, cpb, qin, gates, A, o, idb)
    kb.end_phase()
    kb.begin_phase()
    build_outproj(kb, NT, 1024, o, w_out, g_post, h1, hmid, idb)
    kb.end_phase()
    kb.begin_phase()
    outs = build_ffn(kb, NT, hmid, fw_in, fw_out, fg_pre, fg_post, out, idb)
    kb.end_phase()
    kb.finish(outs); kb.close()
    return nc


def _run(nc, in_maps):
    res = run_bass_kernel_spmd(nc, in_maps, core_ids=list(range(len(in_maps))))
    return res.results


def kernel(x, mix_norm_pre, mix_norm_post, ffn_norm_pre, ffn_norm_post, ffn_w_in, ffn_w_out, ret_w_in, ret_w_out,
           kv_norm, kv_w, cmp_pe_k, cmp_w1_k, cmp_w2_k, cmp_pe_v, cmp_w1_v, cmp_w2_v, nsa_w_in, nsa_w_out, rel_bias):
    f = lambda a: np.ascontiguousarray(np.asarray(a, dtype=np.float32))
    x = f(x)
    B, SEQ = x.shape[0], x.shape[1]
    NCORE = B * CPB; NTC = SEQ // CPB; NQT = NTC // 128
    assert NCORE <= 8 and NTC % 256 == 0
    eye = np.eye(128, dtype=np.float32)
    gi, g2i, maskT = ret_consts()
    w_in_p = perm_ret_w_in(f(ret_w_in)[0])
    base = []
    for c in range(NCORE):
        b, j = divmod(c, CPB)
        pos = np.arange(j * NTC, (j + 1) * NTC)
        cT, sT, ck, sk = ret_tables(pos)
        base.append({"x": np.ascontiguousarray(x[b, j * NTC:(j + 1) * NTC]), "ret_w_in": w_in_p,
                     "g_mix_pre": bcast128(f(mix_norm_pre)[0]), "cosT": cT, "sinT": sT, "cosk": ck, "sink": sk,
                     "gi": gi, "g2i": g2i, "maskT": maskT, "ident": eye})
    r1 = _run(prog_state(NTC), base)
    L = [np.asarray(r["s_out"], np.float32) for r in r1]
    gam128 = np.exp(128.0 * LOGG)
    ims = []
    for c in range(NCORE):
        b, j = divmod(c, CPB)
        slots = np.zeros((3, 2, 128, 4, 512), np.float32); coef = np.zeros((3, 4), np.float64)
        for i in range(j):
            slots[i] = L[b * CPB + i]
            coef[i] = gam128 ** (NQT * (j - 1 - i))
        im = dict(base[c])
        im.update({"s_slots": slots, "coef": bcast128(coef.reshape(-1).astype(np.float32)), "ret_w_out": f(ret_w_out)[0],
                   "g_mix_post": bcast128(f(mix_norm_post)[0]), "ffn_w_in": f(ffn_w_in)[0], "ffn_w_out": f(ffn_w_out)[0],
                   "g_ffn_pre": bcast128(f(ffn_norm_pre)[0]), "g_ffn_post": bcast128(f(ffn_norm_post)[0]),
                   "g_kv": bcast128(f(kv_norm)), "kv_w": f(kv_w)})
        ims.append(im)
    r2 = _run(prog_layer0(NTC), ims)
    h1 = np.stack([np.concatenate([np.asarray(r2[b * CPB + j]["h1"]) for j in range(CPB)]) for b in range(B)])
    kv = np.stack([np.concatenate([np.asarray(r2[b * CPB + j]["kv"]) for j in range(CPB)]) for b in range(B)])
    W1 = np.stack([f(cmp_w1_k), f(cmp_w1_v)]); W2 = np.stack([f(cmp_w2_k), f(cmp_w2_v)])
    PEc = pecol(np.stack([f(cmp_pe_k), f(cmp_pe_v)]))
    ims = []
    for c in range(NCORE):
        b, j = divmod(c, CPB)
        kcv_tok = kv[b].reshape(SEQ, 6, 4, 64)[:, 0:2]
        ims.append({"XT": im2col(kcv_tok, (NTC // 16) * j, NTC // 16), "W1": W1, "W2": W2, "PEc": PEc})
    r3 = _run(prog_compress(NTC // 16), ims)
    n_cmp = (SEQ - 32) // 16 + 1
    kcs = [np.concatenate([np.asarray(r3[b * CPB + j]["kcv"]) for j in range(CPB)], axis=1)[:, :n_cmp] for b in range(B)]
    consts = nsa_consts(NQT, CPB, f(rel_bias))
    ims = []; rows_all = []
    for c in range(NCORE):
        b, j = divmod(c, CPB)
        rows = np.concatenate([np.arange(128) + 128 * (CPB * l + j) for l in range(NQT)])
        rows_all.append(rows)
        im = nsa_core_inputs(NQT, CPB, j, SEQ, kv[b], kcs[b][0], kcs[b][1])
        im.update(consts)
        im.update({"h1rr": np.ascontiguousarray(h1[b][rows]), "g_mix_pre": bcast128(f(mix_norm_pre)[1]), "nsa_w_in": f(nsa_w_in)[0],
                   "ident": eye, "nsa_w_out": f(nsa_w_out)[0], "g_mix_post": bcast128(f(mix_norm_post)[1]),
                   "ffn_w_in": f(ffn_w_in)[1], "ffn_w_out": f(ffn_w_out)[1], "g_ffn_pre": bcast128(f(ffn_norm_pre)[1]),
                   "g_ffn_post": bcast128(f(ffn_norm_post)[1])})
        ims.append(im)
    r4 = _run(prog_layer1(NQT), ims)
    out = np.zeros((B, SEQ, D), np.float32)
    for c in range(NCORE):
        b, j = divmod(c, CPB)
        out[b, rows_all[c]] = np.asarray(r4[c]["out"])
    return out
```

```python
import math
import numpy as np
import ml_dtypes
from contextlib import ExitStack
import concourse.bass as bass
import concourse.mybir as mybir
from concourse.bass_utils import run_bass_kernel_spmd

F32 = mybir.dt.float32
BF16 = mybir.dt.bfloat16
ACT = mybir.ActivationFunctionType
ALU = mybir.AluOpType
AX = mybir.AxisListType

COMPUTE = ("pe", "dve", "act", "pool")


class Buf:
    def __init__(self, kb, t, name, dram=False):
        self.kb = kb
        self.t = t
        self.name = name
        self.dram = dram
        self.last_w = None
        self.reads = []
        self.sem = None
        self.semcnt = 0
        self.psum = False

    def __getitem__(self, idx):
        return self.t[idx]

    def dsem(self, q="sp"):
        if self.sem is None:
            self.sw = (q == "pool")
            if self.kb.sem_pool and not self.sw:
                self.sem, self.semcnt = self.kb.sem_pool.pop()
            else:
                self.sem = self.kb.new_sem("d")
            self.kb.phase_bufs.append(self)
        return self.sem


class KB:
    def __init__(self, nc, same_eng_sync=True):
        self.nc = nc
        self.es = ExitStack()
        self.engs = {"pe": nc.tensor, "dve": nc.vector, "act": nc.scalar,
                     "pool": nc.gpsimd, "sp": nc.sync}
        self.esem = {}
        self.ecnt = {}
        for e in COMPUTE:
            self.esem[e] = self.es.enter_context(nc.semaphore("e_" + e))
            self.ecnt[e] = 0
        self.waited = {e: {} for e in self.engs}
        self.same = same_eng_sync
        self.nsem = 4
        self.nbuf = 0
        self.ninstr = 0
        self.final_tokens = []
        self.cur = self.es
        self.sem_pool = []
        self.phase_bufs = []

    def new_sem(self, name):
        self.nsem += 1
        return self.es.enter_context(self.nc.semaphore(name + "_%d" % self.nsem))

    def sb(self, shape, dtype, name=None):
        self.nbuf += 1
        name = (name or "b") + "_%d" % self.nbuf
        t = self.cur.enter_context(self.nc.sbuf_tensor(name, list(shape), dtype))
        return Buf(self, t, name)

    def ps(self, shape, dtype, name=None):
        self.nbuf += 1
        name = (name or "p") + "_%d" % self.nbuf
        t = self.cur.enter_context(self.nc.psum_tensor(name, list(shape), dtype))
        b = Buf(self, t, name)
        b.psum = True
        return b

    def dram(self, ap, name):
        return Buf(self, ap, name, dram=True)

    def _wait(self, eng, tok):
        if tok is None:
            return
        sem, val, peng = tok
        if peng == eng and (eng == "pe" or not self.same):
            return
        key = id(sem)
        if self.waited[eng].get(key, 0) >= val:
            return
        self.engs[eng].wait_ge(sem, val)
        self.waited[eng][key] = val

    def _deps(self, eng, reads, writes):
        for b in reads:
            self._wait(eng, b.last_w)
            if b.psum:
                for r in self._compact(b.reads):
                    if r[2] != eng:
                        self._wait(eng, r)
        for b in writes:
            self._wait(eng, b.last_w)
            for r in self._compact(b.reads):
                self._wait(eng, r)

    def op(self, eng, fn, reads=(), writes=()):
        self._deps(eng, reads, writes)
        ins = fn()
        self.ecnt[eng] += 1
        ins.then_inc(self.esem[eng], 1)
        tok = (self.esem[eng], self.ecnt[eng], eng)
        for b in reads:
            b.reads.append(tok)
            if len(b.reads) > 24:
                b.reads = self._compact(b.reads)
        for b in writes:
            b.last_w = tok
            b.reads = []
        self.ninstr += 1
        return tok

    @staticmethod
    def _compact(reads):
        best = {}
        for (s, v, e) in reads:
            k = id(s)
            if k not in best or best[k][1] < v:
                best[k] = (s, v, e)
        return list(best.values())

    def dma(self, q, out_ap, in_ap, reads=(), writes=(), **kw):
        self._deps(q, reads, writes)
        owner = writes[0] if writes else reads[0]
        sem = owner.dsem(q)
        owner.semcnt += 16
        self.engs[q].dma_start(out=out_ap, in_=in_ap, **kw).then_inc(sem, 16)
        tok = (sem, owner.semcnt, "dma")
        for b in reads:
            b.reads.append(tok)
        for b in writes:
            b.last_w = tok
            b.reads = []
        self.ninstr += 1
        return tok

    def finish(self, toks, eng="sp"):
        for t in self._compact(toks):
            sem, val, _ = t
            key = id(sem)
            if self.waited[eng].get(key, 0) >= val:
                continue
            self.engs[eng].wait_ge(sem, val)
            self.waited[eng][key] = val

    def barrier(self):
        toks = [(self.esem[e], self.ecnt[e], "x") for e in COMPUTE if self.ecnt[e] > 0]
        toks += [(b.sem, b.semcnt, "dma") for b in self.phase_bufs if b.semcnt > 0]
        for e in self.engs:
            self.finish(toks, eng=e)

    def begin_phase(self):
        self.cur = ExitStack()
        self.phase_bufs = [b for b in self.phase_bufs if b.dram]

    def end_phase(self):
        self.barrier()
        for b in self.phase_bufs:
            if not b.dram and b.sem is not None:
                if not getattr(b, "sw", False):
                    self.sem_pool.append((b.sem, b.semcnt))
                b.sem = None
        self.phase_bufs = [b for b in self.phase_bufs if b.dram]
        self.cur.close()
        self.cur = self.es

    def close(self):
        self.es.close()


D = 1024; H = 2816; HC = 22; KC = 8

def load_ident(kb, ident_ap):
    idb = kb.sb([128, 128], BF16, "ident")
    kb.dma("pool", idb[:], ident_ap, writes=[idb])
    return idb

def rstd_from_ms(kb, rs, ms, eps=1e-6):
    nc = kb.nc
    kb.op("act", lambda: nc.scalar.activation(out=rs[:], in_=ms[:], func=ACT.Sqrt, bias=eps, scale=1.0),
          reads=[ms], writes=[rs])
    kb.op("dve", lambda: nc.vector.reciprocal(out=rs[:], in_=rs[:]), reads=[rs], writes=[rs])

def build_ffn(kb, NT, hin, w_in, w_out, g_pre, g_post, hout, ident, TT=256):
    nc = kb.nc
    NS = TT // 128
    Win = kb.sb([128, KC, 2 * H], BF16, "Win")
    Wout = kb.sb([128, HC, D], BF16, "Wout")
    Gpre = kb.sb([128, D], F32, "Gpre")
    Gpost = kb.sb([128, D], F32, "Gpost")
    for kc in range(KC):
        kb.dma("pool", Win[:, kc, :], w_in[kc * 128:(kc + 1) * 128, :], writes=[Win])
    for hc in range(HC):
        kb.dma("pool", Wout[:, hc, :], w_out[hc * 128:(hc + 1) * 128, :], writes=[Wout])
    kb.dma("sp", Gpre[:], g_pre, writes=[Gpre])
    kb.dma("sp", Gpost[:], g_post, writes=[Gpost])
    XA = [kb.sb([128, D], F32, "xa") for _ in range(2)]
    XB = [kb.sb([128, D], F32, "xb") for _ in range(2)]
    XN = [kb.sb([128, D], BF16, "xn") for _ in range(2)]
    OT = [kb.sb([128, D], F32, "ot") for _ in range(2)]
    junk = kb.sb([128, D], BF16, "junk")
    SS = [kb.sb([128, 1], F32, "ss") for _ in range(2)]
    RS = [kb.sb([128, 1], F32, "rs") for _ in range(2)]
    SS2 = [kb.sb([128, 1], F32, "ss2") for _ in range(2)]
    RS2 = [kb.sb([128, 1], F32, "rs2") for _ in range(2)]
    xnT = kb.sb([128, KC, TT], BF16, "xnT")
    hT = kb.sb([128, HC, TT], BF16, "hT")
    SG = [kb.sb([128, TT], F32, "sg") for _ in range(2)]
    tp = kb.ps([128, KC, 128], BF16, "tp")
    PG = [kb.ps([128, 2, TT], F32, "pg") for _ in range(3)]
    PY = [kb.ps([128, D], F32, "py") for _ in range(NS)]
    outs = []
    n = 0
    for ti in range(NT // TT):
        for s in range(NS):
            r0 = ti * TT + s * 128
            xa = XA[n % 2]; xn = XN[n % 2]; ss = SS[n % 2]; rs = RS[n % 2]
            n += 1
            kb.dma("sp", xa[:], hin[r0:r0 + 128, :], writes=[xa])
            kb.op("dve", lambda: nc.vector.memset(ss[:], 0.0), writes=[ss])
            kb.op("act", lambda: nc.scalar.activation(out=junk[:], in_=xa[:], func=ACT.Square, scale=float(D) ** -0.5,
                                                      accum_out=ss[:, 0:1]), reads=[xa, ss], writes=[junk, ss])
            rstd_from_ms(kb, rs, ss)
            kb.op("dve", lambda: nc.vector.scalar_tensor_tensor(out=xn[:], in0=xa[:], scalar=rs[:, 0:1], in1=Gpre[:],
                                                                op0=ALU.mult, op1=ALU.mult),
                  reads=[xa, rs, Gpre], writes=[xn])
            for kc in range(KC):
                kb.op("pe", lambda: nc.tensor.transpose(out=tp[:, kc, :], in_=xn[:, kc * 128:(kc + 1) * 128],
                                                        identity=ident[:]), reads=[xn, ident], writes=[tp])
            kb.op("act", lambda: nc.scalar.copy(out=xnT[:, :, s * 128:(s + 1) * 128], in_=tp[:]),
                  reads=[tp], writes=[xnT])
        for hc in range(HC):
            pg = PG[hc % 3]; sg = SG[hc % 2]
            for half in range(2):
                c0 = half * H + hc * 128
                for kc in range(KC):
                    kb.op("pe", lambda: nc.tensor.matmul(out=pg[:, half, :], lhsT=Win[:, kc, c0:c0 + 128],
                                                         rhs=xnT[:, kc, :], start=(kc == 0), stop=(kc == KC - 1)),
                          reads=[Win, xnT], writes=[pg])
            kb.op("act", lambda: nc.scalar.activation(out=sg[:], in_=pg[:, 0, :], func=ACT.Silu),
                  reads=[pg], writes=[sg])
            kb.op("dve", lambda: nc.vector.tensor_tensor(out=hT[:, hc, :], in0=sg[:], in1=pg[:, 1, :], op=ALU.mult),
                  reads=[sg, pg], writes=[hT])
        for s in range(NS):
            py = PY[s]
            for cb in range(2):
                for hc in range(HC):
                    kb.op("pe", lambda: nc.tensor.matmul(out=py[:, cb * 512:(cb + 1) * 512],
                                                         lhsT=hT[:, hc, s * 128:(s + 1) * 128],
                                                         rhs=Wout[:, hc, cb * 512:(cb + 1) * 512],
                                                         start=(hc == 0), stop=(hc == HC - 1)),
                          reads=[hT, Wout], writes=[py])
        for s in range(NS):
            r0 = ti * TT + s * 128
            py = PY[s]; xb = XB[s % 2]; ot = OT[s % 2]; ss2 = SS2[s % 2]; rs2 = RS2[s % 2]
            kb.dma("sp", xb[:], hin[r0:r0 + 128, :], writes=[xb])
            kb.op("dve", lambda: nc.vector.memset(ss2[:], 0.0), writes=[ss2])
            kb.op("act", lambda: nc.scalar.activation(out=junk[:], in_=py[:], func=ACT.Square, scale=float(D) ** -0.5,
                                                      accum_out=ss2[:, 0:1]), reads=[py, ss2], writes=[junk, ss2])
            rstd_from_ms(kb, rs2, ss2)
            kb.op("dve", lambda: nc.vector.scalar_tensor_tensor(out=ot[:], in0=py[:], scalar=rs2[:, 0:1], in1=Gpost[:],
                                                                op0=ALU.mult, op1=ALU.mult),
                  reads=[py, rs2, Gpost], writes=[ot])
            kb.op("pool", lambda: nc.gpsimd.tensor_tensor(out=ot[:], in0=ot[:], in1=xb[:], op=ALU.add),
                  reads=[ot, xb], writes=[ot])
            outs.append(kb.dma("sp", hout[r0:r0 + 128, :], ot[:], reads=[ot]))
    return outs


D = 1024
RH = 4; DK = 256; DV = 512
GAM = [1.0 - 2.0 ** (-5.0 - h) for h in range(RH)]


def build_ret(kb, NT, x_ap, w_in, g_pre, cosT, sinT, cosk, sink, gi, g2i, maskT_ap, s_slots, coef, og_out, s_out,
              ident, state_only=False, stage=9):
    nc = kb.nc
    NCH = NT // 128
    KC = 8
    if state_only:
        c_lo, c_hi = 1024, 4096
    else:
        c_lo, c_hi = 0, 6144
    WC = c_hi - c_lo
    Win = kb.sb([128, KC, WC], BF16, "Win")
    for kc in range(KC):
        for cc in range(0, WC, 1024):
            kb.dma("pool", Win[:, kc, cc:cc + 1024], w_in[kc * 128:(kc + 1) * 128, c_lo + cc:c_lo + cc + 1024],
                   writes=[Win])
    KCOL = 1024 - c_lo; VCOL = 2048 - c_lo; GCOL = 4096 - c_lo
    Gpre = kb.sb([128, D], F32, "Gpre")
    kb.dma("sp", Gpre[:], g_pre, writes=[Gpre])
    S = kb.sb([128, 2, RH, DV], F32, "S")
    if state_only:
        kb.op("dve", lambda: nc.vector.memset(S[:], 0.0), writes=[S])
    else:
        osb = kb.sb([128, RH, DV], F32, "osb")
        coef_sb = kb.sb([128, 3 * RH], F32, "coef")
        kb.dma("sp", coef_sb[:], coef, writes=[coef_sb])
        for dc in range(2):
            for slot in range(3):
                kb.dma("sp", osb[:], s_slots[slot, dc], writes=[osb])
                for h in range(RH):
                    cs = coef_sb[:, slot * RH + h:slot * RH + h + 1]
                    if slot == 0:
                        kb.op("dve", lambda: nc.vector.tensor_scalar(out=S[:, dc, h, :], in0=osb[:, h, :], scalar1=cs, scalar2=None,
                                                                     op0=ALU.mult), reads=[osb, coef_sb], writes=[S])
                    else:
                        kb.op("dve", lambda: nc.vector.scalar_tensor_tensor(out=S[:, dc, h, :], in0=osb[:, h, :], scalar=cs,
                                                                            in1=S[:, dc, h, :], op0=ALU.mult, op1=ALU.add),
                              reads=[osb, coef_sb, S], writes=[S])
    XA = [kb.sb([128, D], F32, "xa") for _ in range(2)]
    xn = kb.sb([128, D], BF16, "xn")
    xnT = kb.sb([128, KC, 128], BF16, "xnT")
    junk = kb.sb([128, D], BF16, "junk")
    ss = kb.sb([128, 1], F32, "ss"); rs = kb.sb([128, 1], F32, "rs")
    COSK = [kb.sb([128, RH, 128], F32, "cosk") for _ in range(2)]
    SINK = [kb.sb([128, RH, 128], F32, "sink") for _ in range(2)]
    kraw = kb.sb([128, 2, 2, 128], F32, "kraw")
    kt = kb.sb([128, RH, 2, 128], BF16, "kt")
    v_sb = kb.sb([128, RH, DV], BF16, "v")
    T = [kb.sb([128, 2, 128], F32, "t%d" % i) for i in range(4)]
    P = [kb.ps([128, 512], F32, "bank%d" % i) for i in range(8)]
    p0b = P[0][:].bitcast(BF16).rearrange("p (a b) -> p a b", b=128)
    if not state_only:
        Sb = kb.sb([128, 2, RH, DV], BF16, "Sb")
        for dc in range(2):
            for h in range(RH):
                kb.op("act", lambda: nc.scalar.activation(out=Sb[:, dc, h, :], in_=S[:, dc, h, :], func=ACT.Copy,
                                                          scale=GAM[h]), reads=[S], writes=[Sb])
        COST = [kb.sb([128, 128], F32, "cosT") for _ in range(2)]
        SINT = [kb.sb([128, 128], F32, "sinT") for _ in range(2)]
        GI = kb.sb([128, RH], F32, "gi"); G2I = kb.sb([128, RH], F32, "g2i")
        kb.dma("sp", GI[:], gi, writes=[GI]); kb.dma("sp", G2I[:], g2i, writes=[G2I])
        maskT = kb.sb([128, 128], F32, "maskT")
        kb.dma("sp", maskT[:], maskT_ap, writes=[maskT])
        qraw = kb.sb([128, 2, 2, 128], F32, "qraw")
        qT = kb.sb([128, RH, 2, 128], BF16, "qT")
        kT = kb.sb([128, RH, 2, 128], BF16, "kT")
        gs = kb.sb([128, RH * DV], BF16, "gs")
        PT = kb.sb([128, RH, 128], BF16, "PT")
        OG = [kb.sb([128, RH * DV], BF16, "og") for _ in range(2)]
        ms4 = kb.sb([128, RH], F32, "ms4"); f4 = kb.sb([128, RH], F32, "f4")
    outs = []
    bk = 0
    for c in range(NCH):
        r0 = c * 128
        xa = XA[c % 2]; cosk_t = COSK[c % 2]; sink_t = SINK[c % 2]
        kb.dma("sp", xa[:], x_ap[r0:r0 + 128, :], writes=[xa])
        kb.dma("sp", cosk_t[:], cosk[r0:r0 + 128], writes=[cosk_t])
        kb.dma("sp", sink_t[:], sink[r0:r0 + 128], writes=[sink_t])
        if not state_only:
            cosT_t = COST[c % 2]; sinT_t = SINT[c % 2]
            kb.dma("sp", cosT_t[:], cosT[:, r0:r0 + 128], writes=[cosT_t])
            kb.dma("sp", sinT_t[:], sinT[:, r0:r0 + 128], writes=[sinT_t])
        kb.op("dve", lambda: nc.vector.memset(ss[:], 0.0), writes=[ss])
        kb.op("act", lambda: nc.scalar.activation(out=junk[:], in_=xa[:], func=ACT.Square, scale=float(D) ** -0.5,
                                                  accum_out=ss[:, 0:1]), reads=[xa, ss], writes=[junk, ss])
        rstd_from_ms(kb, rs, ss)
        kb.op("dve", lambda: nc.vector.scalar_tensor_tensor(out=xn[:], in0=xa[:], scalar=rs[:, 0:1], in1=Gpre[:],
                                                            op0=ALU.mult, op1=ALU.mult),
              reads=[xa, rs, Gpre], writes=[xn])
        for kc in range(KC):
            kb.op("pe", lambda: nc.tensor.transpose(out=p0b[:, kc, :], in_=xn[:, kc * 128:(kc + 1) * 128],
                                                    identity=ident[:]), reads=[xn, ident], writes=[P[0]])
        kb.op("act", lambda: nc.scalar.copy(out=xnT[:], in_=p0b), reads=[P[0]], writes=[xnT])
        if not state_only:
            for hb in range(2):
                pq = P[1 + hb]
                pqv = pq[:].rearrange("p (a b t) -> p a b t", a=2, b=2)
                for hh in range(2):
                    h = 2 * hb + hh
                    for blk in range(2):
                        col0 = h * DK + blk * 128
                        for kc in range(KC):
                            kb.op("pe", lambda: nc.tensor.matmul(out=pqv[:, hh, blk, :], lhsT=Win[:, kc, col0:col0 + 128],
                                                                 rhs=xnT[:, kc, :], start=(kc == 0), stop=(kc == KC - 1)),
                                  reads=[Win, xnT], writes=[pq])
                kb.op("act", lambda: nc.scalar.copy(out=qraw[:].rearrange("p a b t -> p (a b t)"), in_=pq[:]),
                      reads=[pq], writes=[qraw])
                A = qraw[:, :, 0, :]; B = qraw[:, :, 1, :]
                cb = cosT_t[:].unsqueeze(1).broadcast_to([128, 2, 128])
                sb_ = sinT_t[:].unsqueeze(1).broadcast_to([128, 2, 128])
                kb.op("dve", lambda: nc.vector.tensor_tensor(out=T[0][:], in0=A, in1=cb, op=ALU.mult),
                      reads=[qraw, cosT_t], writes=[T[0]])
                kb.op("dve", lambda: nc.vector.tensor_tensor(out=T[1][:], in0=B, in1=sb_, op=ALU.mult),
                      reads=[qraw, sinT_t], writes=[T[1]])
                kb.op("dve", lambda: nc.vector.tensor_tensor(out=T[2][:], in0=A, in1=sb_, op=ALU.mult),
                      reads=[qraw, sinT_t], writes=[T[2]])
                kb.op("dve", lambda: nc.vector.tensor_tensor(out=T[3][:], in0=B, in1=cb, op=ALU.mult),
                      reads=[qraw, cosT_t], writes=[T[3]])
                kb.op("dve", lambda: nc.vector.tensor_tensor(out=qT[:, 2 * hb:2 * hb + 2, 0, :], in0=T[0][:], in1=T[1][:],
                                                             op=ALU.subtract), reads=[T[0], T[1]], writes=[qT])
                kb.op("dve", lambda: nc.vector.tensor_tensor(out=qT[:, 2 * hb:2 * hb + 2, 1, :], in0=T[2][:], in1=T[3][:],
                                                              op=ALU.add), reads=[T[2], T[3]], writes=[qT])
        for kb_ in range(2):
            pk = P[3 + bk % 2]; bk += 1
            for kc in range(KC):
                kb.op("pe", lambda: nc.tensor.matmul(out=pk[:], lhsT=xnT[:, kc, :],
                                                     rhs=Win[:, kc, KCOL + kb_ * 512:KCOL + (kb_ + 1) * 512],
                                                     start=(kc == 0), stop=(kc == KC - 1)),
                      reads=[Win, xnT], writes=[pk])
            kb.op("act", lambda: nc.scalar.copy(out=kraw[:].rearrange("p a b t -> p (a b t)"), in_=pk[:]),
                  reads=[pk], writes=[kraw])
            A = kraw[:, :, 0, :]; B = kraw[:, :, 1, :]
            ck = cosk_t[:, 2 * kb_:2 * kb_ + 2, :]; sk = sink_t[:, 2 * kb_:2 * kb_ + 2, :]
            kb.op("dve", lambda: nc.vector.tensor_tensor(out=T[0][:], in0=A, in1=ck, op=ALU.mult),
                  reads=[kraw, cosk_t], writes=[T[0]])
            kb.op("dve", lambda: nc.vector.tensor_tensor(out=T[1][:], in0=B, in1=sk, op=ALU.mult),
                  reads=[kraw, sink_t], writes=[T[1]])
            kb.op("dve", lambda: nc.vector.tensor_tensor(out=T[2][:], in0=A, in1=sk, op=ALU.mult),
                  reads=[kraw, sink_t], writes=[T[2]])
            kb.op("dve", lambda: nc.vector.tensor_tensor(out=T[3][:], in0=B, in1=ck, op=ALU.mult),
                  reads=[kraw, cosk_t], writes=[T[3]])
            kb.op("dve", lambda: nc.vector.tensor_tensor(out=kt[:, 2 * kb_:2 * kb_ + 2, 0, :], in0=T[0][:], in1=T[1][:],
                                                         op=ALU.subtract), reads=[T[0], T[1]], writes=[kt])
            kb.op("dve", lambda: nc.vector.tensor_tensor(out=kt[:, 2 * kb_:2 * kb_ + 2, 1, :], in0=T[2][:], in1=T[3][:],
                                                          op=ALU.add), reads=[T[2], T[3]], writes=[kt])
        for h in range(RH):
            pv = P[3 + bk % 2]; bk += 1
            for kc in range(KC):
                kb.op("pe", lambda: nc.tensor.matmul(out=pv[:], lhsT=xnT[:, kc, :],
                                                     rhs=Win[:, kc, VCOL + h * 512:VCOL + (h + 1) * 512],
                                                     start=(kc == 0), stop=(kc == KC - 1)),
                      reads=[Win, xnT], writes=[pv])
            kb.op("act", lambda: nc.scalar.copy(out=v_sb[:, h, :], in_=pv[:]), reads=[pv], writes=[v_sb])
        if not state_only:
            for h in range(RH if stage >= 2 else 0):
                pg = P[3 + bk % 2]; bk += 1
                for kc in range(KC):
                    kb.op("pe", lambda: nc.tensor.matmul(out=pg[:], lhsT=xnT[:, kc, :],
                                                         rhs=Win[:, kc, GCOL + h * 512:GCOL + (h + 1) * 512],
                                                         start=(kc == 0), stop=(kc == KC - 1)),
                          reads=[Win, xnT], writes=[pg])
                kb.op("act", lambda: nc.scalar.activation(out=gs[:, h * 512:(h + 1) * 512], in_=pg[:], func=ACT.Silu),
                      reads=[pg], writes=[gs])
            for h in range(RH if stage >= 2 else 0):
                for blk in range(2):
                    kb.op("pe", lambda: nc.tensor.transpose(out=p0b[:, h * 2 + blk, :], in_=kt[:, h, blk, :],
                                                            identity=ident[:]), reads=[kt, ident], writes=[P[0]])
            if stage >= 2:
                kb.op("dve", lambda: nc.vector.tensor_copy(out=kT[:].rearrange("p h b t -> p (h b) t"), in_=p0b),
                      reads=[P[0]], writes=[kT])
            p5 = P[5]; p5v = p5[:].rearrange("p (h t) -> p h t", h=RH)
            for h in range(RH if stage >= 3 else 0):
                for blk in range(2):
                    kb.op("pe", lambda: nc.tensor.matmul(out=p5v[:, h, :], lhsT=kT[:, h, blk, :], rhs=qT[:, h, blk, :],
                                                         start=(blk == 0), stop=(blk == 1)),
                          reads=[kT, qT], writes=[p5])
            if stage >= 3:
              kb.op("dve", lambda: nc.vector.tensor_tensor(out=PT[:], in0=p5v,
                                                         in1=maskT[:].unsqueeze(1).broadcast_to([128, RH, 128]),
                                                         op=ALU.mult), reads=[p5, maskT], writes=[PT])
            kb.op("dve", lambda: nc.vector.memset(ms4[:], 0.0), writes=[ms4])
            for h in range(RH if stage >= 4 else 0):
                po = P[6 + h % 2]
                kb.op("pe", lambda: nc.tensor.matmul(out=po[:], lhsT=PT[:, h, :], rhs=v_sb[:, h, :], start=True, stop=False),
                      reads=[PT, v_sb], writes=[po])
                kb.op("pe", lambda: nc.tensor.matmul(out=po[:], lhsT=qT[:, h, 0, :], rhs=Sb[:, 0, h, :], start=False, stop=False),
                      reads=[qT, Sb], writes=[po])
                kb.op("pe", lambda: nc.tensor.matmul(out=po[:], lhsT=qT[:, h, 1, :], rhs=Sb[:, 1, h, :], start=False, stop=True),
                      reads=[qT, Sb], writes=[po])
                kb.op("act", lambda: nc.scalar.activation(out=junk[:, 0:DV], in_=po[:], func=ACT.Square, scale=float(DV) ** -0.5,
                                                          accum_out=ms4[:, h:h + 1]), reads=[po, ms4], writes=[junk, ms4])
                kb.op("dve", lambda: nc.vector.tensor_copy(out=osb[:, h, :], in_=po[:]), reads=[po], writes=[osb])
        for h in range(RH):
            for dc in range(2):
                pkv = P[3 + bk % 2]; bk += 1
                kb.op("pe", lambda: nc.tensor.matmul(out=pkv[:], lhsT=kt[:, h, dc, :], rhs=v_sb[:, h, :], start=True, stop=True),
                      reads=[kt, v_sb], writes=[pkv])
                kb.op("act", lambda: nc.scalar.activation(out=S[:, dc, h, :], in_=S[:, dc, h, :], func=ACT.Copy,
                                                          scale=GAM[h] ** 128), reads=[S], writes=[S])
                kb.op("dve", lambda: nc.vector.scalar_tensor_tensor(out=S[:, dc, h, :], in0=pkv[:], scalar=GAM[h] ** 127,
                                                                    in1=S[:, dc, h, :], op0=ALU.mult, op1=ALU.add),
                      reads=[pkv, S], writes=[S])
                if not state_only:
                    kb.op("act", lambda: nc.scalar.activation(out=Sb[:, dc, h, :], in_=S[:, dc, h, :], func=ACT.Copy,
                                                              scale=GAM[h]), reads=[S], writes=[Sb])
        if not state_only and stage >= 5:
            og = OG[c % 2]
            kb.op("dve", lambda: nc.vector.tensor_tensor(out=f4[:], in0=ms4[:], in1=G2I[:], op=ALU.mult),
                  reads=[ms4, G2I], writes=[f4])
            rstd_from_ms(kb, f4, f4)
            kb.op("dve", lambda: nc.vector.tensor_tensor(out=f4[:], in0=f4[:], in1=GI[:], op=ALU.mult),
                  reads=[f4, GI], writes=[f4])
            for h in range(RH):
                kb.op("dve", lambda: nc.vector.scalar_tensor_tensor(out=og[:, h * DV:(h + 1) * DV], in0=osb[:, h, :], scalar=f4[:, h:h + 1],
                                                          in1=gs[:, h * DV:(h + 1) * DV], op0=ALU.mult, op1=ALU.mult),
                      reads=[osb, f4, gs], writes=[og])
            outs.append(kb.dma("sp", og_out[r0:r0 + 128, :], og[:], reads=[og]))
    for dc in range(2):
        outs.append(kb.dma("sp", s_out[dc], S[:, dc, :, :], reads=[S]))
    return outs

D = 1024

def post_norm_residual(kb, py, x_rows_ap, Gpost, out_rows_ap, xb, ot, ss2, rs2, junk):
    nc = kb.nc
    kb.dma("sp", xb[:], x_rows_ap, writes=[xb])
    kb.op("dve", lambda: nc.vector.memset(ss2[:], 0.0), writes=[ss2])
    kb.op("act", lambda: nc.scalar.activation(out=junk[:], in_=py[:], func=ACT.Square, scale=float(D) ** -0.5,
                                              accum_out=ss2[:, 0:1]), reads=[py, ss2], writes=[junk, ss2])
    rstd_from_ms(kb, rs2, ss2)
    kb.op("dve", lambda: nc.vector.scalar_tensor_tensor(out=ot[:], in0=py[:], scalar=rs2[:, 0:1], in1=Gpost[:],
                                                        op0=ALU.mult, op1=ALU.mult), reads=[py, rs2, Gpost], writes=[ot])
    kb.op("pool", lambda: nc.gpsimd.tensor_tensor(out=ot[:], in0=ot[:], in1=xb[:], op=ALU.add), reads=[ot, xb], writes=[ot])
    return kb.dma("sp", out_rows_ap, ot[:], reads=[ot])


def build_outproj(kb, NT, KD, og_in, w_out, g_post, x_in, h_out, ident, og_buf=None, x_buf=None, h_buf=None):
    nc = kb.nc
    KC = KD // 128
    Wout = kb.sb([128, KC, D], BF16, "Wo")
    for kc in range(KC):
        kb.dma("pool", Wout[:, kc, :], w_out[kc * 128:(kc + 1) * 128, :], writes=[Wout])
    Gpost = kb.sb([128, D], F32, "Gpost")
    kb.dma("sp", Gpost[:], g_post, writes=[Gpost])
    OGT = [kb.sb([128, KD], BF16, "ogt") for _ in range(2)]
    ogT = [kb.sb([128, KC, 128], BF16, "ogT") for _ in range(2)]
    XB = [kb.sb([128, D], F32, "xb") for _ in range(2)]
    OT = [kb.sb([128, D], F32, "ot") for _ in range(2)]
    SS = [kb.sb([128, 1], F32, "ss") for _ in range(2)]
    RS = [kb.sb([128, 1], F32, "rs") for _ in range(2)]
    junk = kb.sb([128, D], BF16, "junk")
    TP = [kb.ps([128, 512], F32, "tp") for _ in range(2)]
    PY = [kb.ps([128, D], F32, "py") for _ in range(2)]
    outs = []
    rd = [og_buf] if og_buf is not None else []
    rdx = [x_buf] if x_buf is not None else []
    for t in range(NT // 128):
        r0 = t * 128
        og = OGT[t % 2]; oT = ogT[t % 2]; py = PY[t % 2]
        kb.dma("sp", og[:], og_in[r0:r0 + 128, :], reads=rd, writes=[og])
        for kc in range(KC):
            tp = TP[(kc // 8) % 2]
            tpb = tp[:].bitcast(BF16).rearrange("p (a b) -> p a b", b=128)
            kb.op("pe", lambda: nc.tensor.transpose(out=tpb[:, kc % 8, :], in_=og[:, kc * 128:(kc + 1) * 128],
                                                    identity=ident[:]), reads=[og, ident], writes=[tp])
            if kc % 8 == 7:
                kb.op("act", lambda: nc.scalar.copy(out=oT[:, kc - 7:kc + 1, :], in_=tpb), reads=[tp], writes=[oT])
        for cb in range(2):
            for kc in range(KC):
                kb.op("pe", lambda: nc.tensor.matmul(out=py[:, cb * 512:(cb + 1) * 512], lhsT=oT[:, kc, :],
                                                     rhs=Wout[:, kc, cb * 512:(cb + 1) * 512],
                                                     start=(kc == 0), stop=(kc == KC - 1)), reads=[oT, Wout], writes=[py])
        if x_buf is not None:
            pass
        tok = post_norm_residual(kb, py, x_in[r0:r0 + 128, :], Gpost, h_out[r0:r0 + 128, :], XB[t % 2], OT[t % 2],
                                 SS[t % 2], RS[t % 2], junk)
        if h_buf is not None:
            h_buf.last_w = tok
        outs.append(tok)
    return outs


def build_normproj(kb, NT, x_in, g_pre, w, C, out_ap, ident, sig_from=None, sig_out=None, x_buf=None):
    nc = kb.nc
    KC = 8
    W = kb.sb([128, KC, C], BF16, "Wp")
    for kc in range(KC):
        kb.dma("pool", W[:, kc, :], w[kc * 128:(kc + 1) * 128, :], writes=[W])
    G = kb.sb([128, D], F32, "G")
    kb.dma("sp", G[:], g_pre, writes=[G])
    XA = [kb.sb([128, D], F32, "xa") for _ in range(2)]
    xn = kb.sb([128, D], BF16, "xn")
    xnT = kb.sb([128, KC, 128], BF16, "xnT")
    junk = kb.sb([128, D], BF16, "junk")
    ss = kb.sb([128, 1], F32, "ss"); rs = kb.sb([128, 1], F32, "rs")
    CM = C if sig_from is None else sig_from
    OB = [kb.sb([128, CM], BF16, "ob") for _ in range(2)]
    if sig_from is not None:
        SG = [kb.sb([128, C - sig_from], F32, "sgo") for _ in range(2)]
    tp = kb.ps([128, 512], F32, "tp")
    tpb = tp[:].bitcast(BF16).rearrange("p (a b) -> p a b", b=128)
    PB = [kb.ps([128, 512], F32, "pb") for _ in range(3)]
    outs = []
    nb = 0
    rdx = [x_buf] if x_buf is not None else []
    for t in range(NT // 128):
        r0 = t * 128
        xa = XA[t % 2]; ob = OB[t % 2]
        kb.dma("sp", xa[:], x_in[r0:r0 + 128, :], reads=rdx, writes=[xa])
        kb.op("dve", lambda: nc.vector.memset(ss[:], 0.0), writes=[ss])
        kb.op("act", lambda: nc.scalar.activation(out=junk[:], in_=xa[:], func=ACT.Square, scale=float(D) ** -0.5,
                                                  accum_out=ss[:, 0:1]), reads=[xa, ss], writes=[junk, ss])
        rstd_from_ms(kb, rs, ss)
        kb.op("dve", lambda: nc.vector.scalar_tensor_tensor(out=xn[:], in0=xa[:], scalar=rs[:, 0:1], in1=G[:],
                                                            op0=ALU.mult, op1=ALU.mult), reads=[xa, rs, G], writes=[xn])
        for kc in range(KC):
            kb.op("pe", lambda: nc.tensor.transpose(out=tpb[:, kc, :], in_=xn[:, kc * 128:(kc + 1) * 128],
                                                    identity=ident[:]), reads=[xn, ident], writes=[tp])
        kb.op("act", lambda: nc.scalar.copy(out=xnT[:], in_=tpb), reads=[tp], writes=[xnT])
        c0 = 0
        while c0 < C:
            cw = min(512, C - c0)
            if sig_from is not None and c0 < sig_from:
                cw = min(cw, sig_from - c0)
            pb = PB[nb % 3]; nb += 1
            for kc in range(KC):
                kb.op("pe", lambda: nc.tensor.matmul(out=pb[:, 0:cw], lhsT=xnT[:, kc, :], rhs=W[:, kc, c0:c0 + cw],
                                                     start=(kc == 0), stop=(kc == KC - 1)), reads=[xnT, W], writes=[pb])
            if sig_from is not None and c0 >= sig_from:
                sg = SG[t % 2]
                kb.op("act", lambda: nc.scalar.activation(out=sg[:, c0 - sig_from:c0 - sig_from + cw], in_=pb[:, 0:cw],
                                                          func=ACT.Sigmoid), reads=[pb], writes=[sg])
            else:
                kb.op("act", lambda: nc.scalar.copy(out=ob[:, c0:c0 + cw], in_=pb[:, 0:cw]), reads=[pb], writes=[ob])
            c0 += cw
        outs.append(kb.dma("sp", out_ap[r0:r0 + 128, :], ob[:], reads=[ob]))
        if sig_from is not None:
            outs.append(kb.dma("sp", sig_out[r0:r0 + 128, :], SG[t % 2][:], reads=[SG[t % 2]]))
    return outs


NEG = -30000.0
HD = 64; GR = 4; NG = 4


def build_compress(kb, NB, XT, W1, W2, PEcol, kcv_out):
    nc = kb.nc
    W1s = kb.sb([128, 2, 16, 256], BF16, "W1s")
    W2s = kb.sb([128, 2, 2, 64], BF16, "W2s")
    PEc = kb.sb([128, 2, 16], BF16, "PEc")
    for kv in range(2):
        kb.dma("pool", W1s[:, kv, :, :], W1[kv].rearrange("(c p) n -> p c n", p=128), writes=[W1s])
        kb.dma("pool", W2s[:, kv, :, :], W2[kv].rearrange("(c p) n -> p c n", p=128), writes=[W2s])
        kb.dma("pool", PEc[:, kv, :], PEcol[kv], writes=[PEc])
    pebs = kb.sb([128, 2, 2], F32, "pebs")
    pp = kb.ps([128, 512], F32, "pp")
    for kv in range(2):
        for hc in range(2):
            for cc in range(16):
                kb.op("pe", lambda: nc.tensor.matmul(out=pp[:, 0:1], lhsT=W1s[:, kv, cc, hc * 128:(hc + 1) * 128],
                                                     rhs=PEc[:, kv, cc:cc + 1], start=(cc == 0), stop=(cc == 15)),
                      reads=[W1s, PEc], writes=[pp])
            kb.op("dve", lambda: nc.vector.tensor_copy(out=pebs[:, kv, hc:hc + 1], in_=pp[:, 0:1]), reads=[pp], writes=[pebs])
    NW = min(128, NB)
    XTt = [kb.sb([128, 16, NW], BF16, "XTt") for _ in range(2)]
    hT = [kb.sb([128, 2, NW], BF16, "hT") for _ in range(2)]
    ob = [kb.sb([128, 64], BF16, "ob") for _ in range(2)]
    PH = [kb.ps([128, 512], F32, "ph") for _ in range(2)]
    PO = [kb.ps([128, 512], F32, "po") for _ in range(2)]
    outs = []
    it = 0
    for kv in range(2):
        for g in range(NG):
            for nt in range((NB + 127) // 128):
                xt = XTt[it % 2]; ht = hT[it % 2]; o = ob[it % 2]; ph = PH[it % 2]; po = PO[it % 2]; it += 1
                kb.dma("sp", xt[:], XT[kv, g, :, :, nt * 128:nt * 128 + NW].rearrange("c p n -> p c n"), writes=[xt])
                for hc in range(2):
                    for cc in range(16):
                        kb.op("pe", lambda: nc.tensor.matmul(out=ph[:, hc * 128:hc * 128 + NW],
                                                             lhsT=W1s[:, kv, cc, hc * 128:(hc + 1) * 128], rhs=xt[:, cc, :],
                                                             start=(cc == 0), stop=(cc == 15)), reads=[W1s, xt], writes=[ph])
                    kb.op("act", lambda: nc.scalar.activation(out=ht[:, hc, :], in_=ph[:, hc * 128:hc * 128 + NW],
                                                              func=ACT.Silu, bias=pebs[:, kv, hc:hc + 1]),
                          reads=[ph, pebs], writes=[ht])
                for hc in range(2):
                    kb.op("pe", lambda: nc.tensor.matmul(out=po[0:NW, 0:64], lhsT=ht[:, hc, :], rhs=W2s[:, kv, hc, :],
                                                         start=(hc == 0), stop=(hc == 1)), reads=[ht, W2s], writes=[po])
                kb.op("dve", lambda: nc.vector.tensor_copy(out=o[0:NW, :], in_=po[0:NW, 0:64]), reads=[po], writes=[o])
                outs.append(kb.dma("sp", kcv_out[kv, nt * 128:nt * 128 + NW, g, :], o[0:NW, :], reads=[o]))
    return outs


def build_nsa_attn(kb, NQT, CPB, qin, gates, A, o_out, identb, q_buf=None):
    nc = kb.nc
    NVT = NQT * CPB + CPB - 1
    NSB = (NVT + 15) // 16
    NBLK = NSB * 32
    NCT = (8 * NVT + 16 + 127) // 128
    NKV = NVT * 128
    rdq = [q_buf] if q_buf is not None else []
    identf = kb.sb([128, 128], F32, "identf")
    kb.dma("sp", identf[:], A["identf"], writes=[identf])
    Mmap = kb.sb([128, NCT, NBLK], BF16, "Mmap")
    kb.dma("pool", Mmap[:], A["Mmap"], writes=[Mmap])
    MNear = kb.sb([16, NQT, NBLK], BF16, "MNear")
    kb.dma("pool", MNear[:], A["MNear"], writes=[MNear])
    bmax = kb.sb([128, 1], F32, "bmax")
    rbf = kb.sb([128, 512], F32, "rbf")
    kb.dma("sp", rbf[:], A["relb_rep"], writes=[rbf])
    kb.op("dve", lambda: nc.vector.tensor_reduce(out=bmax[:], in_=rbf[:], axis=AX.X, op=ALU.max, apply_absolute_value=True),
          reads=[rbf], writes=[bmax])
    ones64 = kb.sb([64, 128], F32, "ones64")
    kb.op("dve", lambda: nc.vector.memset(ones64[:], 1.0), writes=[ones64])
    KsT = kb.sb([128, NKV], BF16, "KsT")
    Vs = kb.sb([128, NVT, 65], BF16, "Vs")
    KcT = kb.sb([128, NCT * 128], BF16, "KcT")
    Vc = kb.sb([128, NCT, 65], BF16, "Vc")
    VcN = kb.sb([16, NQT, 65], BF16, "VcN")
    TS = kb.sb([128, 2, 2, 512], BF16, "TS")
    TW = kb.sb([128, 5, 2, 512], BF16, "TW")
    TC = kb.sb([16, 2, 512], BF16, "TC")
    tf = kb.sb([128, 512], F32, "tf"); tr = kb.sb([128, 512], F32, "tr")
    kd = kb.sb([64, 3], F32, "kd"); kd1 = kb.sb([64, 1], F32, "kd1"); dg = kb.sb([64, 64], F32, "dg")
    KDrow = kb.sb([128, 64], F32, "KDrow")
    kwscr = kb.sb([64, 2048], BF16, "kwscr"); kdw = kb.sb([64, 16], F32, "kdw")
    b31 = kb.sb([128, 4], F32, "b31"); b31h = kb.sb([128, 4], BF16, "b31h"); b31r = kb.sb([128, 4], F32, "b31r")
    AUG1 = [kb.sb([128, GR, 128], BF16, "AUG1") for _ in range(2)]
    AUG2 = [kb.sb([128, NSB, 128], BF16, "AUG2") for _ in range(2)]
    QA = [[kb.sb([128, 512], BF16, "QA%d" % i) for i in range(NSB)] for _ in range(2)]
    QA0 = [kb.sb([128, 512], BF16, "QA0") for _ in range(2)]
    QT = [kb.sb([128, GR * HD], BF16, "qt") for _ in range(2)]
    GT = [kb.sb([128, 48], F32, "gt") for _ in range(2)]
    MS = [kb.sb([128, 3, NBLK], F32, "ms") for _ in range(2)]
    KwT = [kb.sb([128, 5 * 128], BF16, "KwT") for _ in range(2)]
    Vw = [kb.sb([128, 5, 65], BF16, "Vw") for _ in range(2)]
    absq = kb.sb([128, GR, HD], F32, "absq")
    U4 = kb.sb([128, GR], F32, "U4")
    PTc = kb.sb([128, NCT + 1, 512], BF16, "PTc")
    PTn = kb.sb([16, 512], BF16, "PTn")
    PTr = [kb.sb([128, 1024], BF16, "PTr") for _ in range(3)]
    oTs = [kb.sb([65, 512], F32, "oT") for _ in range(2)]
    otok = [kb.sb([128, 3, GR, 65], F32, "otok") for _ in range(2)]
    rinv = [kb.sb([128, 3, GR], F32, "rinv") for _ in range(2)]
    fb = kb.sb([128, 3, GR], F32, "fb")
    imp = kb.sb([128, NBLK], F32, "imp")
    imw = kb.sb([128, NBLK], F32, "imw")
    m8 = kb.sb([128, 8], F32, "m8"); thr = kb.sb([128, 1], F32, "thr")
    oacc = kb.sb([128, GR, HD], F32, "oacc"); otmp = kb.sb([128, GR, HD], F32, "otmp")
    OB = [kb.sb([128, GR * HD], BF16, "obo") for _ in range(2)]
    PSB = [kb.ps([128, 1024], F32, "psS%d" % i) for i in range(2)]
    PO_ = [kb.ps([128, 512], F32, "psO%d" % i) for i in range(2)]
    PY = kb.ps([128, 512], F32, "psYQ")
    PQ = PY
    PX = kb.ps([128, 512], F32, "psX")
    outs = []
    cnt = {"s": 0, "p": 0, "it": 0}

    def split_hilo(dst_hi, dst_lo, src_f32, rows):
        kb.op("dve", lambda: nc.vector.tensor_copy(out=dst_hi, in_=src_f32[0:rows, :]), reads=[tf], writes=[TS, TW, TC])
        kb.op("dve", lambda: nc.vector.tensor_copy(out=tr[0:rows, :], in_=dst_hi), reads=[TS, TW, TC], writes=[tr])
        kb.op("dve", lambda: nc.vector.tensor_tensor(out=dst_lo, in0=src_f32[0:rows, :], in1=tr[0:rows, :], op=ALU.subtract),
              reads=[tf, tr], writes=[TS, TW, TC])

    def softmax_tile(lhsT_ap, rhs_ap, KR, rows, toep=None, identrows=None, rd=None, dst=None):
        psb, ps = dst
        kb.op("pe", lambda: nc.tensor.matmul(out=ps[0:rows, :], lhsT=lhsT_ap, rhs=rhs_ap, start=True, stop=(toep is None)),
              reads=rd, writes=[psb])
        if toep is not None:
            hi, lo = toep
            kb.op("pe", lambda: nc.tensor.matmul(out=ps[0:rows, :], lhsT=identb[0:rows, 0:rows], rhs=hi, start=False, stop=False),
                  reads=[identb, TS, TW, TC], writes=[psb])
            kb.op("pe", lambda: nc.tensor.matmul(out=ps[0:rows, :], lhsT=identb[0:rows, 0:rows], rhs=lo, start=False, stop=True),
                  reads=[identb, TS, TW, TC], writes=[psb])
        return ps

    for g in range(NG):
        kb.dma("sp", KsT[:], A["KsT"][g], writes=[KsT])
        kb.dma("sp", Vs[:], A["Vs"][g].rearrange("(t p) e -> p t e", p=128), writes=[Vs])
        kb.dma("sp", KcT[:], A["KcT"][g], writes=[KcT])
        kb.dma("sp", Vc[:], A["Vc"][g].rearrange("(t p) e -> p t e", p=128), writes=[Vc])
        kb.dma("sp", VcN[:], A["VcN"][g].rearrange("l u e -> u l e"), writes=[VcN])
        for m in range(2):
            kb.dma("sp", tf[:], A["TOEPS"][g, m], writes=[tf])
            split_hilo(TS[:, m, 0, :], TS[:, m, 1, :], tf, 128)
        for m in range(5):
            kb.dma("sp", tf[:], A["TOEPW"][g, m], writes=[tf])
            split_hilo(TW[:, m, 0, :], TW[:, m, 1, :], tf, 128)
        kb.dma("sp", tf[0:16, :], A["TOEPC"][g], writes=[tf])
        split_hilo(TC[:, 0, :], TC[:, 1, :], tf, 16)
        kb.dma("sp", b31[:], A["B31"][g], writes=[b31])
        kb.op("dve", lambda: nc.vector.tensor_copy(out=b31h[:], in_=b31[:]), reads=[b31], writes=[b31h])
        kb.op("dve", lambda: nc.vector.tensor_copy(out=b31r[:], in_=b31h[:]), reads=[b31h], writes=[b31r])
        kb.op("dve", lambda: nc.vector.tensor_tensor(out=b31r[:], in0=b31[:], in1=b31r[:], op=ALU.subtract),
              reads=[b31, b31r], writes=[b31r])
        for pp_ in range(2):
            a1 = AUG1[pp_]; a2 = AUG2[pp_]
            kb.op("dve", lambda: nc.vector.memset(a1[:], 0.0), writes=[a1])
            kb.op("dve", lambda: nc.vector.memset(a2[:], 0.0), writes=[a2])
            kb.op("dve", lambda: nc.vector.memset(a1[:, :, 97:98], 1.0), writes=[a1])
            kb.op("dve", lambda: nc.vector.tensor_copy(out=a1[:, :, 98:99], in_=b31h[:].unsqueeze(2)), reads=[b31h], writes=[a1])
            kb.op("dve", lambda: nc.vector.tensor_copy(out=a1[:, :, 99:100], in_=b31r[:].unsqueeze(2)), reads=[b31r], writes=[a1])
        kb.op("dve", lambda: nc.vector.tensor_reduce(out=kd[:, 0:1], in_=KsT[0:64, :], axis=AX.X, op=ALU.max, apply_absolute_value=True),
              reads=[KsT], writes=[kd])
        kb.op("dve", lambda: nc.vector.tensor_reduce(out=kd[:, 1:2], in_=KcT[0:64, :], axis=AX.X, op=ALU.max, apply_absolute_value=True),
              reads=[KcT], writes=[kd])
        nch = (NKV + 2047) // 2048
        for c in range(nch):
            w = min(2048, NKV - c * 2048)
            kb.dma("sp", kwscr[:, 0:w], A["KwT"][g][0:64, c * 2048:c * 2048 + w], writes=[kwscr])
            kb.op("dve", lambda: nc.vector.tensor_reduce(out=kdw[:, c:c + 1], in_=kwscr[:, 0:w], axis=AX.X, op=ALU.max,
                                                         apply_absolute_value=True), reads=[kwscr], writes=[kdw])
        kb.op("dve", lambda: nc.vector.tensor_reduce(out=kd[:, 2:3], in_=kdw[:, 0:nch], axis=AX.X, op=ALU.max), reads=[kdw], writes=[kd])
        kb.op("dve", lambda: nc.vector.tensor_reduce(out=kd1[:], in_=kd[:], axis=AX.X, op=ALU.max), reads=[kd], writes=[kd1])
        kb.op("dve", lambda: nc.vector.tensor_scalar(out=dg[:], in0=identf[0:64, 0:64], scalar1=kd1[:, 0:1], scalar2=None, op0=ALU.mult),
              reads=[identf, kd1], writes=[dg])
        kb.op("pe", lambda: nc.tensor.matmul(out=PX[:, 0:64], lhsT=ones64[:], rhs=dg[:], start=True, stop=True),
              reads=[ones64, dg], writes=[PX])
        kb.op("dve", lambda: nc.vector.tensor_copy(out=KDrow[:], in_=PX[:, 0:64]), reads=[PX], writes=[KDrow])
        def pre(l, par):
            V = CPB * l + CPB - 1
            qt = QT[par]; gt = GT[par]; ms = MS[par]; kw = KwT[par]; vw = Vw[par]
            aug1 = AUG1[par]; aug2 = AUG2[par]; qa0 = QA0[par]; qa = QA[par]; otk = otok[par]; rnv = rinv[par]
            kb.dma("sp", qt[:], qin[l * 128:(l + 1) * 128, g * 256:(g + 1) * 256], reads=rdq, writes=[qt])
            kb.dma("sp", gt[:], gates[l * 128:(l + 1) * 128, :], reads=rdq, writes=[gt])
            kb.dma("sp", ms[:], A["MSEL"][l].rearrange("c q b -> q c b"), writes=[ms])
            wt0 = max(0, V - 4); nwt = V - wt0 + 1
            kb.dma("sp", kw[:, 0:nwt * 128], A["KwT"][g][:, wt0 * 128:(V + 1) * 128], writes=[kw])
            kb.dma("sp", vw[:, 0:nwt, :], A["Vw"][g][wt0 * 128:(V + 1) * 128, :].rearrange("(t p) e -> p t e", p=128), writes=[vw])
            qv = qt[:].rearrange("p (h d) -> p h d", h=GR)
            kb.op("act", lambda: nc.scalar.activation(out=aug1[:, :, 0:HD], in_=qv, func=ACT.Copy, scale=HD ** -0.5),
                  reads=[qt], writes=[aug1])
            kb.op("act", lambda: nc.scalar.activation(out=absq[:], in_=qv, func=ACT.Abs), reads=[qt], writes=[absq])
            kb.op("dve", lambda: nc.vector.tensor_tensor(out=absq[:], in0=absq[:], in1=KDrow[:].unsqueeze(1).broadcast_to([128, GR, HD]),
                                                         op=ALU.mult), reads=[absq, KDrow], writes=[absq])
            kb.op("dve", lambda: nc.vector.tensor_reduce(out=U4[:], in_=absq[:], axis=AX.X, op=ALU.add), reads=[absq], writes=[U4])
            kb.op("dve", lambda: nc.vector.tensor_scalar(out=U4[:], in0=U4[:], scalar1=-(HD ** -0.5), scalar2=bmax[:, 0:1],
                                                         op0=ALU.mult, op1=ALU.subtract), reads=[U4, bmax], writes=[U4])
            kb.op("dve", lambda: nc.vector.tensor_copy(out=aug1[:, :, 96:97], in_=U4[:].unsqueeze(2)), reads=[U4], writes=[aug1])
            yield
            for h in range(GR):
                kb.op("pe", lambda: nc.tensor.matmul(out=PQ[:, h * 128:(h + 1) * 128], lhsT=aug1[:, h, :], rhs=identb[:],
                                                     start=True, stop=True), reads=[aug1, identb], writes=[PQ])
            kb.op("act", lambda: nc.scalar.copy(out=qa0[:], in_=PQ[:]), reads=[PQ], writes=[qa0])
            yield
            nfar = 8 * V - 9
            oc = PO_[cnt["p"] % 2]; cnt["p"] += 1
            tiles = []
            n0 = 0
            while n0 < nfar:
                rows = min(128, nfar - n0)
                tiles.append((n0 // 128, rows))
                n0 += 128
            first = True
            for (tix, rows) in tiles:
                ps = softmax_tile(KcT[0:100, tix * 128:tix * 128 + rows], qa0[0:100, :], 100, rows, rd=[KcT, qa0], dst=(PY, PY[:, :]))
                kb.op("act", lambda: nc.scalar.activation(out=PTc[0:rows, tix, :], in_=ps[0:rows, :], func=ACT.Exp),
                      reads=[PY], writes=[PTc])
                kb.op("pe", lambda: nc.tensor.matmul(out=oc[0:65, :], lhsT=Vc[0:rows, tix, :], rhs=PTc[0:rows, tix, :],
                                                     start=first, stop=False), reads=[Vc, PTc], writes=[oc])
                first = False
                yield
            ps = softmax_tile(KcT[0:98, nfar:nfar + 16], qa0[0:98, :], 98, 16, toep=(TC[:, 0, :], TC[:, 1, :]), rd=[KcT, qa0], dst=(PY, PY[:, :]))
            kb.op("act", lambda: nc.scalar.activation(out=PTn[:], in_=ps[0:16, :], func=ACT.Exp), reads=[PY], writes=[PTn])
            kb.op("pe", lambda: nc.tensor.matmul(out=oc[0:65, :], lhsT=VcN[:, l, :], rhs=PTn[:], start=first, stop=True),
                  reads=[VcN, PTn], writes=[oc])
            yield
            finish_branch(0, oc, otk, rnv, oTs[0])
            yield
            for h in range(GR):
                fst = True
                for (tix, rows) in tiles:
                    kb.op("pe", lambda: nc.tensor.matmul(out=PY[:, 0:NBLK], lhsT=PTc[0:rows, tix, h * 128:(h + 1) * 128],
                                                         rhs=Mmap[0:rows, tix, :], start=fst, stop=False),
                          reads=[PTc, Mmap], writes=[PY])
                    fst = False
                assert not fst
                kb.op("pe", lambda: nc.tensor.matmul(out=PY[:, 0:NBLK], lhsT=PTn[:, h * 128:(h + 1) * 128], rhs=MNear[:, l, :],
                                                     start=False, stop=True), reads=[PTn, MNear], writes=[PY])
                if h == 0:
                    kb.op("dve", lambda: nc.vector.tensor_scalar(out=imp[:], in0=PY[:, 0:NBLK], scalar1=rnv[:, 0, 0:1], scalar2=None,
                                                                 op0=ALU.mult), reads=[PY, rnv], writes=[imp])
                else:
                    kb.op("dve", lambda: nc.vector.scalar_tensor_tensor(out=imp[:], in0=PY[:, 0:NBLK], scalar=rnv[:, 0, h:h + 1],
                                                                        in1=imp[:], op0=ALU.mult, op1=ALU.add),
                          reads=[PY, rnv, imp], writes=[imp])
                yield
            kb.op("dve", lambda: nc.vector.tensor_tensor(out=imp[:], in0=imp[:], in1=ms[:, 0, :], op=ALU.mult), reads=[imp, ms], writes=[imp])
            kb.op("dve", lambda: nc.vector.tensor_tensor(out=imp[:], in0=imp[:], in1=ms[:, 1, :], op=ALU.add), reads=[imp, ms], writes=[imp])
            kb.op("dve", lambda: nc.vector.max(out=m8[:], in_=imp[:]), reads=[imp], writes=[m8])
            kb.op("dve", lambda: nc.vector.match_replace(out=imw[:], in_to_replace=m8[:], in_values=imp[:], imm_value=-3.0e38),
                  reads=[m8, imp], writes=[imw])
            yield
            kb.op("dve", lambda: nc.vector.max(out=m8[:], in_=imw[:]), reads=[imw], writes=[m8])
            kb.op("dve", lambda: nc.vector.tensor_reduce(out=thr[:], in_=m8[:], axis=AX.X, op=ALU.min), reads=[m8], writes=[thr])
            kb.op("dve", lambda: nc.vector.tensor_scalar(out=imw[:], in0=imp[:], scalar1=thr[:, 0:1], scalar2=None, op0=ALU.is_ge),
                  reads=[imp, thr], writes=[imw])
            kb.op("dve", lambda: nc.vector.tensor_tensor(out=imw[:], in0=imw[:], in1=ms[:, 2, :], op=ALU.mult), reads=[imw, ms], writes=[imw])
            kb.op("dve", lambda: nc.vector.tensor_scalar(out=aug2[:, :, 64:96], in0=imw[:].rearrange("p (s j) -> p s j", j=32),
                                                         scalar1=-NEG, scalar2=NEG, op0=ALU.mult, op1=ALU.add),
                  reads=[imw], writes=[aug2])
            yield
            nsb = (V + 1 + 15) // 16
            for sb in range(nsb):
                for h in range(GR):
                    kb.op("pe", lambda: nc.tensor.matmul(out=PQ[:, h * 128:(h + 1) * 128], lhsT=aug1[:, h, :], rhs=identb[:],
                                                         start=True, stop=False), reads=[aug1, identb], writes=[PQ])
                    kb.op("pe", lambda: nc.tensor.matmul(out=PQ[:, h * 128:(h + 1) * 128], lhsT=aug2[:, sb, :], rhs=identb[:],
                                                         start=False, stop=True), reads=[aug2, identb], writes=[PQ])
                kb.op("dve", lambda: nc.vector.tensor_copy(out=qa[sb][:], in_=PQ[:]), reads=[PQ], writes=[qa[sb]])
                yield

        def finish_branch(b, acc, otk, rnv, oT):
            kb.op("act", lambda: nc.scalar.copy(out=oT[:], in_=acc[0:65, :]), reads=[acc], writes=[oT])
            pxv = PX[:, 0:GR * 65].rearrange("p (h e) -> p h e", h=GR)
            for h in range(GR):
                kb.op("pe", lambda: nc.tensor.matmul(out=pxv[:, h, :], lhsT=oT[:, h * 128:(h + 1) * 128], rhs=identf[0:65, 0:65],
                                                     start=True, stop=True), reads=[oT, identf], writes=[PX])
            kb.op("dve", lambda: nc.vector.tensor_copy(out=otk[:, b, :, :], in_=pxv), reads=[PX], writes=[otk])
            kb.op("dve", lambda: nc.vector.tensor_scalar(out=rnv[:, b, :], in0=otk[:, b, :, 64], scalar1=1e-30, scalar2=None,
                                                         op0=ALU.max), reads=[otk], writes=[rnv])
            kb.op("dve", lambda: nc.vector.reciprocal(out=rnv[:, b, :], in_=rnv[:, b, :]), reads=[rnv], writes=[rnv])

        def step(gen):
            if gen is not None:
                next(gen, None)

        def post(l, par, nxt):
            V = CPB * l + CPB - 1
            gt = GT[par]; kw = KwT[par]; vw = Vw[par]; ob = OB[par]
            qa0 = QA0[par]; qa = QA[par]; otk = otok[par]; rnv = rinv[par]
            wt0 = max(0, V - 4); nwt = V - wt0 + 1
            osel = PO_[cnt["p"] % 2]; cnt["p"] += 1

            def sel_tail(p, psb):
                pt = PTr[p % 3]
                kb.op("act", lambda: nc.scalar.activation(out=pt[:], in_=psb[:], func=ACT.Exp), reads=[psb], writes=[pt])
                for hf in range(2):
                    v = 2 * p + hf
                    kb.op("pe", lambda: nc.tensor.matmul(out=osel[0:65, :], lhsT=Vs[:, v, :], rhs=pt[:, hf * 512:(hf + 1) * 512],
                                                         start=(v == 0), stop=(v == V)), reads=[Vs, pt], writes=[osel])

            pend = None
            for p in range((V + 1) // 2):
                psb = PSB[cnt["s"] % 2]; cnt["s"] += 1
                for hf in range(2):
                    v = 2 * p + hf
                    sb = v // 16
                    dst = (psb, psb[:, hf * 512:(hf + 1) * 512])
                    if v >= V - 1:
                        m = v - (V - 1)
                        softmax_tile(KsT[0:98, v * 128:(v + 1) * 128], qa[sb][0:98, :], 98, 128, toep=(TS[:, m, 0, :], TS[:, m, 1, :]),
                                     rd=[KsT, qa[sb]], dst=dst)
                    else:
                        softmax_tile(KsT[0:100, v * 128:(v + 1) * 128], qa[sb][0:100, :], 100, 128, rd=[KsT, qa[sb]], dst=dst)
                if pend is not None:
                    sel_tail(*pend)
                pend = (p, psb)
                step(nxt)
                step(nxt)
            sel_tail(*pend)
            finish_branch(1, osel, otk, rnv, oTs[1])
            ow = PO_[cnt["p"] % 2]; cnt["p"] += 1

            def win_tail(p, psb, n):
                pt = PTr[p % 3]
                kb.op("act", lambda: nc.scalar.activation(out=pt[:, 0:n * 512], in_=psb[:, 0:n * 512], func=ACT.Exp),
                      reads=[psb], writes=[pt])
                for hf in range(n):
                    i = 2 * p + hf
                    kb.op("pe", lambda: nc.tensor.matmul(out=ow[0:65, :], lhsT=vw[:, i, :], rhs=pt[:, hf * 512:(hf + 1) * 512],
                                                         start=(i == 0), stop=(i == nwt - 1)), reads=[vw, pt], writes=[ow])

            pend = None
            for p in range((nwt + 1) // 2):
                psb = PSB[cnt["s"] % 2]; cnt["s"] += 1
                n = min(2, nwt - 2 * p)
                for hf in range(n):
                    i = 2 * p + hf
                    m = wt0 + i - (V - 4)
                    softmax_tile(kw[0:98, i * 128:(i + 1) * 128], qa0[0:98, :], 98, 128, toep=(TW[:, m, 0, :], TW[:, m, 1, :]),
                                 rd=[kw, qa0], dst=(psb, psb[:, hf * 512:(hf + 1) * 512]))
                if pend is not None:
                    win_tail(*pend)
                pend = (p, psb, n)
                step(nxt)
            win_tail(*pend)
            finish_branch(2, ow, otk, rnv, oTs[1])
            gv = gt[:, g * 12:(g + 1) * 12].rearrange("p (r b) -> p b r", b=3)
            kb.op("dve", lambda: nc.vector.tensor_tensor(out=fb[:], in0=rnv[:], in1=gv, op=ALU.mult), reads=[rnv, gt], writes=[fb])
            for b in range(3):
                fbb = fb[:, b, :].unsqueeze(2).broadcast_to([128, GR, HD])
                dst = oacc if b == 0 else otmp
                kb.op("dve", lambda: nc.vector.tensor_tensor(out=dst[:], in0=otk[:, b, :, 0:HD], in1=fbb, op=ALU.mult),
                      reads=[otk, fb], writes=[dst])
                if b > 0:
                    kb.op("dve", lambda: nc.vector.tensor_tensor(out=oacc[:], in0=oacc[:], in1=otmp[:], op=ALU.add),
                          reads=[oacc, otmp], writes=[oacc])
            kb.op("dve", lambda: nc.vector.tensor_copy(out=ob[:].rearrange("p (h d) -> p h d", h=GR), in_=oacc[:]),
                  reads=[oacc], writes=[ob])
            outs.append(kb.dma("sp", o_out[l * 128:(l + 1) * 128, g * 256:(g + 1) * 256], ob[:], reads=[ob]))
            if nxt is not None:
                for _ in nxt:
                    pass

        par = 0
        for _ in pre(0, par):
            pass
        for l in range(NQT):
            nxt = pre(l + 1, 1 - par) if l + 1 < NQT else None
            post(l, par, nxt)
            par = 1 - par
    return outs

RH = 4; DK = 256; DV = 512
LOGG = np.log(1.0 - 2.0 ** (-5.0 - np.arange(RH, dtype=np.float64)))

def perm_ret_w_in(w):
    idx = []
    for blk in range(2):
        for h in range(RH):
            base = blk * 1024 + h * DK
            idx += [base + 2 * m for m in range(128)] + [base + 2 * m + 1 for m in range(128)]
    idx += list(range(2048, 6144))
    return np.ascontiguousarray(w[:, idx])

def ret_tables(pos):
    theta = (1.0 / (10000.0 ** np.linspace(0.0, 1.0, DK // 2, dtype=np.float32))).astype(np.float32)
    ang = pos.astype(np.float32)[:, None] * theta[None, :]
    cos = np.cos(ang).astype(np.float32); sin = np.sin(ang).astype(np.float32)
    jloc = (pos % 128).astype(np.float64)
    sc = (DK ** -0.5) * np.exp(-jloc[:, None] * LOGG[None, :])
    cosk = (cos[:, None, :].astype(np.float64) * sc[:, :, None]).astype(np.float32)
    sink = (sin[:, None, :].astype(np.float64) * sc[:, :, None]).astype(np.float32)
    return (np.ascontiguousarray(cos.T), np.ascontiguousarray(sin.T), np.ascontiguousarray(cosk),
            np.ascontiguousarray(sink))

def ret_consts():
    i = np.arange(128, dtype=np.float64)
    gi = np.exp(i[:, None] * LOGG[None, :]).astype(np.float32)
    g2i = np.exp(2 * i[:, None] * LOGG[None, :]).astype(np.float32)
    maskT = (np.arange(128)[None, :] >= np.arange(128)[:, None]).astype(np.float32)
    return gi, g2i, maskT

def state_to_dev(S):
    return np.ascontiguousarray(S.reshape(RH, 128, 2, DV).transpose(2, 1, 0, 3))

def state_from_dev(Sd):
    return np.ascontiguousarray(Sd.transpose(2, 1, 0, 3).reshape(RH, DK, DV))

def bcast128(v):
    return np.ascontiguousarray(np.broadcast_to(v.reshape(1, -1), (128, v.size))).astype(np.float32)

BF = ml_dtypes.bfloat16
NEG = -30000.0

def t5_bucket_np(dist):
    n = np.maximum(dist, 0).astype(np.int64)
    nf = np.maximum(n, 1).astype(np.float32)
    val = (np.log(nf / np.float32(16)) / np.float32(math.log(128 / 16))).astype(np.float32) * np.float32(16)
    large = 16 + val.astype(np.int32)
    large = np.minimum(large, 31)
    return np.where(n < 16, n, large).astype(np.int64)

def nsa_consts(NQT, CPB, rel_bias):
    NVT = NQT * CPB + CPB - 1
    NSB = (NVT + 15) // 16; NBLK = NSB * 32
    NCT = (8 * NVT + 16 + 127) // 128
    rb = np.asarray(rel_bias, np.float32)
    k = np.arange(128)[:, None]; q = np.arange(128)[None, :]
    TOEPS = np.zeros((4, 2, 128, 4, 128), np.float32)
    TOEPW = np.zeros((4, 5, 128, 4, 128), np.float32)
    TOEPC = np.zeros((4, 16, 4, 128), np.float32)
    for g in range(4):
        for r in range(4):
            h = g * 4 + r
            for m in range(2):
                d = 128 * (1 - m) + q - k
                TOEPS[g, m, :, r, :] = np.where(d >= 0, rb[t5_bucket_np(d), h], NEG)
            for m in range(5):
                d = 128 * (4 - m) + q - k
                TOEPW[g, m, :, r, :] = np.where((d >= 0) & (d < 512), rb[t5_bucket_np(d), h], NEG)
            u = np.arange(16)[:, None]; qq = np.arange(128)[None, :]
            d = qq + 113 - 16 * u
            TOEPC[g, :, r, :] = np.where(d >= 0, rb[t5_bucket_np(d), h], NEG)
    B31 = np.zeros((4, 128, 4), np.float32)
    for g in range(4):
        B31[g] = rb[31, g * 4:(g + 1) * 4][None, :]
    relb_rep = np.ascontiguousarray(np.broadcast_to(rb.reshape(1, 512), (128, 512)))
    npr = (np.arange(NCT)[None, :] * 128 + np.arange(128)[:, None])
    b = np.arange(NBLK)[None, None, :]
    Mmap = ((npr[:, :, None] >= 4 * b - 1) & (npr[:, :, None] <= 4 * b + 3)).astype(np.float32)
    MNear = np.zeros((16, NQT, NBLK), np.float32)
    for l in range(NQT):
        V = CPB * l + CPB - 1
        npn = 8 * V - 9 + np.arange(16)[:, None]
        bb = np.arange(NBLK)[None, :]
        MNear[:, l, :] = ((npn >= 4 * bb - 1) & (npn <= 4 * bb + 3))
    return dict(TOEPS=TOEPS.reshape(4, 2, 128, 512), TOEPW=TOEPW.reshape(4, 5, 128, 512), TOEPC=TOEPC.reshape(4, 16, 512),
                B31=B31, relb_rep=relb_rep, Mmap=Mmap, MNear=MNear, identf=np.eye(128, dtype=np.float32))

def nsa_core_inputs(NQT, CPB, j, S, kv_tok, kc, vc):
    shift = CPB - 1 - j
    NVT = NQT * CPB + CPB - 1
    NSB = (NVT + 15) // 16; NBLK = NSB * 32
    NCT = (8 * NVT + 16 + 127) // 128
    NKV = NVT * 128
    n_cmp = kc.shape[0]
    kvt = kv_tok.reshape(S, 6, 4, 64)
    KsT = np.zeros((4, 128, NKV), BF); KwT = np.zeros((4, 128, NKV), BF)
    Vs = np.zeros((4, NKV, 65), BF); Vw = np.zeros((4, NKV, 65), BF)
    KcT = np.zeros((4, 128, NCT * 128), BF); Vc = np.zeros((4, NCT * 128, 65), BF)
    t0 = 128 * shift
    tp = np.arange(NKV)
    ind = ((tp // 64) % 32)
    for g in range(4):
        KsT[g, 0:64, t0:t0 + S] = kvt[:, 2, g, :].T
        KwT[g, 0:64, t0:t0 + S] = kvt[:, 4, g, :].T
        Vs[g, t0:t0 + S, 0:64] = kvt[:, 3, g, :]
        Vw[g, t0:t0 + S, 0:64] = kvt[:, 5, g, :]
        Vs[g, :, 64] = 1.0; Vw[g, :, 64] = 1.0
        for jj in range(32):
            KsT[g, 64 + jj, :] = (ind == jj).astype(np.float32)
        KsT[g, 96] = 1.0; KsT[g, 98] = 1.0; KsT[g, 99] = 1.0
        KwT[g, 96] = 1.0
        KwT[g, 97, :t0] = NEG
        c0 = 8 * shift
        KcT[g, 0:64, c0:c0 + n_cmp] = kc[:, g, :].T
        Vc[g, c0:c0 + n_cmp, 0:64] = vc[:, g, :]
        Vc[g, :, 64] = 1.0
        KcT[g, 96] = 1.0; KcT[g, 98] = 1.0; KcT[g, 99] = 1.0
        KcT[g, 97, :] = NEG
        KcT[g, 97, c0:c0 + n_cmp] = 0.0
    VcN = np.zeros((4, NQT, 16, 65), BF)
    for l in range(NQT):
        V = CPB * l + CPB - 1
        VcN[:, l] = Vc[:, 8 * V - 9:8 * V + 7, :]
    MSEL = np.zeros((NQT, 3, 128, NBLK), np.float32)
    n_sel = S // 64
    for l in range(NQT):
        T = CPB * l + j
        t = 128 * T + np.arange(128)[:, None]
        b = np.arange(NBLK)[None, :] - 2 * shift
        valid = (b >= 0) & (b < n_sel) & (64 * b <= t)
        cur = t // 64
        f0 = (b == 0); f1 = (b == cur); f2 = (b == cur - 1)
        forced = (f0 | f1 | f2) & valid
        MSEL[l, 0] = (valid & ~forced)
        add = np.where(valid, 0.0, -1e30)
        add = np.where(f2 & valid, 1e30, add); add = np.where(f1 & valid, 2e30, add); add = np.where(f0 & valid, 3e30, add)
        MSEL[l, 1] = add
        MSEL[l, 2] = valid
    return dict(KsT=KsT, KwT=KwT, Vs=Vs, Vw=Vw, KcT=KcT, Vc=Vc, VcN=VcN, MSEL=MSEL)

def im2col(kvtok, n0, NB):
    S = kvtok.shape[0]
    idx = 16 * (n0 + np.arange(NB))[None, None, :] + 2 * np.arange(16)[:, None, None] + np.arange(2)[None, :, None]
    ok = idx < S
    g = kvtok[np.minimum(idx, S - 1)]
    g = np.where(ok[..., None, None, None], g, np.zeros((), kvtok.dtype))
    return np.ascontiguousarray(g.transpose(3, 4, 0, 1, 5, 2).reshape(2, 4, 16, 128, NB))

def pecol(pe):
    return np.ascontiguousarray(pe.reshape(2, 16, 2, 64).transpose(0, 2, 3, 1).reshape(2, 128, 16))


def pecol(pe):
    return np.ascontiguousarray(pe.reshape(2, 16, 2, 64).transpose(0, 2, 3, 1).reshape(2, 128, 16))

CPB = 4; FH = 2816


def _di(nc, n, s, dt=F32):
    return nc.dram_tensor(n, list(s), dt, kind="ExternalInput").ap()


def _do(nc, n, s, dt=F32):
    return nc.dram_tensor(n, list(s), dt, kind="ExternalOutput").ap()


def _dint(nc, n, s, dt=F32):
    return nc.dram_tensor(n, list(s), dt, kind="Internal").ap()


def _ret_inputs(nc, NT):
    return dict(x=_di(nc, "x", [NT, D]), w_in=_di(nc, "ret_w_in", [D, 6144]), g_pre=_di(nc, "g_mix_pre", [128, D]),
                cosT=_di(nc, "cosT", [128, NT]), sinT=_di(nc, "sinT", [128, NT]), cosk=_di(nc, "cosk", [NT, 4, 128]),
                sink=_di(nc, "sink", [NT, 4, 128]), gi=_di(nc, "gi", [128, 4]), g2i=_di(nc, "g2i", [128, 4]),
                maskT=_di(nc, "maskT", [128, 128]), ident=_di(nc, "ident", [128, 128]))


def prog_state(NT):
    nc = bass.Bass("TRN2", target_bir_lowering=False)
    a = _ret_inputs(nc, NT)
    s_out = _do(nc, "s_out", [2, 128, 4, 512])
    kb = KB(nc)
    idb = load_ident(kb, a["ident"])
    kb.begin_phase()
    outs = build_ret(kb, NT, a["x"], a["w_in"], a["g_pre"], a["cosT"], a["sinT"], a["cosk"], a["sink"], a["gi"], a["g2i"],
                     a["maskT"], None, None, None, s_out, idb, state_only=True)
    kb.end_phase()
    kb.finish(outs); kb.close()
    return nc


def prog_layer0(NT):
    nc = bass.Bass("TRN2", target_bir_lowering=False)
    a = _ret_inputs(nc, NT)
    s_slots = _di(nc, "s_slots", [3, 2, 128, 4, 512]); coef = _di(nc, "coef", [128, 12])
    ret_w_out = _di(nc, "ret_w_out", [2048, D]); g_mix_post = _di(nc, "g_mix_post", [128, D])
    fw_in = _di(nc, "ffn_w_in", [D, 2 * FH]); fw_out = _di(nc, "ffn_w_out", [FH, D])
    fg_pre = _di(nc, "g_ffn_pre", [128, D]); fg_post = _di(nc, "g_ffn_post", [128, D])
    kv_g = _di(nc, "g_kv", [128, D]); kv_w = _di(nc, "kv_w", [D, 1536])
    og = _dint(nc, "og_scr", [NT, 2048], BF16); hmid = _dint(nc, "hmid_scr", [NT, D])
    s_dummy = _dint(nc, "s_scr", [2, 128, 4, 512])
    h1 = _do(nc, "h1", [NT, D]); kv = _do(nc, "kv", [NT, 1536], BF16)
    kb = KB(nc)
    idb = load_ident(kb, a["ident"])
    kb.begin_phase()
    build_ret(kb, NT, a["x"], a["w_in"], a["g_pre"], a["cosT"], a["sinT"], a["cosk"], a["sink"], a["gi"], a["g2i"],
              a["maskT"], s_slots, coef, og, s_dummy, idb)
    kb.end_phase()
    kb.begin_phase()
    build_outproj(kb, NT, 2048, og, ret_w_out, g_mix_post, a["x"], hmid, idb)
    kb.end_phase()
    kb.begin_phase()
    o1 = build_ffn(kb, NT, hmid, fw_in, fw_out, fg_pre, fg_post, h1, idb)
    kb.end_phase()
    kb.begin_phase()
    o2 = build_normproj(kb, NT, h1, kv_g, kv_w, 1536, kv, idb)
    kb.end_phase()
    kb.finish(o1 + o2); kb.close()
    return nc


def prog_compress(NB=256):
    nc = bass.Bass("TRN2", target_bir_lowering=False)
    XT = _di(nc, "XT", (2, 4, 16, 128, NB), BF16); W1 = _di(nc, "W1", (2, 2048, 256)); W2 = _di(nc, "W2", (2, 256, 64))
    PEc = _di(nc, "PEc", (2, 128, 16))
    out = _do(nc, "kcv", [2, NB, 4, 64], BF16)
    kb = KB(nc)
    kb.begin_phase()
    outs = build_compress(kb, NB, XT, W1, W2, PEc, out)
    kb.end_phase()
    kb.finish(outs); kb.close()
    return nc


def prog_layer1(nqt, cpb=CPB):
    nc = bass.Bass("TRN2", target_bir_lowering=False)
    NT = nqt * 128
    NVT = nqt * cpb + cpb - 1; NSB = (NVT + 15) // 16; NBLK = NSB * 32; NCT = (8 * NVT + 16 + 127) // 128; NKV = NVT * 128
    h1 = _di(nc, "h1rr", [NT, D]); g_pre = _di(nc, "g_mix_pre", [128, D]); w_in = _di(nc, "nsa_w_in", [D, 1072])
    ident = _di(nc, "ident", [128, 128])
    A = {}
    A["KsT"] = _di(nc, "KsT", (4, 128, NKV), BF16); A["KwT"] = _di(nc, "KwT", (4, 128, NKV), BF16)
    A["Vs"] = _di(nc, "Vs", (4, NKV, 65), BF16); A["Vw"] = _di(nc, "Vw", (4, NKV, 65), BF16)
    A["KcT"] = _di(nc, "KcT", (4, 128, NCT * 128), BF16); A["Vc"] = _di(nc, "Vc", (4, NCT * 128, 65), BF16)
    A["VcN"] = _di(nc, "VcN", (4, nqt, 16, 65), BF16); A["MSEL"] = _di(nc, "MSEL", (nqt, 3, 128, NBLK))
    A["TOEPS"] = _di(nc, "TOEPS", (4, 2, 128, 512)); A["TOEPW"] = _di(nc, "TOEPW", (4, 5, 128, 512)); A["TOEPC"] = _di(nc, "TOEPC", (4, 16, 512))
    A["B31"] = _di(nc, "B31", (4, 128, 4)); A["relb_rep"] = _di(nc, "relb_rep", (128, 512)); A["Mmap"] = _di(nc, "Mmap", (128, NCT, NBLK))
    A["MNear"] = _di(nc, "MNear", (16, nqt, NBLK)); A["identf"] = _di(nc, "identf", (128, 128))
    w_out = _di(nc, "nsa_w_out", [1024, D]); g_post = _di(nc, "g_mix_post", [128, D])
    fw_in = _di(nc, "ffn_w_in", [D, 2 * FH]); fw_out = _di(nc, "ffn_w_out", [FH, D])
    fg_pre = _di(nc, "g_ffn_pre", [128, D]); fg_post = _di(nc, "g_ffn_post", [128, D])
    qin = _dint(nc, "q_scr", [NT, 1024], BF16); gates = _dint(nc, "gate_scr", [NT, 48])
    o = _dint(nc, "o_scr", [NT, 1024], BF16); hmid = _dint(nc, "hmid_scr", [NT, D])
    out = _do(nc, "out", [NT, D])
    kb = KB(nc)
    idb = load_ident(kb, ident)
    kb.begin_phase()
    build_normproj(kb, NT, h1, g_pre, w_in, 1072, qin, idb, sig_from=1024, sig_out=gates)
    kb.end_phase()
    kb.begin_phase()
    build_nsa_attn(kb, nqt, cpb, qin, gates, A, o, idb)
    kb.end_phase()
    kb.begin_phase()
    build_outproj(kb, NT, 1024, o, w_out, g_post, h1, hmid, idb)
    kb.end_phase()
    kb.begin_phase()
    outs = build_ffn(kb, NT, hmid, fw_in, fw_out, fg_pre, fg_post, out, idb)
    kb.end_phase()
    kb.finish(outs); kb.close()
    return nc


def _run(nc, in_maps):
    res = run_bass_kernel_spmd(nc, in_maps, core_ids=list(range(len(in_maps))))
    return res.results


def kernel(x, mix_norm_pre, mix_norm_post, ffn_norm_pre, ffn_norm_post, ffn_w_in, ffn_w_out, ret_w_in, ret_w_out,
           kv_norm, kv_w, cmp_pe_k, cmp_w1_k, cmp_w2_k, cmp_pe_v, cmp_w1_v, cmp_w2_v, nsa_w_in, nsa_w_out, rel_bias):
    f = lambda a: np.ascontiguousarray(np.asarray(a, dtype=np.float32))
    x = f(x)
    B, SEQ = x.shape[0], x.shape[1]
    NCORE = B * CPB; NTC = SEQ // CPB; NQT = NTC // 128
    assert NCORE <= 8 and NTC % 256 == 0
    eye = np.eye(128, dtype=np.float32)
    gi, g2i, maskT = ret_consts()
    w_in_p = perm_ret_w_in(f(ret_w_in)[0])
    base = []
    for c in range(NCORE):
        b, j = divmod(c, CPB)
        pos = np.arange(j * NTC, (j + 1) * NTC)
        cT, sT, ck, sk = ret_tables(pos)
        base.append({"x": np.ascontiguousarray(x[b, j * NTC:(j + 1) * NTC]), "ret_w_in": w_in_p,
                     "g_mix_pre": bcast128(f(mix_norm_pre)[0]), "cosT": cT, "sinT": sT, "cosk": ck, "sink": sk,
                     "gi": gi, "g2i": g2i, "maskT": maskT, "ident": eye})
    r1 = _run(prog_state(NTC), base)
    L = [np.asarray(r["s_out"], np.float32) for r in r1]
    gam128 = np.exp(128.0 * LOGG)
    ims = []
    for c in range(NCORE):
        b, j = divmod(c, CPB)
        slots = np.zeros((3, 2, 128, 4, 512), np.float32); coef = np.zeros((3, 4), np.float64)
        for i in range(j):
            slots[i] = L[b * CPB + i]
            coef[i] = gam128 ** (NQT * (j - 1 - i))
        im = dict(base[c])
        im.update({"s_slots": slots, "coef": bcast128(coef.reshape(-1).astype(np.float32)), "ret_w_out": f(ret_w_out)[0],
                   "g_mix_post": bcast128(f(mix_norm_post)[0]), "ffn_w_in": f(ffn_w_in)[0], "ffn_w_out": f(ffn_w_out)[0],
                   "g_ffn_pre": bcast128(f(ffn_norm_pre)[0]), "g_ffn_post": bcast128(f(ffn_norm_post)[0]),
                   "g_kv": bcast128(f(kv_norm)), "kv_w": f(kv_w)})
        ims.append(im)
    r2 = _run(prog_layer0(NTC), ims)
    h1 = np.stack([np.concatenate([np.asarray(r2[b * CPB + j]["h1"]) for j in range(CPB)]) for b in range(B)])
    kv = np.stack([np.concatenate([np.asarray(r2[b * CPB + j]["kv"]) for j in range(CPB)]) for b in range(B)])
    W1 = np.stack([f(cmp_w1_k), f(cmp_w1_v)]); W2 = np.stack([f(cmp_w2_k), f(cmp_w2_v)])
    PEc = pecol(np.stack([f(cmp_pe_k), f(cmp_pe_v)]))
    ims = []
    for c in range(NCORE):
        b, j = divmod(c, CPB)
        kcv_tok = kv[b].reshape(SEQ, 6, 4, 64)[:, 0:2]
        ims.append({"XT": im2col(kcv_tok, (NTC // 16) * j, NTC // 16), "W1": W1, "W2": W2, "PEc": PEc})
    r3 = _run(prog_compress(NTC // 16), ims)
    n_cmp = (SEQ - 32) // 16 + 1
    kcs = [np.concatenate([np.asarray(r3[b * CPB + j]["kcv"]) for j in range(CPB)], axis=1)[:, :n_cmp] for b in range(B)]
    consts = nsa_consts(NQT, CPB, f(rel_bias))
    ims = []; rows_all = []
    for c in range(NCORE):
        b, j = divmod(c, CPB)
        rows = np.concatenate([np.arange(128) + 128 * (CPB * l + j) for l in range(NQT)])
        rows_all.append(rows)
        im = nsa_core_inputs(NQT, CPB, j, SEQ, kv[b], kcs[b][0], kcs[b][1])
        im.update(consts)
        im.update({"h1rr": np.ascontiguousarray(h1[b][rows]), "g_mix_pre": bcast128(f(mix_norm_pre)[1]), "nsa_w_in": f(nsa_w_in)[0],
                   "ident": eye, "nsa_w_out": f(nsa_w_out)[0], "g_mix_post": bcast128(f(mix_norm_post)[1]),
                   "ffn_w_in": f(ffn_w_in)[1], "ffn_w_out": f(ffn_w_out)[1], "g_ffn_pre": bcast128(f(ffn_norm_pre)[1]),
                   "g_ffn_post": bcast128(f(ffn_norm_post)[1])})
        ims.append(im)
    r4 = _run(prog_layer1(NQT), ims)
    out = np.zeros((B, SEQ, D), np.float32)
    for c in range(NCORE):
        b, j = divmod(c, CPB)
        out[b, rows_all[c]] = np.asarray(r4[c]["out"])
    return out
```

```python
import math
import numpy as np
import ml_dtypes
from contextlib import ExitStack
import concourse.bass as bass
import concourse.mybir as mybir
from concourse.bass_utils import run_bass_kernel_spmd

F32 = mybir.dt.float32
BF16 = mybir.dt.bfloat16
ACT = mybir.ActivationFunctionType
ALU = mybir.AluOpType
AX = mybir.AxisListType

COMPUTE = ("pe", "dve", "act", "pool")


class Buf:
    def __init__(self, kb, t, name, dram=False):
        self.kb = kb
        self.t = t
        self.name = name
        self.dram = dram
        self.last_w = None
        self.reads = []
        self.sem = None
        self.semcnt = 0
        self.psum = False

    def __getitem__(self, idx):
        return self.t[idx]

    def dsem(self, q="sp"):
        if self.sem is None:
            self.sw = (q == "pool")
            if self.kb.sem_pool and not self.sw:
                self.sem, self.semcnt = self.kb.sem_pool.pop()
            else:
                self.sem = self.kb.new_sem("d")
            self.kb.phase_bufs.append(self)
        return self.sem


class KB:
    def __init__(self, nc, same_eng_sync=True):
        self.nc = nc
        self.es = ExitStack()
        self.engs = {"pe": nc.tensor, "dve": nc.vector, "act": nc.scalar,
                     "pool": nc.gpsimd, "sp": nc.sync}
        self.esem = {}
        self.ecnt = {}
        for e in COMPUTE:
            self.esem[e] = self.es.enter_context(nc.semaphore("e_" + e))
            self.ecnt[e] = 0
        self.waited = {e: {} for e in self.engs}
        self.same = same_eng_sync
        self.nsem = 4
        self.nbuf = 0
        self.ninstr = 0
        self.final_tokens = []
        self.cur = self.es
        self.sem_pool = []
        self.phase_bufs = []

    def new_sem(self, name):
        self.nsem += 1
        return self.es.enter_context(self.nc.semaphore(name + "_%d" % self.nsem))

    def sb(self, shape, dtype, name=None):
        self.nbuf += 1
        name = (name or "b") + "_%d" % self.nbuf
        t = self.cur.enter_context(self.nc.sbuf_tensor(name, list(shape), dtype))
        return Buf(self, t, name)

    def ps(self, shape, dtype, name=None):
        self.nbuf += 1
        name = (name or "p") + "_%d" % self.nbuf
        t = self.cur.enter_context(self.nc.psum_tensor(name, list(shape), dtype))
        b = Buf(self, t, name)
        b.psum = True
        return b

    def dram(self, ap, name):
        return Buf(self, ap, name, dram=True)

    def _wait(self, eng, tok):
        if tok is None:
            return
        sem, val, peng = tok
        if peng == eng and (eng == "pe" or not self.same):
            return
        key = id(sem)
        if self.waited[eng].get(key, 0) >= val:
            return
        self.engs[eng].wait_ge(sem, val)
        self.waited[eng][key] = val

    def _deps(self, eng, reads, writes):
        for b in reads:
            self._wait(eng, b.last_w)
            if b.psum:
                for r in self._compact(b.reads):
                    if r[2] != eng:
                        self._wait(eng, r)
        for b in writes:
            self._wait(eng, b.last_w)
            for r in self._compact(b.reads):
                self._wait(eng, r)

    def op(self, eng, fn, reads=(), writes=()):
        self._deps(eng, reads, writes)
        ins = fn()
        self.ecnt[eng] += 1
        ins.then_inc(self.esem[eng], 1)
        tok = (self.esem[eng], self.ecnt[eng], eng)
        for b in reads:
            b.reads.append(tok)
            if len(b.reads) > 24:
                b.reads = self._compact(b.reads)
        for b in writes:
            b.last_w = tok
            b.reads = []
        self.ninstr += 1
        return tok

    @staticmethod
    def _compact(reads):
        best = {}
        for (s, v, e) in reads:
            k = id(s)
            if k not in best or best[k][1] < v:
                best[k] = (s, v, e)
        return list(best.values())

    def dma(self, q, out_ap, in_ap, reads=(), writes=(), **kw):
        self._deps(q, reads, writes)
        owner = writes[0] if writes else reads[0]
        sem = owner.dsem(q)
        owner.semcnt += 16
        self.engs[q].dma_start(out=out_ap, in_=in_ap, **kw).then_inc(sem, 16)
        tok = (sem, owner.semcnt, "dma")
        for b in reads:
            b.reads.append(tok)
        for b in writes:
            b.last_w = tok
            b.reads = []
        self.ninstr += 1
        return tok

    def finish(self, toks, eng="sp"):
        for t in self._compact(toks):
            sem, val, _ = t
            key = id(sem)
            if self.waited[eng].get(key, 0) >= val:
                continue
            self.engs[eng].wait_ge(sem, val)
            self.waited[eng][key] = val

    def barrier(self):
        toks = [(self.esem[e], self.ecnt[e], "x") for e in COMPUTE if self.ecnt[e] > 0]
        toks += [(b.sem, b.semcnt, "dma") for b in self.phase_bufs if b.semcnt > 0]
        for e in self.engs:
            self.finish(toks, eng=e)

    def begin_phase(self):
        self.cur = ExitStack()
        self.phase_bufs = [b for b in self.phase_bufs if b.dram]

    def end_phase(self):
        self.barrier()
        for b in self.phase_bufs:
            if not b.dram and b.sem is not None:
                if not getattr(b, "sw", False):
                    self.sem_pool.append((b.sem, b.semcnt))
                b.sem = None
        self.phase_bufs = [b for b in self.phase_bufs if b.dram]
        self.cur.close()
        self.cur = self.es

    def close(self):
        self.es.close()


D = 1024; H = 2816; HC = 22; KC = 8

def load_ident(kb, ident_ap):
    idb = kb.sb([128, 128], BF16, "ident")
    kb.dma("pool", idb[:], ident_ap, writes=[idb])
    return idb

def rstd_from_ms(kb, rs, ms, eps=1e-6):
    nc = kb.nc
    kb.op("act", lambda: nc.scalar.activation(out=rs[:], in_=ms[:], func=ACT.Sqrt, bias=eps, scale=1.0),
          reads=[ms], writes=[rs])
    kb.op("dve", lambda: nc.vector.reciprocal(out=rs[:], in_=rs[:]), reads=[rs], writes=[rs])

def build_ffn(kb, NT, hin, w_in, w_out, g_pre, g_post, hout, ident, TT=256):
    nc = kb.nc
    NS = TT // 128
    Win = kb.sb([128, KC, 2 * H], BF16, "Win")
    Wout = kb.sb([128, HC, D], BF16, "Wout")
    Gpre = kb.sb([128, D], F32, "Gpre")
    Gpost = kb.sb([128, D], F32, "Gpost")
    for kc in range(KC):
        kb.dma("pool", Win[:, kc, :], w_in[kc * 128:(kc + 1) * 128, :], writes=[Win])
    for hc in range(HC):
        kb.dma("pool", Wout[:, hc, :], w_out[hc * 128:(hc + 1) * 128, :], writes=[Wout])
    kb.dma("sp", Gpre[:], g_pre, writes=[Gpre])
    kb.dma("sp", Gpost[:], g_post, writes=[Gpost])
    XA = [kb.sb([128, D], F32, "xa") for _ in range(2)]
    XB = [kb.sb([128, D], F32, "xb") for _ in range(2)]
    XN = [kb.sb([128, D], BF16, "xn") for _ in range(2)]
    OT = [kb.sb([128, D], F32, "ot") for _ in range(2)]
    junk = kb.sb([128, D], BF16, "junk")
    SS = [kb.sb([128, 1], F32, "ss") for _ in range(2)]
    RS = [kb.sb([128, 1], F32, "rs") for _ in range(2)]
    SS2 = [kb.sb([128, 1], F32, "ss2") for _ in range(2)]
    RS2 = [kb.sb([128, 1], F32, "rs2") for _ in range(2)]
    xnT = kb.sb([128, KC, TT], BF16, "xnT")
    hT = kb.sb([128, HC, TT], BF16, "hT")
    SG = [kb.sb([128, TT], F32, "sg") for _ in range(2)]
    tp = kb.ps([128, KC, 128], BF16, "tp")
    PG = [kb.ps([128, 2, TT], F32, "pg") for _ in range(3)]
    PY = [kb.ps([128, D], F32, "py") for _ in range(NS)]
    outs = []
    n = 0
    for ti in range(NT // TT):
        for s in range(NS):
            r0 = ti * TT + s * 128
            xa = XA[n % 2]; xn = XN[n % 2]; ss = SS[n % 2]; rs = RS[n % 2]
            n += 1
            kb.dma("sp", xa[:], hin[r0:r0 + 128, :], writes=[xa])
            kb.op("dve", lambda: nc.vector.memset(ss[:], 0.0), writes=[ss])
            kb.op("act", lambda: nc.scalar.activation(out=junk[:], in_=xa[:], func=ACT.Square, scale=float(D) ** -0.5,
                                                      accum_out=ss[:, 0:1]), reads=[xa, ss], writes=[junk, ss])
            rstd_from_ms(kb, rs, ss)
            kb.op("dve", lambda: nc.vector.scalar_tensor_tensor(out=xn[:], in0=xa[:], scalar=rs[:, 0:1], in1=Gpre[:],
                                                                op0=ALU.mult, op1=ALU.mult),
                  reads=[xa, rs, Gpre], writes=[xn])
            for kc in range(KC):
                kb.op("pe", lambda: nc.tensor.transpose(out=tp[:, kc, :], in_=xn[:, kc * 128:(kc + 1) * 128],
                                                        identity=ident[:]), reads=[xn, ident], writes=[tp])
            kb.op("act", lambda: nc.scalar.copy(out=xnT[:, :, s * 128:(s + 1) * 128], in_=tp[:]),
                  reads=[tp], writes=[xnT])
        for hc in range(HC):
            pg = PG[hc % 3]; sg = SG[hc % 2]
            for half in range(2):
                c0 = half * H + hc * 128
                for kc in range(KC):
                    kb.op("pe", lambda: nc.tensor.matmul(out=pg[:, half, :], lhsT=Win[:, kc, c0:c0 + 128],
                                                         rhs=xnT[:, kc, :], start=(kc == 0), stop=(kc == KC - 1)),
                          reads=[Win, xnT], writes=[pg])
            kb.op("act", lambda: nc.scalar.activation(out=sg[:], in_=pg[:, 0, :], func=ACT.Silu),
                  reads=[pg], writes=[sg])
            kb.op("dve", lambda: nc.vector.tensor_tensor(out=hT[:, hc, :], in0=sg[:], in1=pg[:, 1, :], op=ALU.mult),
                  reads=[sg, pg], writes=[hT])
        for s in range(NS):
            py = PY[s]
            for cb in range(2):
                for hc in range(HC):
                    kb.op("pe", lambda: nc.tensor.matmul(out=py[:, cb * 512:(cb + 1) * 512],
                                                         lhsT=hT[:, hc, s * 128:(s + 1) * 128],
                                                         rhs=Wout[:, hc, cb * 512:(cb + 1) * 512],
                                                         start=(hc == 0), stop=(hc == HC - 1)),
                          reads=[hT, Wout], writes=[py])
        for s in range(NS):
            r0 = ti * TT + s * 128
            py = PY[s]; xb = XB[s % 2]; ot = OT[s % 2]; ss2 = SS2[s % 2]; rs2 = RS2[s % 2]
            kb.dma("sp", xb[:], hin[r0:r0 + 128, :], writes=[xb])
            kb.op("dve", lambda: nc.vector.memset(ss2[:], 0.0), writes=[ss2])
            kb.op("act", lambda: nc.scalar.activation(out=junk[:], in_=py[:], func=ACT.Square, scale=float(D) ** -0.5,
                                                      accum_out=ss2[:, 0:1]), reads=[py, ss2], writes=[junk, ss2])
            rstd_from_ms(kb, rs2, ss2)
            kb.op("dve", lambda: nc.vector.scalar_tensor_tensor(out=ot[:], in0=py[:], scalar=rs2[:, 0:1], in1=Gpost[:],
                                                                op0=ALU.mult, op1=ALU.mult),
                  reads=[py, rs2, Gpost], writes=[ot])
            kb.op("pool", lambda: nc.gpsimd.tensor_tensor(out=ot[:], in0=ot[:], in1=xb[:], op=ALU.add),
                  reads=[ot, xb], writes=[ot])
            outs.append(kb.dma("sp", hout[r0:r0 + 128, :], ot[:], reads=[ot]))
    return outs


D = 1024
RH = 4; DK = 256; DV = 512
GAM = [1.0 - 2.0 ** (-5.0 - h) for h in range(RH)]


def build_ret(kb, NT, x_ap, w_in, g_pre, cosT, sinT, cosk, sink, gi, g2i, maskT_ap, s_slots, coef, og_out, s_out,
              ident, state_only=False, stage=9):
    nc = kb.nc
    NCH = NT // 128
    KC = 8
    if state_only:
        c_lo, c_hi = 1024, 4096
    else:
        c_lo, c_hi = 0, 6144
    WC = c_hi - c_lo
    Win = kb.sb([128, KC, WC], BF16, "Win")
    for kc in range(KC):
        for cc in range(0, WC, 1024):
            kb.dma("pool", Win[:, kc, cc:cc + 1024], w_in[kc * 128:(kc + 1) * 128, c_lo + cc:c_lo + cc + 1024],
                   writes=[Win])
    KCOL = 1024 - c_lo; VCOL = 2048 - c_lo; GCOL = 4096 - c_lo
    Gpre = kb.sb([128, D], F32, "Gpre")
    kb.dma("sp", Gpre[:], g_pre, writes=[Gpre])
    S = kb.sb([128, 2, RH, DV], F32, "S")
    if state_only:
        kb.op("dve", lambda: nc.vector.memset(S[:], 0.0), writes=[S])
    else:
        osb = kb.sb([128, RH, DV], F32, "osb")
        coef_sb = kb.sb([128, 3 * RH], F32, "coef")
        kb.dma("sp", coef_sb[:], coef, writes=[coef_sb])
        for dc in range(2):
            for slot in range(3):
                kb.dma("sp", osb[:], s_slots[slot, dc], writes=[osb])
                for h in range(RH):
                    cs = coef_sb[:, slot * RH + h:slot * RH + h + 1]
                    if slot == 0:
                        kb.op("dve", lambda: nc.vector.tensor_scalar(out=S[:, dc, h, :], in0=osb[:, h, :], scalar1=cs, scalar2=None,
                                                                     op0=ALU.mult), reads=[osb, coef_sb], writes=[S])
                    else:
                        kb.op("dve", lambda: nc.vector.scalar_tensor_tensor(out=S[:, dc, h, :], in0=osb[:, h, :], scalar=cs,
                                                                            in1=S[:, dc, h, :], op0=ALU.mult, op1=ALU.add),
                              reads=[osb, coef_sb, S], writes=[S])
    XA = [kb.sb([128, D], F32, "xa") for _ in range(2)]
    xn = kb.sb([128, D], BF16, "xn")
    xnT = kb.sb([128, KC, 128], BF16, "xnT")
    junk = kb.sb([128, D], BF16, "junk")
    ss = kb.sb([128, 1], F32, "ss"); rs = kb.sb([128, 1], F32, "rs")
    COSK = [kb.sb([128, RH, 128], F32, "cosk") for _ in range(2)]
    SINK = [kb.sb([128, RH, 128], F32, "sink") for _ in range(2)]
    kraw = kb.sb([128, 2, 2, 128], F32, "kraw")
    kt = kb.sb([128, RH, 2, 128], BF16, "kt")
    v_sb = kb.sb([128, RH, DV], BF16, "v")
    T = [kb.sb([128, 2, 128], F32, "t%d" % i) for i in range(4)]
    P = [kb.ps([128, 512], F32, "bank%d" % i) for i in range(8)]
    p0b = P[0][:].bitcast(BF16).rearrange("p (a b) -> p a b", b=128)
    if not state_only:
        Sb = kb.sb([128, 2, RH, DV], BF16, "Sb")
        for dc in range(2):
            for h in range(RH):
                kb.op("act", lambda: nc.scalar.activation(out=Sb[:, dc, h, :], in_=S[:, dc, h, :], func=ACT.Copy,
                                                          scale=GAM[h]), reads=[S], writes=[Sb])
        COST = [kb.sb([128, 128], F32, "cosT") for _ in range(2)]
        SINT = [kb.sb([128, 128], F32, "sinT") for _ in range(2)]
        GI = kb.sb([128, RH], F32, "gi"); G2I = kb.sb([128, RH], F32, "g2i")
        kb.dma("sp", GI[:], gi, writes=[GI]); kb.dma("sp", G2I[:], g2i, writes=[G2I])
        maskT = kb.sb([128, 128], F32, "maskT")
        kb.dma("sp", maskT[:], maskT_ap, writes=[maskT])
        qraw = kb.sb([128, 2, 2, 128], F32, "qraw")
        qT = kb.sb([128, RH, 2, 128], BF16, "qT")
        kT = kb.sb([128, RH, 2, 128], BF16, "kT")
        gs = kb.sb([128, RH * DV], BF16, "gs")
        PT = kb.sb([128, RH, 128], BF16, "PT")
        OG = [kb.sb([128, RH * DV], BF16, "og") for _ in range(2)]
        ms4 = kb.sb([128, RH], F32, "ms4"); f4 = kb.sb([128, RH], F32, "f4")
    outs = []
    bk = 0
    for c in range(NCH):
        r0 = c * 128
        xa = XA[c % 2]; cosk_t = COSK[c % 2]; sink_t = SINK[c % 2]
        kb.dma("sp", xa[:], x_ap[r0:r0 + 128, :], writes=[xa])
        kb.dma("sp", cosk_t[:], cosk[r0:r0 + 128], writes=[cosk_t])
        kb.dma("sp", sink_t[:], sink[r0:r0 + 128], writes=[sink_t])
        if not state_only:
            cosT_t = COST[c % 2]; sinT_t = SINT[c % 2]
            kb.dma("sp", cosT_t[:], cosT[:, r0:r0 + 128], writes=[cosT_t])
            kb.dma("sp", sinT_t[:], sinT[:, r0:r0 + 128], writes=[sinT_t])
        kb.op("dve", lambda: nc.vector.memset(ss[:], 0.0), writes=[ss])
        kb.op("act", lambda: nc.scalar.activation(out=junk[:], in_=xa[:], func=ACT.Square, scale=float(D) ** -0.5,
                                                  accum_out=ss[:, 0:1]), reads=[xa, ss], writes=[junk, ss])
        rstd_from_ms(kb, rs, ss)
        kb.op("dve", lambda: nc.vector.scalar_tensor_tensor(out=xn[:], in0=xa[:], scalar=rs[:, 0:1], in1=Gpre[:],
                                                            op0=ALU.mult, op1=ALU.mult),
              reads=[xa, rs, Gpre], writes=[xn])
        for kc in range(KC):
            kb.op("pe", lambda: nc.tensor.transpose(out=p0b[:, kc, :], in_=xn[:, kc * 128:(kc + 1) * 128],
                                                    identity=ident[:]), reads=[xn, ident], writes=[P[0]])
        kb.op("act", lambda: nc.scalar.copy(out=xnT[:], in_=p0b), reads=[P[0]], writes=[xnT])
        if not state_only:
            for hb in range(2):
                pq = P[1 + hb]
                pqv = pq[:].rearrange("p (a b t) -> p a b t", a=2, b=2)
                for hh in range(2):
                    h = 2 * hb + hh
                    for blk in range(2):
                        col0 = h * DK + blk * 128
                        for kc in range(KC):
                            kb.op("pe", lambda: nc.tensor.matmul(out=pqv[:, hh, blk, :], lhsT=Win[:, kc, col0:col0 + 128],
                                                                 rhs=xnT[:, kc, :], start=(kc == 0), stop=(kc == KC - 1)),
                                  reads=[Win, xnT], writes=[pq])
                kb.op("act", lambda: nc.scalar.copy(out=qraw[:].rearrange("p a b t -> p (a b t)"), in_=pq[:]),
                      reads=[pq], writes=[qraw])
                A = qraw[:, :, 0, :]; B = qraw[:, :, 1, :]
                cb = cosT_t[:].unsqueeze(1).broadcast_to([128, 2, 128])
                sb_ = sinT_t[:].unsqueeze(1).broadcast_to([128, 2, 128])
                kb.op("dve", lambda: nc.vector.tensor_tensor(out=T[0][:], in0=A, in1=cb, op=ALU.mult),
                      reads=[qraw, cosT_t], writes=[T[0]])
                kb.op("dve", lambda: nc.vector.tensor_tensor(out=T[1][:], in0=B, in1=sb_, op=ALU.mult),
                      reads=[qraw, sinT_t], writes=[T[1]])
                kb.op("dve", lambda: nc.vector.tensor_tensor(out=T[2][:], in0=A, in1=sb_, op=ALU.mult),
                      reads=[qraw, sinT_t], writes=[T[2]])
                kb.op("dve", lambda: nc.vector.tensor_tensor(out=T[3][:], in0=B, in1=cb, op=ALU.mult),
                      reads=[qraw, cosT_t], writes=[T[3]])
                kb.op("dve", lambda: nc.vector.tensor_tensor(out=qT[:, 2 * hb:2 * hb + 2, 0, :], in0=T[0][:], in1=T[1][:],
                                                             op=ALU.subtract), reads=[T[0], T[1]], writes=[qT])
                kb.op("dve", lambda: nc.vector.tensor_tensor(out=qT[:, 2 * hb:2 * hb + 2, 1, :], in0=T[2][:], in1=T[3][:],
                                                              op=ALU.add), reads=[T[2], T[3]], writes=[qT])
        for kb_ in range(2):
            pk = P[3 + bk % 2]; bk += 1
            for kc in range(KC):
                kb.op("pe", lambda: nc.tensor.matmul(out=pk[:], lhsT=xnT[:, kc, :],
                                                     rhs=Win[:, kc, KCOL + kb_ * 512:KCOL + (kb_ + 1) * 512],
                                                     start=(kc == 0), stop=(kc == KC - 1)),
                      reads=[Win, xnT], writes=[pk])
            kb.op("act", lambda: nc.scalar.copy(out=kraw[:].rearrange("p a b t -> p (a b t)"), in_=pk[:]),
                  reads=[pk], writes=[kraw])
            A = kraw[:, :, 0, :]; B = kraw[:, :, 1, :]
            ck = cosk_t[:, 2 * kb_:2 * kb_ + 2, :]; sk = sink_t[:, 2 * kb_:2 * kb_ + 2, :]
            kb.op("dve", lambda: nc.vector.tensor_tensor(out=T[0][:], in0=A, in1=ck, op=ALU.mult),
                  reads=[kraw, cosk_t], writes=[T[0]])
            kb.op("dve", lambda: nc.vector.tensor_tensor(out=T[1][:], in0=B, in1=sk, op=ALU.mult),
                  reads=[kraw, sink_t], writes=[T[1]])
            kb.op("dve", lambda: nc.vector.tensor_tensor(out=T[2][:], in0=A, in1=sk, op=ALU.mult),
                  reads=[kraw, sink_t], writes=[T[2]])
            kb.op("dve", lambda: nc.vector.tensor_tensor(out=T[3][:], in0=B, in1=ck, op=ALU.mult),
                  reads=[kraw, cosk_t], writes=[T[3]])
            kb.op("dve", lambda: nc.vector.tensor_tensor(out=kt[:, 2 * kb_:2 * kb_ + 2, 0, :], in0=T[0][:], in1=T[1][:],
                                                         op=ALU.subtract), reads=[T[0], T[1]], writes=[kt])
            kb.op("dve", lambda: nc.vector.tensor_tensor(out=kt[:, 2 * kb_:2 * kb_ + 2, 1, :], in0=T[2][:], in1=T[3][:],
                                                          op=ALU.add), reads=[T[2], T[3]], writes=[kt])
        for h in range(RH):
            pv = P[3 + bk % 2]; bk += 1
            for kc in range(KC):
                kb.op("pe", lambda: nc.tensor.matmul(out=pv[:], lhsT=xnT[:, kc, :],
                                                     rhs=Win[:, kc, VCOL + h * 512:VCOL + (h + 1) * 512],
                                                     start=(kc == 0), stop=(kc == KC - 1)),
                      reads=[Win, xnT], writes=[pv])
            kb.op("act", lambda: nc.scalar.copy(out=v_sb[:, h, :], in_=pv[:]), reads=[pv], writes=[v_sb])
        if not state_only:
            for h in range(RH if stage >= 2 else 0):
                pg = P[3 + bk % 2]; bk += 1
                for kc in range(KC):
                    kb.op("pe", lambda: nc.tensor.matmul(out=pg[:], lhsT=xnT[:, kc, :],
                                                         rhs=Win[:, kc, GCOL + h * 512:GCOL + (h + 1) * 512],
                                                         start=(kc == 0), stop=(kc == KC - 1)),
                          reads=[Win, xnT], writes=[pg])
                kb.op("act", lambda: nc.scalar.activation(out=gs[:, h * 512:(h + 1) * 512], in_=pg[:], func=ACT.Silu),
                      reads=[pg], writes=[gs])
            for h in range(RH if stage >= 2 else 0):
                for blk in range(2):
                    kb.op("pe", lambda: nc.tensor.transpose(out=p0b[:, h * 2 + blk, :], in_=kt[:, h, blk, :],
                                                            identity=ident[:]), reads=[kt, ident], writes=[P[0]])
            if stage >= 2:
                kb.op("dve", lambda: nc.vector.tensor_copy(out=kT[:].rearrange("p h b t -> p (h b) t"), in_=p0b),
                      reads=[P[0]], writes=[kT])
            p5 = P[5]; p5v = p5[:].rearrange("p (h t) -> p h t", h=RH)
            for h in range(RH if stage >= 3 else 0):
                for blk in range(2):
                    kb.op("pe", lambda: nc.tensor.matmul(out=p5v[:, h, :], lhsT=kT[:, h, blk, :], rhs=qT[:, h, blk, :],
                                                         start=(blk == 0), stop=(blk == 1)),
                          reads=[kT, qT], writes=[p5])
            if stage >= 3:
              kb.op("dve", lambda: nc.vector.tensor_tensor(out=PT[:], in0=p5v,
                                                         in1=maskT[:].unsqueeze(1).broadcast_to([128, RH, 128]),
                                                         op=ALU.mult), reads=[p5, maskT], writes=[PT])
            kb.op("dve", lambda: nc.vector.memset(ms4[:], 0.0), writes=[ms4])
            for h in range(RH if stage >= 4 else 0):
                po = P[6 + h % 2]
                kb.op("pe", lambda: nc.tensor.matmul(out=po[:], lhsT=PT[:, h, :], rhs=v_sb[:, h, :], start=True, stop=False),
                      reads=[PT, v_sb], writes=[po])
                kb.op("pe", lambda: nc.tensor.matmul(out=po[:], lhsT=qT[:, h, 0, :], rhs=Sb[:, 0, h, :], start=False, stop=False),
                      reads=[qT, Sb], writes=[po])
                kb.op("pe", lambda: nc.tensor.matmul(out=po[:], lhsT=qT[:, h, 1, :], rhs=Sb[:, 1, h, :], start=False, stop=True),
                      reads=[qT, Sb], writes=[po])
                kb.op("act", lambda: nc.scalar.activation(out=junk[:, 0:DV], in_=po[:], func=ACT.Square, scale=float(DV) ** -0.5,
                                                          accum_out=ms4[:, h:h + 1]), reads=[po, ms4], writes=[junk, ms4])
                kb.op("dve", lambda: nc.vector.tensor_copy(out=osb[:, h, :], in_=po[:]), reads=[po], writes=[osb])
        for h in range(RH):
            for dc in range(2):
                pkv = P[3 + bk % 2]; bk += 1
                kb.op("pe", lambda: nc.tensor.matmul(out=pkv[:], lhsT=kt[:, h, dc, :], rhs=v_sb[:, h, :], start=True, stop=True),
                      reads=[kt, v_sb], writes=[pkv])
                kb.op("act", lambda: nc.scalar.activation(out=S[:, dc, h, :], in_=S[:, dc, h, :], func=ACT.Copy,
                                                          scale=GAM[h] ** 128), reads=[S], writes=[S])
                kb.op("dve", lambda: nc.vector.scalar_tensor_tensor(out=S[:, dc, h, :], in0=pkv[:], scalar=GAM[h] ** 127,
                                                                    in1=S[:, dc, h, :], op0=ALU.mult, op1=ALU.add),
                      reads=[pkv, S], writes=[S])
                if not state_only:
                    kb.op("act", lambda: nc.scalar.activation(out=Sb[:, dc, h, :], in_=S[:, dc, h, :], func=ACT.Copy,
                                                              scale=GAM[h]), reads=[S], writes=[Sb])
        if not state_only and stage >= 5:
            og = OG[c % 2]
            kb.op("dve", lambda: nc.vector.tensor_tensor(out=f4[:], in0=ms4[:], in1=G2I[:], op=ALU.mult),
                  reads=[ms4, G2I], writes=[f4])
            rstd_from_ms(kb, f4, f4)
            kb.op("dve", lambda: nc.vector.tensor_tensor(out=f4[:], in0=f4[:], in1=GI[:], op=ALU.mult),
                  reads=[f4, GI], writes=[f4])
            for h in range(RH):
                kb.op("dve", lambda: nc.vector.scalar_tensor_tensor(out=og[:, h * DV:(h + 1) * DV], in0=osb[:, h, :], scalar=f4[:, h:h + 1],
                                                          in1=gs[:, h * DV:(h + 1) * DV], op0=ALU.mult, op1=ALU.mult),
                      reads=[osb, f4, gs], writes=[og])
            outs.append(kb.dma("sp", og_out[r0:r0 + 128, :], og[:], reads=[og]))
    for dc in range(2):
        outs.append(kb.dma("sp", s_out[dc], S[:, dc, :, :], reads=[S]))
    return outs

D = 1024

def post_norm_residual(kb, py, x_rows_ap, Gpost, out_rows_ap, xb, ot, ss2, rs2, junk):
    nc = kb.nc
    kb.dma("sp", xb[:], x_rows_ap, writes=[xb])
    kb.op("dve", lambda: nc.vector.memset(ss2[:], 0.0), writes=[ss2])
    kb.op("act", lambda: nc.scalar.activation(out=junk[:], in_=py[:], func=ACT.Square, scale=float(D) ** -0.5,
                                              accum_out=ss2[:, 0:1]), reads=[py, ss2], writes=[junk, ss2])
    rstd_from_ms(kb, rs2, ss2)
    kb.op("dve", lambda: nc.vector.scalar_tensor_tensor(out=ot[:], in0=py[:], scalar=rs2[:, 0:1], in1=Gpost[:],
                                                        op0=ALU.mult, op1=ALU.mult), reads=[py, rs2, Gpost], writes=[ot])
    kb.op("pool", lambda: nc.gpsimd.tensor_tensor(out=ot[:], in0=ot[:], in1=xb[:], op=ALU.add), reads=[ot, xb], writes=[ot])
    return kb.dma("sp", out_rows_ap, ot[:], reads=[ot])


def build_outproj(kb, NT, KD, og_in, w_out, g_post, x_in, h_out, ident, og_buf=None, x_buf=None, h_buf=None):
    nc = kb.nc
    KC = KD // 128
    Wout = kb.sb([128, KC, D], BF16, "Wo")
    for kc in range(KC):
        kb.dma("pool", Wout[:, kc, :], w_out[kc * 128:(kc + 1) * 128, :], writes=[Wout])
    Gpost = kb.sb([128, D], F32, "Gpost")
    kb.dma("sp", Gpost[:], g_post, writes=[Gpost])
    OGT = [kb.sb([128, KD], BF16, "ogt") for _ in range(2)]
    ogT = [kb.sb([128, KC, 128], BF16, "ogT") for _ in range(2)]
    XB = [kb.sb([128, D], F32, "xb") for _ in range(2)]
    OT = [kb.sb([128, D], F32, "ot") for _ in range(2)]
    SS = [kb.sb([128, 1], F32, "ss") for _ in range(2)]
    RS = [kb.sb([128, 1], F32, "rs") for _ in range(2)]
    junk = kb.sb([128, D], BF16, "junk")
    TP = [kb.ps([128, 512], F32, "tp") for _ in range(2)]
    PY = [kb.ps([128, D], F32, "py") for _ in range(2)]
    outs = []
    rd = [og_buf] if og_buf is not None else []
    rdx = [x_buf] if x_buf is not None else []
    for t in range(NT // 128):
        r0 = t * 128
        og = OGT[t % 2]; oT = ogT[t % 2]; py = PY[t % 2]
        kb.dma("sp", og[:], og_in[r0:r0 + 128, :], reads=rd, writes=[og])
        for kc in range(KC):
            tp = TP[(kc // 8) % 2]
            tpb = tp[:].bitcast(BF16).rearrange("p (a b) -> p a b", b=128)
            kb.op("pe", lambda: nc.tensor.transpose(out=tpb[:, kc % 8, :], in_=og[:, kc * 128:(kc + 1) * 128],
                                                    identity=ident[:]), reads=[og, ident], writes=[tp])
            if kc % 8 == 7:
                kb.op("act", lambda: nc.scalar.copy(out=oT[:, kc - 7:kc + 1, :], in_=tpb), reads=[tp], writes=[oT])
        for cb in range(2):
            for kc in range(KC):
                kb.op("pe", lambda: nc.tensor.matmul(out=py[:, cb * 512:(cb + 1) * 512], lhsT=oT[:, kc, :],
                                                     rhs=Wout[:, kc, cb * 512:(cb + 1) * 512],
                                                     start=(kc == 0), stop=(kc == KC - 1)), reads=[oT, Wout], writes=[py])
        if x_buf is not None:
            pass
        tok = post_norm_residual(kb, py, x_in[r0:r0 + 128, :], Gpost, h_out[r0:r0 + 128, :], XB[t % 2], OT[t % 2],
                                 SS[t % 2], RS[t % 2], junk)
        if h_buf is not None:
            h_buf.last_w = tok
        outs.append(tok)
    return outs


def build_normproj(kb, NT, x_in, g_pre, w, C, out_ap, ident, sig_from=None, sig_out=None, x_buf=None):
    nc = kb.nc
    KC = 8
    W = kb.sb([128, KC, C], BF16, "Wp")
    for kc in range(KC):
        kb.dma("pool", W[:, kc, :], w[kc * 128:(kc + 1) * 128, :], writes=[W])
    G = kb.sb([128, D], F32, "G")
    kb.dma("sp", G[:], g_pre, writes=[G])
    XA = [kb.sb([128, D], F32, "xa") for _ in range(2)]
    xn = kb.sb([128, D], BF16, "xn")
    xnT = kb.sb([128, KC, 128], BF16, "xnT")
    junk = kb.sb([128, D], BF16, "junk")
    ss = kb.sb([128, 1], F32, "ss"); rs = kb.sb([128, 1], F32, "rs")
    CM = C if sig_from is None else sig_from
    OB = [kb.sb([128, CM], BF16, "ob") for _ in range(2)]
    if sig_from is not None:
        SG = [kb.sb([128, C - sig_from], F32, "sgo") for _ in range(2)]
    tp = kb.ps([128, 512], F32, "tp")
    tpb = tp[:].bitcast(BF16).rearrange("p (a b) -> p a b", b=128)
    PB = [kb.ps([128, 512], F32, "pb") for _ in range(3)]
    outs = []
    nb = 0
    rdx = [x_buf] if x_buf is not None else []
    for t in range(NT // 128):
        r0 = t * 128
        xa = XA[t % 2]; ob = OB[t % 2]
        kb.dma("sp", xa[:], x_in[r0:r0 + 128, :], reads=rdx, writes=[xa])
        kb.op("dve", lambda: nc.vector.memset(ss[:], 0.0), writes=[ss])
        kb.op("act", lambda: nc.scalar.activation(out=junk[:], in_=xa[:], func=ACT.Square, scale=float(D) ** -0.5,
                                                  accum_out=ss[:, 0:1]), reads=[xa, ss], writes=[junk, ss])
        rstd_from_ms(kb, rs, ss)
        kb.op("dve", lambda: nc.vector.scalar_tensor_tensor(out=xn[:], in0=xa[:], scalar=rs[:, 0:1], in1=G[:],
                                                            op0=ALU.mult, op1=ALU.mult), reads=[xa, rs, G], writes=[xn])
        for kc in range(KC):
            kb.op("pe", lambda: nc.tensor.transpose(out=tpb[:, kc, :], in_=xn[:, kc * 128:(kc + 1) * 128],
                                                    identity=ident[:]), reads=[xn, ident], writes=[tp])
        kb.op("act", lambda: nc.scalar.copy(out=xnT[:], in_=tpb), reads=[tp], writes=[xnT])
        c0 = 0
        while c0 < C:
            cw = min(512, C - c0)
            if sig_from is not None and c0 < sig_from:
                cw = min(cw, sig_from - c0)
            pb = PB[nb % 3]; nb += 1
            for kc in range(KC):
                kb.op("pe", lambda: nc.tensor.matmul(out=pb[:, 0:cw], lhsT=xnT[:, kc, :], rhs=W[:, kc, c0:c0 + cw],
                                                     start=(kc == 0), stop=(kc == KC - 1)), reads=[xnT, W], writes=[pb])
            if sig_from is not None and c0 >= sig_from:
                sg = SG[t % 2]
                kb.op("act", lambda: nc.scalar.activation(out=sg[:, c0 - sig_from:c0 - sig_from + cw], in_=pb[:, 0:cw],
                                                          func=ACT.Sigmoid), reads=[pb], writes=[sg])
            else:
                kb.op("act", lambda: nc.scalar.copy(out=ob[:, c0:c0 + cw], in_=pb[:, 0:cw]), reads=[pb], writes=[ob])
            c0 += cw
        outs.append(kb.dma("sp", out_ap[r0:r0 + 128, :], ob[:], reads=[ob]))
        if sig_from is not None:
            outs.append(kb.dma("sp", sig_out[r0:r0 + 128, :], SG[t % 2][:], reads=[SG[t % 2]]))
    return outs


NEG = -30000.0
HD = 64; GR = 4; NG = 4


def build_compress(kb, NB, XT, W1, W2, PEcol, kcv_out):
    nc = kb.nc
    W1s = kb.sb([128, 2, 16, 256], BF16, "W1s")
    W2s = kb.sb([128, 2, 2, 64], BF16, "W2s")
    PEc = kb.sb([128, 2, 16], BF16, "PEc")
    for kv in range(2):
        kb.dma("pool", W1s[:, kv, :, :], W1[kv].rearrange("(c p) n -> p c n", p=128), writes=[W1s])
        kb.dma("pool", W2s[:, kv, :, :], W2[kv].rearrange("(c p) n -> p c n", p=128), writes=[W2s])
        kb.dma("pool", PEc[:, kv, :], PEcol[kv], writes=[PEc])
    pebs = kb.sb([128, 2, 2], F32, "pebs")
    pp = kb.ps([128, 512], F32, "pp")
    for kv in range(2):
        for hc in range(2):
            for cc in range(16):
                kb.op("pe", lambda: nc.tensor.matmul(out=pp[:, 0:1], lhsT=W1s[:, kv, cc, hc * 128:(hc + 1) * 128],
                                                     rhs=PEc[:, kv, cc:cc + 1], start=(cc == 0), stop=(cc == 15)),
                      reads=[W1s, PEc], writes=[pp])
            kb.op("dve", lambda: nc.vector.tensor_copy(out=pebs[:, kv, hc:hc + 1], in_=pp[:, 0:1]), reads=[pp], writes=[pebs])
    NW = min(128, NB)
    XTt = [kb.sb([128, 16, NW], BF16, "XTt") for _ in range(2)]
    hT = [kb.sb([128, 2, NW], BF16, "hT") for _ in range(2)]
    ob = [kb.sb([128, 64], BF16, "ob") for _ in range(2)]
    PH = [kb.ps([128, 512], F32, "ph") for _ in range(2)]
    PO = [kb.ps([128, 512], F32, "po") for _ in range(2)]
    outs = []
    it = 0
    for kv in range(2):
        for g in range(NG):
            for nt in range((NB + 127) // 128):
                xt = XTt[it % 2]; ht = hT[it % 2]; o = ob[it % 2]; ph = PH[it % 2]; po = PO[it % 2]; it += 1
                kb.dma("sp", xt[:], XT[kv, g, :, :, nt * 128:nt * 128 + NW].rearrange("c p n -> p c n"), writes=[xt])
                for hc in range(2):
                    for cc in range(16):
                        kb.op("pe", lambda: nc.tensor.matmul(out=ph[:, hc * 128:hc * 128 + NW],
                                                             lhsT=W1s[:, kv, cc, hc * 128:(hc + 1) * 128], rhs=xt[:, cc, :],
                                                             start=(cc == 0), stop=(cc == 15)), reads=[W1s, xt], writes=[ph])
                    kb.op("act", lambda: nc.scalar.activation(out=ht[:, hc, :], in_=ph[:, hc * 128:hc * 128 + NW],
                                                              func=ACT.Silu, bias=pebs[:, kv, hc:hc + 1]),
                          reads=[ph, pebs], writes=[ht])
                for hc in range(2):
                    kb.op("pe", lambda: nc.tensor.matmul(out=po[0:NW, 0:64], lhsT=ht[:, hc, :], rhs=W2s[:, kv, hc, :],
                                                         start=(hc == 0), stop=(hc == 1)), reads=[ht, W2s], writes=[po])
                kb.op("dve", lambda: nc.vector.tensor_copy(out=o[0:NW, :], in_=po[0:NW, 0:64]), reads=[po], writes=[o])
                outs.append(kb.dma("sp", kcv_out[kv, nt * 128:nt * 128 + NW, g, :], o[0:NW, :], reads=[o]))
    return outs


def build_nsa_attn(kb, NQT, CPB, qin, gates, A, o_out, identb, q_buf=None):
    nc = kb.nc
    NVT = NQT * CPB + CPB - 1
    NSB = (NVT + 15) // 16
    NBLK = NSB * 32
    NCT = (8 * NVT + 16 + 127) // 128
    NKV = NVT * 128
    rdq = [q_buf] if q_buf is not None else []
    identf = kb.sb([128, 128], F32, "identf")
    kb.dma("sp", identf[:], A["identf"], writes=[identf])
    Mmap = kb.sb([128, NCT, NBLK], BF16, "Mmap")
    kb.dma("pool", Mmap[:], A["Mmap"], writes=[Mmap])
    MNear = kb.sb([16, NQT, NBLK], BF16, "MNear")
    kb.dma("pool", MNear[:], A["MNear"], writes=[MNear])
    bmax = kb.sb([128, 1], F32, "bmax")
    rbf = kb.sb([128, 512], F32, "rbf")
    kb.dma("sp", rbf[:], A["relb_rep"], writes=[rbf])
    kb.op("dve", lambda: nc.vector.tensor_reduce(out=bmax[:], in_=rbf[:], axis=AX.X, op=ALU.max, apply_absolute_value=True),
          reads=[rbf], writes=[bmax])
    ones64 = kb.sb([64, 128], F32, "ones64")
    kb.op("dve", lambda: nc.vector.memset(ones64[:], 1.0), writes=[ones64])
    KsT = kb.sb([128, NKV], BF16, "KsT")
    Vs = kb.sb([128, NVT, 65], BF16, "Vs")
    KcT = kb.sb([128, NCT * 128], BF16, "KcT")
    Vc = kb.sb([128, NCT, 65], BF16, "Vc")
    VcN = kb.sb([16, NQT, 65], BF16, "VcN")
    TS = kb.sb([128, 2, 2, 512], BF16, "TS")
    TW = kb.sb([128, 5, 2, 512], BF16, "TW")
    TC = kb.sb([16, 2, 512], BF16, "TC")
    tf = kb.sb([128, 512], F32, "tf"); tr = kb.sb([128, 512], F32, "tr")
    kd = kb.sb([64, 3], F32, "kd"); kd1 = kb.sb([64, 1], F32, "kd1"); dg = kb.sb([64, 64], F32, "dg")
    KDrow = kb.sb([128, 64], F32, "KDrow")
    kwscr = kb.sb([64, 2048], BF16, "kwscr"); kdw = kb.sb([64, 16], F32, "kdw")
    b31 = kb.sb([128, 4], F32, "b31"); b31h = kb.sb([128, 4], BF16, "b31h"); b31r = kb.sb([128, 4], F32, "b31r")
    AUG1 = [kb.sb([128, GR, 128], BF16, "AUG1") for _ in range(2)]
    AUG2 = [kb.sb([128, NSB, 128], BF16, "AUG2") for _ in range(2)]
    QA = [[kb.sb([128, 512], BF16, "QA%d" % i) for i in range(NSB)] for _ in range(2)]
    QA0 = [kb.sb([128, 512], BF16, "QA0") for _ in range(2)]
    QT = [kb.sb([128, GR * HD], BF16, "qt") for _ in range(2)]
    GT = [kb.sb([128, 48], F32, "gt") for _ in range(2)]
    MS = [kb.sb([128, 3, NBLK], F32, "ms") for _ in range(2)]
    KwT = [kb.sb([128, 5 * 128], BF16, "KwT") for _ in range(2)]
    Vw = [kb.sb([128, 5, 65], BF16, "Vw") for _ in range(2)]
    absq = kb.sb([128, GR, HD], F32, "absq")
    U4 = kb.sb([128, GR], F32, "U4")
    PTc = kb.sb([128, NCT + 1, 512], BF16, "PTc")
    PTn = kb.sb([16, 512], BF16, "PTn")
    PTr = [kb.sb([128, 1024], BF16, "PTr") for _ in range(3)]
    oTs = [kb.sb([65, 512], F32, "oT") for _ in range(2)]
    otok = [kb.sb([128, 3, GR, 65], F32, "otok") for _ in range(2)]
    rinv = [kb.sb([128, 3, GR], F32, "rinv") for _ in range(2)]
    fb = kb.sb([128, 3, GR], F32, "fb")
    imp = kb.sb([128, NBLK], F32, "imp")
    imw = kb.sb([128, NBLK], F32, "imw")
    m8 = kb.sb([128, 8], F32, "m8"); thr = kb.sb([128, 1], F32, "thr")
    oacc = kb.sb([128, GR, HD], F32, "oacc"); otmp = kb.sb([128, GR, HD], F32, "otmp")
    OB = [kb.sb([128, GR * HD], BF16, "obo") for _ in range(2)]
    PSB = [kb.ps([128, 1024], F32, "psS%d" % i) for i in range(2)]
    PO_ = [kb.ps([128, 512], F32, "psO%d" % i) for i in range(2)]
    PY = kb.ps([128, 512], F32, "psYQ")
    PQ = PY
    PX = kb.ps([128, 512], F32, "psX")
    outs = []
    cnt = {"s": 0, "p": 0, "it": 0}

    def split_hilo(dst_hi, dst_lo, src_f32, rows):
        kb.op("dve", lambda: nc.vector.tensor_copy(out=dst_hi, in_=src_f32[0:rows, :]), reads=[tf], writes=[TS, TW, TC])
        kb.op("dve", lambda: nc.vector.tensor_copy(out=tr[0:rows, :], in_=dst_hi), reads=[TS, TW, TC], writes=[tr])
        kb.op("dve", lambda: nc.vector.tensor_tensor(out=dst_lo, in0=src_f32[0:rows, :], in1=tr[0:rows, :], op=ALU.subtract),
              reads=[tf, tr], writes=[TS, TW, TC])

    def softmax_tile(lhsT_ap, rhs_ap, KR, rows, toep=None, identrows=None, rd=None, dst=None):
        psb, ps = dst
        kb.op("pe", lambda: nc.tensor.matmul(out=ps[0:rows, :], lhsT=lhsT_ap, rhs=rhs_ap, start=True, stop=(toep is None)),
              reads=rd, writes=[psb])
        if toep is not None:
            hi, lo = toep
            kb.op("pe", lambda: nc.tensor.matmul(out=ps[0:rows, :], lhsT=identb[0:rows, 0:rows], rhs=hi, start=False, stop=False),
                  reads=[identb, TS, TW, TC], writes=[psb])
            kb.op("pe", lambda: nc.tensor.matmul(out=ps[0:rows, :], lhsT=identb[0:rows, 0:rows], rhs=lo, start=False, stop=True),
                  reads=[identb, TS, TW, TC], writes=[psb])
        return ps

    for g in range(NG):
        kb.dma("sp", KsT[:], A["KsT"][g], writes=[KsT])
        kb.dma("sp", Vs[:], A["Vs"][g].rearrange("(t p) e -> p t e", p=128), writes=[Vs])
        kb.dma("sp", KcT[:], A["KcT"][g], writes=[KcT])
        kb.dma("sp", Vc[:], A["Vc"][g].rearrange("(t p) e -> p t e", p=128), writes=[Vc])
        kb.dma("sp", VcN[:], A["VcN"][g].rearrange("l u e -> u l e"), writes=[VcN])
        for m in range(2):
            kb.dma("sp", tf[:], A["TOEPS"][g, m], writes=[tf])
            split_hilo(TS[:, m, 0, :], TS[:, m, 1, :], tf, 128)
        for m in range(5):
            kb.dma("sp", tf[:], A["TOEPW"][g, m], writes=[tf])
            split_hilo(TW[:, m, 0, :], TW[:, m, 1, :], tf, 128)
        kb.dma("sp", tf[0:16, :], A["TOEPC"][g], writes=[tf])
        split_hilo(TC[:, 0, :], TC[:, 1, :], tf, 16)
        kb.dma("sp", b31[:], A["B31"][g], writes=[b31])
        kb.op("dve", lambda: nc.vector.tensor_copy(out=b31h[:], in_=b31[:]), reads=[b31], writes=[b31h])
        kb.op("dve", lambda: nc.vector.tensor_copy(out=b31r[:], in_=b31h[:]), reads=[b31h], writes=[b31r])
        kb.op("dve", lambda: nc.vector.tensor_tensor(out=b31r[:], in0=b31[:], in1=b31r[:], op=ALU.subtract),
              reads=[b31, b31r], writes=[b31r])
        for pp_ in range(2):
            a1 = AUG1[pp_]; a2 = AUG2[pp_]
            kb.op("dve", lambda: nc.vector.memset(a1[:], 0.0), writes=[a1])
            kb.op("dve", lambda: nc.vector.memset(a2[:], 0.0), writes=[a2])
            kb.op("dve", lambda: nc.vector.memset(a1[:, :, 97:98], 1.0), writes=[a1])
            kb.op("dve", lambda: nc.vector.tensor_copy(out=a1[:, :, 98:99], in_=b31h[:].unsqueeze(2)), reads=[b31h], writes=[a1])
            kb.op("dve", lambda: nc.vector.tensor_copy(out=a1[:, :, 99:100], in_=b31r[:].unsqueeze(2)), reads=[b31r], writes=[a1])
        kb.op("dve", lambda: nc.vector.tensor_reduce(out=kd[:, 0:1], in_=KsT[0:64, :], axis=AX.X, op=ALU.max, apply_absolute_value=True),
              reads=[KsT], writes=[kd])
        kb.op("dve", lambda: nc.vector.tensor_reduce(out=kd[:, 1:2], in_=KcT[0:64, :], axis=AX.X, op=ALU.max, apply_absolute_value=True),
              reads=[KcT], writes=[kd])
        nch = (NKV + 2047) // 2048
        for c in range(nch):
            w = min(2048, NKV - c * 2048)
            kb.dma("sp", kwscr[:, 0:w], A["KwT"][g][0:64, c * 2048:c * 2048 + w], writes=[kwscr])
            kb.op("dve", lambda: nc.vector.tensor_reduce(out=kdw[:, c:c + 1], in_=kwscr[:, 0:w], axis=AX.X, op=ALU.max,
                                                         apply_absolute_value=True), reads=[kwscr], writes=[kdw])
        kb.op("dve", lambda: nc.vector.tensor_reduce(out=kd[:, 2:3], in_=kdw[:, 0:nch], axis=AX.X, op=ALU.max), reads=[kdw], writes=[kd])
        kb.op("dve", lambda: nc.vector.tensor_reduce(out=kd1[:], in_=kd[:], axis=AX.X, op=ALU.max), reads=[kd], writes=[kd1])
        kb.op("dve", lambda: nc.vector.tensor_scalar(out=dg[:], in0=identf[0:64, 0:64], scalar1=kd1[:, 0:1], scalar2=None, op0=ALU.mult),
              reads=[identf, kd1], writes=[dg])
        kb.op("pe", lambda: nc.tensor.matmul(out=PX[:, 0:64], lhsT=ones64[:], rhs=dg[:], start=True, stop=True),
              reads=[ones64, dg], writes=[PX])
        kb.op("dve", lambda: nc.vector.tensor_copy(out=KDrow[:], in_=PX[:, 0:64]), reads=[PX], writes=[KDrow])
        def pre(l, par):
            V = CPB * l + CPB - 1
            qt = QT[par]; gt = GT[par]; ms = MS[par]; kw = KwT[par]; vw = Vw[par]
            aug1 = AUG1[par]; aug2 = AUG2[par]; qa0 = QA0[par]; qa = QA[par]; otk = otok[par]; rnv = rinv[par]
            kb.dma("sp", qt[:], qin[l * 128:(l + 1) * 128, g * 256:(g + 1) * 256], reads=rdq, writes=[qt])
            kb.dma("sp", gt[:], gates[l * 128:(l + 1) * 128, :], reads=rdq, writes=[gt])
            kb.dma("sp", ms[:], A["MSEL"][l].rearrange("c q b -> q c b"), writes=[ms])
            wt0 = max(0, V - 4); nwt = V - wt0 + 1
            kb.dma("sp", kw[:, 0:nwt * 128], A["KwT"][g][:, wt0 * 128:(V + 1) * 128], writes=[kw])
            kb.dma("sp", vw[:, 0:nwt, :], A["Vw"][g][wt0 * 128:(V + 1) * 128, :].rearrange("(t p) e -> p t e", p=128), writes=[vw])
            qv = qt[:].rearrange("p (h d) -> p h d", h=GR)
            kb.op("act", lambda: nc.scalar.activation(out=aug1[:, :, 0:HD], in_=qv, func=ACT.Copy, scale=HD ** -0.5),
                  reads=[qt], writes=[aug1])
            kb.op("act", lambda: nc.scalar.activation(out=absq[:], in_=qv, func=ACT.Abs), reads=[qt], writes=[absq])
            kb.op("dve", lambda: nc.vector.tensor_tensor(out=absq[:], in0=absq[:], in1=KDrow[:].unsqueeze(1).broadcast_to([128, GR, HD]),
                                                         op=ALU.mult), reads=[absq, KDrow], writes=[absq])
            kb.op("dve", lambda: nc.vector.tensor_reduce(out=U4[:], in_=absq[:], axis=AX.X, op=ALU.add), reads=[absq], writes=[U4])
            kb.op("dve", lambda: nc.vector.tensor_scalar(out=U4[:], in0=U4[:], scalar1=-(HD ** -0.5), scalar2=bmax[:, 0:1],
                                                         op0=ALU.mult, op1=ALU.subtract), reads=[U4, bmax], writes=[U4])
            kb.op("dve", lambda: nc.vector.tensor_copy(out=aug1[:, :, 96:97], in_=U4[:].unsqueeze(2)), reads=[U4], writes=[aug1])
            yield
            yield
            for h in range(GR):
                kb.op("pe", lambda: nc.tensor.matmul(out=PQ[:, h * 128:(h + 1) * 128], lhsT=aug1[:, h, :], rhs=identb[:],
                                                     start=True, stop=True), reads=[aug1, identb], writes=[PQ])
            yield
            kb.op("act", lambda: nc.scalar.copy(out=qa0[:], in_=PQ[:]), reads=[PQ], writes=[qa0])
            yield
            nfar = 8 * V - 9
            oc = PO_[cnt["p"] % 2]; cnt["p"] += 1
            tiles = []
            n0 = 0
            while n0 < nfar:
                rows = min(128, nfar - n0)
                tiles.append((n0 // 128, rows))
                n0 += 128
            first = True
            for (tix, rows) in tiles:
                ps = softmax_tile(KcT[0:100, tix * 128:tix * 128 + rows], qa0[0:100, :], 100, rows, rd=[KcT, qa0], dst=(PY, PY[:, :]))
                yield
                kb.op("act", lambda: nc.scalar.activation(out=PTc[0:rows, tix, :], in_=ps[0:rows, :], func=ACT.Exp),
                      reads=[PY], writes=[PTc])
                yield
                kb.op("pe", lambda: nc.tensor.matmul(out=oc[0:65, :], lhsT=Vc[0:rows, tix, :], rhs=PTc[0:rows, tix, :],
                                                     start=first, stop=False), reads=[Vc, PTc], writes=[oc])
                first = False
                yield
            ps = softmax_tile(KcT[0:98, nfar:nfar + 16], qa0[0:98, :], 98, 16, toep=(TC[:, 0, :], TC[:, 1, :]), rd=[KcT, qa0], dst=(PY, PY[:, :]))
            yield
            kb.op("act", lambda: nc.scalar.activation(out=PTn[:], in_=ps[0:16, :], func=ACT.Exp), reads=[PY], writes=[PTn])
            yield
            kb.op("pe", lambda: nc.tensor.matmul(out=oc[0:65, :], lhsT=VcN[:, l, :], rhs=PTn[:], start=first, stop=True),
                  reads=[VcN, PTn], writes=[oc])
            yield
            yield from finish_branch(0, oc, otk, rnv, oTs[0])
            yield
            for h in range(GR):
                fst = True
                for (tix, rows) in tiles:
                    kb.op("pe", lambda: nc.tensor.matmul(out=PY[:, 0:NBLK], lhsT=PTc[0:rows, tix, h * 128:(h + 1) * 128],
                                                         rhs=Mmap[0:rows, tix, :], start=fst, stop=False),
                          reads=[PTc, Mmap], writes=[PY])
                    fst = False
                assert not fst
                kb.op("pe", lambda: nc.tensor.matmul(out=PY[:, 0:NBLK], lhsT=PTn[:, h * 128:(h + 1) * 128], rhs=MNear[:, l, :],
                                                     start=False, stop=True), reads=[PTn, MNear], writes=[PY])
                yield
                if h == 0:
                    kb.op("dve", lambda: nc.vector.tensor_scalar(out=imp[:], in0=PY[:, 0:NBLK], scalar1=rnv[:, 0, 0:1], scalar2=None,
                                                                 op0=ALU.mult), reads=[PY, rnv], writes=[imp])
                else:
                    kb.op("dve", lambda: nc.vector.scalar_tensor_tensor(out=imp[:], in0=PY[:, 0:NBLK], scalar=rnv[:, 0, h:h + 1],
                                                                        in1=imp[:], op0=ALU.mult, op1=ALU.add),
                          reads=[PY, rnv, imp], writes=[imp])
                yield
            kb.op("dve", lambda: nc.vector.tensor_tensor(out=imp[:], in0=imp[:], in1=ms[:, 0, :], op=ALU.mult), reads=[imp, ms], writes=[imp])
            kb.op("dve", lambda: nc.vector.tensor_tensor(out=imp[:], in0=imp[:], in1=ms[:, 1, :], op=ALU.add), reads=[imp, ms], writes=[imp])
            kb.op("dve", lambda: nc.vector.max(out=m8[:], in_=imp[:]), reads=[imp], writes=[m8])
            kb.op("dve", lambda: nc.vector.match_replace(out=imw[:], in_to_replace=m8[:], in_values=imp[:], imm_value=-3.0e38),
                  reads=[m8, imp], writes=[imw])
            yield
            kb.op("dve", lambda: nc.vector.max(out=m8[:], in_=imw[:]), reads=[imw], writes=[m8])
            kb.op("dve", lambda: nc.vector.tensor_reduce(out=thr[:], in_=m8[:], axis=AX.X, op=ALU.min), reads=[m8], writes=[thr])
            kb.op("dve", lambda: nc.vector.tensor_scalar(out=imw[:], in0=imp[:], scalar1=thr[:, 0:1], scalar2=None, op0=ALU.is_ge),
                  reads=[imp, thr], writes=[imw])
            kb.op("dve", lambda: nc.vector.tensor_tensor(out=imw[:], in0=imw[:], in1=ms[:, 2, :], op=ALU.mult), reads=[imw, ms], writes=[imw])
            kb.op("dve", lambda: nc.vector.tensor_scalar(out=aug2[:, :, 64:96], in0=imw[:].rearrange("p (s j) -> p s j", j=32),
                                                         scalar1=-NEG, scalar2=NEG, op0=ALU.mult, op1=ALU.add),
                  reads=[imw], writes=[aug2])
            yield
            nsb = (V + 1 + 15) // 16
            for sb in range(nsb):
                for h in range(GR):
                    kb.op("pe", lambda: nc.tensor.matmul(out=PQ[:, h * 128:(h + 1) * 128], lhsT=aug1[:, h, :], rhs=identb[:],
                                                         start=True, stop=False), reads=[aug1, identb], writes=[PQ])
                    kb.op("pe", lambda: nc.tensor.matmul(out=PQ[:, h * 128:(h + 1) * 128], lhsT=aug2[:, sb, :], rhs=identb[:],
                                                         start=False, stop=True), reads=[aug2, identb], writes=[PQ])
                yield
                kb.op("dve", lambda: nc.vector.tensor_copy(out=qa[sb][:], in_=PQ[:]), reads=[PQ], writes=[qa[sb]])
                yield

        def finish_branch(b, acc, otk, rnv, oT):
            kb.op("act", lambda: nc.scalar.copy(out=oT[:], in_=acc[0:65, :]), reads=[acc], writes=[oT])
            yield
            pxv = PX[:, 0:GR * 65].rearrange("p (h e) -> p h e", h=GR)
            for h in range(GR):
                kb.op("pe", lambda: nc.tensor.matmul(out=pxv[:, h, :], lhsT=oT[:, h * 128:(h + 1) * 128], rhs=identf[0:65, 0:65],
                                                     start=True, stop=True), reads=[oT, identf], writes=[PX])
            yield
            kb.op("dve", lambda: nc.vector.tensor_copy(out=otk[:, b, :, :], in_=pxv), reads=[PX], writes=[otk])
            kb.op("dve", lambda: nc.vector.tensor_scalar(out=rnv[:, b, :], in0=otk[:, b, :, 64], scalar1=1e-30, scalar2=None,
                                                         op0=ALU.max), reads=[otk], writes=[rnv])
            kb.op("dve", lambda: nc.vector.reciprocal(out=rnv[:, b, :], in_=rnv[:, b, :]), reads=[rnv], writes=[rnv])

        def step(gen):
            if gen is not None:
                next(gen, None)

        def post(l, par, nxt):
            V = CPB * l + CPB - 1
            gt = GT[par]; kw = KwT[par]; vw = Vw[par]; ob = OB[par]
            qa0 = QA0[par]; qa = QA[par]; otk = otok[par]; rnv = rinv[par]
            wt0 = max(0, V - 4); nwt = V - wt0 + 1
            osel = PO_[cnt["p"] % 2]; cnt["p"] += 1

            def sel_tail(p, psb):
                pt = PTr[p % 3]
                kb.op("act", lambda: nc.scalar.activation(out=pt[:], in_=psb[:], func=ACT.Exp), reads=[psb], writes=[pt])
                for hf in range(2):
                    v = 2 * p + hf
                    kb.op("pe", lambda: nc.tensor.matmul(out=osel[0:65, :], lhsT=Vs[:, v, :], rhs=pt[:, hf * 512:(hf + 1) * 512],
                                                         start=(v == 0), stop=(v == V)), reads=[Vs, pt], writes=[osel])

            pend = None
            for p in range((V + 1) // 2):
                psb = PSB[cnt["s"] % 2]; cnt["s"] += 1
                for hf in range(2):
                    v = 2 * p + hf
                    sb = v // 16
                    dst = (psb, psb[:, hf * 512:(hf + 1) * 512])
                    if v >= V - 1:
                        m = v - (V - 1)
                        softmax_tile(KsT[0:98, v * 128:(v + 1) * 128], qa[sb][0:98, :], 98, 128, toep=(TS[:, m, 0, :], TS[:, m, 1, :]),
                                     rd=[KsT, qa[sb]], dst=dst)
                    else:
                        softmax_tile(KsT[0:100, v * 128:(v + 1) * 128], qa[sb][0:100, :], 100, 128, rd=[KsT, qa[sb]], dst=dst)
                if pend is not None:
                    sel_tail(*pend)
                pend = (p, psb)
                step(nxt)
                step(nxt)
            sel_tail(*pend)
            for _ in finish_branch(1, osel, otk, rnv, oTs[1]):
                step(nxt)
            ow = PO_[cnt["p"] % 2]; cnt["p"] += 1

            def win_tail(p, psb, n):
                pt = PTr[p % 3]
                kb.op("act", lambda: nc.scalar.activation(out=pt[:, 0:n * 512], in_=psb[:, 0:n * 512], func=ACT.Exp),
                      reads=[psb], writes=[pt])
                for hf in range(n):
                    i = 2 * p + hf
                    kb.op("pe", lambda: nc.tensor.matmul(out=ow[0:65, :], lhsT=vw[:, i, :], rhs=pt[:, hf * 512:(hf + 1) * 512],
                                                         start=(i == 0), stop=(i == nwt - 1)), reads=[vw, pt], writes=[ow])

            pend = None
            for p in range((nwt + 1) // 2):
                psb = PSB[cnt["s"] % 2]; cnt["s"] += 1
                n = min(2, nwt - 2 * p)
                for hf in range(n):
                    i = 2 * p + hf
                    m = wt0 + i - (V - 4)
                    softmax_tile(kw[0:98, i * 128:(i + 1) * 128], qa0[0:98, :], 98, 128, toep=(TW[:, m, 0, :], TW[:, m, 1, :]),
                                 rd=[kw, qa0], dst=(psb, psb[:, hf * 512:(hf + 1) * 512]))
                if pend is not None:
                    win_tail(*pend)
                pend = (p, psb, n)
                step(nxt)
            win_tail(*pend)
            for _ in finish_branch(2, ow, otk, rnv, oTs[1]):
                step(nxt)
            gv = gt[:, g * 12:(g + 1) * 12].rearrange("p (r b) -> p b r", b=3)
            kb.op("dve", lambda: nc.vector.tensor_tensor(out=fb[:], in0=rnv[:], in1=gv, op=ALU.mult), reads=[rnv, gt], writes=[fb])
            for b in range(3):
                fbb = fb[:, b, :].unsqueeze(2).broadcast_to([128, GR, HD])
                dst = oacc if b == 0 else otmp
                kb.op("dve", lambda: nc.vector.tensor_tensor(out=dst[:], in0=otk[:, b, :, 0:HD], in1=fbb, op=ALU.mult),
                      reads=[otk, fb], writes=[dst])
                if b > 0:
                    kb.op("dve", lambda: nc.vector.tensor_tensor(out=oacc[:], in0=oacc[:], in1=otmp[:], op=ALU.add),
                          reads=[oacc, otmp], writes=[oacc])
            kb.op("dve", lambda: nc.vector.tensor_copy(out=ob[:].rearrange("p (h d) -> p h d", h=GR), in_=oacc[:]),
                  reads=[oacc], writes=[ob])
            outs.append(kb.dma("sp", o_out[l * 128:(l + 1) * 128, g * 256:(g + 1) * 256], ob[:], reads=[ob]))
            if nxt is not None:
                for _ in nxt:
                    pass

        par = 0
        for _ in pre(0, par):
            pass
        for l in range(NQT):
            nxt = pre(l + 1, 1 - par) if l + 1 < NQT else None
            post(l, par, nxt)
            par = 1 - par
    return outs

RH = 4; DK = 256; DV = 512
LOGG = np.log(1.0 - 2.0 ** (-5.0 - np.arange(RH, dtype=np.float64)))

def perm_ret_w_in(w):
    idx = []
    for blk in range(2):
        for h in range(RH):
            base = blk * 1024 + h * DK
            idx += [base + 2 * m for m in range(128)] + [base + 2 * m + 1 for m in range(128)]
    idx += list(range(2048, 6144))
    return np.ascontiguousarray(w[:, idx])

def ret_tables(pos):
    theta = (1.0 / (10000.0 ** np.linspace(0.0, 1.0, DK // 2, dtype=np.float32))).astype(np.float32)
    ang = pos.astype(np.float32)[:, None] * theta[None, :]
    cos = np.cos(ang).astype(np.float32); sin = np.sin(ang).astype(np.float32)
    jloc = (pos % 128).astype(np.float64)
    sc = (DK ** -0.5) * np.exp(-jloc[:, None] * LOGG[None, :])
    cosk = (cos[:, None, :].astype(np.float64) * sc[:, :, None]).astype(np.float32)
    sink = (sin[:, None, :].astype(np.float64) * sc[:, :, None]).astype(np.float32)
    return (np.ascontiguousarray(cos.T), np.ascontiguousarray(sin.T), np.ascontiguousarray(cosk),
            np.ascontiguousarray(sink))

def ret_consts():
    i = np.arange(128, dtype=np.float64)
    gi = np.exp(i[:, None] * LOGG[None, :]).astype(np.float32)
    g2i = np.exp(2 * i[:, None] * LOGG[None, :]).astype(np.float32)
    maskT = (np.arange(128)[None, :] >= np.arange(128)[:, None]).astype(np.float32)
    return gi, g2i, maskT

def state_to_dev(S):
    return np.ascontiguousarray(S.reshape(RH, 128, 2, DV).transpose(2, 1, 0, 3))

def state_from_dev(Sd):
    return np.ascontiguousarray(Sd.transpose(2, 1, 0, 3).reshape(RH, DK, DV))

def bcast128(v):
    return np.ascontiguousarray(np.broadcast_to(v.reshape(1, -1), (128, v.size))).astype(np.float32)

BF = ml_dtypes.bfloat16
NEG = -30000.0

def t5_bucket_np(dist):
    n = np.maximum(dist, 0).astype(np.int64)
    nf = np.maximum(n, 1).astype(np.float32)
    val = (np.log(nf / np.float32(16)) / np.float32(math.log(128 / 16))).astype(np.float32) * np.float32(16)
    large = 16 + val.astype(np.int32)
    large = np.minimum(large, 31)
    return np.where(n < 16, n, large).astype(np.int64)

def nsa_consts(NQT, CPB, rel_bias):
    NVT = NQT * CPB + CPB - 1
    NSB = (NVT + 15) // 16; NBLK = NSB * 32
    NCT = (8 * NVT + 16 + 127) // 128
    rb = np.asarray(rel_bias, np.float32)
    k = np.arange(128)[:, None]; q = np.arange(128)[None, :]
    TOEPS = np.zeros((4, 2, 128, 4, 128), np.float32)
    TOEPW = np.zeros((4, 5, 128, 4, 128), np.float32)
    TOEPC = np.zeros((4, 16, 4, 128), np.float32)
    for g in range(4):
        for r in range(4):
            h = g * 4 + r
            for m in range(2):
                d = 128 * (1 - m) + q - k
                TOEPS[g, m, :, r, :] = np.where(d >= 0, rb[t5_bucket_np(d), h], NEG)
            for m in range(5):
                d = 128 * (4 - m) + q - k
                TOEPW[g, m, :, r, :] = np.where((d >= 0) & (d < 512), rb[t5_bucket_np(d), h], NEG)
            u = np.arange(16)[:, None]; qq = np.arange(128)[None, :]
            d = qq + 113 - 16 * u
            TOEPC[g, :, r, :] = np.where(d >= 0, rb[t5_bucket_np(d), h], NEG)
    B31 = np.zeros((4, 128, 4), np.float32)
    for g in range(4):
        B31[g] = rb[31, g * 4:(g + 1) * 4][None, :]
    relb_rep = np.ascontiguousarray(np.broadcast_to(rb.reshape(1, 512), (128, 512)))
    npr = (np.arange(NCT)[None, :] * 128 + np.arange(128)[:, None])
    b = np.arange(NBLK)[None, None, :]
    Mmap = ((npr[:, :, None] >= 4 * b - 1) & (npr[:, :, None] <= 4 * b + 3)).astype(np.float32)
    MNear = np.zeros((16, NQT, NBLK), np.float32)
    for l in range(NQT):
        V = CPB * l + CPB - 1
        npn = 8 * V - 9 + np.arange(16)[:, None]
        bb = np.arange(NBLK)[None, :]
        MNear[:, l, :] = ((npn >= 4 * bb - 1) & (npn <= 4 * bb + 3))
    return dict(TOEPS=TOEPS.reshape(4, 2, 128, 512), TOEPW=TOEPW.reshape(4, 5, 128, 512), TOEPC=TOEPC.reshape(4, 16, 512),
                B31=B31, relb_rep=relb_rep, Mmap=Mmap, MNear=MNear, identf=np.eye(128, dtype=np.float32))

def nsa_core_inputs(NQT, CPB, j, S, kv_tok, kc, vc):
    shift = CPB - 1 - j
    NVT = NQT * CPB + CPB - 1
    NSB = (NVT + 15) // 16; NBLK = NSB * 32
    NCT = (8 * NVT + 16 + 127) // 128
    NKV = NVT * 128
    n_cmp = kc.shape[0]
    kvt = kv_tok.reshape(S, 6, 4, 64)
    KsT = np.zeros((4, 128, NKV), BF); KwT = np.zeros((4, 128, NKV), BF)
    Vs = np.zeros((4, NKV, 65), BF); Vw = np.zeros((4, NKV, 65), BF)
    KcT = np.zeros((4, 128, NCT * 128), BF); Vc = np.zeros((4, NCT * 128, 65), BF)
    t0 = 128 * shift
    tp = np.arange(NKV)
    ind = ((tp // 64) % 32)
    for g in range(4):
        KsT[g, 0:64, t0:t0 + S] = kvt[:, 2, g, :].T
        KwT[g, 0:64, t0:t0 + S] = kvt[:, 4, g, :].T
        Vs[g, t0:t0 + S, 0:64] = kvt[:, 3, g, :]
        Vw[g, t0:t0 + S, 0:64] = kvt[:, 5, g, :]
        Vs[g, :, 64] = 1.0; Vw[g, :, 64] = 1.0
        for jj in range(32):
            KsT[g, 64 + jj, :] = (ind == jj).astype(np.float32)
        KsT[g, 96] = 1.0; KsT[g, 98] = 1.0; KsT[g, 99] = 1.0
        KwT[g, 96] = 1.0
        KwT[g, 97, :t0] = NEG
        c0 = 8 * shift
        KcT[g, 0:64, c0:c0 + n_cmp] = kc[:, g, :].T
        Vc[g, c0:c0 + n_cmp, 0:64] = vc[:, g, :]
        Vc[g, :, 64] = 1.0
        KcT[g, 96] = 1.0; KcT[g, 98] = 1.0; KcT[g, 99] = 1.0
        KcT[g, 97, :] = NEG
        KcT[g, 97, c0:c0 + n_cmp] = 0.0
    VcN = np.zeros((4, NQT, 16, 65), BF)
    for l in range(NQT):
        V = CPB * l + CPB - 1
        VcN[:, l] = Vc[:, 8 * V - 9:8 * V + 7, :]
    MSEL = np.zeros((NQT, 3, 128, NBLK), np.float32)
    n_sel = S // 64
    for l in range(NQT):
        T = CPB * l + j
        t = 128 * T + np.arange(128)[:, None]
        b = np.arange(NBLK)[None, :] - 2 * shift
        valid = (b >= 0) & (b < n_sel) & (64 * b <= t)
        cur = t // 64
        f0 = (b == 0); f1 = (b == cur); f2 = (b == cur - 1)
        forced = (f0 | f1 | f2) & valid
        MSEL[l, 0] = (valid & ~forced)
        add = np.where(valid, 0.0, -1e30)
        add = np.where(f2 & valid, 1e30, add); add = np.where(f1 & valid, 2e30, add); add = np.where(f0 & valid, 3e30, add)
        MSEL[l, 1] = add
        MSEL[l, 2] = valid
    return dict(KsT=KsT, KwT=KwT, Vs=Vs, Vw=Vw, KcT=KcT, Vc=Vc, VcN=VcN, MSEL=MSEL)

def im2col(kvtok, n0, NB):
    S = kvtok.shape[0]
    idx = 16 * (n0 + np.arange(NB))[None, None, :] + 2 * np.arange(16)[:, None, None] + np.arange(2)[None, :, None]
    ok = idx < S
    g = kvtok[np.minimum(idx, S - 1)]
    g = np.where(ok[..., None, None, None], g, np.zeros((), kvtok.dtype))
    return np.ascontiguousarray(g.transpose(3, 4, 0, 1, 5, 2).reshape(2, 4, 16, 128, NB))

def pecol(pe):
    return np.ascontiguousarray(pe.reshape(2, 16, 2, 64).transpose(0, 2, 3, 1).reshape(2, 128, 16))


def pecol(pe):
    return np.ascontiguousarray(pe.reshape(2, 16, 2, 64).transpose(0, 2, 3, 1).reshape(2, 128, 16))

CPB = 4; FH = 2816


def _di(nc, n, s, dt=F32):
    return nc.dram_tensor(n, list(s), dt, kind="ExternalInput").ap()


def _do(nc, n, s, dt=F32):
    return nc.dram_tensor(n, list(s), dt, kind="ExternalOutput").ap()


def _dint(nc, n, s, dt=F32):
    return nc.dram_tensor(n, list(s), dt, kind="Internal").ap()


def _ret_inputs(nc, NT):
    return dict(x=_di(nc, "x", [NT, D]), w_in=_di(nc, "ret_w_in", [D, 6144]), g_pre=_di(nc, "g_mix_pre", [128, D]),
                cosT=_di(nc, "cosT", [128, NT]), sinT=_di(nc, "sinT", [128, NT]), cosk=_di(nc, "cosk", [NT, 4, 128]),
                sink=_di(nc, "sink", [NT, 4, 128]), gi=_di(nc, "gi", [128, 4]), g2i=_di(nc, "g2i", [128, 4]),
                maskT=_di(nc, "maskT", [128, 128]), ident=_di(nc, "ident", [128, 128]))


def prog_state(NT):
    nc = bass.Bass("TRN2", target_bir_lowering=False)
    a = _ret_inputs(nc, NT)
    s_out = _do(nc, "s_out", [2, 128, 4, 512])
    kb = KB(nc)
    idb = load_ident(kb, a["ident"])
    kb.begin_phase()
    outs = build_ret(kb, NT, a["x"], a["w_in"], a["g_pre"], a["cosT"], a["sinT"], a["cosk"], a["sink"], a["gi"], a["g2i"],
                     a["maskT"], None, None, None, s_out, idb, state_only=True)
    kb.end_phase()
    kb.finish(outs); kb.close()
    return nc


def prog_layer0(NT):
    nc = bass.Bass("TRN2", target_bir_lowering=False)
    a = _ret_inputs(nc, NT)
    s_slots = _di(nc, "s_slots", [3, 2, 128, 4, 512]); coef = _di(nc, "coef", [128, 12])
    ret_w_out = _di(nc, "ret_w_out", [2048, D]); g_mix_post = _di(nc, "g_mix_post", [128, D])
    fw_in = _di(nc, "ffn_w_in", [D, 2 * FH]); fw_out = _di(nc, "ffn_w_out", [FH, D])
    fg_pre = _di(nc, "g_ffn_pre", [128, D]); fg_post = _di(nc, "g_ffn_post", [128, D])
    kv_g = _di(nc, "g_kv", [128, D]); kv_w = _di(nc, "kv_w", [D, 1536])
    og = _dint(nc, "og_scr", [NT, 2048], BF16); hmid = _dint(nc, "hmid_scr", [NT, D])
    s_dummy = _dint(nc, "s_scr", [2, 128, 4, 512])
    h1 = _do(nc, "h1", [NT, D]); kv = _do(nc, "kv", [NT, 1536], BF16)
    kb = KB(nc)
    idb = load_ident(kb, a["ident"])
    kb.begin_phase()
    build_ret(kb, NT, a["x"], a["w_in"], a["g_pre"], a["cosT"], a["sinT"], a["cosk"], a["sink"], a["gi"], a["g2i"],
              a["maskT"], s_slots, coef, og, s_dummy, idb)
    kb.end_phase()
    kb.begin_phase()
    build_outproj(kb, NT, 2048, og, ret_w_out, g_mix_post, a["x"], hmid, idb)
    kb.end_phase()
    kb.begin_phase()
    o1 = build_ffn(kb, NT, hmid, fw_in, fw_out, fg_pre, fg_post, h1, idb)
    kb.end_phase()
    kb.begin_phase()
    o2 = build_normproj(kb, NT, h1, kv_g, kv_w, 1536, kv, idb)
    kb.end_phase()
    kb.finish(o1 + o2); kb.close()
    return nc


def prog_compress(NB=256):
    nc = bass.Bass("TRN2", target_bir_lowering=False)
    XT = _di(nc, "XT", (2, 4, 16, 128, NB), BF16); W1 = _di(nc, "W1", (2, 2048, 256)); W2 = _di(nc, "W2", (2, 256, 64))
    PEc = _di(nc, "PEc", (2, 128, 16))
    out = _do(nc, "kcv", [2, NB, 4, 64], BF16)
    kb = KB(nc)
    kb.begin_phase()
    outs = build_compress(kb, NB, XT, W1, W2, PEc, out)
    kb.end_phase()
    kb.finish(outs); kb.close()
    return nc


def prog_layer1(nqt, cpb=CPB):
    nc = bass.Bass("TRN2", target_bir_lowering=False)
    NT = nqt * 128
    NVT = nqt * cpb + cpb - 1; NSB = (NVT + 15) // 16; NBLK = NSB * 32; NCT = (8 * NVT + 16 + 127) // 128; NKV = NVT * 128
    h1 = _di(nc, "h1rr", [NT, D]); g_pre = _di(nc, "g_mix_pre", [128, D]); w_in = _di(nc, "nsa_w_in", [D, 1072])
    ident = _di(nc, "ident", [128, 128])
    A = {}
    A["KsT"] = _di(nc, "KsT", (4, 128, NKV), BF16); A["KwT"] = _di(nc, "KwT", (4, 128, NKV), BF16)
    A["Vs"] = _di(nc, "Vs", (4, NKV, 65), BF16); A["Vw"] = _di(nc, "Vw", (4, NKV, 65), BF16)
    A["KcT"] = _di(nc, "KcT", (4, 128, NCT * 128), BF16); A["Vc"] = _di(nc, "Vc", (4, NCT * 128, 65), BF16)
    A["VcN"] = _di(nc, "VcN", (4, nqt, 16, 65), BF16); A["MSEL"] = _di(nc, "MSEL", (nqt, 3, 128, NBLK))
    A["TOEPS"] = _di(nc, "TOEPS", (4, 2, 128, 512)); A["TOEPW"] = _di(nc, "TOEPW", (4, 5, 128, 512)); A["TOEPC"] = _di(nc, "TOEPC", (4, 16, 512))
    A["B31"] = _di(nc, "B31", (4, 128, 4)); A["relb_rep"] = _di(nc, "relb_rep", (128, 512)); A["Mmap"] = _di(nc, "Mmap", (128, NCT, NBLK))
    A["MNear"] = _di(nc, "MNear", (16, nqt, NBLK)); A["identf"] = _di(nc, "identf", (128, 128))
    w_out = _di(nc, "nsa_w_out", [1024, D]); g_post = _di(nc, "g_mix_post", [128, D])
    fw_in = _di(nc, "ffn_w_in", [D, 2 * FH]); fw_out = _di(nc, "ffn_w_out", [FH, D])
    fg_pre = _di(nc, "g_ffn_pre", [128, D]); fg_post = _di(nc, "g_ffn_post", [128, D])
    qin = _dint(nc, "q_scr", [NT, 1024], BF16); gates = _dint(nc, "gate_scr", [NT, 48])
    o = _dint(nc, "o_scr", [NT, 1024], BF16); hmid = _dint(nc, "hmid_scr", [NT, D])
    out = _do(nc, "out", [NT, D])
    kb = KB(nc)
    idb = load_ident(kb, ident)
    kb.begin_phase()
    build_normproj(kb, NT, h1, g_pre, w_in, 1072, qin, idb, sig_from=1024, sig_out=gates)
    kb.end_phase()
    kb.begin_phase()
    build_nsa_attn(kb, nqt, cpb, qin, gates, A, o, idb)
    kb.end_phase()
    kb.begin_phase()
    build_outproj(kb, NT, 1024, o, w_out, g_post, h1, hmid, idb)
    kb.end_phase()
    kb.begin_phase()
    outs = build_ffn(kb, NT, hmid, fw_in, fw_out, fg_pre, fg_post, out, idb)
    kb.end_phase()
    kb.finish(outs); kb.close()
    return nc


def _run(nc, in_maps):
    res = run_bass_kernel_spmd(nc, in_maps, core_ids=list(range(len(in_maps))))
    return res.results


def kernel(x, mix_norm_pre, mix_norm_post, ffn_norm_pre, ffn_norm_post, ffn_w_in, ffn_w_out, ret_w_in, ret_w_out,
           kv_norm, kv_w, cmp_pe_k, cmp_w1_k, cmp_w2_k, cmp_pe_v, cmp_w1_v, cmp_w2_v, nsa_w_in, nsa_w_out, rel_bias):
    f = lambda a: np.ascontiguousarray(np.asarray(a, dtype=np.float32))
    x = f(x)
    B, SEQ = x.shape[0], x.shape[1]
    NCORE = B * CPB; NTC = SEQ // CPB; NQT = NTC // 128
    assert NCORE <= 8 and NTC % 256 == 0
    eye = np.eye(128, dtype=np.float32)
    gi, g2i, maskT = ret_consts()
    w_in_p = perm_ret_w_in(f(ret_w_in)[0])
    base = []
    for c in range(NCORE):
        b, j = divmod(c, CPB)
        pos = np.arange(j * NTC, (j + 1) * NTC)
        cT, sT, ck, sk = ret_tables(pos)
        base.append({"x": np.ascontiguousarray(x[b, j * NTC:(j + 1) * NTC]), "ret_w_in": w_in_p,
                     "g_mix_pre": bcast128(f(mix_norm_pre)[0]), "cosT": cT, "sinT": sT, "cosk": ck, "sink": sk,
                     "gi": gi, "g2i": g2i, "maskT": maskT, "ident": eye})
    r1 = _run(prog_state(NTC), base)
    L = [np.asarray(r["s_out"], np.float32) for r in r1]
    gam128 = np.exp(128.0 * LOGG)
    ims = []
    for c in range(NCORE):
        b, j = divmod(c, CPB)
        slots = np.zeros((3, 2, 128, 4, 512), np.float32); coef = np.zeros((3, 4), np.float64)
        for i in range(j):
            slots[i] = L[b * CPB + i]
            coef[i] = gam128 ** (NQT * (j - 1 - i))
        im = dict(base[c])
        im.update({"s_slots": slots, "coef": bcast128(coef.reshape(-1).astype(np.float32)), "ret_w_out": f(ret_w_out)[0],
                   "g_mix_post": bcast128(f(mix_norm_post)[0]), "ffn_w_in": f(ffn_w_in)[0], "ffn_w_out": f(ffn_w_out)[0],
                   "g_ffn_pre": bcast128(f(ffn_norm_pre)[0]), "g_ffn_post": bcast128(f(ffn_norm_post)[0]),
                   "g_kv": bcast128(f(kv_norm)), "kv_w": f(kv_w)})
        ims.append(im)
    r2 = _run(prog_layer0(NTC), ims)
    h1 = np.stack([np.concatenate([np.asarray(r2[b * CPB + j]["h1"]) for j in range(CPB)]) for b in range(B)])
    kv = np.stack([np.concatenate([np.asarray(r2[b * CPB + j]["kv"]) for j in range(CPB)]) for b in range(B)])
    W1 = np.stack([f(cmp_w1_k), f(cmp_w1_v)]); W2 = np.stack([f(cmp_w2_k), f(cmp_w2_v)])
    PEc = pecol(np.stack([f(cmp_pe_k), f(cmp_pe_v)]))
    ims = []
    for c in range(NCORE):
        b, j = divmod(c, CPB)
        kcv_tok = kv[b].reshape(SEQ, 6, 4, 64)[:, 0:2]
        ims.append({"XT": im2col(kcv_tok, (NTC // 16) * j, NTC // 16), "W1": W1, "W2": W2, "PEc": PEc})
    r3 = _run(prog_compress(NTC // 16), ims)
    n_cmp = (SEQ - 32) // 16 + 1
    kcs = [np.concatenate([np.asarray(r3[b * CPB + j]["kcv"]) for j in range(CPB)], axis=1)[:, :n_cmp] for b in range(B)]
    consts = nsa_consts(NQT, CPB, f(rel_bias))
    ims = []; rows_all = []
    for c in range(NCORE):
        b, j = divmod(c, CPB)
        rows = np.concatenate([np.arange(128) + 128 * (CPB * l + j) for l in range(NQT)])
        rows_all.append(rows)
        im = nsa_core_inputs(NQT, CPB, j, SEQ, kv[b], kcs[b][0], kcs[b][1])
        im.update(consts)
        im.update({"h1rr": np.ascontiguousarray(h1[b][rows]), "g_mix_pre": bcast128(f(mix_norm_pre)[1]), "nsa_w_in": f(nsa_w_in)[0],
                   "ident": eye, "nsa_w_out": f(nsa_w_out)[0], "g_mix_post": bcast128(f(mix_norm_post)[1]),
                   "ffn_w_in": f(ffn_w_in)[1], "ffn_w_out": f(ffn_w_out)[1], "g_ffn_pre": bcast128(f(ffn_norm_pre)[1]),
                   "g_ffn_post": bcast128(f(ffn_norm_post)[1])})
        ims.append(im)
    r4 = _run(prog_layer1(NQT), ims)
    out = np.zeros((B, SEQ, D), np.float32)
    for c in range(NCORE):
        b, j = divmod(c, CPB)
        out[b, rows_all[c]] = np.asarray(r4[c]["out"])
    return out
```
